# Optimizing a Trainium2 kernel written in Bass

```python
import jax, jax.numpy as jnp
from jax import lax
import numpy as np

D_MODEL = 1024
BATCH = 2
SEQ = 8192
DEPTH = 2

GRID_W = 64
CTX_LEN = 256
N_MIXERS = 2
EPS = 1e-6
D_RNN = D_MODEL
RG_BLOCKS = 8
RG_BW = D_RNN // RG_BLOCKS
CONV_W = 4
LRU_C = 8.0
HEAD_DIM = 128
N_HEADS = D_MODEL // HEAD_DIM
N_KV = 2
GROUPS = N_HEADS // N_KV
Q_BLOCK = 128
ROPE_THETA = 10000.0
ROPE_AXIS_DIM = HEAD_DIM // 2
ROPE_NFREQ = ROPE_AXIS_DIM // 2
D_FF = 2816
N_EXPERTS = 8
TOP_K = 2
D_FF_EXPERT = 3584
MOE_BLOCK = 128
N_A = (DEPTH + 1) // 2
N_B = DEPTH // 2
N_DENSE = (DEPTH + 1) // 2
N_MOE = DEPTH // 2

kernel_name = "hybrid_rglru_gqa_moe_diffusion_block"


def rmsnorm(x, g):
    xf = x.astype(jnp.float32)
    y = xf * lax.rsqrt(jnp.mean(xf * xf, axis=-1, keepdims=True) + EPS)
    return (y * g).astype(x.dtype)


def modulate(h, shift, scale):
    return h * (1 + scale) + shift


def dwconv_centered(x, w, b):
    L = x.shape[1]
    left = CONV_W // 2
    right = CONV_W - 1 - left
    xp = jnp.pad(x, ((0, 0), (left, right), (0, 0)))
    y = b
    for k in range(CONV_W):
        y = y + xp[:, k:k + L] * w[k]
    return y


def block_diag(x, w):
    B, L, _ = x.shape
    xb = x.reshape(B, L, RG_BLOCKS, RG_BW)
    return jnp.einsum('blnc,ncd->blnd', xb, w).reshape(B, L, RG_BLOCKS * RG_BW)


def lru_coeffs(xc, w_a, b_a, w_i, b_i, lam):
    xf = xc.astype(jnp.float32)
    r = jax.nn.sigmoid(block_diag(xf, w_a) + b_a)
    i = jax.nn.sigmoid(block_diag(xf, w_i) + b_i)
    log_a = -LRU_C * r * jax.nn.softplus(-lam.astype(jnp.float32))
    a = jnp.exp(log_a)
    mult = jnp.sqrt(-jnp.expm1(2.0 * log_a))
    return a, mult * (i * xf)


def linear_scan(a, b, h0, reverse):
    def combine(e1, e2):
        a1, b1 = e1
        a2, b2 = e2
        return a1 * a2, a2 * b1 + b2
    A, Bc = lax.associative_scan(combine, (a, b), reverse=reverse, axis=1)
    return A * h0[:, None, :] + Bc


def rglru_mixer(h_lat, h_ctx, w_in, conv_w, conv_b, w_a, b_a, w_i, b_i, lam, w_out, need_ctx):
    xb_l, gb_l = jnp.split(h_lat @ w_in, 2, axis=-1)
    xb_c, gb_c = jnp.split(h_ctx @ w_in, 2, axis=-1)
    xc_l = dwconv_centered(xb_l, conv_w, conv_b)
    xc_c = dwconv_centered(xb_c, conv_w, conv_b)
    ys_l, ys_c = [], []
    for d, reverse in enumerate((False, True)):
        a_c, b_c = lru_coeffs(xc_c, w_a[d], b_a[d], w_i[d], b_i[d], lam[d])
        h_c = linear_scan(a_c, b_c, jnp.zeros_like(a_c[:, 0]), reverse)
        h0 = h_c[:, 0] if reverse else h_c[:, -1]
        a_l, b_l = lru_coeffs(xc_l, w_a[d], b_a[d], w_i[d], b_i[d], lam[d])
        ys_l.append(linear_scan(a_l, b_l, h0, reverse))
        ys_c.append(h_c)
    y_l = (ys_l[0] + ys_l[1]).astype(h_lat.dtype)
    out_lat = (y_l * jax.nn.gelu(gb_l)) @ w_out
    out_ctx = None
    if need_ctx:
        y_c = (ys_c[0] + ys_c[1]).astype(h_ctx.dtype)
        out_ctx = (y_c * jax.nn.gelu(gb_c)) @ w_out
    return out_lat, out_ctx


def axial_rope_angles(L):
    rows = L // GRID_W
    row = jnp.broadcast_to(jnp.arange(rows)[:, None], (rows, GRID_W)).reshape(L).astype(jnp.float32)
    col = jnp.broadcast_to(jnp.arange(GRID_W)[None, :], (rows, GRID_W)).reshape(L).astype(jnp.float32)
    freqs = ROPE_THETA ** (-jnp.arange(ROPE_NFREQ, dtype=jnp.float32) / ROPE_NFREQ)
    return row[:, None] * freqs, col[:, None] * freqs


def rotate(x, ang):
    cos = jnp.cos(ang)[None, :, None, :].astype(x.dtype)
    sin = jnp.sin(ang)[None, :, None, :].astype(x.dtype)
    x1, x2 = x[..., :ROPE_NFREQ], x[..., ROPE_NFREQ:]
    return jnp.concatenate([x1 * cos - x2 * sin, x2 * cos + x1 * sin], axis=-1)


def apply_axial_rope(x, ang_row, ang_col):
    return jnp.concatenate([rotate(x[..., :ROPE_AXIS_DIM], ang_row),
                            rotate(x[..., ROPE_AXIS_DIM:], ang_col)], axis=-1)


def gqa_attend(q, k, v):
    s = jnp.einsum('bqkgd,bskd->bkgqs', q, k).astype(jnp.float32) * (HEAD_DIM ** -0.5)
    p = jax.nn.softmax(s, axis=-1).astype(v.dtype)
    return jnp.einsum('bkgqs,bskd->bqkgd', p, v)


def attn_mixer(h_lat, h_ctx, w_qkv, q_g, k_g, w_o, need_ctx):
    B, L, _ = h_lat.shape
    C = h_ctx.shape[1]
    nq = N_HEADS * HEAD_DIM
    nkv = N_KV * HEAD_DIM
    q_l, k_l, v_l = jnp.split(h_lat @ w_qkv, [nq, nq + nkv], axis=-1)
    q_l = rmsnorm(q_l.reshape(B, L, N_HEADS, HEAD_DIM), q_g)
    k_l = rmsnorm(k_l.reshape(B, L, N_KV, HEAD_DIM), k_g)
    v_l = v_l.reshape(B, L, N_KV, HEAD_DIM)
    ang_row, ang_col = axial_rope_angles(L)
    q_l = apply_axial_rope(q_l, ang_row, ang_col)
    k_l = apply_axial_rope(k_l, ang_row, ang_col)
    if need_ctx:
        q_c, k_c, v_c = jnp.split(h_ctx @ w_qkv, [nq, nq + nkv], axis=-1)
    else:
        k_c, v_c = jnp.split(h_ctx @ w_qkv[:, nq:], [nkv], axis=-1)
    k_c = rmsnorm(k_c.reshape(B, C, N_KV, HEAD_DIM), k_g)
    v_c = v_c.reshape(B, C, N_KV, HEAD_DIM)
    k_all = jnp.concatenate([k_c, k_l], axis=1)
    v_all = jnp.concatenate([v_c, v_l], axis=1)
    nb = L // Q_BLOCK
    qb = q_l.reshape(B, nb, Q_BLOCK, N_KV, GROUPS, HEAD_DIM).transpose(1, 0, 2, 3, 4, 5)
    o = lax.map(lambda qblk: gqa_attend(qblk, k_all, v_all), qb)
    o = o.transpose(1, 0, 2, 3, 4, 5).reshape(B, L, nq)
    out_lat = o @ w_o
    out_ctx = None
    if need_ctx:
        q_c = rmsnorm(q_c.reshape(B, C, N_HEADS, HEAD_DIM), q_g).reshape(B, C, N_KV, GROUPS, HEAD_DIM)
        out_ctx = gqa_attend(q_c, k_c, v_c).reshape(B, C, nq) @ w_o
    return out_lat, out_ctx


def swiglu(h, w1, w3, w2):
    return (jax.nn.silu(h @ w1) * (h @ w3)) @ w2


def moe_ffn(h, router, w1, w3, w2):
    shp = h.shape
    xt = h.reshape(-1, shp[-1])
    T = xt.shape[0]
    logits = (xt @ router).astype(jnp.float32)
    top_logit, top_idx = lax.top_k(logits, TOP_K)
    gates = jax.nn.softmax(top_logit, axis=-1).astype(h.dtype)
    TK = T * TOP_K
    slot_e = top_idx.reshape(TK)
    slot_tok = jnp.arange(TK, dtype=jnp.int32) // TOP_K
    slot_w = gates.reshape(TK)
    order = jnp.argsort(slot_e)
    e_sorted = slot_e[order]
    counts = jnp.bincount(slot_e, length=N_EXPERTS)
    padded = ((counts + MOE_BLOCK - 1) // MOE_BLOCK) * MOE_BLOCK
    start = jnp.cumsum(counts) - counts
    pend = jnp.cumsum(padded)
    pstart = pend - padded
    dest = pstart[e_sorted] + (jnp.arange(TK) - start[e_sorted])
    n_blocks = -(-TK // MOE_BLOCK) + N_EXPERTS
    P = n_blocks * MOE_BLOCK
    buf_tok = jnp.zeros((P,), jnp.int32).at[dest].set(slot_tok[order])
    buf_w = jnp.zeros((P,), h.dtype).at[dest].set(slot_w[order])
    blk_e = jnp.minimum(jnp.searchsorted(pend, jnp.arange(n_blocks) * MOE_BLOCK, side='right'), N_EXPERTS - 1)

    def run(args):
        tok, w, e = args
        xb = xt[tok]
        hid = jax.nn.silu(xb @ w1[e]) * (xb @ w3[e])
        return (hid @ w2[e]) * w[:, None]

    y = lax.map(run, (buf_tok.reshape(n_blocks, MOE_BLOCK), buf_w.reshape(n_blocks, MOE_BLOCK), blk_e))
    out = jnp.zeros_like(xt).at[buf_tok].add(y.reshape(P, shp[-1]))
    return out.reshape(shp)


def setup_inputs(seed: int = 0) -> dict:
    key = jax.random.key(seed)
    ks = jax.random.split(key, 32)
    f32 = jnp.float32

    def nrm(k, shape, fan_in, mult=1.0):
        return jax.random.normal(k, shape, f32) * (mult * fan_in ** -0.5)

    def gain(k, shape):
        return 1.0 + 0.1 * jax.random.normal(k, shape, f32)

    def bias(k, shape):
        return 0.02 * jax.random.normal(k, shape, f32)

    u = jax.random.uniform(ks[16], (N_A, 2, D_RNN), f32, minval=0.9, maxval=0.999)
    s = u ** (1.0 / LRU_C)
    rg_lam = jnp.log(s) - jnp.log1p(-s)
    qkv_out = (N_HEADS + 2 * N_KV) * HEAD_DIM
    return {
        "x": jax.random.normal(ks[0], (BATCH, SEQ, D_MODEL), f32),
        "c": jax.random.normal(ks[1], (BATCH, D_MODEL), f32),
        "ctx": jax.random.normal(ks[2], (BATCH, CTX_LEN, D_MODEL), f32),
        "c_ctx": jax.random.normal(ks[3], (D_MODEL,), f32),
        "ada_w": nrm(ks[4], (DEPTH, D_MODEL, 6 * D_MODEL), D_MODEL, 0.5),
        "ada_b": bias(ks[5], (DEPTH, 6 * D_MODEL)),
        "norm1_g": gain(ks[6], (DEPTH, D_MODEL)),
        "norm2_g": gain(ks[7], (DEPTH, D_MODEL)),
        "rg_w_in": nrm(ks[8], (N_A, D_MODEL, 2 * D_RNN), D_MODEL),
        "rg_conv_w": nrm(ks[9], (N_A, CONV_W, D_RNN), CONV_W),
        "rg_conv_b": bias(ks[10], (N_A, D_RNN)),
        "rg_w_a": nrm(ks[11], (N_A, 2, RG_BLOCKS, RG_BW, RG_BW), RG_BW),
        "rg_b_a": bias(ks[12], (N_A, 2, D_RNN)),
        "rg_w_i": nrm(ks[13], (N_A, 2, RG_BLOCKS, RG_BW, RG_BW), RG_BW),
        "rg_b_i": bias(ks[14], (N_A, 2, D_RNN)),
        "rg_lam": rg_lam,
        "rg_w_out": nrm(ks[15], (N_A, D_RNN, D_MODEL), D_RNN),
        "attn_w_qkv": nrm(ks[17], (N_B, D_MODEL, qkv_out), D_MODEL),
        "attn_q_g": gain(ks[18], (N_B, HEAD_DIM)),
        "attn_k_g": gain(ks[19], (N_B, HEAD_DIM)),
        "attn_w_o": nrm(ks[20], (N_B, N_HEADS * HEAD_DIM, D_MODEL), N_HEADS * HEAD_DIM),
        "ffn_w1": nrm(ks[21], (N_DENSE, D_MODEL, D_FF), D_MODEL),
        "ffn_w3": nrm(ks[22], (N_DENSE, D_MODEL, D_FF), D_MODEL),
        "ffn_w2": nrm(ks[23], (N_DENSE, D_FF, D_MODEL), D_FF),
        "moe_router": nrm(ks[24], (N_MOE, D_MODEL, N_EXPERTS), D_MODEL),
        "moe_w1": nrm(ks[25], (N_MOE, N_EXPERTS, D_MODEL, D_FF_EXPERT), D_MODEL),
        "moe_w3": nrm(ks[26], (N_MOE, N_EXPERTS, D_MODEL, D_FF_EXPERT), D_MODEL),
        "moe_w2": nrm(ks[27], (N_MOE, N_EXPERTS, D_FF_EXPERT, D_MODEL), D_FF_EXPERT),
        "final_g": gain(ks[28], (D_MODEL,)),
    }


def reference(x, c, ctx, c_ctx, ada_w, ada_b, norm1_g, norm2_g,
              rg_w_in, rg_conv_w, rg_conv_b, rg_w_a, rg_b_a, rg_w_i, rg_b_i, rg_lam, rg_w_out,
              attn_w_qkv, attn_q_g, attn_k_g, attn_w_o,
              ffn_w1, ffn_w3, ffn_w2,
              moe_router, moe_w1, moe_w3, moe_w2, final_g):
    silu_c = jax.nn.silu(c)
    silu_cc = jax.nn.silu(c_ctx)
    for i in range(DEPTH):
        need_ctx = i < DEPTH - 1
        j = i // N_MIXERS
        mod_l = (silu_c @ ada_w[i] + ada_b[i])[:, None, :]
        sh1, sc1, g1, sh2, sc2, g2 = jnp.split(mod_l, 6, axis=-1)
        mod_c = silu_cc @ ada_w[i] + ada_b[i]
        csh1, csc1, cg1, csh2, csc2, cg2 = jnp.split(mod_c, 6, axis=-1)
        h_l = modulate(rmsnorm(x, norm1_g[i]), sh1, sc1)
        h_c = modulate(rmsnorm(ctx, norm1_g[i]), csh1, csc1)
        if i % N_MIXERS == 0:
            m_l, m_c = rglru_mixer(h_l, h_c, rg_w_in[j], rg_conv_w[j], rg_conv_b[j], rg_w_a[j], rg_b_a[j],
                                   rg_w_i[j], rg_b_i[j], rg_lam[j], rg_w_out[j], need_ctx)
        else:
            m_l, m_c = attn_mixer(h_l, h_c, attn_w_qkv[j], attn_q_g[j], attn_k_g[j], attn_w_o[j], need_ctx)
        x = x + g1 * m_l
        h_l = modulate(rmsnorm(x, norm2_g[i]), sh2, sc2)
        if i % 2 == 0:
            ffn = lambda h: swiglu(h, ffn_w1[j], ffn_w3[j], ffn_w2[j])
        else:
            ffn = lambda h: moe_ffn(h, moe_router[j], moe_w1[j], moe_w3[j], moe_w2[j])
        x = x + g2 * ffn(h_l)
        if need_ctx:
            ctx = ctx + cg1 * m_c
            h_c = modulate(rmsnorm(ctx, norm2_g[i]), csh2, csc2)
            ctx = ctx + cg2 * ffn(h_c)
    return rmsnorm(x, final_g)
```

```python
from contextlib import ExitStack
import numpy as np
import ml_dtypes
import concourse.bass as bass
import concourse.mybir as mybir
from concourse.bass_utils import run_bass_kernel_spmd

F32 = mybir.dt.float32
BF16 = mybir.dt.bfloat16
AF = mybir.ActivationFunctionType
ALU = mybir.AluOpType
AX = mybir.AxisListType

D = 1024
KC = 8
NT = 2048
NCX = 256
LAT0, CTX0, HAL0 = 0, NT, NT + NCX
TOT = NT + NCX + 128
DFF = 2816
DFE = 3584
NE = 8
EPS = 1e-6
SEQ = 8192
NKEY = SEQ + NCX
NKT = NKEY // 128


class Buf:
    __slots__ = ("name", "w", "r", "sem", "cnt")

    def __init__(self, name):
        self.name = name
        self.w = None
        self.r = []
        self.sem = None
        self.cnt = 0


class Sched:
    ENG = ("tensor", "vector", "scalar", "gpsimd", "sync")

    def __init__(self, nc, es):
        self.nc = nc
        self.es = es
        self.es_sem = es
        self.ops = {e: [] for e in self.ENG}
        self.cnt = {e: 0 for e in self.ENG}
        self.seen = {e: {} for e in self.ENG}
        self.esem = {e: es.enter_context(nc.semaphore("s_" + e)) for e in self.ENG}
        self.dsems = []
        self.out_tokens = []
        self.nbuf = 0
        self.cond = None
        self.regs = {e: [es.enter_context(getattr(nc, e).register(f"r{i}_" + e)) for i in range(3)] for e in self.ENG}

    def sbuf(self, name, shape, dtype, side=None):
        self.nbuf += 1
        name = f"sb{self.nbuf}_{name}"
        if side is None:
            return self.es.enter_context(self.nc.sbuf_tensor(name, list(shape), dtype))
        return self.es.enter_context(self.nc.sbuf_tensor(name, list(shape), dtype, side=side))

    def psum(self, name, shape, dtype=F32):
        self.nbuf += 1
        name = f"pp{self.nbuf}_{name}"
        return self.es.enter_context(self.nc.psum_tensor(name, list(shape), dtype))

    def buf(self, name="b"):
        self.nbuf += 1
        return Buf(f"{name}{self.nbuf}")

    def _waits(self, engine, deps):
        need = {}
        for tok in deps:
            if tok is None:
                continue
            key, val = tok
            if isinstance(key, str):
                if key == engine and engine in ("tensor", "sync"):
                    continue
                sem = self.esem[key]
                assert val <= self.cnt[key], f"dep on unsignaled op of {key}"
            else:
                sem = key
            k = id(sem)
            if self.seen[engine].get(k, 0) >= val:
                continue
            if k not in need or need[k][1] < val:
                need[k] = (sem, val)
        for k, (sem, val) in need.items():
            self.seen[engine][k] = val
        return list(need.values())

    def op(self, engine, fn, reads=(), writes=(), sig=True):
        deps = []
        for b in reads:
            deps.append(b.w)
        for b in writes:
            deps.append(b.w)
            deps.extend(b.r)
        waits = self._waits(engine, deps)
        if sig:
            self.cnt[engine] += 1
            tok = (engine, self.cnt[engine])
            inc = (self.esem[engine], 1)
        else:
            tok = (engine, self.cnt[engine] + 1)
            inc = None
        self.ops[engine].append((fn, waits, inc, None))
        for b in reads:
            b.r.append(tok)
        for b in writes:
            b.w = tok
            b.r = []
        return tok

    def collective(self, kind, groups, in_ap, out_ap, reads, out_buf):
        deps = [b.w for b in reads] + [out_buf.w] + list(out_buf.r)
        waits = self._waits("gpsimd", deps)
        if out_buf.sem is None:
            out_buf.sem = self.es_sem.enter_context(self.nc.semaphore("c_" + out_buf.name))
            self.dsems.append(out_buf)
        out_buf.cnt += 1
        tok = (out_buf.sem, out_buf.cnt)

        def fn(eng):
            return eng.collective_compute(kind, ALU.bypass, replica_groups=groups, ins=[in_ap], outs=[out_ap])
        self.ops["gpsimd"].append((fn, waits, (out_buf.sem, 1), out_buf))
        for b in reads:
            b.r.append(tok)
        out_buf.w = tok
        out_buf.r = []
        return tok

    def dma(self, queue, out, in_, reads=(), writes=(), out_sem_buf=None, nowaw=False, **kw):
        deps = []
        for b in reads:
            deps.append(b.w)
        for b in writes:
            if not (nowaw and b.w is not None and b.sem is not None and b.w[0] is b.sem):
                deps.append(b.w)
            deps.extend(b.r)
        waits = self._waits(queue, deps)
        holder = writes[0] if writes else out_sem_buf
        if holder.sem is None:
            holder.sem = self.es_sem.enter_context(self.nc.semaphore("d_" + holder.name))
            self.dsems.append(holder)
        holder.cnt += 16
        tok = (holder.sem, holder.cnt)

        def fn(eng, out=out, in_=in_, kw=kw):
            return eng.dma_start(out=out, in_=in_, **kw)
        self.ops[queue].append((fn, waits, (holder.sem, 16), holder))
        for b in reads:
            b.r.append(tok)
        for b in writes:
            b.w = tok
            b.r = []
        if not writes:
            self.out_tokens.append(tok)
        return tok

    def barrier(self):
        toks = [(e, self.cnt[e]) for e in self.ENG if self.cnt[e] > 0]
        toks += [(h.sem, h.cnt) for h in self.dsems]
        for e in self.ENG:
            waits = self._waits(e, toks)
            if waits:
                self.ops[e].append((None, waits, None, None))

    def finish(self):
        waits = self._waits("sync", self.out_tokens)
        self.ops["sync"].append((None, waits, None, None))

    def begin_cond(self, cnt_ap, cnt_buf, thr, key=None):
        if self.cond is None:
            self.cond = []
        self.cond.append(dict(ap=cnt_ap, buf=cnt_buf, thr=thr, key=key, start={e: len(self.ops[e]) for e in self.ENG},
                              cnt0=dict(self.cnt), hcnt0={id(h): h.cnt for h in self.dsems},
                              seen0={e: dict(self.seen[e]) for e in self.ENG}))

    def end_cond(self):
        c = self.cond.pop()

        def collect(body, hinc):
            for (fn, w, inc, holder) in body:
                if fn == "cond":
                    collect(inc[2], hinc)
                elif fn is not None and holder is not None:
                    k = id(holder)
                    if k not in hinc:
                        hinc[k] = [holder, c["hcnt0"].get(k, 0), 0]
                    hinc[k][2] += inc[1]
        for e in self.ENG:
            body = self.ops[e][c["start"][e]:]
            if not body:
                continue
            del self.ops[e][c["start"][e]:]
            self.seen[e] = c["seen0"][e]
            waits = self._waits(e, [c["buf"].w])
            nsig = self.cnt[e] - c["cnt0"][e]
            hinc = {}
            collect(body, hinc)
            self.ops[e].append(("cond", waits, (c["ap"], c["thr"], body, c["cnt0"][e], nsig, list(hinc.values()), c["key"]), None))

    def emit(self):
        with self.nc.Block() as block:
            loaded = {}

            def mk(engine):
                def run(eng, ops, depth=0):
                    for fn, waits, inc, _h in ops:
                        for sem, val in waits:
                            eng.wait_ge(sem, val)
                        if fn is None:
                            continue
                        if fn == "cond":
                            ap, thr, sub, cnt0, nsig, hincs, key = inc
                            reg = self.regs[engine][0]
                            if key is None or loaded.get(engine) != key:
                                eng.reg_load(reg, ap)
                                loaded[engine] = key if depth == 0 else None
                            with eng.If_lt(reg, thr + 1):
                                if nsig:
                                    if cnt0:
                                        eng.wait_ge(self.esem[engine], cnt0)
                                    eng.sem_inc(self.esem[engine], nsig)
                                for holder, before, tot in hincs:
                                    if before:
                                        eng.wait_ge(holder.sem, before)
                                    eng.sem_inc(holder.sem, tot)
                            with eng.Else():
                                run(eng, sub, depth + 1)
                            continue
                        ins = fn(eng)
                        if inc is not None:
                            ins.then_inc(inc[0], inc[1])

                def body(eng):
                    run(eng, self.ops[engine])
                return body
            for e in self.ENG:
                if self.ops[e]:
                    getattr(block, e)(mk(e))


class Ring:
    def __init__(self, S, name, shape, dtype, n, psum=False):
        self.tiles = []
        S.nbuf += 1
        name = f"{name}_{S.nbuf}_"
        for i in range(n):
            t = S.psum(f"{name}{i}", shape, dtype) if psum else S.sbuf(f"{name}{i}", shape, dtype)
            self.tiles.append((t, S.buf(name)))
        self.i = 0

    def next(self):
        t = self.tiles[self.i % len(self.tiles)]
        self.i += 1
        return t


def bcast_mid(ap2d, n):
    a = ap2d.ap
    return bass.AP(ap2d.tensor, ap2d.offset, [list(a[0]), [0, n], list(a[1])])


def rev_ap(ap2d):
    a = [list(p) for p in ap2d.ap]
    n = a[-1][1]
    a[-1] = [-a[-1][0], n]
    return bass.AP(ap2d.tensor, ap2d.offset + (n - 1) * ap2d.ap[-1][0], a)


class VecPack:
    def __init__(self):
        self.cols = {}
        self.n = 0

    def add(self, name, ncols):
        self.cols[name] = (self.n, ncols)
        self.n += ncols

    def sl(self, name, j=0, w=1):
        o, n = self.cols[name]
        assert j + w <= n
        return slice(o + j, o + j + w)


def chunked(v):
    v = np.asarray(v, np.float32)
    return np.ascontiguousarray(v.reshape(-1, 128).T)


VP = VecPack()
for _n, _c in [("cv", 16), ("ada_b0", 48), ("ada_b1", 48), ("n1g0", 8), ("n2g0", 8), ("n1g1", 8), ("n2g1", 8),
               ("convw", 32), ("convb", 8), ("b_a", 16), ("b_i", 16), ("lam", 16), ("q_g", 1), ("k_g", 1),
               ("hmask", 3), ("sel_f", 4), ("sel_r", 4)]:
    VP.add(_n, _c)


def pack_vecs(inp, b, j):
    v = np.zeros((128, VP.n), np.float32)

    def put(name, arr):
        o, n = VP.cols[name]
        arr = np.asarray(arr, np.float32).reshape(128, n)
        v[:, o:o + n] = arr
    cl = chunked(inp["c"][b])
    cc = chunked(inp["c_ctx"])
    put("cv", np.stack([cl, cc], axis=2).reshape(128, 16))
    put("ada_b0", chunked(inp["ada_b"][0]))
    put("ada_b1", chunked(inp["ada_b"][1]))
    put("n1g0", chunked(inp["norm1_g"][0]))
    put("n2g0", chunked(inp["norm2_g"][0]))
    put("n1g1", chunked(inp["norm1_g"][1]))
    put("n2g1", chunked(inp["norm2_g"][1]))
    put("convw", np.concatenate([chunked(inp["rg_conv_w"][0, k]) for k in range(4)], axis=1))
    put("convb", chunked(inp["rg_conv_b"][0]))
    put("b_a", np.concatenate([chunked(inp["rg_b_a"][0, d]) for d in range(2)], axis=1))
    put("b_i", np.concatenate([chunked(inp["rg_b_i"][0, d]) for d in range(2)], axis=1))
    put("lam", np.concatenate([chunked(inp["rg_lam"][0, d]) for d in range(2)], axis=1))
    put("q_g", np.asarray(inp["attn_q_g"][0]).reshape(128, 1))
    put("k_g", np.asarray(inp["attn_k_g"][0]).reshape(128, 1))
    hm = np.array([1.0 if j > 0 else 0.0, 1.0 if j > 0 else 0.0, 1.0 if j < 3 else 0.0], np.float32)
    put("hmask", np.broadcast_to(hm, (128, 3)))
    put("sel_f", np.broadcast_to(np.array([1.0 if i < j else 0.0 for i in range(4)], np.float32), (128, 4)))
    put("sel_r", np.broadcast_to(np.array([1.0 if i > j else 0.0 for i in range(4)], np.float32), (128, 4)))
    return v


class Ctx:
    pass


def setup_common(nc, es, vecs_d, ident_d, ps_ring=True):
    C = Ctx()
    C.nc = nc
    S = C.S = Sched(nc, es)
    if ps_ring:
        C.ps = Ring(S, "ps", [128, 512], F32, 8, psum=True)
    C.vec = S.sbuf("vec", [128, VP.n], F32)
    C.bvec = S.buf("vec")
    S.dma("sync", C.vec[:], vecs_d, writes=[C.bvec])
    C.ident = S.sbuf("ident", [128, 128], F32)
    C.bident = S.buf("ident")
    S.dma("sync", C.ident[:], ident_d, writes=[C.bident])
    C.ones = S.sbuf("ones", [128, 128], BF16)
    C.bones = S.buf("ones")
    S.op("vector", lambda e: e.memset(C.ones[:], 1.0), writes=[C.bones])
    C.epsb = S.sbuf("epsb", [128, 1], F32)
    C.bepsb = S.buf("epsb")
    S.op("vector", lambda e: e.memset(C.epsb[:], EPS), writes=[C.bepsb])
    C.alt = 0
    C.ada_queue = "sync"
    C.ada_dt = F32
    C.ada_ring = 2
    return C


def V(C, name, j=0, w=1):
    return C.vec[:, VP.sl(name, j, w)]


def evac_engine(C):
    C.alt += 1
    return "vector" if C.alt % 2 else "scalar"


def copy_op(C, eng, out, in_, reads, writes):
    S = C.S
    if eng == "scalar":
        S.op("scalar", lambda e: e.copy(out, in_), reads=reads, writes=writes)
    else:
        S.op(eng, lambda e: e.tensor_copy(out, in_), reads=reads, writes=writes)


def emit_ada(C, ada_w_d, layer, bias_name, out_mod, bmod):
    S = C.S
    nc = C.nc
    ps, bps = C.ps.next()
    wr = Ring(S, f"adaw{layer}", [128, KC, 768], C.ada_dt, C.ada_ring)
    for g in range(8):
        wt, bw = wr.next()
        src = ada_w_d[layer].rearrange("(k p) n -> p k n", p=128)[:, :, g * 768:(g + 1) * 768]
        S.dma(C.ada_queue, wt[:], src, writes=[bw])
        for fi in range(6):
            f = g * 6 + fi
            for k in range(KC):
                S.op("tensor", lambda e, wt=wt, fi=fi, k=k, f=f: e.matmul(
                    ps[:, f * 2:f * 2 + 2], wt[:, k, fi * 128:(fi + 1) * 128], C.sv_mm[:, k * 2:k * 2 + 2],
                    start=(k == 0), stop=(k == KC - 1)),
                    reads=[bw, C.bsv], writes=[bps], sig=(k == KC - 1))
    bo, bn = VP.cols[bias_name]
    S.op("vector", lambda e: e.tensor_tensor(
        out_mod[:], ps[:, 0:96].rearrange("p (f c) -> p f c", c=2),
        bass.AP(C.vec[:].tensor, C.vec[:, bo:bo + 48].offset, [list(C.vec[:].ap[0]), [1, 48], [0, 2]]), ALU.add),
        reads=[bps, C.bvec], writes=[bmod])


def emit_silu_c(C):
    S = C.S
    C.sv = S.sbuf("sv", [128, 16], F32)
    C.bsv = S.buf("sv")
    S.op("scalar", lambda e: e.activation(C.sv[:], V(C, "cv", 0, 16), AF.Silu), reads=[C.bvec], writes=[C.bsv])
    C.sv_mm = C.sv
    if C.ada_dt != F32:
        C.sv_mm = S.sbuf("svb", [128, 16], C.ada_dt)
        S.op("vector", lambda e: e.tensor_copy(C.sv_mm[:], C.sv[:]), reads=[C.bsv], writes=[C.bsv])


def emit_gs(C, mod, bmod, sc_idx, gname, name):
    S = C.S
    gs = S.sbuf(name, [128, KC, 2], F32)
    bgs = S.buf(name)
    go, _ = VP.cols[gname]
    gb = bass.AP(C.vec[:].tensor, C.vec[:, go:go + 8].offset, [list(C.vec[:].ap[0]), [1, 8], [0, 2]])
    S.op("vector", lambda e: e.scalar_tensor_tensor(
        gs[:], mod[:, sc_idx * 8:(sc_idx + 1) * 8, :], 1.0, gb, ALU.add, ALU.mult),
        reads=[bmod, C.bvec], writes=[bgs])
    return gs, bgs


def emit_ada_dma(C, ada_w_d, layer, g, wr):
    wt, bw = wr.next()
    src = ada_w_d[layer].rearrange("(k p) n -> p k n", p=128)[:, :, g * 768:(g + 1) * 768]
    C.S.dma(C.ada_queue, wt[:], src, writes=[bw])
    return wt, bw


def emit_ada_mm(C, g, wt, bw, bias_name, out_mod, bmod):
    S = C.S
    ps, bps = C.ps.next()
    for fi in range(6):
        for k in range(KC):
            S.op("tensor", lambda e, fi=fi, k=k: e.matmul(
                ps[:, fi * 2:fi * 2 + 2], wt[:, k, fi * 128:(fi + 1) * 128], C.sv_mm[:, k * 2:k * 2 + 2],
                start=(k == 0), stop=(k == KC - 1)),
                reads=[bw, C.bsv], writes=[bps], sig=(k == KC - 1))
    bo, bn = VP.cols[bias_name]
    S.op("vector", lambda e: e.tensor_tensor(
        out_mod[:, g * 6:(g + 1) * 6, :], ps[:, 0:12].rearrange("p (f c) -> p f c", c=2),
        bass.AP(C.vec[:].tensor, C.vec[:, bo + g * 6:bo + g * 6 + 6].offset, [list(C.vec[:].ap[0]), [1, 6], [0, 2]]), ALU.add),
        reads=[bps, C.bvec], writes=[bmod])


def emit_gs_into(C, gs, bgs, mod, bmod, sc_idx, gname):
    S = C.S
    go, _ = VP.cols[gname]
    gb = bass.AP(C.vec[:].tensor, C.vec[:, go:go + 8].offset, [list(C.vec[:].ap[0]), [1, 8], [0, 2]])
    S.op("vector", lambda e: e.scalar_tensor_tensor(
        gs[:], mod[:, sc_idx * 8:(sc_idx + 1) * 8, :], 1.0, gb, ALU.add, ALU.mult),
        reads=[bmod, C.bvec], writes=[bgs])


def tile_bufs(bufs, c0, n):
    return bufs[c0 // 128:(c0 + n + 127) // 128]


def bcast_cols(ap_col, n):
    return bass.AP(ap_col.tensor, ap_col.offset, [list(ap_col.ap[0]), [0, n]])


def scoped(S):
    es = ExitStack()
    S.es = es
    return es


def alloc_norm_tmps(C):
    S = C.S
    C.sq = Ring(S, "sq", [128, 512], BF16, 2)
    C.rstd = Ring(S, "rstd", [128, 512], F32, 3)
    C.ntmp = Ring(S, "ntmp", [128, 512], F32, 2)


def emit_norm_stats(C, xs, bxs, s0, n):
    S = C.S
    rx = tile_bufs(bxs, s0, n)
    ps, bps = C.ps.next()
    for k in range(KC):
        sq, bsq = C.sq.next()
        if k % 2 == 0:
            S.op("gpsimd", lambda e, sq=sq, k=k: e.tensor_tensor(sq[:, :n], xs[:, k, s0:s0 + n], xs[:, k, s0:s0 + n], ALU.mult),
                 reads=rx, writes=[bsq])
        else:
            S.op("scalar", lambda e, sq=sq, k=k: e.activation(sq[:, :n], xs[:, k, s0:s0 + n], AF.Square), reads=rx, writes=[bsq])
        S.op("tensor", lambda e, sq=sq, k=k: e.matmul(ps[:, :n], C.ones[:], sq[:, :n], start=(k == 0), stop=(k == KC - 1)),
             reads=[bsq, C.bones], writes=[bps])
    rs, brs = C.rstd.next()
    S.op("scalar", lambda e: e.activation(rs[:, :n], ps[:, :n], AF.Sqrt, bias=C.epsb[:, 0:1], scale=1.0 / D), reads=[bps, C.bepsb], writes=[brs])
    S.op("vector", lambda e: e.reciprocal(rs[:, :n], rs[:, :n]), reads=[brs], writes=[brs])
    return rs, brs


def emit_norm_apply(C, rs, brs, xs, bxs, s0, hT, bh, c0, n, gs, bgs, mod, bmod, sh_idx, which, hf=None, bhf=None):
    S = C.S
    rx = tile_bufs(bxs, s0, n)
    wh = tile_bufs(bh, c0, n)
    for k in range(KC):
        tmp, btmp = C.ntmp.next()
        S.op("vector", lambda e, tmp=tmp, k=k: e.tensor_tensor(tmp[:, :n], xs[:, k, s0:s0 + n], rs[:, :n], ALU.mult),
             reads=rx + [brs], writes=[btmp])
        S.op("scalar", lambda e, tmp=tmp, k=k: e.activation(
            hT[:, k, c0:c0 + n], tmp[:, :n], AF.Identity,
            bias=mod[:, sh_idx * 8 + k, which:which + 1], scale=gs[:, k, which:which + 1]),
            reads=[btmp, bgs, bmod], writes=wh)
        if hf is not None:
            S.op("vector", lambda e, tmp=tmp, k=k: e.scalar_tensor_tensor(
                hf[:, k, c0:c0 + n], tmp[:, :n], gs[:, k, which:which + 1],
                bcast_cols(mod[:, sh_idx * 8 + k, which:which + 1], n), ALU.mult, ALU.add),
                reads=[btmp, bgs, bmod], writes=tile_bufs(bhf, c0, n))


def emit_norm(C, xs, bxs, s0, hT, bh, c0, n, gs, bgs, mod, bmod, sh_idx, which, hf=None, bhf=None):
    rs, brs = emit_norm_stats(C, xs, bxs, s0, n)
    emit_norm_apply(C, rs, brs, xs, bxs, s0, hT, bh, c0, n, gs, bgs, mod, bmod, sh_idx, which, hf, bhf)


def emit_norm_blocks(C, xs, bxs, hT, bh, blocks, gs, bgs, mod, bmod, sh_idx, hf=None, bhf=None):
    st = emit_norm_stats(C, xs, bxs, blocks[0][0], blocks[0][1])
    for i, (c0, n, which) in enumerate(blocks):
        cur = st
        if i + 1 < len(blocks):
            st = emit_norm_stats(C, xs, bxs, blocks[i + 1][0], blocks[i + 1][1])
        emit_norm_apply(C, cur[0], cur[1], xs, bxs, c0, hT, bh, c0, n, gs, bgs, mod, bmod, sh_idx, which, hf, bhf)


def emit_load_xT(C, x_d, row0, ntiles, dst, bdst, dcol0):
    S = C.S
    for i in range(ntiles):
        xt, bxt = C.xtile.next()
        S.dma("sync", xt[:], x_d[row0 + i * 128:row0 + (i + 1) * 128, :], writes=[bxt])
        c0 = dcol0 + i * 128
        for half in range(2):
            ps, bps = C.ps.next()
            for j in range(4):
                kc = half * 4 + j
                S.op("tensor", lambda e, ps=ps, j=j, kc=kc, xt=xt: e.transpose(
                    ps[:, j * 128:(j + 1) * 128], xt[:, kc * 128:(kc + 1) * 128], C.ident[:]),
                    reads=[bxt, C.bident], writes=[bps], sig=(j == 3))
            copy_op(C, evac_engine(C), dst[:, half * 4:half * 4 + 4, c0:c0 + 128],
                    ps[:].rearrange("p (j t) -> p j t", j=4), [bps], [bdst[c0 // 128]])


def load_w(C, ring, w_d, rows, c0, ncols, queue="gpsimd"):
    wt, bw = ring.next()
    kc = rows // 128
    src = w_d.rearrange("(k p) n -> p k n", p=128)[:, :, c0:c0 + ncols]
    C.S.dma(queue, wt[:, :kc, :ncols], src, writes=[bw])
    return wt, bw


def alloc_ffn(C, SL):
    S = C.S
    C.SL = SL
    C.w13 = Ring(S, "w13", [128, KC, SL * 128], BF16, 4)
    C.w2r = Ring(S, "w2", [128, SL, D], BF16, 2)
    C.hid = Ring(S, "hid", [128, SL, 512], BF16, 2)
    C.sil = Ring(S, "sil", [128, 512], F32, 3)


def emit_ffn(C, hT, bh, xT, bx, blocks, w1_d, w3_d, w2_d, dff, mod, bmod, g_idx, gate=None):
    S = C.S
    nfc = dff // 128
    SL = C.SL
    pending = [None]
    for s0 in range(0, nfc, SL):
        sl = min(SL, nfc - s0)
        w1t, bw1 = load_w(C, C.w13, w1_d, D, s0 * 128, sl * 128)
        w3t, bw3 = load_w(C, C.w13, w3_d, D, s0 * 128, sl * 128)
        w2t, bw2 = C.w2r.next()
        S.dma("gpsimd", w2t[:, :sl, :], w2_d[s0 * 128:(s0 + sl) * 128, :].rearrange("(f p) n -> p f n", p=128), writes=[bw2])
        for (c0, n, which) in blocks:
            rh = tile_bufs(bh, c0, n)
            hid, bhid = C.hid.next()
            for fi in range(sl):
                pa, bpa = C.ps.next()
                for k in range(KC):
                    S.op("tensor", lambda e, pa=pa, k=k, fi=fi, w1t=w1t, c0=c0, n=n: e.matmul(
                        pa[:, :n], w1t[:, k, fi * 128:(fi + 1) * 128], hT[:, k, c0:c0 + n], start=(k == 0), stop=(k == KC - 1)),
                        reads=rh + [bw1], writes=[bpa], sig=(k == KC - 1))
                pb, bpb = C.ps.next()
                for k in range(KC):
                    S.op("tensor", lambda e, pb=pb, k=k, fi=fi, w3t=w3t, c0=c0, n=n: e.matmul(
                        pb[:, :n], w3t[:, k, fi * 128:(fi + 1) * 128], hT[:, k, c0:c0 + n], start=(k == 0), stop=(k == KC - 1)),
                        reads=rh + [bw3], writes=[bpb], sig=(k == KC - 1))
                sa, bsa = C.sil.next()
                S.op("scalar", lambda e, sa=sa, pa=pa, n=n: e.activation(sa[:, :n], pa[:, :n], AF.Silu), reads=[bpa], writes=[bsa])
                if gate is None:
                    S.op("vector", lambda e, sa=sa, pb=pb, hid=hid, fi=fi, n=n: e.tensor_tensor(hid[:, fi, :n], pb[:, :n], sa[:, :n], ALU.mult),
                         reads=[bpb, bsa], writes=[bhid])
                else:
                    gt, bgt = gate
                    S.op("vector", lambda e, sa=sa, pb=pb, n=n: e.tensor_tensor(sa[:, :n], pb[:, :n], sa[:, :n], ALU.mult),
                         reads=[bpb, bsa], writes=[bsa])
                    S.op("gpsimd", lambda e, sa=sa, hid=hid, fi=fi, gt=gt, c0=c0, n=n: e.tensor_tensor(hid[:, fi, :n], sa[:, :n], gt[:, c0:c0 + n], ALU.mult),
                         reads=[bsa, bgt], writes=[bhid])
            def down(c0=c0, n=n, which=which, hid=hid, bhid=bhid, w2t=w2t, bw2=bw2, sl=sl):
                wx = tile_bufs(bx, c0, n)
                for dc in range(KC):
                    po, bpo = C.ps.next()
                    for fi in range(sl):
                        S.op("tensor", lambda e, po=po, fi=fi, dc=dc: e.matmul(
                            po[:, :n], w2t[:, fi, dc * 128:(dc + 1) * 128], hid[:, fi, :n], start=(fi == 0), stop=(fi == sl - 1)),
                            reads=[bhid, bw2], writes=[bpo], sig=(fi == sl - 1))
                    S.op("vector", lambda e, po=po, dc=dc: e.scalar_tensor_tensor(
                        xT[:, dc, c0:c0 + n], po[:, :n], mod[:, g_idx * 8 + dc, which:which + 1], xT[:, dc, c0:c0 + n], ALU.mult, ALU.add),
                        reads=[bpo, bmod], writes=wx)
            if pending[0] is not None:
                pending[0]()
            pending[0] = down
    if pending[0] is not None:
        pending[0]()


def emit_proj_residual(C, inT, bin_, nk, w_t, bw, xT, bx, blocks, mod, bmod, g_idx):
    S = C.S
    for (c0, n, which) in blocks:
        ri = tile_bufs(bin_, c0, n)
        wx = tile_bufs(bx, c0, n)
        for dc in range(KC):
            po, bpo = C.ps.next()
            for k in range(nk):
                S.op("tensor", lambda e, po=po, k=k, dc=dc, c0=c0, n=n: e.matmul(
                    po[:, :n], w_t[:, k, dc * 128:(dc + 1) * 128], inT[:, k, c0:c0 + n], start=(k == 0), stop=(k == nk - 1)),
                    reads=ri + [bw], writes=[bpo], sig=(k == nk - 1))
            S.op("vector", lambda e, po=po, dc=dc, c0=c0, n=n, which=which: e.scalar_tensor_tensor(
                xT[:, dc, c0:c0 + n], po[:, :n], mod[:, g_idx * 8 + dc, which:which + 1], xT[:, dc, c0:c0 + n], ALU.mult, ALU.add),
                reads=[bpo, bmod], writes=wx)


def emit_mods(C, es, ada_w, layers):
    S = C.S
    nc = C.nc
    emit_silu_c(C)
    out = {}
    for l in layers:
        out[l] = (es.enter_context(nc.sbuf_tensor(f"mod{l}", [128, 48, 2], F32)), S.buf("mod"))
    es_ada = scoped(S)
    for l in layers:
        emit_ada(C, ada_w, l, f"ada_b{l}", out[l][0], out[l][1])
    S.barrier()
    es_ada.close()
    S.es = es
    return out


LAT_BLOCKS = [(0, 512, 0), (512, 512, 0), (1024, 512, 0), (1536, 512, 0)]
CTX_BLOCK = (CTX0, NCX, 1)
HAL_BLOCK = (HAL0, 128, 0)
NSC = NT + NCX


def build_l0(phase):
    nc = bass.Bass("TRN2", target_bir_lowering=False)

    def din(name, shape, dt=F32):
        return nc.dram_tensor(name, list(shape), dt, kind="ExternalInput").ap()

    def dout(name, shape, dt=F32):
        return nc.dram_tensor(name, list(shape), dt, kind="ExternalOutput").ap()
    xall = din("xall", [TOT, D])
    vecs = din("vecs", [128, VP.n])
    ident = din("ident", [128, 128])
    ada_w = din("ada_w", [2, D, 6 * D])
    w_in = din("w_in", [D, 2 * D])
    w_a = din("w_a", [2, 8, 128, 128])
    w_i = din("w_i", [2, 8, 128, 128])
    if phase == "A":
        summ_o = dout("summ", [128, 32])
    else:
        summ_all = din("summ_all", [128, 4 * 32])
        w_out = din("w_out", [D, D])
        f_w1 = din("f_w1", [D, DFF])
        f_w3 = din("f_w3", [D, DFF])
        f_w2 = din("f_w2", [DFF, D])
        w_qkv = din("w_qkv", [D, 1536])
        cos_d = din("cos", [128, NT])
        sin_d = din("sin", [128, NT])
        rotm_d = din("rotm", [128, 128])
        x1_o = dout("x1T", [128, KC * NT])
        q_o = dout("qT", [128, 8 * NT], BF16)
        k_o = dout("kT", [128, 2 * NSC], BF16)
        v_o = dout("vtm", [NSC, 256], BF16)

    with ExitStack() as es:
        C = setup_common(nc, es, vecs, ident)
        S = C.S
        outb = S.buf("out")
        mods = emit_mods(C, es, ada_w, [0] if phase == "A" else [0, 1])
        mod0, bmod0 = mods[0]
        gs1, bgs1 = emit_gs(C, mod0, bmod0, 1, "n1g0", "gs1")
        hT = S.sbuf("hT", [128, KC, TOT], BF16)
        bh = [S.buf("hT") for _ in range(TOT // 128)]

        es1 = scoped(S)
        C.xtile = Ring(S, "xtile", [128, D], F32, 2)
        alloc_norm_tmps(C)
        xblk = Ring(S, "xblk", [128, KC, 512], F32, 2)
        for (c0, n, which) in LAT_BLOCKS + [CTX_BLOCK, HAL_BLOCK]:
            xb_, bxb_ = xblk.next()
            bl = [bxb_] * 4
            emit_load_xT(C, xall, c0, n // 128, xb_, bl, 0)
            emit_norm(C, xb_, bl, 0, hT, bh, c0, n, gs1, bgs1, mod0, bmod0, 0, which)
        S.barrier()
        es1.close()

        es_yg = scoped(S)
        if phase == "B":
            ygT = S.sbuf("ygT", [128, KC, NSC], BF16)
            byg = [S.buf("yg") for _ in range(NSC // 128)]
        es_mix = scoped(S)
        win_r = Ring(S, "win", [128, KC, 256], BF16, 2)
        wg_r = Ring(S, "wg", [128, 4, 128], BF16, 2)
        xbe = Ring(S, "xbe", [128, NT + 3 + NCX + 3], F32, 1)
        xc_r = Ring(S, "xc", [128, NSC], F32, 1)
        xcb_r = Ring(S, "xcb", [128, NSC], BF16, 1)
        r_r = Ring(S, "rr", [128, NSC], F32, 1)
        b_r = Ring(S, "bb", [128, NSC], F32, 1)
        a_r = Ring(S, "aa", [128, NSC], F32, 1)
        m_r = Ring(S, "mm", [128, NSC], F32, 1)
        y_r = Ring(S, "yy", [128, NSC], F32, 2)
        sm_r = Ring(S, "sm", [128, 8], F32, 2)
        gtmp = Ring(S, "gtmp", [128, 512], F32, 3)
        cneg = S.sbuf("cneg", [128, 32], F32)
        bcneg = S.buf("cneg")
        S.op("scalar", lambda e: e.activation(cneg[:, 0:16], V(C, "lam", 0, 16), AF.Exp, scale=-1.0), reads=[C.bvec], writes=[bcneg])
        S.op("scalar", lambda e: e.activation(cneg[:, 0:16], cneg[:, 0:16], AF.Ln, bias=1.0), reads=[bcneg], writes=[bcneg])
        S.op("vector", lambda e: e.tensor_scalar(cneg[:, 16:32], cneg[:, 0:16], -16.0, None, ALU.mult), reads=[bcneg], writes=[bcneg])
        S.op("vector", lambda e: e.tensor_scalar(cneg[:, 0:16], cneg[:, 0:16], -8.0, None, ALU.mult), reads=[bcneg], writes=[bcneg])
        if phase == "A":
            summ = S.sbuf("summ", [128, 32], F32)
            bsumm = S.buf("summ")
        else:
            sall = S.sbuf("sall", [128, 4 * 32], F32)
            bsall = S.buf("sall")
            S.dma("sync", sall[:], summ_all, writes=[bsall])
        LB = NT + 3
        w_in_v = w_in.rearrange("(k p) n -> p k n", p=128)
        for ct in range(KC):
            wt, bw = win_r.next()
            S.dma("gpsimd", wt[:, :, 0:128], w_in_v[:, :, ct * 128:(ct + 1) * 128], writes=[bw])
            S.dma("gpsimd", wt[:, :, 128:256], w_in_v[:, :, D + ct * 128:D + (ct + 1) * 128], writes=[bw])
            wg, bwg = wg_r.next()
            for d in range(2):
                S.dma("gpsimd", wg[:, d, :], w_a[d, ct], writes=[bwg])
                S.dma("gpsimd", wg[:, 2 + d, :], w_i[d, ct], writes=[bwg])
            xe, bxe = xbe.next()
            S.op("gpsimd", lambda e, xe=xe: e.memset(xe[:, LB:LB + 2], 0.0), writes=[bxe])
            S.op("gpsimd", lambda e, xe=xe: e.memset(xe[:, LB + 2 + NCX:LB + 3 + NCX], 0.0), writes=[bxe])
            for (c0, n, which) in LAT_BLOCKS + [CTX_BLOCK, (HAL0, 3, 0)]:
                ps, bps = C.ps.next()
                for k in range(KC):
                    S.op("tensor", lambda e, ps=ps, k=k, wt=wt, c0=c0, n=n: e.matmul(
                        ps[:, :n], wt[:, k, 0:128], hT[:, k, c0:c0 + n], start=(k == 0), stop=(k == KC - 1)),
                        reads=tile_bufs(bh, c0, n) + [bw], writes=[bps], sig=(k == KC - 1))
                if c0 < CTX0:
                    copy_op(C, evac_engine(C), xe[:, 2 + c0:2 + c0 + n], ps[:, :n], [bps], [bxe])
                elif c0 == CTX0:
                    copy_op(C, evac_engine(C), xe[:, LB + 2:LB + 2 + NCX], ps[:, :n], [bps], [bxe])
                else:
                    S.op("vector", lambda e, ps=ps, xe=xe: e.tensor_tensor(xe[:, 0:2], ps[:, 0:2], V(C, "hmask", 0, 2), ALU.mult),
                         reads=[bps, C.bvec], writes=[bxe])
                    S.op("vector", lambda e, ps=ps, xe=xe: e.tensor_tensor(xe[:, 2 + NT:3 + NT], ps[:, 2:3], V(C, "hmask", 2, 1), ALU.mult),
                         reads=[bps, C.bvec], writes=[bxe])
            xc, bxc = xc_r.next()
            for (dst0, src0, n) in [(0, 0, NT), (NT, LB, NCX)]:
                S.op("scalar", lambda e, xc=xc, xe=xe, dst0=dst0, src0=src0, n=n, ct=ct: e.activation(
                    xc[:, dst0:dst0 + n], xe[:, src0:src0 + n], AF.Identity,
                    bias=V(C, "convb", ct), scale=V(C, "convw", ct)), reads=[bxe, C.bvec], writes=[bxc])
                for k in range(1, 4):
                    S.op("vector", lambda e, xc=xc, xe=xe, dst0=dst0, src0=src0, n=n, k=k, ct=ct: e.scalar_tensor_tensor(
                        xc[:, dst0:dst0 + n], xe[:, src0 + k:src0 + k + n], V(C, "convw", k * 8 + ct), xc[:, dst0:dst0 + n],
                        ALU.mult, ALU.add), reads=[bxe, C.bvec, bxc], writes=[bxc])
            xcb, bxcb = xcb_r.next()
            S.op("gpsimd", lambda e, xcb=xcb, xc=xc: e.tensor_copy(xcb[:], xc[:]), reads=[bxc], writes=[bxcb])
            ys = []
            for d in range(2):
                rr, brr = r_r.next()
                bb, bbb = b_r.next()
                for (c0, n) in [(0, 512), (512, 512), (1024, 512), (1536, 512), (NT, NCX)]:
                    pr, bpr = C.ps.next()
                    S.op("tensor", lambda e, pr=pr, wg=wg, d=d, xcb=xcb, c0=c0, n=n: e.matmul(
                        pr[:, :n], wg[:, d, :], xcb[:, c0:c0 + n], start=True, stop=True), reads=[bwg, bxcb], writes=[bpr])
                    S.op("scalar", lambda e, pr=pr, rr=rr, c0=c0, n=n, d=d, ct=ct: e.activation(
                        rr[:, c0:c0 + n], pr[:, :n], AF.Sigmoid, bias=V(C, "b_a", d * 8 + ct)), reads=[bpr, C.bvec], writes=[brr])
                    pi, bpi = C.ps.next()
                    S.op("tensor", lambda e, pi=pi, wg=wg, d=d, xcb=xcb, c0=c0, n=n: e.matmul(
                        pi[:, :n], wg[:, 2 + d, :], xcb[:, c0:c0 + n], start=True, stop=True), reads=[bwg, bxcb], writes=[bpi])
                    S.op("scalar", lambda e, pi=pi, bb=bb, c0=c0, n=n, d=d, ct=ct: e.activation(
                        bb[:, c0:c0 + n], pi[:, :n], AF.Sigmoid, bias=V(C, "b_i", d * 8 + ct)), reads=[bpi, C.bvec], writes=[bbb])
                aa, baa = a_r.next()
                mm, bmm = m_r.next()
                cn = d * 8 + ct
                S.op("scalar", lambda e, aa=aa, rr=rr, cn=cn: e.activation(aa[:], rr[:], AF.Exp, scale=cneg[:, cn:cn + 1]),
                     reads=[brr, bcneg], writes=[baa])
                S.op("scalar", lambda e, mm=mm, rr=rr, cn=cn: e.activation(mm[:], rr[:], AF.Exp, scale=cneg[:, 16 + cn:17 + cn]),
                     reads=[brr, bcneg], writes=[bmm])
                S.op("scalar", lambda e, mm=mm: e.activation(mm[:], mm[:], AF.Sqrt, bias=1.0, scale=-1.0), reads=[bmm], writes=[bmm])
                S.op("gpsimd", lambda e, bb=bb, xc=xc: e.tensor_tensor(bb[:], bb[:], xc[:], ALU.mult), reads=[bbb, bxc], writes=[bbb])
                S.op("vector", lambda e, bb=bb, mm=mm: e.tensor_tensor(bb[:], bb[:], mm[:], ALU.mult), reads=[bbb, bmm], writes=[bbb])
                yy, byy = y_r.next()
                sm, bsm = sm_r.next()
                if phase == "A":
                    if d == 0:
                        S.op("vector", lambda e, yy=yy, aa=aa, bb=bb: e.tensor_tensor_scan(
                            yy[:, 0:NT], aa[:, 0:NT], bb[:, 0:NT], 0.0, ALU.mult, ALU.add), reads=[baa, bbb], writes=[byy])
                        hend = yy[:, NT - 1:NT]
                    else:
                        S.op("vector", lambda e, yy=yy, aa=aa, bb=bb: e.tensor_tensor_scan(
                            rev_ap(yy[:, 0:NT]), rev_ap(aa[:, 0:NT]), rev_ap(bb[:, 0:NT]), 0.0, ALU.mult, ALU.add),
                            reads=[baa, bbb], writes=[byy])
                        hend = yy[:, 0:1]
                    S.op("vector", lambda e, sm=sm, rr=rr: e.tensor_reduce(sm[:, 0:1], rr[:, 0:NT], AX.X, ALU.add), reads=[brr], writes=[bsm])
                    o = ct * 4 + d * 2
                    S.op("scalar", lambda e, sm=sm, cn=cn, o=o: e.activation(summ[:, o:o + 1], sm[:, 0:1], AF.Exp, scale=cneg[:, cn:cn + 1]),
                         reads=[bsm, bcneg], writes=[bsumm])
                    S.op("vector", lambda e, hend=hend, o=o: e.tensor_copy(summ[:, o + 1:o + 2], hend), reads=[byy], writes=[bsumm])
                else:
                    if d == 0:
                        S.op("vector", lambda e, yy=yy, aa=aa, bb=bb: e.tensor_tensor_scan(
                            yy[:, NT:NSC], aa[:, NT:NSC], bb[:, NT:NSC], 0.0, ALU.mult, ALU.add), reads=[baa, bbb], writes=[byy])
                        st0 = yy[:, NSC - 1:NSC]
                        order = [0, 1, 2, 3]
                        sel = "sel_f"
                    else:
                        S.op("vector", lambda e, yy=yy, aa=aa, bb=bb: e.tensor_tensor_scan(
                            rev_ap(yy[:, NT:NSC]), rev_ap(aa[:, NT:NSC]), rev_ap(bb[:, NT:NSC]), 0.0, ALU.mult, ALU.add),
                            reads=[baa, bbb], writes=[byy])
                        st0 = yy[:, NT:NT + 1]
                        order = [3, 2, 1, 0]
                        sel = "sel_r"
                    S.op("vector", lambda e, sm=sm, st0=st0: e.tensor_copy(sm[:, 0:1], st0), reads=[byy], writes=[bsm])
                    for i in order:
                        o = i * 32 + ct * 4 + d * 2
                        S.op("vector", lambda e, sm=sm, o=o: e.scalar_tensor_tensor(
                            sm[:, 1:2], sm[:, 0:1], sall[:, o:o + 1], sall[:, o + 1:o + 2], ALU.mult, ALU.add), reads=[bsm, bsall], writes=[bsm])
                        S.op("vector", lambda e, sm=sm: e.tensor_tensor(sm[:, 1:2], sm[:, 1:2], sm[:, 0:1], ALU.subtract), reads=[bsm], writes=[bsm])
                        S.op("vector", lambda e, sm=sm, i=i, sel=sel: e.scalar_tensor_tensor(
                            sm[:, 0:1], sm[:, 1:2], V(C, sel, i), sm[:, 0:1], ALU.mult, ALU.add), reads=[bsm, C.bvec], writes=[bsm])
                    if d == 0:
                        S.op("vector", lambda e, yy=yy, aa=aa, bb=bb, sm=sm: e.tensor_tensor_scan(
                            yy[:, 0:NT], aa[:, 0:NT], bb[:, 0:NT], sm[:, 0:1], ALU.mult, ALU.add), reads=[baa, bbb, bsm], writes=[byy])
                    else:
                        S.op("vector", lambda e, yy=yy, aa=aa, bb=bb, sm=sm: e.tensor_tensor_scan(
                            rev_ap(yy[:, 0:NT]), rev_ap(aa[:, 0:NT]), rev_ap(bb[:, 0:NT]), sm[:, 0:1], ALU.mult, ALU.add),
                            reads=[baa, bbb, bsm], writes=[byy])
                    ys.append((yy, byy))
            if phase == "B":
                (y0, by0), (y1, by1) = ys
                S.op("gpsimd", lambda e, y0=y0, y1=y1: e.tensor_tensor(y0[:], y0[:], y1[:], ALU.add), reads=[by0, by1], writes=[by0])
                for (c0, n, which) in LAT_BLOCKS + [CTX_BLOCK]:
                    pg, bpg = C.ps.next()
                    for k in range(KC):
                        S.op("tensor", lambda e, pg=pg, k=k, wt=wt, c0=c0, n=n: e.matmul(
                            pg[:, :n], wt[:, k, 128:256], hT[:, k, c0:c0 + n], start=(k == 0), stop=(k == KC - 1)),
                            reads=tile_bufs(bh, c0, n) + [bw], writes=[bpg], sig=(k == KC - 1))
                    t1, bt1 = gtmp.next()
                    S.op("scalar", lambda e, t1=t1, pg=pg, n=n: e.activation(t1[:, :n], pg[:, :n], AF.Gelu_apprx_tanh), reads=[bpg], writes=[bt1])
                    S.op("vector", lambda e, t1=t1, y0=y0, c0=c0, n=n, ct=ct: e.tensor_tensor(ygT[:, ct, c0:c0 + n], t1[:, :n], y0[:, c0:c0 + n], ALU.mult),
                         reads=[bt1, by0], writes=tile_bufs(byg, c0, n))
        if phase == "A":
            S.dma("sync", summ_o, summ[:], reads=[bsumm], out_sem_buf=outb)
            S.finish()
            S.emit()
            es_mix.close()
            es_yg.close()
            return nc
        S.barrier()
        es_mix.close()

        S.es = es
        xT = S.sbuf("xT", [128, KC, NSC], F32, side="right")
        bx = [S.buf("xT") for _ in range(NSC // 128)]
        es_o = scoped(S)
        C.xtile = Ring(S, "xtile", [128, D], F32, 2)
        emit_load_xT(C, xall, 0, NSC // 128, xT, bx, 0)
        wo_r = Ring(S, "wo", [128, KC, D], BF16, 1)
        wot, bwo = load_w(C, wo_r, w_out, D, 0, D)
        BL = LAT_BLOCKS + [CTX_BLOCK]
        emit_proj_residual(C, ygT, byg, KC, wot, bwo, xT, bx, BL, mod0, bmod0, 2)
        S.barrier()
        es_o.close()
        es_yg.close()

        es_f = scoped(S)
        gs2, bgs2 = emit_gs(C, mod0, bmod0, 4, "n2g0", "gs2")
        alloc_norm_tmps(C)
        for (c0, n, which) in BL:
            emit_norm(C, xT, bx, c0, hT, bh, c0, n, gs2, bgs2, mod0, bmod0, 3, which)
        alloc_ffn(C, 4)
        emit_ffn(C, hT, bh, xT, bx, BL, f_w1, f_w3, f_w2, DFF, mod0, bmod0, 5)
        for k in range(KC):
            S.dma("sync", x1_o[:, k * NT:(k + 1) * NT], xT[:, k, 0:NT], reads=bx[0:NT // 128], out_sem_buf=outb)
        mod1, bmod1 = mods[1]
        gs3, bgs3 = emit_gs(C, mod1, bmod1, 1, "n1g1", "gs3")
        for (c0, n, which) in BL:
            emit_norm(C, xT, bx, c0, hT, bh, c0, n, gs3, bgs3, mod1, bmod1, 0, which)
        S.barrier()
        es_f.close()

        es_q = scoped(S)
        wq_r = Ring(S, "wq", [128, KC, 1536], BF16, 1)
        wqt, bwq = load_w(C, wq_r, w_qkv, D, 0, 1536)
        cosT = S.sbuf("cosT", [128, NT], F32)
        sinT = S.sbuf("sinT", [128, NT], F32)
        bcs = S.buf("cs")
        S.dma("sync", cosT[:], cos_d, writes=[bcs])
        S.dma("sync", sinT[:], sin_d, writes=[bcs])
        rotm = S.sbuf("rotm", [128, 128], F32)
        rotb = S.sbuf("rotb", [128, 128], BF16)
        brot = S.buf("rot")
        S.dma("sync", rotm[:], rotm_d, writes=[brot])
        S.op("vector", lambda e: e.tensor_copy(rotb[:], rotm[:]), reads=[brot], writes=[brot])
        C.rstd = Ring(S, "rstd2", [128, 512], F32, 3)
        qst = Ring(S, "qst", [128, 512], BF16, 3)
        qg = Ring(S, "qg", [128, 512], F32, 2)
        qgb = Ring(S, "qgb", [128, 512], BF16, 2)
        qsq = Ring(S, "qsq", [128, 512], BF16, 2)
        qt1 = Ring(S, "qt1", [128, 512], F32, 2)
        vst = Ring(S, "vst", [128, 256], BF16, 2)
        for (c0, n, which) in BL:
            rh = tile_bufs(bh, c0, n)
            for hd in range(10):
                if hd < 8 and which == 1:
                    continue
                gname = "q_g" if hd < 8 else "k_g"
                pq, bpq = C.ps.next()
                for k in range(KC):
                    S.op("tensor", lambda e, pq=pq, k=k, hd=hd, c0=c0, n=n: e.matmul(
                        pq[:, :n], wqt[:, k, hd * 128:(hd + 1) * 128], hT[:, k, c0:c0 + n], start=(k == 0), stop=(k == KC - 1)),
                        reads=rh + [bwq], writes=[bpq], sig=(k == KC - 1))
                sqt, bsqt = qsq.next()
                S.op("scalar", lambda e, sqt=sqt, pq=pq, n=n: e.activation(sqt[:, :n], pq[:, :n], AF.Square), reads=[bpq], writes=[bsqt])
                pss, bpss = C.ps.next()
                S.op("tensor", lambda e, pss=pss, sqt=sqt, n=n: e.matmul(pss[:, :n], C.ones[:], sqt[:, :n], start=True, stop=True),
                     reads=[bsqt, C.bones], writes=[bpss])
                rs, brs = C.rstd.next()
                S.op("scalar", lambda e, rs=rs, pss=pss, n=n: e.activation(rs[:, :n], pss[:, :n], AF.Sqrt, bias=C.epsb[:, 0:1], scale=1.0 / 128),
                     reads=[bpss, C.bepsb], writes=[brs])
                S.op("vector", lambda e, rs=rs, n=n: e.reciprocal(rs[:, :n], rs[:, :n]), reads=[brs], writes=[brs])
                qn, bqn = qg.next()
                S.op("vector", lambda e, qn=qn, pq=pq, rs=rs, gname=gname, n=n: e.scalar_tensor_tensor(
                    qn[:, :n], pq[:, :n], V(C, gname, 0), rs[:, :n], ALU.mult, ALU.mult), reads=[bpq, C.bvec, brs], writes=[bqn])
                qo, bqo = qst.next()
                if which == 0:
                    qb, bqb = qgb.next()
                    S.op("gpsimd", lambda e, qb=qb, qn=qn, n=n: e.tensor_copy(qb[:, :n], qn[:, :n]), reads=[bqn], writes=[bqb])
                    pr, bpr = C.ps.next()
                    S.op("tensor", lambda e, pr=pr, qb=qb, n=n: e.matmul(pr[:, :n], rotb[:], qb[:, :n], start=True, stop=True),
                         reads=[bqb, brot], writes=[bpr])
                    t1, bt1 = qt1.next()
                    S.op("vector", lambda e, t1=t1, pr=pr, c0=c0, n=n: e.tensor_tensor(t1[:, :n], pr[:, :n], sinT[:, c0:c0 + n], ALU.mult), reads=[bpr, bcs], writes=[bt1])
                    S.op("gpsimd", lambda e, qn=qn, c0=c0, n=n: e.tensor_tensor(qn[:, :n], qn[:, :n], cosT[:, c0:c0 + n], ALU.mult), reads=[bqn, bcs], writes=[bqn])
                    S.op("vector", lambda e, qo=qo, qn=qn, t1=t1, n=n: e.tensor_tensor(qo[:, :n], qn[:, :n], t1[:, :n], ALU.add), reads=[bqn, bt1], writes=[bqo])
                else:
                    S.op("vector", lambda e, qo=qo, qn=qn, n=n: e.tensor_copy(qo[:, :n], qn[:, :n]), reads=[bqn], writes=[bqo])
                if hd < 8:
                    S.dma("sync", q_o[:, hd * NT + c0:hd * NT + c0 + n], qo[:, :n], reads=[bqo], out_sem_buf=outb)
                else:
                    S.dma("sync", k_o[:, (hd - 8) * NSC + c0:(hd - 8) * NSC + c0 + n], qo[:, :n], reads=[bqo], out_sem_buf=outb)
            for t0 in range(0, n, 128):
                pv, bpv = C.ps.next()
                for k in range(KC):
                    S.op("tensor", lambda e, pv=pv, k=k, t0=t0, c0=c0: e.matmul(
                        pv[:, 0:256], hT[:, k, c0 + t0:c0 + t0 + 128], wqt[:, k, 1280:1536], start=(k == 0), stop=(k == KC - 1)),
                        reads=rh + [bwq], writes=[bpv], sig=(k == KC - 1))
                vo, bvo = vst.next()
                copy_op(C, evac_engine(C), vo[:], pv[:, 0:256], [bpv], [bvo])
                S.dma("sync", v_o[c0 + t0:c0 + t0 + 128, :], vo[:], reads=[bvo], out_sem_buf=outb)
        S.finish()
        S.emit()
        es_q.close()
    return nc


def build_l1():
    nc = bass.Bass("TRN2", target_bir_lowering=False)

    def din(name, shape, dt=F32):
        return nc.dram_tensor(name, list(shape), dt, kind="ExternalInput").ap()

    def dout(name, shape, dt=F32):
        return nc.dram_tensor(name, list(shape), dt, kind="ExternalOutput").ap()
    vecs = din("vecs", [128, VP.n])
    ident = din("ident", [128, 128])
    ada_w = din("ada_w", [2, D, 6 * D])
    x1_d = din("x1T", [128, KC * NT])
    q_d = din("qT", [128, 8 * NT], BF16)
    k_d = din("kT", [128, 2 * NKEY], BF16)
    v_d = din("vtm", [NKEY, 256], BF16)
    w_o = din("w_o", [D, D])
    router = din("router", [D, NE])
    m_w1 = din("m_w1", [NE, D, DFE])
    m_w3 = din("m_w3", [NE, D, DFE])
    m_w2 = din("m_w2", [NE, DFE, D])
    fin_g = din("fin_g", [D])
    out_d = dout("out", [NT, D])

    with ExitStack() as es:
        C = setup_common(nc, es, vecs, ident, ps_ring=False)
        S = C.S
        outb = S.buf("out")
        identb = S.sbuf("identb", [128, 128], BF16)
        bidb = S.buf("identb")
        S.op("vector", lambda e: e.tensor_copy(identb[:], C.ident[:]), reads=[C.bident], writes=[bidb])
        es_oT = scoped(S)
        oT = S.sbuf("oT", [128, 8, NT], BF16, side="right")
        boT = [S.buf("oT") for _ in range(NT // 128)]
        S.es = es

        es_a = scoped(S)
        psS = Ring(S, "psS", [128, 512], F32, 3, psum=True)
        psO = Ring(S, "psO", [128, 512], F32, 4, psum=True)
        psT = Ring(S, "psT", [128, 1024], BF16, 1, psum=True)
        kT = S.sbuf("kT", [128, 2, NKEY], BF16)
        bk = S.buf("kT")
        S.dma("sync", kT[:], k_d.rearrange("p (g s) -> p g s", g=2), writes=[bk])
        va = S.sbuf("va", [128, NKT, 2, 130], BF16)
        bva = S.buf("va")
        S.op("vector", lambda e: e.memset(va[:, :, :, 128:130], 1.0), writes=[bva])
        vsrc = v_d.rearrange("(t p) (g d) -> p t g d", p=128, g=2)
        for g in range(2):
            S.dma("sync", va[:, :, g, 0:128], vsrc[:, :, g, :], writes=[bva])
        qT = S.sbuf("qT", [128, 8, NT], BF16)
        bq = S.buf("qT")
        S.dma("sync", qT[:], q_d.rearrange("p (h t) -> p h t", h=8), writes=[bq])
        pT_r = Ring(S, "pT", [128, 512], BF16, 3)
        on_r = Ring(S, "on", [128, 128], BF16, 2)
        ri_r = Ring(S, "ri", [128, 2], F32, 2)
        SCL = 1.0 / float(np.sqrt(128.0))
        for qb in range(NT // 512):
            for h in range(8):
                g = h // 4
                po = [psO.next(), psO.next()]
                for kt in range(NKT):
                    ps, bps = psS.next()
                    S.op("tensor", lambda e, ps=ps, kt=kt, g=g, h=h, qb=qb: e.matmul(
                        ps[:], kT[:, g, kt * 128:(kt + 1) * 128], qT[:, h, qb * 512:(qb + 1) * 512], start=True, stop=True),
                        reads=[bk, bq], writes=[bps])
                    pT, bpT = pT_r.next()
                    S.op("scalar", lambda e, pT=pT, ps=ps: e.activation(pT[:], ps[:], AF.Exp, scale=SCL), reads=[bps], writes=[bpT])
                    for qt in range(4):
                        pot, bpot = po[qt // 2]
                        c = (qt % 2) * 129
                        S.op("tensor", lambda e, pot=pot, c=c, pT=pT, qt=qt, kt=kt, g=g: e.matmul(
                            pot[:, c:c + 129], pT[:, qt * 128:(qt + 1) * 128], va[:, kt, g, 0:129], start=(kt == 0 and qt % 2 == 0), stop=(kt == NKT - 1),
                            skip_group_check=True),
                            reads=[bpT, bva], writes=[bpot], sig=(qt == 3))
                for qt in range(4):
                    pot, bpot = po[qt // 2]
                    c = (qt % 2) * 129
                    ri, bri = ri_r.next()
                    S.op("vector", lambda e, ri=ri, pot=pot, c=c: e.reciprocal(ri[:, 0:1], pot[:, c + 128:c + 129]), reads=[bpot], writes=[bri])
                    on, bon = on_r.next()
                    S.op("vector", lambda e, on=on, pot=pot, c=c, ri=ri: e.tensor_scalar(on[:], pot[:, c:c + 128], ri[:, 0:1], None, ALU.mult),
                         reads=[bpot, bri], writes=[bon])
                    pt, bpt = psT.next()
                    S.op("tensor", lambda e, pt=pt, on=on: e.transpose(pt[:, 0:128], on[:], identb[:]), reads=[bon, bidb], writes=[bpt])
                    col = qb * 512 + qt * 128
                    S.op("scalar", lambda e, pt=pt, h=h, col=col: e.copy(oT[:, h, col:col + 128], pt[:, 0:128]), reads=[bpt], writes=[boT[col // 128]])
        S.barrier()
        es_a.close()

        S.es = es
        C.ps = Ring(S, "ps", [128, 512], F32, 8, psum=True)
        mods = emit_mods(C, es, ada_w, [1])
        mod1, bmod1 = mods[1]
        xT = S.sbuf("xT", [128, KC, NT], F32)
        bx = [S.buf("xT") for _ in range(NT // 128)]
        for k in range(KC):
            S.dma("sync", xT[:, k, :], x1_d[:, k * NT:(k + 1) * NT], writes=bx)
        hT = S.sbuf("hT", [128, KC, NT], BF16)
        bh = [S.buf("hT") for _ in range(NT // 128)]
        es_o = scoped(S)
        wo_r = Ring(S, "wo", [128, KC, D], BF16, 1)
        wot, bwo = load_w(C, wo_r, w_o, D, 0, D)
        emit_proj_residual(C, oT, boT, KC, wot, bwo, xT, bx, LAT_BLOCKS, mod1, bmod1, 2)
        S.barrier()
        es_o.close()
        es_oT.close()

        gates = es.enter_context(nc.sbuf_tensor("gates", [128, NT // 128, NE], F32))
        bgates = S.buf("gates")
        es_r = scoped(S)
        gs2, bgs2 = emit_gs(C, mod1, bmod1, 4, "n2g1", "gs2")
        alloc_norm_tmps(C)
        hf = S.sbuf("hf", [128, KC, NT], F32)
        bhf = [S.buf("hf") for _ in range(NT // 128)]
        for (c0, n, which) in LAT_BLOCKS:
            emit_norm(C, xT, bx, c0, hT, bh, c0, n, gs2, bgs2, mod1, bmod1, 3, which, hf=hf, bhf=bhf)
        rw = S.sbuf("rw", [128, KC, NE], F32)
        brw = S.buf("rw")
        S.dma("sync", rw[:], router.rearrange("(k p) e -> p k e", p=128), writes=[brw])
        lg_r = Ring(S, "lg", [128, 32], F32, 2)
        for t in range(NT // 128):
            pl, bpl = C.ps.next()
            for k in range(KC):
                S.op("tensor", lambda e, pl=pl, k=k, t=t: e.matmul(
                    pl[:, 0:NE], hf[:, k, t * 128:(t + 1) * 128], rw[:, k, :], start=(k == 0), stop=(k == KC - 1)),
                    reads=[bhf[t], brw], writes=[bpl], sig=(k == KC - 1))
            lg, blg = lg_r.next()
            S.op("vector", lambda e, lg=lg, pl=pl: e.tensor_copy(lg[:, 0:8], pl[:, 0:8]), reads=[bpl], writes=[blg])
            S.op("vector", lambda e, lg=lg: e.max(lg[:, 8:16], lg[:, 0:8]), reads=[blg], writes=[blg])
            S.op("vector", lambda e, lg=lg: e.tensor_scalar(lg[:, 16:24], lg[:, 0:8], lg[:, 8:9], None, ALU.subtract), reads=[blg], writes=[blg])
            S.op("scalar", lambda e, lg=lg: e.activation(lg[:, 16:24], lg[:, 16:24], AF.Exp), reads=[blg], writes=[blg])
            S.op("vector", lambda e, lg=lg: e.tensor_tensor(lg[:, 24:25], lg[:, 9:10], lg[:, 8:9], ALU.subtract), reads=[blg], writes=[blg])
            S.op("scalar", lambda e, lg=lg: e.activation(lg[:, 24:25], lg[:, 24:25], AF.Exp), reads=[blg], writes=[blg])
            S.op("vector", lambda e, lg=lg: e.tensor_scalar(lg[:, 24:25], lg[:, 24:25], 1.0, None, ALU.add), reads=[blg], writes=[blg])
            S.op("vector", lambda e, lg=lg: e.reciprocal(lg[:, 24:25], lg[:, 24:25]), reads=[blg], writes=[blg])
            S.op("vector", lambda e, lg=lg: e.tensor_scalar(lg[:, 0:8], lg[:, 0:8], lg[:, 9:10], None, ALU.is_ge), reads=[blg], writes=[blg])
            S.op("vector", lambda e, lg=lg: e.tensor_tensor(lg[:, 0:8], lg[:, 0:8], lg[:, 16:24], ALU.mult), reads=[blg], writes=[blg])
            S.op("vector", lambda e, lg=lg, t=t: e.tensor_scalar(gates[:, t, :], lg[:, 0:8], lg[:, 24:25], None, ALU.mult), reads=[blg], writes=[bgates])
        S.barrier()
        es_r.close()

        es_m = scoped(S)
        alloc_ffn(C, 4)
        gB_r = Ring(S, "gB", [128, NT], F32, 2)
        gl_r = Ring(S, "gl", [128, 128], F32, 2)
        for ex in range(NE):
            gB, bgB = gB_r.next()
            for t4 in range(NT // 512):
                pg, bpg = C.ps.next()
                for j in range(4):
                    t = t4 * 4 + j
                    gl, bgl = gl_r.next()
                    S.op("gpsimd", lambda e, gl=gl, t=t, ex=ex: e.tensor_copy(gl[:], bcast_cols(gates[:, t, ex:ex + 1], 128)), reads=[bgates], writes=[bgl])
                    S.op("tensor", lambda e, pg=pg, gl=gl, j=j: e.matmul(pg[:, j * 128:(j + 1) * 128], gl[:], C.ident[:], start=True, stop=True),
                         reads=[bgl, C.bident], writes=[bpg])
                copy_op(C, "scalar", gB[:, t4 * 512:(t4 + 1) * 512], pg[:], [bpg], [bgB])
            emit_ffn(C, hT, bh, xT, bx, LAT_BLOCKS, m_w1[ex], m_w3[ex], m_w2[ex], DFE, mod1, bmod1, 5, gate=(gB, bgB))
        S.barrier()
        es_m.close()

        es_z = scoped(S)
        gfin = S.sbuf("gfin", [128, D], F32)
        bgf = S.buf("gfin")
        S.dma("sync", gfin[:], bass.AP(fin_g.tensor, fin_g.offset, [[0, 128], [1, D]]), writes=[bgf])
        ot_r = Ring(S, "ot", [128, D], F32, 2)
        sq_r = Ring(S, "fsq", [128, 512], F32, 2)
        ss_r = Ring(S, "fss", [128, 4], F32, 2)
        for t in range(NT // 128):
            pp = [C.ps.next(), C.ps.next()]
            for half in range(2):
                ph, bph = pp[half]
                for j in range(4):
                    kc = half * 4 + j
                    S.op("tensor", lambda e, ph=ph, j=j, kc=kc, t=t: e.transpose(
                        ph[:, j * 128:(j + 1) * 128], xT[:, kc, t * 128:(t + 1) * 128], C.ident[:]),
                        reads=[bx[t], C.bident], writes=[bph], sig=(j == 3))
            ss, bss = ss_r.next()
            S.op("vector", lambda e, ss=ss: e.memset(ss[:], 0.0), writes=[bss])
            for half in range(2):
                ph, bph = pp[half]
                sq, bsq = sq_r.next()
                S.op("scalar", lambda e, sq=sq, ph=ph, ss=ss, half=half: e.activation(sq[:], ph[:], AF.Square, accum_out=ss[:, half:half + 1]),
                     reads=[bph], writes=[bsq, bss])
            S.op("vector", lambda e, ss=ss: e.tensor_tensor(ss[:, 2:3], ss[:, 0:1], ss[:, 1:2], ALU.add), reads=[bss], writes=[bss])
            S.op("scalar", lambda e, ss=ss: e.activation(ss[:, 2:3], ss[:, 2:3], AF.Sqrt, bias=C.epsb[:, 0:1], scale=1.0 / D), reads=[bss, C.bepsb], writes=[bss])
            S.op("vector", lambda e, ss=ss: e.reciprocal(ss[:, 3:4], ss[:, 2:3]), reads=[bss], writes=[bss])
            ot, bot = ot_r.next()
            for half in range(2):
                ph, bph = pp[half]
                S.op("vector", lambda e, ot=ot, ph=ph, ss=ss, half=half: e.scalar_tensor_tensor(
                    ot[:, half * 512:(half + 1) * 512], ph[:], ss[:, 3:4], gfin[:, half * 512:(half + 1) * 512], ALU.mult, ALU.mult),
                    reads=[bph, bss, bgf], writes=[bot])
            S.dma("sync", out_d[t * 128:(t + 1) * 128, :], ot[:], reads=[bot], out_sem_buf=outb)
        S.finish()
        S.emit()
        es_z.close()
    return nc


def emit_router(C, hf, bhf, router, gates, bgates, maskt=None):
    S = C.S
    rw = S.sbuf("rw", [128, KC, NE], F32)
    brw = S.buf("rw")
    S.dma("sync", rw[:], router.rearrange("(k p) e -> p k e", p=128), writes=[brw])
    lg_r = Ring(S, "lg", [128, 32], F32, 2)
    for t in range(NT // 128):
        pl, bpl = C.ps.next()
        for k in range(KC):
            S.op("tensor", lambda e, pl=pl, k=k, t=t: e.matmul(
                pl[:, 0:NE], hf[:, k, t * 128:(t + 1) * 128], rw[:, k, :], start=(k == 0), stop=(k == KC - 1)),
                reads=[bhf[t], brw], writes=[bpl], sig=(k == KC - 1))
        lg, blg = lg_r.next()
        S.op("vector", lambda e, lg=lg, pl=pl: e.tensor_copy(lg[:, 0:8], pl[:, 0:8]), reads=[bpl], writes=[blg])
        S.op("vector", lambda e, lg=lg: e.max(lg[:, 8:16], lg[:, 0:8]), reads=[blg], writes=[blg])
        S.op("vector", lambda e, lg=lg: e.tensor_scalar(lg[:, 16:24], lg[:, 0:8], lg[:, 8:9], None, ALU.subtract), reads=[blg], writes=[blg])
        S.op("scalar", lambda e, lg=lg: e.activation(lg[:, 16:24], lg[:, 16:24], AF.Exp), reads=[blg], writes=[blg])
        S.op("vector", lambda e, lg=lg: e.tensor_tensor(lg[:, 24:25], lg[:, 9:10], lg[:, 8:9], ALU.subtract), reads=[blg], writes=[blg])
        S.op("scalar", lambda e, lg=lg: e.activation(lg[:, 24:25], lg[:, 24:25], AF.Exp), reads=[blg], writes=[blg])
        S.op("vector", lambda e, lg=lg: e.tensor_scalar(lg[:, 24:25], lg[:, 24:25], 1.0, None, ALU.add), reads=[blg], writes=[blg])
        S.op("vector", lambda e, lg=lg: e.reciprocal(lg[:, 24:25], lg[:, 24:25]), reads=[blg], writes=[blg])
        S.op("vector", lambda e, lg=lg: e.tensor_scalar(lg[:, 0:8], lg[:, 0:8], lg[:, 9:10], None, ALU.is_ge), reads=[blg], writes=[blg])
        if maskt is not None:
            S.op("vector", lambda e, lg=lg, t=t: e.tensor_copy(maskt[:, t, :], lg[:, 0:8]), reads=[blg], writes=[bgates])
        S.op("vector", lambda e, lg=lg: e.tensor_tensor(lg[:, 0:8], lg[:, 0:8], lg[:, 16:24], ALU.mult), reads=[blg], writes=[blg])
        S.op("vector", lambda e, lg=lg, t=t: e.tensor_scalar(gates[:, t, :], lg[:, 0:8], lg[:, 24:25], None, ALU.mult), reads=[blg], writes=[bgates])


def emit_final(C, xT, bx, fin_g, out_d, outb):
    S = C.S
    gfin = S.sbuf("gfin", [128, D], F32)
    bgf = S.buf("gfin")
    S.dma("sync", gfin[:], bass.AP(fin_g.tensor, fin_g.offset, [[0, 128], [1, D]]), writes=[bgf])
    ot_r = Ring(S, "ot", [128, D], F32, 2)
    sq_r = Ring(S, "fsq", [128, 512], F32, 2)
    ss_r = Ring(S, "fss", [128, 4], F32, 2)
    for t in range(NT // 128):
        pp = [C.ps.next(), C.ps.next()]
        for half in range(2):
            ph, bph = pp[half]
            for j in range(4):
                kc = half * 4 + j
                S.op("tensor", lambda e, ph=ph, j=j, kc=kc, t=t: e.transpose(
                    ph[:, j * 128:(j + 1) * 128], xT[:, kc, t * 128:(t + 1) * 128], C.ident[:]),
                    reads=[bx[t], C.bident], writes=[bph], sig=(j == 3))
        ss, bss = ss_r.next()
        S.op("vector", lambda e, ss=ss: e.memset(ss[:], 0.0), writes=[bss])
        for half in range(2):
            ph, bph = pp[half]
            sq, bsq = sq_r.next()
            S.op("scalar", lambda e, sq=sq, ph=ph, ss=ss, half=half: e.activation(sq[:], ph[:], AF.Square, accum_out=ss[:, half:half + 1]),
                 reads=[bph], writes=[bsq, bss])
        S.op("vector", lambda e, ss=ss: e.tensor_tensor(ss[:, 2:3], ss[:, 0:1], ss[:, 1:2], ALU.add), reads=[bss], writes=[bss])
        S.op("scalar", lambda e, ss=ss: e.activation(ss[:, 2:3], ss[:, 2:3], AF.Sqrt, bias=C.epsb[:, 0:1], scale=1.0 / D), reads=[bss, C.bepsb], writes=[bss])
        S.op("vector", lambda e, ss=ss: e.reciprocal(ss[:, 3:4], ss[:, 2:3]), reads=[bss], writes=[bss])
        ot, bot = ot_r.next()
        for half in range(2):
            ph, bph = pp[half]
            S.op("vector", lambda e, ot=ot, ph=ph, ss=ss, half=half: e.scalar_tensor_tensor(
                ot[:, half * 512:(half + 1) * 512], ph[:], ss[:, 3:4], gfin[:, half * 512:(half + 1) * 512], ALU.mult, ALU.mult),
                reads=[bph, bss, bgf], writes=[bot])
        S.dma("sync", out_d[t * 128:(t + 1) * 128, :], ot[:], reads=[bot], out_sem_buf=outb)


class SubRing:
    def __init__(self, tiles):
        self.tiles = list(tiles)
        self.i = 0

    def next(self):
        t = self.tiles[self.i % len(self.tiles)]
        self.i += 1
        return t


BS = 256
PASS = 1024
NTT = NT // 128


def emit_htm(C, hT2, bh2, htm_d, identb, bidb):
    S = C.S
    bhtm = S.buf("htm")
    htw = Ring(S, "htw", [128, D], BF16, 2)
    for tt in range(NTT):
        ptr, bptr = C.ps.next()
        ptb = ptr[:].bitcast(BF16)
        for k in range(KC):
            S.op("tensor", lambda e, ptb=ptb, k=k, tt=tt: e.transpose(ptb[:, k * 128:(k + 1) * 128], hT2[:, k, tt * 128:(tt + 1) * 128], identb[:]),
                 reads=[bh2[tt], bidb], writes=[bptr], sig=(k == KC - 1))
        ht, bht = htw.next()
        copy_op(C, evac_engine(C), ht[:], ptb[:, 0:D], [bptr], [bht])
        S.dma("sync", htm_d[tt], ht[:], reads=[bht], writes=[bhtm], nowaw=True)

    return bhtm


def emit_moe_sparse(C, bhtm, xT, bx, maskt, gates, bgates, cst_d, htm_d, m_w1, m_w3, m_w2, mod1, bmod1, identb, bidb):
    S = C.S
    cst = S.sbuf("cst", [128, 385], F32)
    bcst_ = S.buf("cst")
    S.dma("sync", cst[:], cst_d, writes=[bcst_])
    utri, iota_row, iota_p = cst[:, 0:128], cst[:, 128:384], cst[:, 384:385]
    ones_f = S.sbuf("ones_f", [128, 128], F32)
    bof = S.buf("ones_f")
    S.op("vector", lambda e: e.memset(ones_f[:], 1.0), writes=[bof])
    tot = S.sbuf("tot", [128, NTT, NE], F32)
    incl = S.sbuf("incl", [128, NTT, NE], F32)
    pos = S.sbuf("pos", [128, NTT, NE], F32)
    posq = S.sbuf("posq", [128, 8, NTT, NE], F32)
    jp = S.sbuf("jp", [128, 16], F32)
    cnti = S.sbuf("cnti", [128, NE], mybir.dt.int32)
    btab = S.buf("tab")
    bcnt = S.buf("cnt")
    mflat = maskt[:].rearrange("p t e -> p (t e)")
    pw, bpw = C.ps.next()
    S.op("tensor", lambda e: e.matmul(pw[:, 0:128], utri, mflat, start=True, stop=True), reads=[bcst_, bgates], writes=[bpw])
    pt_, bpt_ = C.ps.next()
    S.op("tensor", lambda e: e.matmul(pt_[:, 0:128], ones_f[:], mflat, start=True, stop=True), reads=[bof, bgates], writes=[bpt_])
    S.op("vector", lambda e: e.tensor_copy(tot[:].rearrange("p t e -> p (t e)"), pt_[:, 0:128]), reads=[bpt_], writes=[btab])
    for ex in range(NE):
        S.op("vector", lambda e, ex=ex: e.tensor_tensor_scan(incl[:, :, ex], ones_f[:, 0:NTT], tot[:, :, ex], 0.0, ALU.mult, ALU.add),
             reads=[btab, bof], writes=[btab])
    S.op("vector", lambda e: e.tensor_tensor(pos[:].rearrange("p t e -> p (t e)"), pw[:, 0:128], incl[:].rearrange("p t e -> p (t e)"), ALU.add),
         reads=[bpw, btab], writes=[btab])
    S.op("vector", lambda e: e.tensor_tensor(pos[:], pos[:], tot[:], ALU.subtract), reads=[btab], writes=[btab])
    S.op("vector", lambda e: e.scalar_tensor_tensor(pos[:], pos[:], 1.0, maskt[:], ALU.add, ALU.mult), reads=[btab, bgates], writes=[btab])
    for q in range(8):
        S.op("vector", lambda e, q=q: e.tensor_scalar(posq[:, q], pos[:], -1.0 - BS * q, None, ALU.add), reads=[btab], writes=[btab])
    S.op("vector", lambda e: e.tensor_scalar(pos[:], pos[:], -1.0, None, ALU.add), reads=[btab], writes=[btab])
    for st in range(16):
        S.op("vector", lambda e, st=st: e.tensor_scalar(jp[:, st:st + 1], iota_p, 128.0 * st, None, ALU.add), reads=[bcst_], writes=[btab])
    S.op("vector", lambda e: e.tensor_copy(cnti[:], incl[:, NTT - 1, :]), reads=[btab], writes=[bcnt])

    hg = S.sbuf("hg", [128, KC, PASS], BF16)
    bhg = [S.buf("hg") for _ in range(PASS // BS)]
    ys = S.sbuf("ys", [128, KC, PASS], F32)
    bys = [S.buf("ys") for _ in range(PASS // BS)]
    prow = S.sbuf("prow", [128, NT], F32)
    grow = S.sbuf("grow", [128, NT], F32)
    brow = S.buf("rows")
    gl_r = Ring(S, "gl", [128, 128], F32, 3)
    htr = Ring(S, "htr", [128, D], BF16, 3)
    sct_r = Ring(S, "sct", [128, 512], F32, 2)
    sel_r = Ring(S, "sel", [128, BS], BF16, 3)
    ysb_r = Ring(S, "ysb", [128, KC, BS], BF16, 1)
    ysm_r = Ring(S, "ysm", [128, D], BF16, 2)
    sg_r = Ring(S, "sg", [128, NT], BF16, 2)
    SL = 2
    w13 = Ring(S, "w13s", [128, KC, SL * 128], BF16, 4)
    w2r = Ring(S, "w2s", [128, SL, D], BF16, 2)
    hid_r = [Ring(S, "hids", [128, BS], BF16, PASS // BS) for _ in range(SL)]
    sil_r = Ring(S, "sils", [128, BS], F32, 2)
    nfc = DFE // 128
    nblk = PASS // BS

    def cap_of(ex):
        return cnti[0:1, ex:ex + 1]

    pre = {}

    def issue_slice(ex, s0, tiles):
        w1t, bw1, w3t, bw3, w2t, bw2 = tiles
        S.dma("gpsimd", w1t[:], m_w1[ex].rearrange("(k p) n -> p k n", p=128)[:, :, s0 * 128:(s0 + SL) * 128], writes=[bw1])
        S.dma("gpsimd", w3t[:], m_w3[ex].rearrange("(k p) n -> p k n", p=128)[:, :, s0 * 128:(s0 + SL) * 128], writes=[bw3])
        S.dma("gpsimd", w2t[:], m_w2[ex][s0 * 128:(s0 + SL) * 128, :].rearrange("(f p) n -> p f n", p=128), writes=[bw2])

    def load_slice(ex, s0):
        w1t, bw1 = w13.next()
        w3t, bw3 = w13.next()
        w2t, bw2 = w2r.next()
        tiles = (w1t, bw1, w3t, bw3, w2t, bw2)
        issue_slice(ex, s0, tiles)
        return tiles

    def prefetch(ex):
        for s0 in (0, SL):
            pre[(ex, s0)] = load_slice(ex, s0)

    def do_rowbcast(ex):
        for src, dstrow in ((pos, prow), (gates, grow)):
            for t4 in range(NT // 512):
                pg, bpg = C.ps.next()
                for j in range(4):
                    t = t4 * 4 + j
                    gl, bgl = gl_r.next()
                    S.op("scalar", lambda e, gl=gl, t=t, ex=ex, src=src: e.copy(gl[:], bcast_cols(src[:, t, ex:ex + 1], 128)),
                         reads=[btab, bgates], writes=[bgl])
                    S.op("tensor", lambda e, pg=pg, gl=gl, j=j: e.matmul(pg[:, j * 128:(j + 1) * 128], gl[:], C.ident[:], start=True, stop=True),
                         reads=[bgl, C.bident], writes=[bpg])
                copy_op(C, "vector", dstrow[:, t4 * 512:(t4 + 1) * 512], pg[:], [bpg], [brow])

    def do_gather(ex, p):
        cap = cap_of(ex)
        for b in range(nblk):
            q = p * nblk + b
            if q > 0:
                S.begin_cond(cap, bcnt, BS * q, key=("cap", ex))
            pgk = [C.ps.next() for _ in range(4)]
            for tt in range(NTT):
                ht, bht = htr.next()
                S.dma("sync", ht[:], htm_d[tt], reads=[bhtm], writes=[bht])
                sel, bsel = sel_r.next()
                S.op("vector", lambda e, sel=sel, q=q, tt=tt, ex=ex: e.tensor_scalar(
                    sel[:], iota_row, posq[:, q, tt, ex:ex + 1], None, ALU.is_equal), reads=[bcst_, btab], writes=[bsel])
                for k in range(KC):
                    pk, bpk = pgk[k // 2]
                    S.op("tensor", lambda e, pk=pk, k=k, ht=ht, sel=sel, tt=tt: e.matmul(
                        pk[:, (k % 2) * BS:(k % 2 + 1) * BS], ht[:, k * 128:(k + 1) * 128], sel[:], start=(tt == 0 and k % 2 == 0), stop=(tt == NTT - 1),
                        skip_group_check=True),
                        reads=[bht, bsel], writes=[bpk], sig=(k == KC - 1))
            for k2 in range(4):
                pk, bpk = pgk[k2]
                copy_op(C, evac_engine(C), hg[:, 2 * k2:2 * k2 + 2, b * BS:(b + 1) * BS],
                        pk[:].rearrange("p (a c) -> p a c", a=2), [bpk], [bhg[b]])
            if q > 0:
                S.end_cond()

    def do_ffn(ex, p):
        cap = cap_of(ex)
        for b in range(nblk):
            S.op("gpsimd", lambda e, b=b: e.memset(ys[:, :, b * BS:(b + 1) * BS], 0.0), writes=[bys[b]])
        for s0 in range(0, nfc, SL):
            if p == 0 and (ex, s0) in pre:
                w1t, bw1, w3t, bw3, w2t, bw2 = pre[(ex, s0)]
            else:
                w1t, bw1, w3t, bw3, w2t, bw2 = load_slice(ex, s0)
            def up(b, w1t=w1t, w3t=w3t, bw1=bw1, bw3=bw3):
                c0 = b * BS
                hids = [r.next() for r in hid_r]
                for fi in range(SL):
                    pa, bpa = C.ps.next()
                    for k in range(KC):
                        S.op("tensor", lambda e, pa=pa, k=k, fi=fi, c0=c0: e.matmul(
                            pa[:, :BS], w1t[:, k, fi * 128:(fi + 1) * 128], hg[:, k, c0:c0 + BS], start=(k == 0), stop=(k == KC - 1)),
                            reads=[bhg[b], bw1], writes=[bpa], sig=(k == KC - 1))
                    pb, bpb = C.ps.next()
                    for k in range(KC):
                        S.op("tensor", lambda e, pb=pb, k=k, fi=fi, c0=c0: e.matmul(
                            pb[:, :BS], w3t[:, k, fi * 128:(fi + 1) * 128], hg[:, k, c0:c0 + BS], start=(k == 0), stop=(k == KC - 1)),
                            reads=[bhg[b], bw3], writes=[bpb], sig=(k == KC - 1))
                    sa, bsa = sil_r.next()
                    S.op("scalar", lambda e, sa=sa, pa=pa: e.activation(sa[:], pa[:, :BS], AF.Silu), reads=[bpa], writes=[bsa])
                    S.op("vector", lambda e, sa=sa, pb=pb, hf_=hids[fi][0]: e.tensor_tensor(hf_[:], pb[:, :BS], sa[:], ALU.mult),
                         reads=[bpb, bsa], writes=[hids[fi][1]])
                hid_of[b] = hids

            def down(b, w2t=w2t, bw2=bw2):
                c0 = b * BS
                hids = hid_of[b]
                pos_ = [C.ps.next() for _ in range(KC // 2)]
                for fi in range(SL):
                    for dc in range(KC):
                        po, bpo = pos_[dc // 2]
                        h2 = dc % 2
                        S.op("tensor", lambda e, po=po, fi=fi, dc=dc, h2=h2, hf_=hids[fi][0]: e.matmul(
                            po[:, h2 * BS:(h2 + 1) * BS], w2t[:, fi, dc * 128:(dc + 1) * 128], hf_[:],
                            start=(fi == 0 and h2 == 0), stop=(fi == SL - 1), skip_group_check=True),
                            reads=[hids[fi][1], bw2], writes=[bpo], sig=(fi == SL - 1 and h2 == 1))
                for dc2 in range(KC // 2):
                    po, bpo = pos_[dc2]
                    S.op("vector", lambda e, po=po, dc2=dc2, c0=c0: e.tensor_tensor(
                        ys[:, 2 * dc2:2 * dc2 + 2, c0:c0 + BS], po[:, 0:2 * BS].rearrange("p (a c) -> p a c", a=2), ys[:, 2 * dc2:2 * dc2 + 2, c0:c0 + BS], ALU.add),
                        reads=[bpo], writes=[bys[b]])

            def chain(f):
                opened = 0
                for b in range(nblk):
                    q = p * nblk + b
                    if q > 0:
                        S.begin_cond(cap, bcnt, BS * q, key=("cap", ex))
                        opened += 1
                    f(b)
                for _ in range(opened):
                    S.end_cond()
            hid_of = {}
            for b in range(nblk):
                q = p * nblk + b
                if q > 0:
                    S.begin_cond(cap, bcnt, BS * q, key=("cap", ex))
                up(b)
                down(b)
                if q > 0:
                    S.end_cond()

    def do_scatter(ex, p):
        cap = cap_of(ex)
        for b in range(nblk):
            q = p * nblk + b
            if q > 0:
                S.begin_cond(cap, bcnt, BS * q, key=("cap", ex))
            ysb, bysb = ysb_r.next()
            S.op("gpsimd", lambda e, ysb=ysb, b=b: e.tensor_copy(ysb[:], ys[:, :, b * BS:(b + 1) * BS]), reads=[bys[b]], writes=[bysb])
            tiles = []
            for st2 in range(BS // 128):
                slot = q * (BS // 128) + st2
                ptr, bptr = C.ps.next()
                ptb = ptr[:].bitcast(BF16)
                for dc in range(KC):
                    S.op("tensor", lambda e, ptb=ptb, dc=dc, ysb=ysb, st2=st2: e.transpose(
                        ptb[:, dc * 128:(dc + 1) * 128], ysb[:, dc, st2 * 128:(st2 + 1) * 128], identb[:]),
                        reads=[bysb, bidb], writes=[bptr], sig=(dc == KC - 1))
                ysm, bysm = ysm_r.next()
                S.op("scalar", lambda e, ysm=ysm, ptb=ptb: e.copy(ysm[:], ptb[:, 0:D]), reads=[bptr], writes=[bysm])
                sg, bsg = sg_r.next()
                S.op("vector", lambda e, sg=sg, slot=slot: e.scalar_tensor_tensor(
                    sg[:], prow[:], jp[:, slot:slot + 1], grow[:], ALU.is_equal, ALU.mult), reads=[brow, btab], writes=[bsg])
                tiles.append((ysm, bysm, sg, bsg))
            for dc in range(KC):
                for tb in range(NT // 512):
                    po, bpo = C.ps.next()
                    for i, (ysm, bysm, sg, bsg) in enumerate(tiles):
                        S.op("tensor", lambda e, po=po, ysm=ysm, sg=sg, dc=dc, tb=tb, i=i: e.matmul(
                            po[:], ysm[:, dc * 128:(dc + 1) * 128], sg[:, tb * 512:(tb + 1) * 512], start=(i == 0), stop=(i == len(tiles) - 1)),
                            reads=[bysm, bsg], writes=[bpo], sig=(i == len(tiles) - 1))
                    if (dc * 4 + tb) % 3 != 2:
                        S.op("vector", lambda e, po=po, dc=dc, tb=tb: e.scalar_tensor_tensor(
                            xT[:, dc, tb * 512:(tb + 1) * 512], po[:], mod1[:, 5 * 8 + dc, 0:1], xT[:, dc, tb * 512:(tb + 1) * 512], ALU.mult, ALU.add),
                            reads=[bpo, bmod1], writes=bx[tb * 4:(tb + 1) * 4])
                    else:
                        sc, bsc = sct_r.next()
                        S.op("scalar", lambda e, sc=sc, po=po, dc=dc: e.activation(sc[:], po[:], AF.Copy, scale=mod1[:, 5 * 8 + dc, 0:1]),
                             reads=[bpo, bmod1], writes=[bsc])
                        S.op("gpsimd", lambda e, sc=sc, dc=dc, tb=tb: e.tensor_tensor(
                            xT[:, dc, tb * 512:(tb + 1) * 512], xT[:, dc, tb * 512:(tb + 1) * 512], sc[:], ALU.add),
                            reads=[bsc], writes=bx[tb * 4:(tb + 1) * 4])
            if q > 0:
                S.end_cond()

    do_rowbcast(0)
    do_gather(0, 0)
    for ex in range(NE):
        do_ffn(ex, 0)
        if ex + 1 < NE:
            prefetch(ex + 1)
            do_gather(ex + 1, 0)
        do_scatter(ex, 0)
        for p in range(1, NT // PASS):
            S.begin_cond(cap_of(ex), bcnt, PASS * p, key=("cap", ex))
            do_gather(ex, p)
            do_ffn(ex, p)
            do_scatter(ex, p)
            if ex + 1 < NE and p == NT // PASS - 1:
                do_gather(ex + 1, 0)
                for s0 in (0, SL):
                    issue_slice(ex + 1, s0, pre[(ex + 1, s0)])
            S.end_cond()
        if ex + 1 < NE:
            do_rowbcast(ex + 1)


GROUPS = [[0, 1, 2, 3], [4, 5, 6, 7]]


def build_fused():
    nc = bass.Bass("TRN2", target_bir_lowering=False)

    def din(name, shape, dt=F32):
        return nc.dram_tensor(name, list(shape), dt, kind="ExternalInput").ap()

    def dscr(name, shape, dt=F32):
        return nc.dram_tensor(name, list(shape), dt).ap()
    xall = din("xall", [TOT, D])
    vecs = din("vecs", [128, VP.n])
    ident = din("ident", [128, 128])
    ada_w = din("ada_w", [2, D, 6 * D])
    w_in = din("w_in", [D, 2 * D])
    w_a = din("w_a", [2, 8, 128, 128])
    w_i = din("w_i", [2, 8, 128, 128])
    w_out = din("w_out", [D, D])
    f_w1 = din("f_w1", [D, DFF])
    f_w3 = din("f_w3", [D, DFF])
    f_w2 = din("f_w2", [DFF, D])
    w_qkv = din("w_qkv", [D, 1536])
    cos_d = din("cos", [128, NT])
    sin_d = din("sin", [128, NT])
    rotm_d = din("rotm", [128, 128])
    w_o = din("w_o", [D, D])
    router = din("router", [D, NE])
    m_w1 = din("m_w1", [NE, D, DFE])
    m_w3 = din("m_w3", [NE, D, DFE])
    m_w2 = din("m_w2", [NE, DFE, D])
    fin_g = din("fin_g", [D])
    cst_d = din("cst", [128, 385])
    out_d = nc.dram_tensor("out", [NT, D], F32, kind="ExternalOutput").ap()
    htm_d = dscr("htm_d", [NT // 128, 128, D], BF16)
    a_sp = dscr("a_sp", [16, 128, NT])
    b_sp = dscr("b_sp", [16, 128, NT])
    summ_loc = dscr("summ_loc", [128, 32])
    summ_all_d = dscr("summ_all_d", [4 * 128, 32])
    klat = [dscr(f"klat{g}", [128, NT], BF16) for g in range(2)]
    kall = [dscr(f"kall{g}", [4 * 128, NT], BF16) for g in range(2)]
    kctx = dscr("kctx", [128, 2 * NCX], BF16)
    vlat = [dscr(f"vlat{h}", [NT // 2, 256], BF16) for h in range(2)]
    vall = [dscr(f"vall{h}", [4 * NT // 2, 256], BF16) for h in range(2)]
    vctx = dscr("vctx", [NCX, 256], BF16)

    with ExitStack() as es:
        C = setup_common(nc, es, vecs, ident)
        S = C.S
        outb = S.buf("out")
        C.ada_queue = "gpsimd"
        C.ada_dt = BF16
        C.ada_ring = 4
        emit_silu_c(C)
        mod0, bmod0 = S.sbuf("mod0", [128, 48, 2], F32), S.buf("mod")
        mod1, bmod1 = S.sbuf("mod1", [128, 48, 2], F32), S.buf("mod")
        gs1 = S.sbuf("gs1", [128, KC, 2], F32)
        gs2 = S.sbuf("gs2", [128, KC, 2], F32)
        gs3 = S.sbuf("gs3", [128, KC, 2], F32)
        gs4 = S.sbuf("gs4", [128, KC, 2], F32)
        bgs1, bgs2, bgs3, bgs4 = S.buf("gs"), S.buf("gs"), S.buf("gs"), S.buf("gs")
        identb = S.sbuf("identb", [128, 128], BF16)
        bidb = S.buf("identb")
        S.op("vector", lambda e: e.tensor_copy(identb[:], C.ident[:]), reads=[C.bident], writes=[bidb])
        cst = S.sbuf("cst", [128, 16], F32)
        bcst = S.buf("cst")
        summ = S.sbuf("summ", [128, 32], F32)
        bsumm = S.buf("summ")
        sall = S.sbuf("sall", [128, 4, 32], F32)
        bsall = S.buf("sall")
        cneg = S.sbuf("cneg", [128, 32], F32)
        bcneg = S.buf("cneg")
        S.op("scalar", lambda e: e.activation(cneg[:, 0:16], V(C, "lam", 0, 16), AF.Exp, scale=-1.0), reads=[C.bvec], writes=[bcneg])
        S.op("scalar", lambda e: e.activation(cneg[:, 0:16], cneg[:, 0:16], AF.Ln, bias=1.0), reads=[bcneg], writes=[bcneg])
        S.op("vector", lambda e: e.tensor_scalar(cneg[:, 16:32], cneg[:, 0:16], -16.0, None, ALU.mult), reads=[bcneg], writes=[bcneg])
        S.op("vector", lambda e: e.tensor_scalar(cneg[:, 0:16], cneg[:, 0:16], -8.0, None, ALU.mult), reads=[bcneg], writes=[bcneg])

        es_l0 = scoped(S)
        hT = S.sbuf("hT", [128, KC, TOT], BF16)
        bh = [S.buf("hT") for _ in range(TOT // 128)]
        es_ada = scoped(S)
        emit_ada(C, ada_w, 0, "ada_b0", mod0, bmod0)
        emit_gs_into(C, gs1, bgs1, mod0, bmod0, 1, "n1g0")
        emit_gs_into(C, gs2, bgs2, mod0, bmod0, 4, "n2g0")

        es1 = scoped(S)
        C.xtile = Ring(S, "xtile", [128, D], F32, 2)
        alloc_norm_tmps(C)
        xblk = Ring(S, "xblk", [128, KC, 512], F32, 2)
        for (c0, n, which) in LAT_BLOCKS + [CTX_BLOCK, HAL_BLOCK]:
            xb_, bxb_ = xblk.next()
            bl = [bxb_] * 4
            emit_load_xT(C, xall, c0, n // 128, xb_, bl, 0)
            emit_norm(C, xb_, bl, 0, hT, bh, c0, n, gs1, bgs1, mod0, bmod0, 0, which)
        S.barrier()
        es1.close()
        es_ada.close()

        es_yc = scoped(S)
        yctx = S.sbuf("yctx", [128, KC, NCX], F32)
        byctx = S.buf("yctx")
        es_mix = scoped(S)
        ada1_r = Ring(S, "adaw1s", [128, KC, 768], BF16, 2)
        win_r = Ring(S, "win", [128, KC, 128], BF16, 3)
        wg_r = Ring(S, "wg", [128, 4, 128], BF16, 3)
        xbe = Ring(S, "xbe", [128, NT + 3 + NCX + 3], F32, 2)
        xc_r = Ring(S, "xc", [128, NSC], F32, 2)
        xcb_r = Ring(S, "xcb", [128, NSC], BF16, 2)
        r_r = Ring(S, "rr", [128, NSC], F32, 2)
        b_r = Ring(S, "bb", [128, NSC], F32, 2)
        a_r = Ring(S, "aa", [128, NSC], F32, 2)
        m_r = Ring(S, "mm", [128, NSC], F32, 2)
        yc_r = Ring(S, "yc", [128, NCX], F32, 2)
        sm_r = Ring(S, "sm", [128, 8], F32, 2)
        bsp = [S.buf("sp") for _ in range(16)]
        LB = NT + 3
        w_in_v = w_in.rearrange("(k p) n -> p k n", p=128)
        ada1_w = {0: emit_ada_dma(C, ada_w, 1, 0, ada1_r)}

        def p2a_loadw(ct):
            wt, bw = win_r.next()
            S.dma("gpsimd", wt[:], w_in_v[:, :, ct * 128:(ct + 1) * 128], writes=[bw])
            wg, bwg = wg_r.next()
            for d in range(2):
                S.dma("gpsimd", wg[:, d, :], w_a[d, ct], writes=[bwg], nowaw=True)
                S.dma("gpsimd", wg[:, 2 + d, :], w_i[d, ct], writes=[bwg], nowaw=True)
            return wt, bw, wg, bwg
        p2a_w = {0: p2a_loadw(0)}

        def p2a_front(ct):
            if ct + 1 < KC:
                ada1_w[ct + 1] = emit_ada_dma(C, ada_w, 1, ct + 1, ada1_r)
            if ct + 1 < KC:
                p2a_w[ct + 1] = p2a_loadw(ct + 1)
            emit_ada_mm(C, ct, ada1_w[ct][0], ada1_w[ct][1], "ada_b1", mod1, bmod1)
            wt, bw, wg, bwg = p2a_w[ct]
            xe, bxe = xbe.next()
            S.op("gpsimd", lambda e, xe=xe: e.memset(xe[:, LB:LB + 2], 0.0), writes=[bxe])
            S.op("gpsimd", lambda e, xe=xe: e.memset(xe[:, LB + 2 + NCX:LB + 3 + NCX], 0.0), writes=[bxe])
            for (c0, n, which) in LAT_BLOCKS + [CTX_BLOCK, (HAL0, 3, 0)]:
                ps, bps = C.ps.next()
                for k in range(KC):
                    S.op("tensor", lambda e, ps=ps, k=k, wt=wt, c0=c0, n=n: e.matmul(
                        ps[:, :n], wt[:, k, :], hT[:, k, c0:c0 + n], start=(k == 0), stop=(k == KC - 1)),
                        reads=tile_bufs(bh, c0, n) + [bw], writes=[bps], sig=(k == KC - 1))
                if c0 < CTX0:
                    copy_op(C, evac_engine(C), xe[:, 2 + c0:2 + c0 + n], ps[:, :n], [bps], [bxe])
                elif c0 == CTX0:
                    copy_op(C, evac_engine(C), xe[:, LB + 2:LB + 2 + NCX], ps[:, :n], [bps], [bxe])
                else:
                    S.op("vector", lambda e, ps=ps, xe=xe: e.tensor_tensor(xe[:, 0:2], ps[:, 0:2], V(C, "hmask", 0, 2), ALU.mult),
                         reads=[bps, C.bvec], writes=[bxe])
                    S.op("vector", lambda e, ps=ps, xe=xe: e.tensor_tensor(xe[:, 2 + NT:3 + NT], ps[:, 2:3], V(C, "hmask", 2, 1), ALU.mult),
                         reads=[bps, C.bvec], writes=[bxe])
            xc, bxc = xc_r.next()
            for (dst0, src0, n) in [(0, 0, NT), (NT, LB, NCX)]:
                S.op("scalar", lambda e, xc=xc, xe=xe, dst0=dst0, src0=src0, n=n, ct=ct: e.activation(
                    xc[:, dst0:dst0 + n], xe[:, src0:src0 + n], AF.Identity,
                    bias=V(C, "convb", ct), scale=V(C, "convw", ct)), reads=[bxe, C.bvec], writes=[bxc])
                for k in range(1, 4):
                    S.op("vector", lambda e, xc=xc, xe=xe, dst0=dst0, src0=src0, n=n, k=k, ct=ct: e.scalar_tensor_tensor(
                        xc[:, dst0:dst0 + n], xe[:, src0 + k:src0 + k + n], V(C, "convw", k * 8 + ct), xc[:, dst0:dst0 + n],
                        ALU.mult, ALU.add), reads=[bxe, C.bvec, bxc], writes=[bxc])
            xcb, bxcb = xcb_r.next()
            S.op("gpsimd", lambda e, xcb=xcb, xc=xc: e.tensor_copy(xcb[:], xc[:]), reads=[bxc], writes=[bxcb])
            return (xc, bxc, xcb, bxcb, wg, bwg)
        def p2a_back(ct, xc, bxc, xcb, bxcb, wg, bwg):
            ycs = []
            for d in range(2):
                rr, brr = r_r.next()
                bb, bbb = b_r.next()
                for (c0, n) in [(0, 512), (512, 512), (1024, 512), (1536, 512), (NT, NCX)]:
                    pr, bpr = C.ps.next()
                    S.op("tensor", lambda e, pr=pr, wg=wg, d=d, xcb=xcb, c0=c0, n=n: e.matmul(
                        pr[:, :n], wg[:, d, :], xcb[:, c0:c0 + n], start=True, stop=True), reads=[bwg, bxcb], writes=[bpr])
                    S.op("scalar", lambda e, pr=pr, rr=rr, c0=c0, n=n, d=d, ct=ct: e.activation(
                        rr[:, c0:c0 + n], pr[:, :n], AF.Sigmoid, bias=V(C, "b_a", d * 8 + ct)), reads=[bpr, C.bvec], writes=[brr])
                    pi, bpi = C.ps.next()
                    S.op("tensor", lambda e, pi=pi, wg=wg, d=d, xcb=xcb, c0=c0, n=n: e.matmul(
                        pi[:, :n], wg[:, 2 + d, :], xcb[:, c0:c0 + n], start=True, stop=True), reads=[bwg, bxcb], writes=[bpi])
                    S.op("scalar", lambda e, pi=pi, bb=bb, c0=c0, n=n, d=d, ct=ct: e.activation(
                        bb[:, c0:c0 + n], pi[:, :n], AF.Sigmoid, bias=V(C, "b_i", d * 8 + ct)), reads=[bpi, C.bvec], writes=[bbb])
                aa, baa = a_r.next()
                mm, bmm = m_r.next()
                cn = d * 8 + ct
                sm, bsm = sm_r.next()
                S.op("scalar", lambda e, aa=aa, rr=rr, cn=cn: e.activation(aa[:], rr[:], AF.Exp, scale=cneg[:, cn:cn + 1]),
                     reads=[brr, bcneg], writes=[baa])
                S.op("scalar", lambda e, mm=mm, rr=rr, cn=cn: e.activation(mm[:], rr[:], AF.Exp, scale=cneg[:, 16 + cn:17 + cn]),
                     reads=[brr, bcneg], writes=[bmm])
                S.op("vector", lambda e, sm=sm, rr=rr: e.tensor_reduce(sm[:, 0:1], rr[:, 0:NT], AX.X, ALU.add), reads=[brr], writes=[bsm])
                S.op("scalar", lambda e, mm=mm: e.activation(mm[:], mm[:], AF.Sqrt, bias=1.0, scale=-1.0), reads=[bmm], writes=[bmm])
                S.op("gpsimd", lambda e, bb=bb, xc=xc: e.tensor_tensor(bb[:], bb[:], xc[:], ALU.mult), reads=[bbb, bxc], writes=[bbb])
                S.op("vector", lambda e, bb=bb, mm=mm: e.tensor_tensor(bb[:], bb[:], mm[:], ALU.mult), reads=[bbb, bmm], writes=[bbb])
                S.dma("sync", a_sp[cn], aa[:, 0:NT], reads=[baa], writes=[bsp[cn]])
                S.dma("sync", b_sp[cn], bb[:, 0:NT], reads=[bbb], writes=[bsp[cn]], nowaw=True)
                yc, byc = yc_r.next()
                o = ct * 4 + d * 2
                if d == 0:
                    S.op("vector", lambda e, yc=yc, aa=aa, bb=bb: e.tensor_tensor_scan(
                        yc[:], aa[:, NT:NSC], bb[:, NT:NSC], 0.0, ALU.mult, ALU.add), reads=[baa, bbb], writes=[byc])
                    S.op("vector", lambda e, mm=mm, aa=aa, bb=bb: e.tensor_tensor_scan(
                        mm[:, 0:NT], aa[:, 0:NT], bb[:, 0:NT], 0.0, ALU.mult, ALU.add), reads=[baa, bbb], writes=[bmm])
                    st0, hend = yc[:, NCX - 1:NCX], mm[:, NT - 1:NT]
                else:
                    S.op("vector", lambda e, yc=yc, aa=aa, bb=bb: e.tensor_tensor_scan(
                        rev_ap(yc[:]), rev_ap(aa[:, NT:NSC]), rev_ap(bb[:, NT:NSC]), 0.0, ALU.mult, ALU.add), reads=[baa, bbb], writes=[byc])
                    S.op("vector", lambda e, mm=mm, aa=aa, bb=bb: e.tensor_tensor_scan(
                        rev_ap(mm[:, 0:NT]), rev_ap(aa[:, 0:NT]), rev_ap(bb[:, 0:NT]), 0.0, ALU.mult, ALU.add), reads=[baa, bbb], writes=[bmm])
                    st0, hend = yc[:, 0:1], mm[:, 0:1]
                S.op("vector", lambda e, st0=st0, cn=cn: e.tensor_copy(cst[:, cn:cn + 1], st0), reads=[byc], writes=[bcst])
                S.op("scalar", lambda e, sm=sm, cn=cn, o=o: e.activation(summ[:, o:o + 1], sm[:, 0:1], AF.Exp, scale=cneg[:, cn:cn + 1]),
                     reads=[bsm, bcneg], writes=[bsumm])
                S.op("vector", lambda e, hend=hend, o=o: e.tensor_copy(summ[:, o + 1:o + 2], hend), reads=[bmm], writes=[bsumm])
                ycs.append((yc, byc))
            (yc0, byc0), (yc1, byc1) = ycs
            S.op("gpsimd", lambda e, yc0=yc0, yc1=yc1, ct=ct: e.tensor_tensor(yctx[:, ct, :], yc0[:], yc1[:], ALU.add), reads=[byc0, byc1], writes=[byctx])
        fr = p2a_front(0)
        for ct in range(KC):
            cur = fr
            if ct + 1 < KC:
                fr = p2a_front(ct + 1)
            p2a_back(ct, *cur)
        emit_gs_into(C, gs3, bgs3, mod1, bmod1, 1, "n1g1")
        emit_gs_into(C, gs4, bgs4, mod1, bmod1, 4, "n2g1")
        bsl, bsa = S.buf("summ_loc"), S.buf("summ_all")
        S.dma("sync", summ_loc, summ[:], reads=[bsumm], writes=[bsl])
        S.collective("AllGather", GROUPS, summ_loc, summ_all_d, [bsl], bsa)
        S.dma("sync", sall[:], summ_all_d.rearrange("(r p) c -> p r c", p=128), reads=[bsa], writes=[bsall])
        S.barrier()
        es_mix.close()

        es_yg = scoped(S)
        ygT = S.sbuf("ygT", [128, KC, NSC], BF16)
        byg = [S.buf("yg") for _ in range(NSC // 128)]
        es_p2b = scoped(S)
        a2_r = Ring(S, "a2", [128, NT], F32, 4)
        b2_r = Ring(S, "b2", [128, NT], F32, 4)
        y_r = Ring(S, "yy", [128, NT], F32, 2)
        sm_r = Ring(S, "sm2", [128, 8], F32, 2)
        gtmp = Ring(S, "gtmp", [128, 512], F32, 3)
        win_r = Ring(S, "win2", [128, KC, 128], BF16, 2)
        carr = S.sbuf("carr", [128, 16], F32)
        ctmp = S.sbuf("ctmp", [128, 8], F32)
        bcarr = S.buf("carr")
        S.op("vector", lambda e: e.tensor_copy(carr[:], cst[:]), reads=[bcst], writes=[bcarr])
        for d in range(2):
            order, sel = ([0, 1, 2, 3], "sel_f") if d == 0 else ([3, 2, 1, 0], "sel_r")
            cs_ = carr[:, d * 8:(d + 1) * 8]
            for i in order:
                Pv = bass.AP(sall[:].tensor, sall[:, i, d * 2:d * 2 + 1].offset, [list(sall[:].ap[0]), [4, 8]])
                Hv = bass.AP(sall[:].tensor, sall[:, i, d * 2 + 1:d * 2 + 2].offset, [list(sall[:].ap[0]), [4, 8]])
                S.op("vector", lambda e, cs_=cs_, Pv=Pv: e.tensor_tensor(ctmp[:], cs_, Pv, ALU.mult), reads=[bcarr, bsall], writes=[bcarr])
                S.op("vector", lambda e, Hv=Hv: e.tensor_tensor(ctmp[:], ctmp[:], Hv, ALU.add), reads=[bcarr, bsall], writes=[bcarr])
                S.op("vector", lambda e, cs_=cs_: e.tensor_tensor(ctmp[:], ctmp[:], cs_, ALU.subtract), reads=[bcarr], writes=[bcarr])
                S.op("vector", lambda e, cs_=cs_, i=i, sel=sel: e.scalar_tensor_tensor(
                    cs_, ctmp[:], V(C, sel, i), cs_, ALU.mult, ALU.add), reads=[bcarr, C.bvec], writes=[bcarr])
        for ct in range(KC):
            wt, bw = win_r.next()
            S.dma("gpsimd", wt[:], w_in_v[:, :, D + ct * 128:D + (ct + 1) * 128], writes=[bw])
            ys = []
            for d in range(2):
                cn = d * 8 + ct
                aa, baa = a2_r.next()
                bb, bbb = b2_r.next()
                S.dma("sync", aa[:], a_sp[cn], reads=[bsp[cn]], writes=[baa])
                S.dma("sync", bb[:], b_sp[cn], reads=[bsp[cn]], writes=[bbb])
                yy, byy = y_r.next()
                if d == 0:
                    S.op("vector", lambda e, yy=yy, aa=aa, bb=bb, cn=cn: e.tensor_tensor_scan(
                        yy[:], aa[:], bb[:], carr[:, cn:cn + 1], ALU.mult, ALU.add), reads=[baa, bbb, bcarr], writes=[byy])
                else:
                    S.op("vector", lambda e, yy=yy, aa=aa, bb=bb, cn=cn: e.tensor_tensor_scan(
                        rev_ap(yy[:]), rev_ap(aa[:]), rev_ap(bb[:]), carr[:, cn:cn + 1], ALU.mult, ALU.add), reads=[baa, bbb, bcarr], writes=[byy])
                ys.append((yy, byy))
            (y0, by0), (y1, by1) = ys
            S.op("gpsimd", lambda e, y0=y0, y1=y1: e.tensor_tensor(y0[:], y0[:], y1[:], ALU.add), reads=[by0, by1], writes=[by0])
            for (c0, n, which) in LAT_BLOCKS + [CTX_BLOCK]:
                pg, bpg = C.ps.next()
                for k in range(KC):
                    S.op("tensor", lambda e, pg=pg, k=k, wt=wt, c0=c0, n=n: e.matmul(
                        pg[:, :n], wt[:, k, :], hT[:, k, c0:c0 + n], start=(k == 0), stop=(k == KC - 1)),
                        reads=tile_bufs(bh, c0, n) + [bw], writes=[bpg], sig=(k == KC - 1))
                t1, bt1 = gtmp.next()
                S.op("scalar", lambda e, t1=t1, pg=pg, n=n: e.activation(t1[:, :n], pg[:, :n], AF.Gelu_apprx_tanh), reads=[bpg], writes=[bt1])
                if which == 0:
                    S.op("vector", lambda e, t1=t1, y0=y0, c0=c0, n=n, ct=ct: e.tensor_tensor(ygT[:, ct, c0:c0 + n], t1[:, :n], y0[:, c0:c0 + n], ALU.mult),
                         reads=[bt1, by0], writes=tile_bufs(byg, c0, n))
                else:
                    S.op("vector", lambda e, t1=t1, c0=c0, n=n, ct=ct: e.tensor_tensor(ygT[:, ct, c0:c0 + n], t1[:, :n], yctx[:, ct, :], ALU.mult),
                         reads=[bt1, byctx], writes=tile_bufs(byg, c0, n))
        S.barrier()
        es_p2b.close()

        S.es = es
        xT = S.sbuf("xT", [128, KC, NSC], F32, side="right")
        bx = [S.buf("xT") for _ in range(NSC // 128)]
        es_o = scoped(S)
        C.xtile = Ring(S, "xtile", [128, D], F32, 2)
        emit_load_xT(C, xall, 0, NSC // 128, xT, bx, 0)
        wo_r = Ring(S, "wo", [128, KC, D], BF16, 1)
        wot, bwo = load_w(C, wo_r, w_out, D, 0, D)
        BL = LAT_BLOCKS + [CTX_BLOCK]
        emit_proj_residual(C, ygT, byg, KC, wot, bwo, xT, bx, BL, mod0, bmod0, 2)
        S.barrier()
        es_o.close()
        es_yg.close()
        es_yc.close()

        es_f = scoped(S)
        alloc_norm_tmps(C)
        emit_norm_blocks(C, xT, bx, hT, bh, BL, gs2, bgs2, mod0, bmod0, 3)
        alloc_ffn(C, 4)
        emit_ffn(C, hT, bh, xT, bx, BL, f_w1, f_w3, f_w2, DFF, mod0, bmod0, 5)
        emit_norm_blocks(C, xT, bx, hT, bh, BL, gs3, bgs3, mod1, bmod1, 0)
        S.barrier()
        es_f.close()

        es_qo = scoped(S)
        qoT = S.sbuf("qoT", [128, 8, NT], BF16, side="right")
        bqo = [[S.buf("qo") for _ in range(NT // 512)] for _ in range(8)]
        es_q = scoped(S)
        wq_r = Ring(S, "wq", [128, KC, 1536], BF16, 1)
        wqt, bwq = load_w(C, wq_r, w_qkv, D, 0, 1536)
        cs_r = Ring(S, "cs", [128, 2, 512], F32, 2)
        rotm = S.sbuf("rotm", [128, 128], F32)
        rotb = S.sbuf("rotb", [128, 128], BF16)
        brot = S.buf("rot")
        S.dma("sync", rotm[:], rotm_d, writes=[brot])
        S.op("vector", lambda e: e.tensor_copy(rotb[:], rotm[:]), reads=[brot], writes=[brot])
        C.rstd = Ring(S, "rstd2", [128, 512], F32, 2)
        qst = Ring(S, "qst", [128, 512], BF16, 2)
        qg = Ring(S, "qg", [128, 512], F32, 2)
        qgb = Ring(S, "qgb", [128, 512], BF16, 2)
        qsq = Ring(S, "qsq", [128, 512], BF16, 2)
        qt1 = Ring(S, "qt1", [128, 512], F32, 2)
        vst = Ring(S, "vst", [128, 256], BF16, 2)
        bklat = [S.buf("klat") for _ in range(2)]
        bkall = [S.buf("kall") for _ in range(2)]
        bkctx = S.buf("kctx")
        bvlat = [S.buf("vlat") for _ in range(2)]
        bvall = [S.buf("vall") for _ in range(2)]
        bvctx = S.buf("vctx")

        def head_mm(hd, c0, n, which):
            rh = tile_bufs(bh, c0, n)
            pq, bpq = C.ps.next()
            for k in range(KC):
                S.op("tensor", lambda e, k=k: e.matmul(
                    pq[:, :n], wqt[:, k, hd * 128:(hd + 1) * 128], hT[:, k, c0:c0 + n], start=(k == 0), stop=(k == KC - 1)),
                    reads=rh + [bwq], writes=[bpq], sig=(k == KC - 1))
            return (hd, c0, n, which, pq, bpq)

        def head_proj(hd, c0, n, which, pq=None, bpq=None):
            if pq is None:
                _, _, _, _, pq, bpq = head_mm(hd, c0, n, which)
            gname = "q_g" if hd < 8 else "k_g"
            sqt, bsqt = qsq.next()
            S.op("scalar", lambda e: e.activation(sqt[:, :n], pq[:, :n], AF.Square), reads=[bpq], writes=[bsqt])
            pss, bpss = C.ps.next()
            S.op("tensor", lambda e: e.matmul(pss[:, :n], C.ones[:], sqt[:, :n], start=True, stop=True),
                 reads=[bsqt, C.bones], writes=[bpss])
            rs, brs = C.rstd.next()
            S.op("scalar", lambda e: e.activation(rs[:, :n], pss[:, :n], AF.Sqrt, bias=C.epsb[:, 0:1], scale=1.0 / 128),
                 reads=[bpss, C.bepsb], writes=[brs])
            S.op("vector", lambda e: e.reciprocal(rs[:, :n], rs[:, :n]), reads=[brs], writes=[brs])
            qn, bqn = qg.next()
            S.op("vector", lambda e: e.scalar_tensor_tensor(
                qn[:, :n], pq[:, :n], V(C, gname, 0), rs[:, :n], ALU.mult, ALU.mult), reads=[bpq, C.bvec, brs], writes=[bqn])
            if hd < 8:
                dst, bdst = qoT[:, hd, c0:c0 + n], [bqo[hd][c0 // 512]]
            else:
                qo_, bqo_ = qst.next()
                dst, bdst = qo_[:, :n], [bqo_]
            if which == 0:
                cs, bcs = head_proj.cs
                qb, bqb = qgb.next()
                S.op("gpsimd", lambda e: e.tensor_copy(qb[:, :n], qn[:, :n]), reads=[bqn], writes=[bqb])
                pr, bpr = C.ps.next()
                S.op("tensor", lambda e: e.matmul(pr[:, :n], rotb[:], qb[:, :n], start=True, stop=True),
                     reads=[bqb, brot], writes=[bpr])
                t1, bt1 = qt1.next()
                S.op("vector", lambda e: e.tensor_tensor(t1[:, :n], pr[:, :n], cs[:, 1, :n], ALU.mult), reads=[bpr, bcs], writes=[bt1])
                S.op("gpsimd", lambda e: e.tensor_tensor(qn[:, :n], qn[:, :n], cs[:, 0, :n], ALU.mult), reads=[bqn, bcs], writes=[bqn])
                S.op("vector", lambda e: e.tensor_tensor(dst, qn[:, :n], t1[:, :n], ALU.add), reads=[bqn, bt1], writes=bdst)
            else:
                S.op("vector", lambda e: e.tensor_copy(dst, qn[:, :n]), reads=[bqn], writes=bdst)
            if hd >= 8:
                g = hd - 8
                if which == 0:
                    S.dma("sync", klat[g][:, c0:c0 + n], dst, reads=bdst, writes=[bklat[g]], nowaw=True)
                else:
                    S.dma("sync", kctx[:, g * NCX:(g + 1) * NCX], dst, reads=bdst, writes=[bkctx], nowaw=True)

        def load_cs(c0, n):
            cs, bcs = cs_r.next()
            S.dma("sync", cs[:, 0, :n], cos_d[:, c0:c0 + n], writes=[bcs])
            S.dma("sync", cs[:, 1, :n], sin_d[:, c0:c0 + n], writes=[bcs], nowaw=True)
            head_proj.cs = (cs, bcs)
        for (c0, n, which) in BL:
            if which == 0:
                load_cs(c0, n)
            for hd in (8, 9):
                head_proj(hd, c0, n, which)
        for g in range(2):
            S.collective("AllGather", GROUPS, klat[g], kall[g], [bklat[g]], bkall[g])
        for (c0, n, which) in BL:
            rh = tile_bufs(bh, c0, n)
            for t0 in range(0, n, 128):
                pv, bpv = C.ps.next()
                for k in range(KC):
                    S.op("tensor", lambda e, pv=pv, k=k, t0=t0, c0=c0: e.matmul(
                        pv[:, 0:256], hT[:, k, c0 + t0:c0 + t0 + 128], wqt[:, k, 1280:1536], start=(k == 0), stop=(k == KC - 1)),
                        reads=rh + [bwq], writes=[bpv], sig=(k == KC - 1))
                vo, bvo = vst.next()
                copy_op(C, evac_engine(C), vo[:], pv[:, 0:256], [bpv], [bvo])
                if which == 0:
                    t = c0 + t0
                    hh = t // (NT // 2)
                    r0 = t % (NT // 2)
                    S.dma("sync", vlat[hh][r0:r0 + 128, :], vo[:], reads=[bvo], writes=[bvlat[hh]], nowaw=True)
                else:
                    S.dma("sync", vctx[t0:t0 + 128, :], vo[:], reads=[bvo], writes=[bvctx], nowaw=True)
        for hh in range(2):
            S.collective("AllGather", GROUPS, vlat[hh], vall[hh], [bvlat[hh]], bvall[hh])
        for (c0, n, which) in LAT_BLOCKS:
            load_cs(c0, n)
            pend = head_mm(0, c0, n, which)
            for hd in range(8):
                cur = pend
                if hd + 1 < 8:
                    pend = head_mm(hd + 1, c0, n, which)
                head_proj(*cur)
        S.barrier()
        es_q.close()
        es_l0.close()

        es_a = scoped(S)
        kT = S.sbuf("kT", [128, 2, NKEY], BF16)
        bk = S.buf("kT")
        va = S.sbuf("va", [128, NKT, 2, 130], BF16)
        bva = S.buf("va")
        S.op("vector", lambda e: e.memset(va[:, :, :, 128:130], 1.0), writes=[bva])
        for g in range(2):
            S.dma("sync", kT[:, g, 0:NCX], kctx[:, g * NCX:(g + 1) * NCX], reads=[bkctx], writes=[bk], nowaw=True)
            for r in range(4):
                S.dma("sync", kT[:, g, NCX + r * NT:NCX + (r + 1) * NT], kall[g][r * 128:(r + 1) * 128, :], reads=[bkall[g]], writes=[bk], nowaw=True)
            S.dma("sync", va[:, 0:2, g, 0:128], vctx[:, g * 128:(g + 1) * 128].rearrange("(t p) d -> p t d", p=128), reads=[bvctx], writes=[bva], nowaw=True)
            for hh in range(2):
                for r in range(4):
                    kt0 = 2 + r * 16 + hh * 8
                    S.dma("sync", va[:, kt0:kt0 + 8, g, 0:128],
                          vall[hh][r * (NT // 2):(r + 1) * (NT // 2), g * 128:(g + 1) * 128].rearrange("(t p) d -> p t d", p=128),
                          reads=[bvall[hh]], writes=[bva], nowaw=True)
        pT_r = Ring(S, "pT", [128, 512], BF16, 4)
        on_r = Ring(S, "on", [128, 128], BF16, 2)
        ri_r = Ring(S, "ri", [128, 2], F32, 2)
        SCL = 1.0 / float(np.sqrt(128.0))
        psO = SubRing(C.ps.tiles[0:4])
        psS = SubRing(C.ps.tiles[4:7])
        psT = SubRing(C.ps.tiles[7:8])
        for qb in range(NT // 512):
            for h in range(8):
                g = h // 4
                po = [psO.next(), psO.next()]

                def s_issue(kt, g=g, h=h, qb=qb):
                    ps, bps = psS.next()
                    S.op("tensor", lambda e, ps=ps, kt=kt: e.matmul(
                        ps[:], kT[:, g, kt * 128:(kt + 1) * 128], qoT[:, h, qb * 512:(qb + 1) * 512], start=True, stop=True),
                        reads=[bk, bqo[h][qb]], writes=[bps])
                    pT, bpT = pT_r.next()
                    S.op("scalar", lambda e, pT=pT, ps=ps: e.activation(pT[:], ps[:], AF.Exp, scale=SCL), reads=[bps], writes=[bpT])
                    return pT, bpT
                pend = [s_issue(0), s_issue(1)]
                for kt in range(NKT):
                    pT, bpT = pend.pop(0)
                    if kt + 2 < NKT:
                        pend.append(s_issue(kt + 2))
                    for qt in range(4):
                        pot, bpot = po[qt // 2]
                        c = (qt % 2) * 129
                        S.op("tensor", lambda e, pot=pot, c=c, pT=pT, qt=qt, kt=kt, g=g: e.matmul(
                            pot[:, c:c + 129], pT[:, qt * 128:(qt + 1) * 128], va[:, kt, g, 0:129], start=(kt == 0 and qt % 2 == 0), stop=(kt == NKT - 1),
                            skip_group_check=True),
                            reads=[bpT, bva], writes=[bpot], sig=(qt == 3))
                for qt in range(4):
                    pot, bpot = po[qt // 2]
                    c = (qt % 2) * 129
                    ri, bri = ri_r.next()
                    S.op("vector", lambda e, ri=ri, pot=pot, c=c: e.reciprocal(ri[:, 0:1], pot[:, c + 128:c + 129]), reads=[bpot], writes=[bri])
                    on, bon = on_r.next()
                    S.op("vector", lambda e, on=on, pot=pot, c=c, ri=ri: e.tensor_scalar(on[:], pot[:, c:c + 128], ri[:, 0:1], None, ALU.mult),
                         reads=[bpot, bri], writes=[bon])
                    pt, bpt = psT.next()
                    ptb = pt[:].bitcast(BF16)
                    S.op("tensor", lambda e, ptb=ptb, on=on: e.transpose(ptb[:, 0:128], on[:], identb[:]), reads=[bon, bidb], writes=[bpt])
                    col = qb * 512 + qt * 128
                    S.op("scalar", lambda e, ptb=ptb, h=h, col=col: e.copy(qoT[:, h, col:col + 128], ptb[:, 0:128]), reads=[bpt], writes=[bqo[h][qb]])
        S.barrier()
        es_a.close()

        es_o = scoped(S)
        wo_r = Ring(S, "wo2", [128, KC, D], BF16, 1)
        wot, bwo = load_w(C, wo_r, w_o, D, 0, D)
        bqo_cols = [None] * (NT // 128)
        for (c0, n, which) in LAT_BLOCKS:
            ri = [bqo[h][c0 // 512] for h in range(8)]
            wx = tile_bufs(bx, c0, n)
            for dc in range(KC):
                po_, bpo_ = C.ps.next()
                for k in range(KC):
                    S.op("tensor", lambda e, po_=po_, k=k, dc=dc, c0=c0, n=n: e.matmul(
                        po_[:, :n], wot[:, k, dc * 128:(dc + 1) * 128], qoT[:, k, c0:c0 + n], start=(k == 0), stop=(k == KC - 1)),
                        reads=ri + [bwo], writes=[bpo_], sig=(k == KC - 1))
                S.op("vector", lambda e, po_=po_, dc=dc, c0=c0, n=n: e.scalar_tensor_tensor(
                    xT[:, dc, c0:c0 + n], po_[:, :n], mod1[:, 2 * 8 + dc, 0:1], xT[:, dc, c0:c0 + n], ALU.mult, ALU.add),
                    reads=[bpo_, bmod1], writes=wx)
        S.barrier()
        es_o.close()
        es_qo.close()

        S.es = es
        gates = S.sbuf("gates", [128, NT // 128, NE], F32)
        maskt = S.sbuf("maskt", [128, NT // 128, NE], F32)
        bgates = S.buf("gates")
        es_h2 = scoped(S)
        hT2 = S.sbuf("hT2", [128, KC, NT], BF16)
        bh2 = [S.buf("hT2") for _ in range(NT // 128)]
        es_r = scoped(S)
        alloc_norm_tmps(C)
        hf = S.sbuf("hf", [128, KC, NT], F32)
        bhf = [S.buf("hf") for _ in range(NT // 128)]
        emit_norm_blocks(C, xT, bx, hT2, bh2, LAT_BLOCKS, gs4, bgs4, mod1, bmod1, 3, hf=hf, bhf=bhf)
        emit_router(C, hf, bhf, router, gates, bgates, maskt=maskt)
        S.barrier()
        es_r.close()

        es_t = scoped(S)
        bhtm = emit_htm(C, hT2, bh2, htm_d, identb, bidb)
        S.barrier()
        es_t.close()
        es_h2.close()
        es_m = scoped(S)
        emit_moe_sparse(C, bhtm, xT, bx, maskt, gates, bgates, cst_d, htm_d, m_w1, m_w3, m_w2, mod1, bmod1, identb, bidb)
        S.barrier()
        es_m.close()

        es_z = scoped(S)
        emit_final(C, xT, bx, fin_g, out_d, outb)
        S.finish()
        S.emit()
        es_z.close()
    return nc


def rope_tables():
    nfreq = 32
    t = np.arange(SEQ)
    row = (t // 64).astype(np.float32)
    col = (t % 64).astype(np.float32)
    freqs = (np.float32(10000.0) ** (-np.arange(nfreq, dtype=np.float32) / np.float32(nfreq))).astype(np.float32)
    ang = np.zeros((128, SEQ), np.float32)
    for d in range(128):
        pos = row if d < 64 else col
        ang[d] = pos * freqs[d % 32]
    return np.cos(ang).astype(np.float32), np.sin(ang).astype(np.float32)


def rot_matrix_T():
    R = np.zeros((128, 128), np.float32)
    for m in range(128):
        if m % 64 < 32:
            R[m, m + 32] = -1.0
        else:
            R[m, m - 32] = 1.0
    return np.ascontiguousarray(R.T)


_CACHE = {}
_DEBUG = {}


def _prog(name):
    if name not in _CACHE:
        _CACHE[name] = {"A": lambda: build_l0("A"), "B": lambda: build_l0("B"), "C": build_l1, "F": build_fused}[name]()
    return _CACHE[name]


def kernel_unfused(**inp):
    inp = {k: np.asarray(v) for k, v in inp.items()}
    x = inp["x"].astype(np.float32, copy=False)
    ctx = inp["ctx"].astype(np.float32, copy=False)
    ident = np.eye(128, dtype=np.float32)
    cores = list(range(8))
    maps0 = []
    for c in cores:
        b, j = c // 4, c % 4
        t0 = j * NT
        hal = np.zeros((128, D), np.float32)
        if j > 0:
            hal[0:2] = x[b, t0 - 2:t0]
        if j < 3:
            hal[2] = x[b, t0 + NT]
        xall = np.concatenate([x[b, t0:t0 + NT], ctx[b], hal], axis=0)
        maps0.append({
            "xall": np.ascontiguousarray(xall), "vecs": pack_vecs(inp, b, j), "ident": ident,
            "ada_w": inp["ada_w"], "w_in": inp["rg_w_in"][0], "w_a": inp["rg_w_a"][0], "w_i": inp["rg_w_i"][0],
        })
    resA = run_bass_kernel_spmd(_prog("A"), maps0, core_ids=cores).results
    cosf, sinf = rope_tables()
    rotm = rot_matrix_T()
    maps1 = []
    for c in cores:
        b, j = c // 4, c % 4
        m = dict(maps0[c])
        m["summ_all"] = np.ascontiguousarray(np.concatenate([resA[b * 4 + i]["summ"] for i in range(4)], axis=1))
        m.update({"w_out": inp["rg_w_out"][0], "f_w1": inp["ffn_w1"][0], "f_w3": inp["ffn_w3"][0], "f_w2": inp["ffn_w2"][0],
                  "w_qkv": inp["attn_w_qkv"][0], "cos": np.ascontiguousarray(cosf[:, j * NT:(j + 1) * NT]),
                  "sin": np.ascontiguousarray(sinf[:, j * NT:(j + 1) * NT]), "rotm": rotm})
        maps1.append(m)
    resB = run_bass_kernel_spmd(_prog("B"), maps1, core_ids=cores).results
    if _DEBUG.get("stop") == "B":
        return resA, resB
    maps2 = []
    for c in cores:
        b, j = c // 4, c % 4
        kparts, vparts = [], []
        for g in range(2):
            segs = [resB[b * 4]["kT"][:, g * NSC + NT:(g + 1) * NSC]]
            segs += [resB[b * 4 + i]["kT"][:, g * NSC:g * NSC + NT] for i in range(4)]
            kparts.append(np.concatenate(segs, axis=1))
        kfull = np.ascontiguousarray(np.concatenate(kparts, axis=1))
        vfull = np.ascontiguousarray(np.concatenate([resB[b * 4]["vtm"][NT:NSC]] + [resB[b * 4 + i]["vtm"][0:NT] for i in range(4)], axis=0))
        maps2.append({
            "vecs": maps0[c]["vecs"], "ident": ident, "ada_w": inp["ada_w"],
            "x1T": resB[c]["x1T"], "qT": resB[c]["qT"], "kT": kfull, "vtm": vfull,
            "w_o": inp["attn_w_o"][0], "router": inp["moe_router"][0],
            "m_w1": inp["moe_w1"][0], "m_w3": inp["moe_w3"][0], "m_w2": inp["moe_w2"][0], "fin_g": inp["final_g"],
        })
    resC = run_bass_kernel_spmd(_prog("C"), maps2, core_ids=cores).results
    out = np.zeros((2, SEQ, D), np.float32)
    for c in cores:
        b, j = c // 4, c % 4
        out[b, j * NT:(j + 1) * NT] = resC[c]["out"]
    return out


def kernel(**inp):
    inp = {k: np.asarray(v) for k, v in inp.items()}
    x = inp["x"].astype(np.float32, copy=False)
    ctx = inp["ctx"].astype(np.float32, copy=False)
    ident = np.eye(128, dtype=np.float32)
    cosf, sinf = rope_tables()
    rotm = rot_matrix_T()
    cst = np.zeros((128, 385), np.float32)
    cst[:, 0:128] = np.triu(np.ones((128, 128), np.float32), 1)
    cst[:, 128:384] = np.arange(256, dtype=np.float32)[None, :]
    cst[:, 384] = np.arange(128, dtype=np.float32)
    cores = list(range(8))
    maps = []
    for c in cores:
        b, j = c // 4, c % 4
        t0 = j * NT
        hal = np.zeros((128, D), np.float32)
        if j > 0:
            hal[0:2] = x[b, t0 - 2:t0]
        if j < 3:
            hal[2] = x[b, t0 + NT]
        xall = np.concatenate([x[b, t0:t0 + NT], ctx[b], hal], axis=0)
        maps.append({
            "xall": np.ascontiguousarray(xall), "vecs": pack_vecs(inp, b, j), "ident": ident,
            "ada_w": inp["ada_w"], "w_in": inp["rg_w_in"][0], "w_a": inp["rg_w_a"][0], "w_i": inp["rg_w_i"][0],
            "w_out": inp["rg_w_out"][0], "f_w1": inp["ffn_w1"][0], "f_w3": inp["ffn_w3"][0], "f_w2": inp["ffn_w2"][0],
            "w_qkv": inp["attn_w_qkv"][0], "cos": np.ascontiguousarray(cosf[:, j * NT:(j + 1) * NT]),
            "sin": np.ascontiguousarray(sinf[:, j * NT:(j + 1) * NT]), "rotm": rotm,
            "w_o": inp["attn_w_o"][0], "router": inp["moe_router"][0],
            "m_w1": inp["moe_w1"][0], "m_w3": inp["moe_w3"][0], "m_w2": inp["moe_w2"][0], "fin_g": inp["final_g"],
            "cst": cst,
        })
    res = run_bass_kernel_spmd(_prog("F"), maps, core_ids=cores).results
    out = np.zeros((2, SEQ, D), np.float32)
    for c in cores:
        b, j = c // 4, c % 4
        out[b, j * NT:(j + 1) * NT] = res[c]["out"]
    return out
```

```python
from contextlib import ExitStack
import numpy as np
import ml_dtypes
import concourse.bass as bass
import concourse.mybir as mybir
from concourse.bass_utils import run_bass_kernel_spmd

F32 = mybir.dt.float32
BF16 = mybir.dt.bfloat16
AF = mybir.ActivationFunctionType
ALU = mybir.AluOpType
AX = mybir.AxisListType

D = 1024
KC = 8
NT = 2048
NCX = 256
LAT0, CTX0, HAL0 = 0, NT, NT + NCX
TOT = NT + NCX + 128
DFF = 2816
DFE = 3584
NE = 8
EPS = 1e-6
SEQ = 8192
NKEY = SEQ + NCX
NKT = NKEY // 128


class Buf:
    __slots__ = ("name", "w", "r", "sem", "cnt")

    def __init__(self, name):
        self.name = name
        self.w = None
        self.r = []
        self.sem = None
        self.cnt = 0


class Sched:
    ENG = ("tensor", "vector", "scalar", "gpsimd", "sync")

    def __init__(self, nc, es):
        self.nc = nc
        self.es = es
        self.es_sem = es
        self.ops = {e: [] for e in self.ENG}
        self.cnt = {e: 0 for e in self.ENG}
        self.seen = {e: {} for e in self.ENG}
        self.esem = {e: es.enter_context(nc.semaphore("s_" + e)) for e in self.ENG}
        self.dsems = []
        self.out_tokens = []
        self.nbuf = 0
        self.cond = None
        self.regs = {e: [es.enter_context(getattr(nc, e).register(f"r{i}_" + e)) for i in range(3)] for e in self.ENG}

    def sbuf(self, name, shape, dtype, side=None):
        self.nbuf += 1
        name = f"sb{self.nbuf}_{name}"
        if side is None:
            return self.es.enter_context(self.nc.sbuf_tensor(name, list(shape), dtype))
        return self.es.enter_context(self.nc.sbuf_tensor(name, list(shape), dtype, side=side))

    def psum(self, name, shape, dtype=F32):
        self.nbuf += 1
        name = f"pp{self.nbuf}_{name}"
        return self.es.enter_context(self.nc.psum_tensor(name, list(shape), dtype))

    def buf(self, name="b"):
        self.nbuf += 1
        return Buf(f"{name}{self.nbuf}")

    def _waits(self, engine, deps):
        need = {}
        for tok in deps:
            if tok is None:
                continue
            key, val = tok
            if isinstance(key, str):
                if key == engine and engine in ("tensor", "sync"):
                    continue
                sem = self.esem[key]
                assert val <= self.cnt[key], f"dep on unsignaled op of {key}"
            else:
                sem = key
            k = id(sem)
            if self.seen[engine].get(k, 0) >= val:
                continue
            if k not in need or need[k][1] < val:
                need[k] = (sem, val)
        for k, (sem, val) in need.items():
            self.seen[engine][k] = val
        return list(need.values())

    def op(self, engine, fn, reads=(), writes=(), sig=True):
        deps = []
        for b in reads:
            deps.append(b.w)
        for b in writes:
            deps.append(b.w)
            deps.extend(b.r)
        waits = self._waits(engine, deps)
        if sig:
            self.cnt[engine] += 1
            tok = (engine, self.cnt[engine])
            inc = (self.esem[engine], 1)
        else:
            tok = (engine, self.cnt[engine] + 1)
            inc = None
        self.ops[engine].append((fn, waits, inc, None))
        for b in reads:
            b.r.append(tok)
        for b in writes:
            b.w = tok
            b.r = []
        return tok

    def collective(self, kind, groups, in_ap, out_ap, reads, out_buf):
        deps = [b.w for b in reads] + [out_buf.w] + list(out_buf.r)
        waits = self._waits("gpsimd", deps)
        if out_buf.sem is None:
            out_buf.sem = self.es_sem.enter_context(self.nc.semaphore("c_" + out_buf.name))
            self.dsems.append(out_buf)
        out_buf.cnt += 1
        tok = (out_buf.sem, out_buf.cnt)

        def fn(eng):
            return eng.collective_compute(kind, ALU.bypass, replica_groups=groups, ins=[in_ap], outs=[out_ap])
        self.ops["gpsimd"].append((fn, waits, (out_buf.sem, 1), out_buf))
        for b in reads:
            b.r.append(tok)
        out_buf.w = tok
        out_buf.r = []
        return tok

    def dma(self, queue, out, in_, reads=(), writes=(), out_sem_buf=None, nowaw=False, **kw):
        deps = []
        for b in reads:
            deps.append(b.w)
        for b in writes:
            if not (nowaw and b.w is not None and b.sem is not None and b.w[0] is b.sem):
                deps.append(b.w)
            deps.extend(b.r)
        waits = self._waits(queue, deps)
        holder = writes[0] if writes else out_sem_buf
        if holder.sem is None:
            holder.sem = self.es_sem.enter_context(self.nc.semaphore("d_" + holder.name))
            self.dsems.append(holder)
        holder.cnt += 16
        tok = (holder.sem, holder.cnt)

        def fn(eng, out=out, in_=in_, kw=kw):
            return eng.dma_start(out=out, in_=in_, **kw)
        self.ops[queue].append((fn, waits, (holder.sem, 16), holder))
        for b in reads:
            b.r.append(tok)
        for b in writes:
            b.w = tok
            b.r = []
        if not writes:
            self.out_tokens.append(tok)
        return tok

    def barrier(self):
        toks = [(e, self.cnt[e]) for e in self.ENG if self.cnt[e] > 0]
        toks += [(h.sem, h.cnt) for h in self.dsems]
        for e in self.ENG:
            waits = self._waits(e, toks)
            if waits:
                self.ops[e].append((None, waits, None, None))

    def finish(self):
        waits = self._waits("sync", self.out_tokens)
        self.ops["sync"].append((None, waits, None, None))

    def begin_cond(self, cnt_ap, cnt_buf, thr, key=None):
        if self.cond is None:
            self.cond = []
        self.cond.append(dict(ap=cnt_ap, buf=cnt_buf, thr=thr, key=key, start={e: len(self.ops[e]) for e in self.ENG},
                              cnt0=dict(self.cnt), hcnt0={id(h): h.cnt for h in self.dsems},
                              seen0={e: dict(self.seen[e]) for e in self.ENG}))

    def end_cond(self):
        c = self.cond.pop()

        def collect(body, hinc):
            for (fn, w, inc, holder) in body:
                if fn == "cond":
                    collect(inc[2], hinc)
                elif fn is not None and holder is not None:
                    k = id(holder)
                    if k not in hinc:
                        hinc[k] = [holder, c["hcnt0"].get(k, 0), 0]
                    hinc[k][2] += inc[1]
        for e in self.ENG:
            body = self.ops[e][c["start"][e]:]
            if not body:
                continue
            del self.ops[e][c["start"][e]:]
            self.seen[e] = c["seen0"][e]
            waits = self._waits(e, [c["buf"].w])
            nsig = self.cnt[e] - c["cnt0"][e]
            hinc = {}
            collect(body, hinc)
            self.ops[e].append(("cond", waits, (c["ap"], c["thr"], body, c["cnt0"][e], nsig, list(hinc.values()), c["key"]), None))

    def emit(self):
        with self.nc.Block() as block:
            loaded = {}

            def mk(engine):
                def run(eng, ops, depth=0):
                    for fn, waits, inc, _h in ops:
                        for sem, val in waits:
                            eng.wait_ge(sem, val)
                        if fn is None:
                            continue
                        if fn == "cond":
                            ap, thr, sub, cnt0, nsig, hincs, key = inc
                            reg = self.regs[engine][0]
                            if key is None or loaded.get(engine) != key:
                                eng.reg_load(reg, ap)
                                loaded[engine] = key if depth == 0 else None
                            with eng.If_lt(reg, thr + 1):
                                if nsig:
                                    if cnt0:
                                        eng.wait_ge(self.esem[engine], cnt0)
                                    eng.sem_inc(self.esem[engine], nsig)
                                for holder, before, tot in hincs:
                                    if before:
                                        eng.wait_ge(holder.sem, before)
                                    eng.sem_inc(holder.sem, tot)
                            with eng.Else():
                                run(eng, sub, depth + 1)
                            continue
                        ins = fn(eng)
                        if inc is not None:
                            ins.then_inc(inc[0], inc[1])

                def body(eng):
                    run(eng, self.ops[engine])
                return body
            for e in self.ENG:
                if self.ops[e]:
                    getattr(block, e)(mk(e))


class Ring:
    def __init__(self, S, name, shape, dtype, n, psum=False):
        self.tiles = []
        S.nbuf += 1
        name = f"{name}_{S.nbuf}_"
        for i in range(n):
            t = S.psum(f"{name}{i}", shape, dtype) if psum else S.sbuf(f"{name}{i}", shape, dtype)
            self.tiles.append((t, S.buf(name)))
        self.i = 0

    def next(self):
        t = self.tiles[self.i % len(self.tiles)]
        self.i += 1
        return t


def bcast_mid(ap2d, n):
    a = ap2d.ap
    return bass.AP(ap2d.tensor, ap2d.offset, [list(a[0]), [0, n], list(a[1])])


def rev_ap(ap2d):
    a = [list(p) for p in ap2d.ap]
    n = a[-1][1]
    a[-1] = [-a[-1][0], n]
    return bass.AP(ap2d.tensor, ap2d.offset + (n - 1) * ap2d.ap[-1][0], a)


class VecPack:
    def __init__(self):
        self.cols = {}
        self.n = 0

    def add(self, name, ncols):
        self.cols[name] = (self.n, ncols)
        self.n += ncols

    def sl(self, name, j=0, w=1):
        o, n = self.cols[name]
        assert j + w <= n
        return slice(o + j, o + j + w)


def chunked(v):
    v = np.asarray(v, np.float32)
    return np.ascontiguousarray(v.reshape(-1, 128).T)


VP = VecPack()
for _n, _c in [("cv", 16), ("ada_b0", 48), ("ada_b1", 48), ("n1g0", 8), ("n2g0", 8), ("n1g1", 8), ("n2g1", 8),
               ("convw", 32), ("convb", 8), ("b_a", 16), ("b_i", 16), ("lam", 16), ("q_g", 1), ("k_g", 1),
               ("hmask", 3), ("sel_f", 4), ("sel_r", 4)]:
    VP.add(_n, _c)


def pack_vecs(inp, b, j):
    v = np.zeros((128, VP.n), np.float32)

    def put(name, arr):
        o, n = VP.cols[name]
        arr = np.asarray(arr, np.float32).reshape(128, n)
        v[:, o:o + n] = arr
    cl = chunked(inp["c"][b])
    cc = chunked(inp["c_ctx"])
    put("cv", np.stack([cl, cc], axis=2).reshape(128, 16))
    put("ada_b0", chunked(inp["ada_b"][0]))
    put("ada_b1", chunked(inp["ada_b"][1]))
    put("n1g0", chunked(inp["norm1_g"][0]))
    put("n2g0", chunked(inp["norm2_g"][0]))
    put("n1g1", chunked(inp["norm1_g"][1]))
    put("n2g1", chunked(inp["norm2_g"][1]))
    put("convw", np.concatenate([chunked(inp["rg_conv_w"][0, k]) for k in range(4)], axis=1))
    put("convb", chunked(inp["rg_conv_b"][0]))
    put("b_a", np.concatenate([chunked(inp["rg_b_a"][0, d]) for d in range(2)], axis=1))
    put("b_i", np.concatenate([chunked(inp["rg_b_i"][0, d]) for d in range(2)], axis=1))
    put("lam", np.concatenate([chunked(inp["rg_lam"][0, d]) for d in range(2)], axis=1))
    put("q_g", np.asarray(inp["attn_q_g"][0]).reshape(128, 1))
    put("k_g", np.asarray(inp["attn_k_g"][0]).reshape(128, 1))
    hm = np.array([1.0 if j > 0 else 0.0, 1.0 if j > 0 else 0.0, 1.0 if j < 3 else 0.0], np.float32)
    put("hmask", np.broadcast_to(hm, (128, 3)))
    put("sel_f", np.broadcast_to(np.array([1.0 if i < j else 0.0 for i in range(4)], np.float32), (128, 4)))
    put("sel_r", np.broadcast_to(np.array([1.0 if i > j else 0.0 for i in range(4)], np.float32), (128, 4)))
    return v


class Ctx:
    pass


def setup_common(nc, es, vecs_d, ident_d, ps_ring=True):
    C = Ctx()
    C.nc = nc
    S = C.S = Sched(nc, es)
    if ps_ring:
        C.ps = Ring(S, "ps", [128, 512], F32, 8, psum=True)
    C.vec = S.sbuf("vec", [128, VP.n], F32)
    C.bvec = S.buf("vec")
    S.dma("sync", C.vec[:], vecs_d, writes=[C.bvec])
    C.ident = S.sbuf("ident", [128, 128], F32)
    C.bident = S.buf("ident")
    S.dma("sync", C.ident[:], ident_d, writes=[C.bident])
    C.ones = S.sbuf("ones", [128, 128], BF16)
    C.bones = S.buf("ones")
    S.op("vector", lambda e: e.memset(C.ones[:], 1.0), writes=[C.bones])
    C.epsb = S.sbuf("epsb", [128, 1], F32)
    C.bepsb = S.buf("epsb")
    S.op("vector", lambda e: e.memset(C.epsb[:], EPS), writes=[C.bepsb])
    C.alt = 0
    C.ada_queue = "sync"
    C.ada_dt = F32
    C.ada_ring = 2
    return C


def V(C, name, j=0, w=1):
    return C.vec[:, VP.sl(name, j, w)]


def evac_engine(C):
    C.alt += 1
    return "vector" if C.alt % 2 else "scalar"


def copy_op(C, eng, out, in_, reads, writes):
    S = C.S
    if eng == "scalar":
        S.op("scalar", lambda e: e.copy(out, in_), reads=reads, writes=writes)
    else:
        S.op(eng, lambda e: e.tensor_copy(out, in_), reads=reads, writes=writes)


def emit_ada(C, ada_w_d, layer, bias_name, out_mod, bmod):
    S = C.S
    nc = C.nc
    ps, bps = C.ps.next()
    wr = Ring(S, f"adaw{layer}", [128, KC, 768], C.ada_dt, C.ada_ring)
    for g in range(8):
        wt, bw = wr.next()
        src = ada_w_d[layer].rearrange("(k p) n -> p k n", p=128)[:, :, g * 768:(g + 1) * 768]
        S.dma(C.ada_queue, wt[:], src, writes=[bw])
        for fi in range(6):
            f = g * 6 + fi
            for k in range(KC):
                S.op("tensor", lambda e, wt=wt, fi=fi, k=k, f=f: e.matmul(
                    ps[:, f * 2:f * 2 + 2], wt[:, k, fi * 128:(fi + 1) * 128], C.sv_mm[:, k * 2:k * 2 + 2],
                    start=(k == 0), stop=(k == KC - 1)),
                    reads=[bw, C.bsv], writes=[bps], sig=(k == KC - 1))
    bo, bn = VP.cols[bias_name]
    S.op("vector", lambda e: e.tensor_tensor(
        out_mod[:], ps[:, 0:96].rearrange("p (f c) -> p f c", c=2),
        bass.AP(C.vec[:].tensor, C.vec[:, bo:bo + 48].offset, [list(C.vec[:].ap[0]), [1, 48], [0, 2]]), ALU.add),
        reads=[bps, C.bvec], writes=[bmod])


def emit_silu_c(C):
    S = C.S
    C.sv = S.sbuf("sv", [128, 16], F32)
    C.bsv = S.buf("sv")
    S.op("scalar", lambda e: e.activation(C.sv[:], V(C, "cv", 0, 16), AF.Silu), reads=[C.bvec], writes=[C.bsv])
    C.sv_mm = C.sv
    if C.ada_dt != F32:
        C.sv_mm = S.sbuf("svb", [128, 16], C.ada_dt)
        S.op("vector", lambda e: e.tensor_copy(C.sv_mm[:], C.sv[:]), reads=[C.bsv], writes=[C.bsv])


def emit_gs(C, mod, bmod, sc_idx, gname, name):
    S = C.S
    gs = S.sbuf(name, [128, KC, 2], F32)
    bgs = S.buf(name)
    go, _ = VP.cols[gname]
    gb = bass.AP(C.vec[:].tensor, C.vec[:, go:go + 8].offset, [list(C.vec[:].ap[0]), [1, 8], [0, 2]])
    S.op("vector", lambda e: e.scalar_tensor_tensor(
        gs[:], mod[:, sc_idx * 8:(sc_idx + 1) * 8, :], 1.0, gb, ALU.add, ALU.mult),
        reads=[bmod, C.bvec], writes=[bgs])
    return gs, bgs


def emit_ada_dma(C, ada_w_d, layer, g, wr):
    wt, bw = wr.next()
    src = ada_w_d[layer].rearrange("(k p) n -> p k n", p=128)[:, :, g * 768:(g + 1) * 768]
    C.S.dma(C.ada_queue, wt[:], src, writes=[bw])
    return wt, bw


def emit_ada_mm(C, g, wt, bw, bias_name, out_mod, bmod):
    S = C.S
    ps, bps = C.ps.next()
    for fi in range(6):
        for k in range(KC):
            S.op("tensor", lambda e, fi=fi, k=k: e.matmul(
                ps[:, fi * 2:fi * 2 + 2], wt[:, k, fi * 128:(fi + 1) * 128], C.sv_mm[:, k * 2:k * 2 + 2],
                start=(k == 0), stop=(k == KC - 1)),
                reads=[bw, C.bsv], writes=[bps], sig=(k == KC - 1))
    bo, bn = VP.cols[bias_name]
    S.op("vector", lambda e: e.tensor_tensor(
        out_mod[:, g * 6:(g + 1) * 6, :], ps[:, 0:12].rearrange("p (f c) -> p f c", c=2),
        bass.AP(C.vec[:].tensor, C.vec[:, bo + g * 6:bo + g * 6 + 6].offset, [list(C.vec[:].ap[0]), [1, 6], [0, 2]]), ALU.add),
        reads=[bps, C.bvec], writes=[bmod])


def emit_gs_into(C, gs, bgs, mod, bmod, sc_idx, gname):
    S = C.S
    go, _ = VP.cols[gname]
    gb = bass.AP(C.vec[:].tensor, C.vec[:, go:go + 8].offset, [list(C.vec[:].ap[0]), [1, 8], [0, 2]])
    S.op("vector", lambda e: e.scalar_tensor_tensor(
        gs[:], mod[:, sc_idx * 8:(sc_idx + 1) * 8, :], 1.0, gb, ALU.add, ALU.mult),
        reads=[bmod, C.bvec], writes=[bgs])


def tile_bufs(bufs, c0, n):
    return bufs[c0 // 128:(c0 + n + 127) // 128]


def bcast_cols(ap_col, n):
    return bass.AP(ap_col.tensor, ap_col.offset, [list(ap_col.ap[0]), [0, n]])


def scoped(S):
    es = ExitStack()
    S.es = es
    return es


def alloc_norm_tmps(C):
    S = C.S
    C.sq = Ring(S, "sq", [128, 512], BF16, 2)
    C.rstd = Ring(S, "rstd", [128, 512], F32, 3)
    C.ntmp = Ring(S, "ntmp", [128, 512], F32, 2)


def emit_norm_stats(C, xs, bxs, s0, n):
    S = C.S
    rx = tile_bufs(bxs, s0, n)
    ps, bps = C.ps.next()
    for k in range(KC):
        sq, bsq = C.sq.next()
        if k % 2 == 0:
            S.op("gpsimd", lambda e, sq=sq, k=k: e.tensor_tensor(sq[:, :n], xs[:, k, s0:s0 + n], xs[:, k, s0:s0 + n], ALU.mult),
                 reads=rx, writes=[bsq])
        else:
            S.op("scalar", lambda e, sq=sq, k=k: e.activation(sq[:, :n], xs[:, k, s0:s0 + n], AF.Square), reads=rx, writes=[bsq])
        S.op("tensor", lambda e, sq=sq, k=k: e.matmul(ps[:, :n], C.ones[:], sq[:, :n], start=(k == 0), stop=(k == KC - 1)),
             reads=[bsq, C.bones], writes=[bps])
    rs, brs = C.rstd.next()
    S.op("scalar", lambda e: e.activation(rs[:, :n], ps[:, :n], AF.Sqrt, bias=C.epsb[:, 0:1], scale=1.0 / D), reads=[bps, C.bepsb], writes=[brs])
    S.op("vector", lambda e: e.reciprocal(rs[:, :n], rs[:, :n]), reads=[brs], writes=[brs])
    return rs, brs


def emit_norm_apply(C, rs, brs, xs, bxs, s0, hT, bh, c0, n, gs, bgs, mod, bmod, sh_idx, which, hf=None, bhf=None):
    S = C.S
    rx = tile_bufs(bxs, s0, n)
    wh = tile_bufs(bh, c0, n)
    for k in range(KC):
        tmp, btmp = C.ntmp.next()
        S.op("vector", lambda e, tmp=tmp, k=k: e.tensor_tensor(tmp[:, :n], xs[:, k, s0:s0 + n], rs[:, :n], ALU.mult),
             reads=rx + [brs], writes=[btmp])
        S.op("scalar", lambda e, tmp=tmp, k=k: e.activation(
            hT[:, k, c0:c0 + n], tmp[:, :n], AF.Identity,
            bias=mod[:, sh_idx * 8 + k, which:which + 1], scale=gs[:, k, which:which + 1]),
            reads=[btmp, bgs, bmod], writes=wh)
        if hf is not None:
            S.op("vector", lambda e, tmp=tmp, k=k: e.scalar_tensor_tensor(
                hf[:, k, c0:c0 + n], tmp[:, :n], gs[:, k, which:which + 1],
                bcast_cols(mod[:, sh_idx * 8 + k, which:which + 1], n), ALU.mult, ALU.add),
                reads=[btmp, bgs, bmod], writes=tile_bufs(bhf, c0, n))


def emit_norm(C, xs, bxs, s0, hT, bh, c0, n, gs, bgs, mod, bmod, sh_idx, which, hf=None, bhf=None):
    rs, brs = emit_norm_stats(C, xs, bxs, s0, n)
    emit_norm_apply(C, rs, brs, xs, bxs, s0, hT, bh, c0, n, gs, bgs, mod, bmod, sh_idx, which, hf, bhf)


def emit_norm_blocks(C, xs, bxs, hT, bh, blocks, gs, bgs, mod, bmod, sh_idx, hf=None, bhf=None):
    st = emit_norm_stats(C, xs, bxs, blocks[0][0], blocks[0][1])
    for i, (c0, n, which) in enumerate(blocks):
        cur = st
        if i + 1 < len(blocks):
            st = emit_norm_stats(C, xs, bxs, blocks[i + 1][0], blocks[i + 1][1])
        emit_norm_apply(C, cur[0], cur[1], xs, bxs, c0, hT, bh, c0, n, gs, bgs, mod, bmod, sh_idx, which, hf, bhf)


def emit_load_xT(C, x_d, row0, ntiles, dst, bdst, dcol0):
    S = C.S
    for i in range(ntiles):
        xt, bxt = C.xtile.next()
        S.dma("sync", xt[:], x_d[row0 + i * 128:row0 + (i + 1) * 128, :], writes=[bxt])
        c0 = dcol0 + i * 128
        for half in range(2):
            ps, bps = C.ps.next()
            for j in range(4):
                kc = half * 4 + j
                S.op("tensor", lambda e, ps=ps, j=j, kc=kc, xt=xt: e.transpose(
                    ps[:, j * 128:(j + 1) * 128], xt[:, kc * 128:(kc + 1) * 128], C.ident[:]),
                    reads=[bxt, C.bident], writes=[bps], sig=(j == 3))
            copy_op(C, evac_engine(C), dst[:, half * 4:half * 4 + 4, c0:c0 + 128],
                    ps[:].rearrange("p (j t) -> p j t", j=4), [bps], [bdst[c0 // 128]])


def load_w(C, ring, w_d, rows, c0, ncols, queue="gpsimd"):
    wt, bw = ring.next()
    kc = rows // 128
    src = w_d.rearrange("(k p) n -> p k n", p=128)[:, :, c0:c0 + ncols]
    C.S.dma(queue, wt[:, :kc, :ncols], src, writes=[bw])
    return wt, bw


def alloc_ffn(C, SL):
    S = C.S
    C.SL = SL
    C.w13 = Ring(S, "w13", [128, KC, SL * 128], BF16, 4)
    C.w2r = Ring(S, "w2", [128, SL, D], BF16, 2)
    C.hid = Ring(S, "hid", [128, SL, 512], BF16, 2)
    C.sil = Ring(S, "sil", [128, 512], F32, 3)


def emit_ffn(C, hT, bh, xT, bx, blocks, w1_d, w3_d, w2_d, dff, mod, bmod, g_idx, gate=None):
    S = C.S
    nfc = dff // 128
    SL = C.SL
    pending = [None]
    for s0 in range(0, nfc, SL):
        sl = min(SL, nfc - s0)
        w1t, bw1 = load_w(C, C.w13, w1_d, D, s0 * 128, sl * 128)
        w3t, bw3 = load_w(C, C.w13, w3_d, D, s0 * 128, sl * 128)
        w2t, bw2 = C.w2r.next()
        S.dma("gpsimd", w2t[:, :sl, :], w2_d[s0 * 128:(s0 + sl) * 128, :].rearrange("(f p) n -> p f n", p=128), writes=[bw2])
        for (c0, n, which) in blocks:
            rh = tile_bufs(bh, c0, n)
            hid, bhid = C.hid.next()
            for fi in range(sl):
                pa, bpa = C.ps.next()
                for k in range(KC):
                    S.op("tensor", lambda e, pa=pa, k=k, fi=fi, w1t=w1t, c0=c0, n=n: e.matmul(
                        pa[:, :n], w1t[:, k, fi * 128:(fi + 1) * 128], hT[:, k, c0:c0 + n], start=(k == 0), stop=(k == KC - 1)),
                        reads=rh + [bw1], writes=[bpa], sig=(k == KC - 1))
                pb, bpb = C.ps.next()
                for k in range(KC):
                    S.op("tensor", lambda e, pb=pb, k=k, fi=fi, w3t=w3t, c0=c0, n=n: e.matmul(
                        pb[:, :n], w3t[:, k, fi * 128:(fi + 1) * 128], hT[:, k, c0:c0 + n], start=(k == 0), stop=(k == KC - 1)),
                        reads=rh + [bw3], writes=[bpb], sig=(k == KC - 1))
                sa, bsa = C.sil.next()
                S.op("scalar", lambda e, sa=sa, pa=pa, n=n: e.activation(sa[:, :n], pa[:, :n], AF.Silu), reads=[bpa], writes=[bsa])
                if gate is None:
                    S.op("vector", lambda e, sa=sa, pb=pb, hid=hid, fi=fi, n=n: e.tensor_tensor(hid[:, fi, :n], pb[:, :n], sa[:, :n], ALU.mult),
                         reads=[bpb, bsa], writes=[bhid])
                else:
                    gt, bgt = gate
                    S.op("vector", lambda e, sa=sa, pb=pb, n=n: e.tensor_tensor(sa[:, :n], pb[:, :n], sa[:, :n], ALU.mult),
                         reads=[bpb, bsa], writes=[bsa])
                    S.op("gpsimd", lambda e, sa=sa, hid=hid, fi=fi, gt=gt, c0=c0, n=n: e.tensor_tensor(hid[:, fi, :n], sa[:, :n], gt[:, c0:c0 + n], ALU.mult),
                         reads=[bsa, bgt], writes=[bhid])
            def down(c0=c0, n=n, which=which, hid=hid, bhid=bhid, w2t=w2t, bw2=bw2, sl=sl):
                wx = tile_bufs(bx, c0, n)
                for dc in range(KC):
                    po, bpo = C.ps.next()
                    for fi in range(sl):
                        S.op("tensor", lambda e, po=po, fi=fi, dc=dc: e.matmul(
                            po[:, :n], w2t[:, fi, dc * 128:(dc + 1) * 128], hid[:, fi, :n], start=(fi == 0), stop=(fi == sl - 1)),
                            reads=[bhid, bw2], writes=[bpo], sig=(fi == sl - 1))
                    S.op("vector", lambda e, po=po, dc=dc: e.scalar_tensor_tensor(
                        xT[:, dc, c0:c0 + n], po[:, :n], mod[:, g_idx * 8 + dc, which:which + 1], xT[:, dc, c0:c0 + n], ALU.mult, ALU.add),
                        reads=[bpo, bmod], writes=wx)
            if pending[0] is not None:
                pending[0]()
            pending[0] = down
    if pending[0] is not None:
        pending[0]()


def emit_proj_residual(C, inT, bin_, nk, w_t, bw, xT, bx, blocks, mod, bmod, g_idx):
    S = C.S
    for (c0, n, which) in blocks:
        ri = tile_bufs(bin_, c0, n)
        wx = tile_bufs(bx, c0, n)
        for dc in range(KC):
            po, bpo = C.ps.next()
            for k in range(nk):
                S.op("tensor", lambda e, po=po, k=k, dc=dc, c0=c0, n=n: e.matmul(
                    po[:, :n], w_t[:, k, dc * 128:(dc + 1) * 128], inT[:, k, c0:c0 + n], start=(k == 0), stop=(k == nk - 1)),
                    reads=ri + [bw], writes=[bpo], sig=(k == nk - 1))
            S.op("vector", lambda e, po=po, dc=dc, c0=c0, n=n, which=which: e.scalar_tensor_tensor(
                xT[:, dc, c0:c0 + n], po[:, :n], mod[:, g_idx * 8 + dc, which:which + 1], xT[:, dc, c0:c0 + n], ALU.mult, ALU.add),
                reads=[bpo, bmod], writes=wx)


def emit_mods(C, es, ada_w, layers):
    S = C.S
    nc = C.nc
    emit_silu_c(C)
    out = {}
    for l in layers:
        out[l] = (es.enter_context(nc.sbuf_tensor(f"mod{l}", [128, 48, 2], F32)), S.buf("mod"))
    es_ada = scoped(S)
    for l in layers:
        emit_ada(C, ada_w, l, f"ada_b{l}", out[l][0], out[l][1])
    S.barrier()
    es_ada.close()
    S.es = es
    return out


LAT_BLOCKS = [(0, 512, 0), (512, 512, 0), (1024, 512, 0), (1536, 512, 0)]
CTX_BLOCK = (CTX0, NCX, 1)
HAL_BLOCK = (HAL0, 128, 0)
NSC = NT + NCX


def build_l0(phase):
    nc = bass.Bass("TRN2", target_bir_lowering=False)

    def din(name, shape, dt=F32):
        return nc.dram_tensor(name, list(shape), dt, kind="ExternalInput").ap()

    def dout(name, shape, dt=F32):
        return nc.dram_tensor(name, list(shape), dt, kind="ExternalOutput").ap()
    xall = din("xall", [TOT, D])
    vecs = din("vecs", [128, VP.n])
    ident = din("ident", [128, 128])
    ada_w = din("ada_w", [2, D, 6 * D])
    w_in = din("w_in", [D, 2 * D])
    w_a = din("w_a", [2, 8, 128, 128])
    w_i = din("w_i", [2, 8, 128, 128])
    if phase == "A":
        summ_o = dout("summ", [128, 32])
    else:
        summ_all = din("summ_all", [128, 4 * 32])
        w_out = din("w_out", [D, D])
        f_w1 = din("f_w1", [D, DFF])
        f_w3 = din("f_w3", [D, DFF])
        f_w2 = din("f_w2", [DFF, D])
        w_qkv = din("w_qkv", [D, 1536])
        cos_d = din("cos", [128, NT])
        sin_d = din("sin", [128, NT])
        rotm_d = din("rotm", [128, 128])
        x1_o = dout("x1T", [128, KC * NT])
        q_o = dout("qT", [128, 8 * NT], BF16)
        k_o = dout("kT", [128, 2 * NSC], BF16)
        v_o = dout("vtm", [NSC, 256], BF16)

    with ExitStack() as es:
        C = setup_common(nc, es, vecs, ident)
        S = C.S
        outb = S.buf("out")
        mods = emit_mods(C, es, ada_w, [0] if phase == "A" else [0, 1])
        mod0, bmod0 = mods[0]
        gs1, bgs1 = emit_gs(C, mod0, bmod0, 1, "n1g0", "gs1")
        hT = S.sbuf("hT", [128, KC, TOT], BF16)
        bh = [S.buf("hT") for _ in range(TOT // 128)]

        es1 = scoped(S)
        C.xtile = Ring(S, "xtile", [128, D], F32, 2)
        alloc_norm_tmps(C)
        xblk = Ring(S, "xblk", [128, KC, 512], F32, 2)
        for (c0, n, which) in LAT_BLOCKS + [CTX_BLOCK, HAL_BLOCK]:
            xb_, bxb_ = xblk.next()
            bl = [bxb_] * 4
            emit_load_xT(C, xall, c0, n // 128, xb_, bl, 0)
            emit_norm(C, xb_, bl, 0, hT, bh, c0, n, gs1, bgs1, mod0, bmod0, 0, which)
        S.barrier()
        es1.close()

        es_yg = scoped(S)
        if phase == "B":
            ygT = S.sbuf("ygT", [128, KC, NSC], BF16)
            byg = [S.buf("yg") for _ in range(NSC // 128)]
        es_mix = scoped(S)
        win_r = Ring(S, "win", [128, KC, 256], BF16, 2)
        wg_r = Ring(S, "wg", [128, 4, 128], BF16, 2)
        xbe = Ring(S, "xbe", [128, NT + 3 + NCX + 3], F32, 1)
        xc_r = Ring(S, "xc", [128, NSC], F32, 1)
        xcb_r = Ring(S, "xcb", [128, NSC], BF16, 1)
        r_r = Ring(S, "rr", [128, NSC], F32, 1)
        b_r = Ring(S, "bb", [128, NSC], F32, 1)
        a_r = Ring(S, "aa", [128, NSC], F32, 1)
        m_r = Ring(S, "mm", [128, NSC], F32, 1)
        y_r = Ring(S, "yy", [128, NSC], F32, 2)
        sm_r = Ring(S, "sm", [128, 8], F32, 2)
        gtmp = Ring(S, "gtmp", [128, 512], F32, 3)
        cneg = S.sbuf("cneg", [128, 32], F32)
        bcneg = S.buf("cneg")
        S.op("scalar", lambda e: e.activation(cneg[:, 0:16], V(C, "lam", 0, 16), AF.Exp, scale=-1.0), reads=[C.bvec], writes=[bcneg])
        S.op("scalar", lambda e: e.activation(cneg[:, 0:16], cneg[:, 0:16], AF.Ln, bias=1.0), reads=[bcneg], writes=[bcneg])
        S.op("vector", lambda e: e.tensor_scalar(cneg[:, 16:32], cneg[:, 0:16], -16.0, None, ALU.mult), reads=[bcneg], writes=[bcneg])
        S.op("vector", lambda e: e.tensor_scalar(cneg[:, 0:16], cneg[:, 0:16], -8.0, None, ALU.mult), reads=[bcneg], writes=[bcneg])
        if phase == "A":
            summ = S.sbuf("summ", [128, 32], F32)
            bsumm = S.buf("summ")
        else:
            sall = S.sbuf("sall", [128, 4 * 32], F32)
            bsall = S.buf("sall")
            S.dma("sync", sall[:], summ_all, writes=[bsall])
        LB = NT + 3
        w_in_v = w_in.rearrange("(k p) n -> p k n", p=128)
        for ct in range(KC):
            wt, bw = win_r.next()
            S.dma("gpsimd", wt[:, :, 0:128], w_in_v[:, :, ct * 128:(ct + 1) * 128], writes=[bw])
            S.dma("gpsimd", wt[:, :, 128:256], w_in_v[:, :, D + ct * 128:D + (ct + 1) * 128], writes=[bw])
            wg, bwg = wg_r.next()
            for d in range(2):
                S.dma("gpsimd", wg[:, d, :], w_a[d, ct], writes=[bwg])
                S.dma("gpsimd", wg[:, 2 + d, :], w_i[d, ct], writes=[bwg])
            xe, bxe = xbe.next()
            S.op("gpsimd", lambda e, xe=xe: e.memset(xe[:, LB:LB + 2], 0.0), writes=[bxe])
            S.op("gpsimd", lambda e, xe=xe: e.memset(xe[:, LB + 2 + NCX:LB + 3 + NCX], 0.0), writes=[bxe])
            for (c0, n, which) in LAT_BLOCKS + [CTX_BLOCK, (HAL0, 3, 0)]:
                ps, bps = C.ps.next()
                for k in range(KC):
                    S.op("tensor", lambda e, ps=ps, k=k, wt=wt, c0=c0, n=n: e.matmul(
                        ps[:, :n], wt[:, k, 0:128], hT[:, k, c0:c0 + n], start=(k == 0), stop=(k == KC - 1)),
                        reads=tile_bufs(bh, c0, n) + [bw], writes=[bps], sig=(k == KC - 1))
                if c0 < CTX0:
                    copy_op(C, evac_engine(C), xe[:, 2 + c0:2 + c0 + n], ps[:, :n], [bps], [bxe])
                elif c0 == CTX0:
                    copy_op(C, evac_engine(C), xe[:, LB + 2:LB + 2 + NCX], ps[:, :n], [bps], [bxe])
                else:
                    S.op("vector", lambda e, ps=ps, xe=xe: e.tensor_tensor(xe[:, 0:2], ps[:, 0:2], V(C, "hmask", 0, 2), ALU.mult),
                         reads=[bps, C.bvec], writes=[bxe])
                    S.op("vector", lambda e, ps=ps, xe=xe: e.tensor_tensor(xe[:, 2 + NT:3 + NT], ps[:, 2:3], V(C, "hmask", 2, 1), ALU.mult),
                         reads=[bps, C.bvec], writes=[bxe])
            xc, bxc = xc_r.next()
            for (dst0, src0, n) in [(0, 0, NT), (NT, LB, NCX)]:
                S.op("scalar", lambda e, xc=xc, xe=xe, dst0=dst0, src0=src0, n=n, ct=ct: e.activation(
                    xc[:, dst0:dst0 + n], xe[:, src0:src0 + n], AF.Identity,
                    bias=V(C, "convb", ct), scale=V(C, "convw", ct)), reads=[bxe, C.bvec], writes=[bxc])
                for k in range(1, 4):
                    S.op("vector", lambda e, xc=xc, xe=xe, dst0=dst0, src0=src0, n=n, k=k, ct=ct: e.scalar_tensor_tensor(
                        xc[:, dst0:dst0 + n], xe[:, src0 + k:src0 + k + n], V(C, "convw", k * 8 + ct), xc[:, dst0:dst0 + n],
                        ALU.mult, ALU.add), reads=[bxe, C.bvec, bxc], writes=[bxc])
            xcb, bxcb = xcb_r.next()
            S.op("gpsimd", lambda e, xcb=xcb, xc=xc: e.tensor_copy(xcb[:], xc[:]), reads=[bxc], writes=[bxcb])
            ys = []
            for d in range(2):
                rr, brr = r_r.next()
                bb, bbb = b_r.next()
                for (c0, n) in [(0, 512), (512, 512), (1024, 512), (1536, 512), (NT, NCX)]:
                    pr, bpr = C.ps.next()
                    S.op("tensor", lambda e, pr=pr, wg=wg, d=d, xcb=xcb, c0=c0, n=n: e.matmul(
                        pr[:, :n], wg[:, d, :], xcb[:, c0:c0 + n], start=True, stop=True), reads=[bwg, bxcb], writes=[bpr])
                    S.op("scalar", lambda e, pr=pr, rr=rr, c0=c0, n=n, d=d, ct=ct: e.activation(
                        rr[:, c0:c0 + n], pr[:, :n], AF.Sigmoid, bias=V(C, "b_a", d * 8 + ct)), reads=[bpr, C.bvec], writes=[brr])
                    pi, bpi = C.ps.next()
                    S.op("tensor", lambda e, pi=pi, wg=wg, d=d, xcb=xcb, c0=c0, n=n: e.matmul(
                        pi[:, :n], wg[:, 2 + d, :], xcb[:, c0:c0 + n], start=True, stop=True), reads=[bwg, bxcb], writes=[bpi])
                    S.op("scalar", lambda e, pi=pi, bb=bb, c0=c0, n=n, d=d, ct=ct: e.activation(
                        bb[:, c0:c0 + n], pi[:, :n], AF.Sigmoid, bias=V(C, "b_i", d * 8 + ct)), reads=[bpi, C.bvec], writes=[bbb])
                aa, baa = a_r.next()
                mm, bmm = m_r.next()
                cn = d * 8 + ct
                S.op("scalar", lambda e, aa=aa, rr=rr, cn=cn: e.activation(aa[:], rr[:], AF.Exp, scale=cneg[:, cn:cn + 1]),
                     reads=[brr, bcneg], writes=[baa])
                S.op("scalar", lambda e, mm=mm, rr=rr, cn=cn: e.activation(mm[:], rr[:], AF.Exp, scale=cneg[:, 16 + cn:17 + cn]),
                     reads=[brr, bcneg], writes=[bmm])
                S.op("scalar", lambda e, mm=mm: e.activation(mm[:], mm[:], AF.Sqrt, bias=1.0, scale=-1.0), reads=[bmm], writes=[bmm])
                S.op("gpsimd", lambda e, bb=bb, xc=xc: e.tensor_tensor(bb[:], bb[:], xc[:], ALU.mult), reads=[bbb, bxc], writes=[bbb])
                S.op("vector", lambda e, bb=bb, mm=mm: e.tensor_tensor(bb[:], bb[:], mm[:], ALU.mult), reads=[bbb, bmm], writes=[bbb])
                yy, byy = y_r.next()
                sm, bsm = sm_r.next()
                if phase == "A":
                    if d == 0:
                        S.op("vector", lambda e, yy=yy, aa=aa, bb=bb: e.tensor_tensor_scan(
                            yy[:, 0:NT], aa[:, 0:NT], bb[:, 0:NT], 0.0, ALU.mult, ALU.add), reads=[baa, bbb], writes=[byy])
                        hend = yy[:, NT - 1:NT]
                    else:
                        S.op("vector", lambda e, yy=yy, aa=aa, bb=bb: e.tensor_tensor_scan(
                            rev_ap(yy[:, 0:NT]), rev_ap(aa[:, 0:NT]), rev_ap(bb[:, 0:NT]), 0.0, ALU.mult, ALU.add),
                            reads=[baa, bbb], writes=[byy])
                        hend = yy[:, 0:1]
                    S.op("vector", lambda e, sm=sm, rr=rr: e.tensor_reduce(sm[:, 0:1], rr[:, 0:NT], AX.X, ALU.add), reads=[brr], writes=[bsm])
                    o = ct * 4 + d * 2
                    S.op("scalar", lambda e, sm=sm, cn=cn, o=o: e.activation(summ[:, o:o + 1], sm[:, 0:1], AF.Exp, scale=cneg[:, cn:cn + 1]),
                         reads=[bsm, bcneg], writes=[bsumm])
                    S.op("vector", lambda e, hend=hend, o=o: e.tensor_copy(summ[:, o + 1:o + 2], hend), reads=[byy], writes=[bsumm])
                else:
                    if d == 0:
                        S.op("vector", lambda e, yy=yy, aa=aa, bb=bb: e.tensor_tensor_scan(
                            yy[:, NT:NSC], aa[:, NT:NSC], bb[:, NT:NSC], 0.0, ALU.mult, ALU.add), reads=[baa, bbb], writes=[byy])
                        st0 = yy[:, NSC - 1:NSC]
                        order = [0, 1, 2, 3]
                        sel = "sel_f"
                    else:
                        S.op("vector", lambda e, yy=yy, aa=aa, bb=bb: e.tensor_tensor_scan(
                            rev_ap(yy[:, NT:NSC]), rev_ap(aa[:, NT:NSC]), rev_ap(bb[:, NT:NSC]), 0.0, ALU.mult, ALU.add),
                            reads=[baa, bbb], writes=[byy])
                        st0 = yy[:, NT:NT + 1]
                        order = [3, 2, 1, 0]
                        sel = "sel_r"
                    S.op("vector", lambda e, sm=sm, st0=st0: e.tensor_copy(sm[:, 0:1], st0), reads=[byy], writes=[bsm])
                    for i in order:
                        o = i * 32 + ct * 4 + d * 2
                        S.op("vector", lambda e, sm=sm, o=o: e.scalar_tensor_tensor(
                            sm[:, 1:2], sm[:, 0:1], sall[:, o:o + 1], sall[:, o + 1:o + 2], ALU.mult, ALU.add), reads=[bsm, bsall], writes=[bsm])
                        S.op("vector", lambda e, sm=sm: e.tensor_tensor(sm[:, 1:2], sm[:, 1:2], sm[:, 0:1], ALU.subtract), reads=[bsm], writes=[bsm])
                        S.op("vector", lambda e, sm=sm, i=i, sel=sel: e.scalar_tensor_tensor(
                            sm[:, 0:1], sm[:, 1:2], V(C, sel, i), sm[:, 0:1], ALU.mult, ALU.add), reads=[bsm, C.bvec], writes=[bsm])
                    if d == 0:
                        S.op("vector", lambda e, yy=yy, aa=aa, bb=bb, sm=sm: e.tensor_tensor_scan(
                            yy[:, 0:NT], aa[:, 0:NT], bb[:, 0:NT], sm[:, 0:1], ALU.mult, ALU.add), reads=[baa, bbb, bsm], writes=[byy])
                    else:
                        S.op("vector", lambda e, yy=yy, aa=aa, bb=bb, sm=sm: e.tensor_tensor_scan(
                            rev_ap(yy[:, 0:NT]), rev_ap(aa[:, 0:NT]), rev_ap(bb[:, 0:NT]), sm[:, 0:1], ALU.mult, ALU.add),
                            reads=[baa, bbb, bsm], writes=[byy])
                    ys.append((yy, byy))
            if phase == "B":
                (y0, by0), (y1, by1) = ys
                S.op("gpsimd", lambda e, y0=y0, y1=y1: e.tensor_tensor(y0[:], y0[:], y1[:], ALU.add), reads=[by0, by1], writes=[by0])
                for (c0, n, which) in LAT_BLOCKS + [CTX_BLOCK]:
                    pg, bpg = C.ps.next()
                    for k in range(KC):
                        S.op("tensor", lambda e, pg=pg, k=k, wt=wt, c0=c0, n=n: e.matmul(
                            pg[:, :n], wt[:, k, 128:256], hT[:, k, c0:c0 + n], start=(k == 0), stop=(k == KC - 1)),
                            reads=tile_bufs(bh, c0, n) + [bw], writes=[bpg], sig=(k == KC - 1))
                    t1, bt1 = gtmp.next()
                    S.op("scalar", lambda e, t1=t1, pg=pg, n=n: e.activation(t1[:, :n], pg[:, :n], AF.Gelu_apprx_tanh), reads=[bpg], writes=[bt1])
                    S.op("vector", lambda e, t1=t1, y0=y0, c0=c0, n=n, ct=ct: e.tensor_tensor(ygT[:, ct, c0:c0 + n], t1[:, :n], y0[:, c0:c0 + n], ALU.mult),
                         reads=[bt1, by0], writes=tile_bufs(byg, c0, n))
        if phase == "A":
            S.dma("sync", summ_o, summ[:], reads=[bsumm], out_sem_buf=outb)
            S.finish()
            S.emit()
            es_mix.close()
            es_yg.close()
            return nc
        S.barrier()
        es_mix.close()

        S.es = es
        xT = S.sbuf("xT", [128, KC, NSC], F32, side="right")
        bx = [S.buf("xT") for _ in range(NSC // 128)]
        es_o = scoped(S)
        C.xtile = Ring(S, "xtile", [128, D], F32, 2)
        emit_load_xT(C, xall, 0, NSC // 128, xT, bx, 0)
        wo_r = Ring(S, "wo", [128, KC, D], BF16, 1)
        wot, bwo = load_w(C, wo_r, w_out, D, 0, D)
        BL = LAT_BLOCKS + [CTX_BLOCK]
        emit_proj_residual(C, ygT, byg, KC, wot, bwo, xT, bx, BL, mod0, bmod0, 2)
        S.barrier()
        es_o.close()
        es_yg.close()

        es_f = scoped(S)
        gs2, bgs2 = emit_gs(C, mod0, bmod0, 4, "n2g0", "gs2")
        alloc_norm_tmps(C)
        for (c0, n, which) in BL:
            emit_norm(C, xT, bx, c0, hT, bh, c0, n, gs2, bgs2, mod0, bmod0, 3, which)
        alloc_ffn(C, 4)
        emit_ffn(C, hT, bh, xT, bx, BL, f_w1, f_w3, f_w2, DFF, mod0, bmod0, 5)
        for k in range(KC):
            S.dma("sync", x1_o[:, k * NT:(k + 1) * NT], xT[:, k, 0:NT], reads=bx[0:NT // 128], out_sem_buf=outb)
        mod1, bmod1 = mods[1]
        gs3, bgs3 = emit_gs(C, mod1, bmod1, 1, "n1g1", "gs3")
        for (c0, n, which) in BL:
            emit_norm(C, xT, bx, c0, hT, bh, c0, n, gs3, bgs3, mod1, bmod1, 0, which)
        S.barrier()
        es_f.close()

        es_q = scoped(S)
        wq_r = Ring(S, "wq", [128, KC, 1536], BF16, 1)
        wqt, bwq = load_w(C, wq_r, w_qkv, D, 0, 1536)
        cosT = S.sbuf("cosT", [128, NT], F32)
        sinT = S.sbuf("sinT", [128, NT], F32)
        bcs = S.buf("cs")
        S.dma("sync", cosT[:], cos_d, writes=[bcs])
        S.dma("sync", sinT[:], sin_d, writes=[bcs])
        rotm = S.sbuf("rotm", [128, 128], F32)
        rotb = S.sbuf("rotb", [128, 128], BF16)
        brot = S.buf("rot")
        S.dma("sync", rotm[:], rotm_d, writes=[brot])
        S.op("vector", lambda e: e.tensor_copy(rotb[:], rotm[:]), reads=[brot], writes=[brot])
        C.rstd = Ring(S, "rstd2", [128, 512], F32, 3)
        qst = Ring(S, "qst", [128, 512], BF16, 3)
        qg = Ring(S, "qg", [128, 512], F32, 2)
        qgb = Ring(S, "qgb", [128, 512], BF16, 2)
        qsq = Ring(S, "qsq", [128, 512], BF16, 2)
        qt1 = Ring(S, "qt1", [128, 512], F32, 2)
        vst = Ring(S, "vst", [128, 256], BF16, 2)
        for (c0, n, which) in BL:
            rh = tile_bufs(bh, c0, n)
            for hd in range(10):
                if hd < 8 and which == 1:
                    continue
                gname = "q_g" if hd < 8 else "k_g"
                pq, bpq = C.ps.next()
                for k in range(KC):
                    S.op("tensor", lambda e, pq=pq, k=k, hd=hd, c0=c0, n=n: e.matmul(
                        pq[:, :n], wqt[:, k, hd * 128:(hd + 1) * 128], hT[:, k, c0:c0 + n], start=(k == 0), stop=(k == KC - 1)),
                        reads=rh + [bwq], writes=[bpq], sig=(k == KC - 1))
                sqt, bsqt = qsq.next()
                S.op("scalar", lambda e, sqt=sqt, pq=pq, n=n: e.activation(sqt[:, :n], pq[:, :n], AF.Square), reads=[bpq], writes=[bsqt])
                pss, bpss = C.ps.next()
                S.op("tensor", lambda e, pss=pss, sqt=sqt, n=n: e.matmul(pss[:, :n], C.ones[:], sqt[:, :n], start=True, stop=True),
                     reads=[bsqt, C.bones], writes=[bpss])
                rs, brs = C.rstd.next()
                S.op("scalar", lambda e, rs=rs, pss=pss, n=n: e.activation(rs[:, :n], pss[:, :n], AF.Sqrt, bias=C.epsb[:, 0:1], scale=1.0 / 128),
                     reads=[bpss, C.bepsb], writes=[brs])
                S.op("vector", lambda e, rs=rs, n=n: e.reciprocal(rs[:, :n], rs[:, :n]), reads=[brs], writes=[brs])
                qn, bqn = qg.next()
                S.op("vector", lambda e, qn=qn, pq=pq, rs=rs, gname=gname, n=n: e.scalar_tensor_tensor(
                    qn[:, :n], pq[:, :n], V(C, gname, 0), rs[:, :n], ALU.mult, ALU.mult), reads=[bpq, C.bvec, brs], writes=[bqn])
                qo, bqo = qst.next()
                if which == 0:
                    qb, bqb = qgb.next()
                    S.op("gpsimd", lambda e, qb=qb, qn=qn, n=n: e.tensor_copy(qb[:, :n], qn[:, :n]), reads=[bqn], writes=[bqb])
                    pr, bpr = C.ps.next()
                    S.op("tensor", lambda e, pr=pr, qb=qb, n=n: e.matmul(pr[:, :n], rotb[:], qb[:, :n], start=True, stop=True),
                         reads=[bqb, brot], writes=[bpr])
                    t1, bt1 = qt1.next()
                    S.op("vector", lambda e, t1=t1, pr=pr, c0=c0, n=n: e.tensor_tensor(t1[:, :n], pr[:, :n], sinT[:, c0:c0 + n], ALU.mult), reads=[bpr, bcs], writes=[bt1])
                    S.op("gpsimd", lambda e, qn=qn, c0=c0, n=n: e.tensor_tensor(qn[:, :n], qn[:, :n], cosT[:, c0:c0 + n], ALU.mult), reads=[bqn, bcs], writes=[bqn])
                    S.op("vector", lambda e, qo=qo, qn=qn, t1=t1, n=n: e.tensor_tensor(qo[:, :n], qn[:, :n], t1[:, :n], ALU.add), reads=[bqn, bt1], writes=[bqo])
                else:
                    S.op("vector", lambda e, qo=qo, qn=qn, n=n: e.tensor_copy(qo[:, :n], qn[:, :n]), reads=[bqn], writes=[bqo])
                if hd < 8:
                    S.dma("sync", q_o[:, hd * NT + c0:hd * NT + c0 + n], qo[:, :n], reads=[bqo], out_sem_buf=outb)
                else:
                    S.dma("sync", k_o[:, (hd - 8) * NSC + c0:(hd - 8) * NSC + c0 + n], qo[:, :n], reads=[bqo], out_sem_buf=outb)
            for t0 in range(0, n, 128):
                pv, bpv = C.ps.next()
                for k in range(KC):
                    S.op("tensor", lambda e, pv=pv, k=k, t0=t0, c0=c0: e.matmul(
                        pv[:, 0:256], hT[:, k, c0 + t0:c0 + t0 + 128], wqt[:, k, 1280:1536], start=(k == 0), stop=(k == KC - 1)),
                        reads=rh + [bwq], writes=[bpv], sig=(k == KC - 1))
                vo, bvo = vst.next()
                copy_op(C, evac_engine(C), vo[:], pv[:, 0:256], [bpv], [bvo])
                S.dma("sync", v_o[c0 + t0:c0 + t0 + 128, :], vo[:], reads=[bvo], out_sem_buf=outb)
        S.finish()
        S.emit()
        es_q.close()
    return nc


def build_l1():
    nc = bass.Bass("TRN2", target_bir_lowering=False)

    def din(name, shape, dt=F32):
        return nc.dram_tensor(name, list(shape), dt, kind="ExternalInput").ap()

    def dout(name, shape, dt=F32):
        return nc.dram_tensor(name, list(shape), dt, kind="ExternalOutput").ap()
    vecs = din("vecs", [128, VP.n])
    ident = din("ident", [128, 128])
    ada_w = din("ada_w", [2, D, 6 * D])
    x1_d = din("x1T", [128, KC * NT])
    q_d = din("qT", [128, 8 * NT], BF16)
    k_d = din("kT", [128, 2 * NKEY], BF16)
    v_d = din("vtm", [NKEY, 256], BF16)
    w_o = din("w_o", [D, D])
    router = din("router", [D, NE])
    m_w1 = din("m_w1", [NE, D, DFE])
    m_w3 = din("m_w3", [NE, D, DFE])
    m_w2 = din("m_w2", [NE, DFE, D])
    fin_g = din("fin_g", [D])
    out_d = dout("out", [NT, D])

    with ExitStack() as es:
        C = setup_common(nc, es, vecs, ident, ps_ring=False)
        S = C.S
        outb = S.buf("out")
        identb = S.sbuf("identb", [128, 128], BF16)
        bidb = S.buf("identb")
        S.op("vector", lambda e: e.tensor_copy(identb[:], C.ident[:]), reads=[C.bident], writes=[bidb])
        es_oT = scoped(S)
        oT = S.sbuf("oT", [128, 8, NT], BF16, side="right")
        boT = [S.buf("oT") for _ in range(NT // 128)]
        S.es = es

        es_a = scoped(S)
        psS = Ring(S, "psS", [128, 512], F32, 3, psum=True)
        psO = Ring(S, "psO", [128, 512], F32, 4, psum=True)
        psT = Ring(S, "psT", [128, 1024], BF16, 1, psum=True)
        kT = S.sbuf("kT", [128, 2, NKEY], BF16)
        bk = S.buf("kT")
        S.dma("sync", kT[:], k_d.rearrange("p (g s) -> p g s", g=2), writes=[bk])
        va = S.sbuf("va", [128, NKT, 2, 130], BF16)
        bva = S.buf("va")
        S.op("vector", lambda e: e.memset(va[:, :, :, 128:130], 1.0), writes=[bva])
        vsrc = v_d.rearrange("(t p) (g d) -> p t g d", p=128, g=2)
        for g in range(2):
            S.dma("sync", va[:, :, g, 0:128], vsrc[:, :, g, :], writes=[bva])
        qT = S.sbuf("qT", [128, 8, NT], BF16)
        bq = S.buf("qT")
        S.dma("sync", qT[:], q_d.rearrange("p (h t) -> p h t", h=8), writes=[bq])
        pT_r = Ring(S, "pT", [128, 512], BF16, 3)
        on_r = Ring(S, "on", [128, 128], BF16, 2)
        ri_r = Ring(S, "ri", [128, 2], F32, 2)
        SCL = 1.0 / float(np.sqrt(128.0))
        for qb in range(NT // 512):
            for h in range(8):
                g = h // 4
                po = [psO.next(), psO.next()]
                for kt in range(NKT):
                    ps, bps = psS.next()
                    S.op("tensor", lambda e, ps=ps, kt=kt, g=g, h=h, qb=qb: e.matmul(
                        ps[:], kT[:, g, kt * 128:(kt + 1) * 128], qT[:, h, qb * 512:(qb + 1) * 512], start=True, stop=True),
                        reads=[bk, bq], writes=[bps])
                    pT, bpT = pT_r.next()
                    S.op("scalar", lambda e, pT=pT, ps=ps: e.activation(pT[:], ps[:], AF.Exp, scale=SCL), reads=[bps], writes=[bpT])
                    for qt in range(4):
                        pot, bpot = po[qt // 2]
                        c = (qt % 2) * 129
                        S.op("tensor", lambda e, pot=pot, c=c, pT=pT, qt=qt, kt=kt, g=g: e.matmul(
                            pot[:, c:c + 129], pT[:, qt * 128:(qt + 1) * 128], va[:, kt, g, 0:129], start=(kt == 0 and qt % 2 == 0), stop=(kt == NKT - 1),
                            skip_group_check=True),
                            reads=[bpT, bva], writes=[bpot], sig=(qt == 3))
                for qt in range(4):
                    pot, bpot = po[qt // 2]
                    c = (qt % 2) * 129
                    ri, bri = ri_r.next()
                    S.op("vector", lambda e, ri=ri, pot=pot, c=c: e.reciprocal(ri[:, 0:1], pot[:, c + 128:c + 129]), reads=[bpot], writes=[bri])
                    on, bon = on_r.next()
                    S.op("vector", lambda e, on=on, pot=pot, c=c, ri=ri: e.tensor_scalar(on[:], pot[:, c:c + 128], ri[:, 0:1], None, ALU.mult),
                         reads=[bpot, bri], writes=[bon])
                    pt, bpt = psT.next()
                    S.op("tensor", lambda e, pt=pt, on=on: e.transpose(pt[:, 0:128], on[:], identb[:]), reads=[bon, bidb], writes=[bpt])
                    col = qb * 512 + qt * 128
                    S.op("scalar", lambda e, pt=pt, h=h, col=col: e.copy(oT[:, h, col:col + 128], pt[:, 0:128]), reads=[bpt], writes=[boT[col // 128]])
        S.barrier()
        es_a.close()

        S.es = es
        C.ps = Ring(S, "ps", [128, 512], F32, 8, psum=True)
        mods = emit_mods(C, es, ada_w, [1])
        mod1, bmod1 = mods[1]
        xT = S.sbuf("xT", [128, KC, NT], F32)
        bx = [S.buf("xT") for _ in range(NT // 128)]
        for k in range(KC):
            S.dma("sync", xT[:, k, :], x1_d[:, k * NT:(k + 1) * NT], writes=bx)
        hT = S.sbuf("hT", [128, KC, NT], BF16)
        bh = [S.buf("hT") for _ in range(NT // 128)]
        es_o = scoped(S)
        wo_r = Ring(S, "wo", [128, KC, D], BF16, 1)
        wot, bwo = load_w(C, wo_r, w_o, D, 0, D)
        emit_proj_residual(C, oT, boT, KC, wot, bwo, xT, bx, LAT_BLOCKS, mod1, bmod1, 2)
        S.barrier()
        es_o.close()
        es_oT.close()

        gates = es.enter_context(nc.sbuf_tensor("gates", [128, NT // 128, NE], F32))
        bgates = S.buf("gates")
        es_r = scoped(S)
        gs2, bgs2 = emit_gs(C, mod1, bmod1, 4, "n2g1", "gs2")
        alloc_norm_tmps(C)
        hf = S.sbuf("hf", [128, KC, NT], F32)
        bhf = [S.buf("hf") for _ in range(NT // 128)]
        for (c0, n, which) in LAT_BLOCKS:
            emit_norm(C, xT, bx, c0, hT, bh, c0, n, gs2, bgs2, mod1, bmod1, 3, which, hf=hf, bhf=bhf)
        rw = S.sbuf("rw", [128, KC, NE], F32)
        brw = S.buf("rw")
        S.dma("sync", rw[:], router.rearrange("(k p) e -> p k e", p=128), writes=[brw])
        lg_r = Ring(S, "lg", [128, 32], F32, 2)
        for t in range(NT // 128):
            pl, bpl = C.ps.next()
            for k in range(KC):
                S.op("tensor", lambda e, pl=pl, k=k, t=t: e.matmul(
                    pl[:, 0:NE], hf[:, k, t * 128:(t + 1) * 128], rw[:, k, :], start=(k == 0), stop=(k == KC - 1)),
                    reads=[bhf[t], brw], writes=[bpl], sig=(k == KC - 1))
            lg, blg = lg_r.next()
            S.op("vector", lambda e, lg=lg, pl=pl: e.tensor_copy(lg[:, 0:8], pl[:, 0:8]), reads=[bpl], writes=[blg])
            S.op("vector", lambda e, lg=lg: e.max(lg[:, 8:16], lg[:, 0:8]), reads=[blg], writes=[blg])
            S.op("vector", lambda e, lg=lg: e.tensor_scalar(lg[:, 16:24], lg[:, 0:8], lg[:, 8:9], None, ALU.subtract), reads=[blg], writes=[blg])
            S.op("scalar", lambda e, lg=lg: e.activation(lg[:, 16:24], lg[:, 16:24], AF.Exp), reads=[blg], writes=[blg])
            S.op("vector", lambda e, lg=lg: e.tensor_tensor(lg[:, 24:25], lg[:, 9:10], lg[:, 8:9], ALU.subtract), reads=[blg], writes=[blg])
            S.op("scalar", lambda e, lg=lg: e.activation(lg[:, 24:25], lg[:, 24:25], AF.Exp), reads=[blg], writes=[blg])
            S.op("vector", lambda e, lg=lg: e.tensor_scalar(lg[:, 24:25], lg[:, 24:25], 1.0, None, ALU.add), reads=[blg], writes=[blg])
            S.op("vector", lambda e, lg=lg: e.reciprocal(lg[:, 24:25], lg[:, 24:25]), reads=[blg], writes=[blg])
            S.op("vector", lambda e, lg=lg: e.tensor_scalar(lg[:, 0:8], lg[:, 0:8], lg[:, 9:10], None, ALU.is_ge), reads=[blg], writes=[blg])
            S.op("vector", lambda e, lg=lg: e.tensor_tensor(lg[:, 0:8], lg[:, 0:8], lg[:, 16:24], ALU.mult), reads=[blg], writes=[blg])
            S.op("vector", lambda e, lg=lg, t=t: e.tensor_scalar(gates[:, t, :], lg[:, 0:8], lg[:, 24:25], None, ALU.mult), reads=[blg], writes=[bgates])
        S.barrier()
        es_r.close()

        es_m = scoped(S)
        alloc_ffn(C, 4)
        gB_r = Ring(S, "gB", [128, NT], F32, 2)
        gl_r = Ring(S, "gl", [128, 128], F32, 2)
        for ex in range(NE):
            gB, bgB = gB_r.next()
            for t4 in range(NT // 512):
                pg, bpg = C.ps.next()
                for j in range(4):
                    t = t4 * 4 + j
                    gl, bgl = gl_r.next()
                    S.op("gpsimd", lambda e, gl=gl, t=t, ex=ex: e.tensor_copy(gl[:], bcast_cols(gates[:, t, ex:ex + 1], 128)), reads=[bgates], writes=[bgl])
                    S.op("tensor", lambda e, pg=pg, gl=gl, j=j: e.matmul(pg[:, j * 128:(j + 1) * 128], gl[:], C.ident[:], start=True, stop=True),
                         reads=[bgl, C.bident], writes=[bpg])
                copy_op(C, "scalar", gB[:, t4 * 512:(t4 + 1) * 512], pg[:], [bpg], [bgB])
            emit_ffn(C, hT, bh, xT, bx, LAT_BLOCKS, m_w1[ex], m_w3[ex], m_w2[ex], DFE, mod1, bmod1, 5, gate=(gB, bgB))
        S.barrier()
        es_m.close()

        es_z = scoped(S)
        gfin = S.sbuf("gfin", [128, D], F32)
        bgf = S.buf("gfin")
        S.dma("sync", gfin[:], bass.AP(fin_g.tensor, fin_g.offset, [[0, 128], [1, D]]), writes=[bgf])
        ot_r = Ring(S, "ot", [128, D], F32, 2)
        sq_r = Ring(S, "fsq", [128, 512], F32, 2)
        ss_r = Ring(S, "fss", [128, 4], F32, 2)
        for t in range(NT // 128):
            pp = [C.ps.next(), C.ps.next()]
            for half in range(2):
                ph, bph = pp[half]
                for j in range(4):
                    kc = half * 4 + j
                    S.op("tensor", lambda e, ph=ph, j=j, kc=kc, t=t: e.transpose(
                        ph[:, j * 128:(j + 1) * 128], xT[:, kc, t * 128:(t + 1) * 128], C.ident[:]),
                        reads=[bx[t], C.bident], writes=[bph], sig=(j == 3))
            ss, bss = ss_r.next()
            S.op("vector", lambda e, ss=ss: e.memset(ss[:], 0.0), writes=[bss])
            for half in range(2):
                ph, bph = pp[half]
                sq, bsq = sq_r.next()
                S.op("scalar", lambda e, sq=sq, ph=ph, ss=ss, half=half: e.activation(sq[:], ph[:], AF.Square, accum_out=ss[:, half:half + 1]),
                     reads=[bph], writes=[bsq, bss])
            S.op("vector", lambda e, ss=ss: e.tensor_tensor(ss[:, 2:3], ss[:, 0:1], ss[:, 1:2], ALU.add), reads=[bss], writes=[bss])
            S.op("scalar", lambda e, ss=ss: e.activation(ss[:, 2:3], ss[:, 2:3], AF.Sqrt, bias=C.epsb[:, 0:1], scale=1.0 / D), reads=[bss, C.bepsb], writes=[bss])
            S.op("vector", lambda e, ss=ss: e.reciprocal(ss[:, 3:4], ss[:, 2:3]), reads=[bss], writes=[bss])
            ot, bot = ot_r.next()
            for half in range(2):
                ph, bph = pp[half]
                S.op("vector", lambda e, ot=ot, ph=ph, ss=ss, half=half: e.scalar_tensor_tensor(
                    ot[:, half * 512:(half + 1) * 512], ph[:], ss[:, 3:4], gfin[:, half * 512:(half + 1) * 512], ALU.mult, ALU.mult),
                    reads=[bph, bss, bgf], writes=[bot])
            S.dma("sync", out_d[t * 128:(t + 1) * 128, :], ot[:], reads=[bot], out_sem_buf=outb)
        S.finish()
        S.emit()
        es_z.close()
    return nc


def emit_router(C, hf, bhf, router, gates, bgates, maskt=None):
    S = C.S
    rw = S.sbuf("rw", [128, KC, NE], F32)
    brw = S.buf("rw")
    S.dma("sync", rw[:], router.rearrange("(k p) e -> p k e", p=128), writes=[brw])
    lg_r = Ring(S, "lg", [128, 32], F32, 2)
    for t in range(NT // 128):
        pl, bpl = C.ps.next()
        for k in range(KC):
            S.op("tensor", lambda e, pl=pl, k=k, t=t: e.matmul(
                pl[:, 0:NE], hf[:, k, t * 128:(t + 1) * 128], rw[:, k, :], start=(k == 0), stop=(k == KC - 1)),
                reads=[bhf[t], brw], writes=[bpl], sig=(k == KC - 1))
        lg, blg = lg_r.next()
        S.op("vector", lambda e, lg=lg, pl=pl: e.tensor_copy(lg[:, 0:8], pl[:, 0:8]), reads=[bpl], writes=[blg])
        S.op("vector", lambda e, lg=lg: e.max(lg[:, 8:16], lg[:, 0:8]), reads=[blg], writes=[blg])
        S.op("vector", lambda e, lg=lg: e.tensor_scalar(lg[:, 16:24], lg[:, 0:8], lg[:, 8:9], None, ALU.subtract), reads=[blg], writes=[blg])
        S.op("scalar", lambda e, lg=lg: e.activation(lg[:, 16:24], lg[:, 16:24], AF.Exp), reads=[blg], writes=[blg])
        S.op("vector", lambda e, lg=lg: e.tensor_tensor(lg[:, 24:25], lg[:, 9:10], lg[:, 8:9], ALU.subtract), reads=[blg], writes=[blg])
        S.op("scalar", lambda e, lg=lg: e.activation(lg[:, 24:25], lg[:, 24:25], AF.Exp), reads=[blg], writes=[blg])
        S.op("vector", lambda e, lg=lg: e.tensor_scalar(lg[:, 24:25], lg[:, 24:25], 1.0, None, ALU.add), reads=[blg], writes=[blg])
        S.op("vector", lambda e, lg=lg: e.reciprocal(lg[:, 24:25], lg[:, 24:25]), reads=[blg], writes=[blg])
        S.op("vector", lambda e, lg=lg: e.tensor_scalar(lg[:, 0:8], lg[:, 0:8], lg[:, 9:10], None, ALU.is_ge), reads=[blg], writes=[blg])
        if maskt is not None:
            S.op("vector", lambda e, lg=lg, t=t: e.tensor_copy(maskt[:, t, :], lg[:, 0:8]), reads=[blg], writes=[bgates])
        S.op("vector", lambda e, lg=lg: e.tensor_tensor(lg[:, 0:8], lg[:, 0:8], lg[:, 16:24], ALU.mult), reads=[blg], writes=[blg])
        S.op("vector", lambda e, lg=lg, t=t: e.tensor_scalar(gates[:, t, :], lg[:, 0:8], lg[:, 24:25], None, ALU.mult), reads=[blg], writes=[bgates])


def emit_final(C, xT, bx, fin_g, out_d, outb):
    S = C.S
    gfin = S.sbuf("gfin", [128, D], F32)
    bgf = S.buf("gfin")
    S.dma("sync", gfin[:], bass.AP(fin_g.tensor, fin_g.offset, [[0, 128], [1, D]]), writes=[bgf])
    ot_r = Ring(S, "ot", [128, D], F32, 2)
    sq_r = Ring(S, "fsq", [128, 512], F32, 2)
    ss_r = Ring(S, "fss", [128, 4], F32, 2)
    for t in range(NT // 128):
        pp = [C.ps.next(), C.ps.next()]
        for half in range(2):
            ph, bph = pp[half]
            for j in range(4):
                kc = half * 4 + j
                S.op("tensor", lambda e, ph=ph, j=j, kc=kc, t=t: e.transpose(
                    ph[:, j * 128:(j + 1) * 128], xT[:, kc, t * 128:(t + 1) * 128], C.ident[:]),
                    reads=[bx[t], C.bident], writes=[bph], sig=(j == 3))
        ss, bss = ss_r.next()
        S.op("vector", lambda e, ss=ss: e.memset(ss[:], 0.0), writes=[bss])
        for half in range(2):
            ph, bph = pp[half]
            sq, bsq = sq_r.next()
            S.op("scalar", lambda e, sq=sq, ph=ph, ss=ss, half=half: e.activation(sq[:], ph[:], AF.Square, accum_out=ss[:, half:half + 1]),
                 reads=[bph], writes=[bsq, bss])
        S.op("vector", lambda e, ss=ss: e.tensor_tensor(ss[:, 2:3], ss[:, 0:1], ss[:, 1:2], ALU.add), reads=[bss], writes=[bss])
        S.op("scalar", lambda e, ss=ss: e.activation(ss[:, 2:3], ss[:, 2:3], AF.Sqrt, bias=C.epsb[:, 0:1], scale=1.0 / D), reads=[bss, C.bepsb], writes=[bss])
        S.op("vector", lambda e, ss=ss: e.reciprocal(ss[:, 3:4], ss[:, 2:3]), reads=[bss], writes=[bss])
        ot, bot = ot_r.next()
        for half in range(2):
            ph, bph = pp[half]
            S.op("vector", lambda e, ot=ot, ph=ph, ss=ss, half=half: e.scalar_tensor_tensor(
                ot[:, half * 512:(half + 1) * 512], ph[:], ss[:, 3:4], gfin[:, half * 512:(half + 1) * 512], ALU.mult, ALU.mult),
                reads=[bph, bss, bgf], writes=[bot])
        S.dma("sync", out_d[t * 128:(t + 1) * 128, :], ot[:], reads=[bot], out_sem_buf=outb)


class SubRing:
    def __init__(self, tiles):
        self.tiles = list(tiles)
        self.i = 0

    def next(self):
        t = self.tiles[self.i % len(self.tiles)]
        self.i += 1
        return t


BS = 256
PASS = 1024
NTT = NT // 128


def emit_htm(C, hT2, bh2, htm_d, identb, bidb):
    S = C.S
    bhtm = S.buf("htm")
    htw = Ring(S, "htw", [128, D], BF16, 2)
    for tt in range(NTT):
        ptr, bptr = C.ps.next()
        ptb = ptr[:].bitcast(BF16)
        for k in range(KC):
            S.op("tensor", lambda e, ptb=ptb, k=k, tt=tt: e.transpose(ptb[:, k * 128:(k + 1) * 128], hT2[:, k, tt * 128:(tt + 1) * 128], identb[:]),
                 reads=[bh2[tt], bidb], writes=[bptr], sig=(k == KC - 1))
        ht, bht = htw.next()
        copy_op(C, evac_engine(C), ht[:], ptb[:, 0:D], [bptr], [bht])
        S.dma("sync", htm_d[tt], ht[:], reads=[bht], writes=[bhtm], nowaw=True)

    return bhtm


def emit_moe_sparse(C, bhtm, xT, bx, maskt, gates, bgates, cst_d, htm_d, m_w1, m_w3, m_w2, mod1, bmod1, identb, bidb):
    S = C.S
    cst = S.sbuf("cst", [128, 385], F32)
    bcst_ = S.buf("cst")
    S.dma("sync", cst[:], cst_d, writes=[bcst_])
    utri, iota_row, iota_p = cst[:, 0:128], cst[:, 128:384], cst[:, 384:385]
    ones_f = S.sbuf("ones_f", [128, 128], F32)
    bof = S.buf("ones_f")
    S.op("vector", lambda e: e.memset(ones_f[:], 1.0), writes=[bof])
    tot = S.sbuf("tot", [128, NTT, NE], F32)
    incl = S.sbuf("incl", [128, NTT, NE], F32)
    pos = S.sbuf("pos", [128, NTT, NE], F32)
    posq = S.sbuf("posq", [128, 8, NTT, NE], F32)
    jp = S.sbuf("jp", [128, 16], F32)
    cnti = S.sbuf("cnti", [128, NE], mybir.dt.int32)
    btab = S.buf("tab")
    bcnt = S.buf("cnt")
    mflat = maskt[:].rearrange("p t e -> p (t e)")
    pw, bpw = C.ps.next()
    S.op("tensor", lambda e: e.matmul(pw[:, 0:128], utri, mflat, start=True, stop=True), reads=[bcst_, bgates], writes=[bpw])
    pt_, bpt_ = C.ps.next()
    S.op("tensor", lambda e: e.matmul(pt_[:, 0:128], ones_f[:], mflat, start=True, stop=True), reads=[bof, bgates], writes=[bpt_])
    S.op("vector", lambda e: e.tensor_copy(tot[:].rearrange("p t e -> p (t e)"), pt_[:, 0:128]), reads=[bpt_], writes=[btab])
    for ex in range(NE):
        S.op("vector", lambda e, ex=ex: e.tensor_tensor_scan(incl[:, :, ex], ones_f[:, 0:NTT], tot[:, :, ex], 0.0, ALU.mult, ALU.add),
             reads=[btab, bof], writes=[btab])
    S.op("vector", lambda e: e.tensor_tensor(pos[:].rearrange("p t e -> p (t e)"), pw[:, 0:128], incl[:].rearrange("p t e -> p (t e)"), ALU.add),
         reads=[bpw, btab], writes=[btab])
    S.op("vector", lambda e: e.tensor_tensor(pos[:], pos[:], tot[:], ALU.subtract), reads=[btab], writes=[btab])
    S.op("vector", lambda e: e.scalar_tensor_tensor(pos[:], pos[:], 1.0, maskt[:], ALU.add, ALU.mult), reads=[btab, bgates], writes=[btab])
    for q in range(8):
        S.op("vector", lambda e, q=q: e.tensor_scalar(posq[:, q], pos[:], -1.0 - BS * q, None, ALU.add), reads=[btab], writes=[btab])
    S.op("vector", lambda e: e.tensor_scalar(pos[:], pos[:], -1.0, None, ALU.add), reads=[btab], writes=[btab])
    for st in range(16):
        S.op("vector", lambda e, st=st: e.tensor_scalar(jp[:, st:st + 1], iota_p, 128.0 * st, None, ALU.add), reads=[bcst_], writes=[btab])
    S.op("vector", lambda e: e.tensor_copy(cnti[:], incl[:, NTT - 1, :]), reads=[btab], writes=[bcnt])

    hg = S.sbuf("hg", [128, KC, PASS], BF16)
    bhg = [S.buf("hg") for _ in range(PASS // BS)]
    ys = S.sbuf("ys", [128, KC, PASS], F32)
    bys = [S.buf("ys") for _ in range(PASS // BS)]
    prow = S.sbuf("prow", [128, NT], F32)
    grow = S.sbuf("grow", [128, NT], F32)
    brow = S.buf("rows")
    gl_r = Ring(S, "gl", [128, 128], F32, 3)
    htr = Ring(S, "htr", [128, D], BF16, 3)
    sct_r = Ring(S, "sct", [128, 512], F32, 2)
    sel_r = Ring(S, "sel", [128, BS], BF16, 3)
    ysb_r = Ring(S, "ysb", [128, KC, BS], BF16, 1)
    ysm_r = Ring(S, "ysm", [128, D], BF16, 2)
    sg_r = Ring(S, "sg", [128, NT], BF16, 2)
    SL = 2
    w13 = Ring(S, "w13s", [128, KC, SL * 128], BF16, 4)
    w2r = Ring(S, "w2s", [128, SL, D], BF16, 2)
    hid_r = [Ring(S, "hids", [128, BS], BF16, PASS // BS) for _ in range(SL)]
    sil_r = Ring(S, "sils", [128, BS], F32, 2)
    nfc = DFE // 128
    nblk = PASS // BS

    def cap_of(ex):
        return cnti[0:1, ex:ex + 1]

    pre = {}

    def issue_slice(ex, s0, tiles):
        w1t, bw1, w3t, bw3, w2t, bw2 = tiles
        S.dma("gpsimd", w1t[:], m_w1[ex].rearrange("(k p) n -> p k n", p=128)[:, :, s0 * 128:(s0 + SL) * 128], writes=[bw1])
        S.dma("gpsimd", w3t[:], m_w3[ex].rearrange("(k p) n -> p k n", p=128)[:, :, s0 * 128:(s0 + SL) * 128], writes=[bw3])
        S.dma("gpsimd", w2t[:], m_w2[ex][s0 * 128:(s0 + SL) * 128, :].rearrange("(f p) n -> p f n", p=128), writes=[bw2])

    def load_slice(ex, s0):
        w1t, bw1 = w13.next()
        w3t, bw3 = w13.next()
        w2t, bw2 = w2r.next()
        tiles = (w1t, bw1, w3t, bw3, w2t, bw2)
        issue_slice(ex, s0, tiles)
        return tiles

    def prefetch(ex):
        for s0 in (0, SL):
            pre[(ex, s0)] = load_slice(ex, s0)

    def do_rowbcast(ex):
        for src, dstrow in ((pos, prow), (gates, grow)):
            for t4 in range(NT // 512):
                pg, bpg = C.ps.next()
                for j in range(4):
                    t = t4 * 4 + j
                    gl, bgl = gl_r.next()
                    S.op("scalar", lambda e, gl=gl, t=t, ex=ex, src=src: e.copy(gl[:], bcast_cols(src[:, t, ex:ex + 1], 128)),
                         reads=[btab, bgates], writes=[bgl])
                    S.op("tensor", lambda e, pg=pg, gl=gl, j=j: e.matmul(pg[:, j * 128:(j + 1) * 128], gl[:], C.ident[:], start=True, stop=True),
                         reads=[bgl, C.bident], writes=[bpg])
                copy_op(C, "vector", dstrow[:, t4 * 512:(t4 + 1) * 512], pg[:], [bpg], [brow])

    def do_gather(ex, p):
        cap = cap_of(ex)
        for b in range(nblk):
            q = p * nblk + b
            if q > 0:
                S.begin_cond(cap, bcnt, BS * q, key=("cap", ex))
            pgk = [C.ps.next() for _ in range(4)]
            for tt in range(NTT):
                ht, bht = htr.next()
                S.dma("sync", ht[:], htm_d[tt], reads=[bhtm], writes=[bht])
                sel, bsel = sel_r.next()
                S.op("vector", lambda e, sel=sel, q=q, tt=tt, ex=ex: e.tensor_scalar(
                    sel[:], iota_row, posq[:, q, tt, ex:ex + 1], None, ALU.is_equal), reads=[bcst_, btab], writes=[bsel])
                for k in range(KC):
                    pk, bpk = pgk[k // 2]
                    S.op("tensor", lambda e, pk=pk, k=k, ht=ht, sel=sel, tt=tt: e.matmul(
                        pk[:, (k % 2) * BS:(k % 2 + 1) * BS], ht[:, k * 128:(k + 1) * 128], sel[:], start=(tt == 0 and k % 2 == 0), stop=(tt == NTT - 1),
                        skip_group_check=True),
                        reads=[bht, bsel], writes=[bpk], sig=(k == KC - 1))
            for k2 in range(4):
                pk, bpk = pgk[k2]
                copy_op(C, evac_engine(C), hg[:, 2 * k2:2 * k2 + 2, b * BS:(b + 1) * BS],
                        pk[:].rearrange("p (a c) -> p a c", a=2), [bpk], [bhg[b]])
            if q > 0:
                S.end_cond()

    def do_ffn(ex, p):
        cap = cap_of(ex)
        for b in range(nblk):
            S.op("gpsimd", lambda e, b=b: e.memset(ys[:, :, b * BS:(b + 1) * BS], 0.0), writes=[bys[b]])
        for s0 in range(0, nfc, SL):
            if p == 0 and (ex, s0) in pre:
                w1t, bw1, w3t, bw3, w2t, bw2 = pre[(ex, s0)]
            else:
                w1t, bw1, w3t, bw3, w2t, bw2 = load_slice(ex, s0)
            def up(b, w1t=w1t, w3t=w3t, bw1=bw1, bw3=bw3):
                c0 = b * BS
                hids = [r.next() for r in hid_r]
                for fi in range(SL):
                    pa, bpa = C.ps.next()
                    for k in range(KC):
                        S.op("tensor", lambda e, pa=pa, k=k, fi=fi, c0=c0: e.matmul(
                            pa[:, :BS], w1t[:, k, fi * 128:(fi + 1) * 128], hg[:, k, c0:c0 + BS], start=(k == 0), stop=(k == KC - 1)),
                            reads=[bhg[b], bw1], writes=[bpa], sig=(k == KC - 1))
                    pb, bpb = C.ps.next()
                    for k in range(KC):
                        S.op("tensor", lambda e, pb=pb, k=k, fi=fi, c0=c0: e.matmul(
                            pb[:, :BS], w3t[:, k, fi * 128:(fi + 1) * 128], hg[:, k, c0:c0 + BS], start=(k == 0), stop=(k == KC - 1)),
                            reads=[bhg[b], bw3], writes=[bpb], sig=(k == KC - 1))
                    sa, bsa = sil_r.next()
                    S.op("scalar", lambda e, sa=sa, pa=pa: e.activation(sa[:], pa[:, :BS], AF.Silu), reads=[bpa], writes=[bsa])
                    S.op("vector", lambda e, sa=sa, pb=pb, hf_=hids[fi][0]: e.tensor_tensor(hf_[:], pb[:, :BS], sa[:], ALU.mult),
                         reads=[bpb, bsa], writes=[hids[fi][1]])
                hid_of[b] = hids

            def down(b, w2t=w2t, bw2=bw2):
                c0 = b * BS
                hids = hid_of[b]
                pos_ = [C.ps.next() for _ in range(KC // 2)]
                for fi in range(SL):
                    for dc in range(KC):
                        po, bpo = pos_[dc // 2]
                        h2 = dc % 2
                        S.op("tensor", lambda e, po=po, fi=fi, dc=dc, h2=h2, hf_=hids[fi][0]: e.matmul(
                            po[:, h2 * BS:(h2 + 1) * BS], w2t[:, fi, dc * 128:(dc + 1) * 128], hf_[:],
                            start=(fi == 0 and h2 == 0), stop=(fi == SL - 1), skip_group_check=True),
                            reads=[hids[fi][1], bw2], writes=[bpo], sig=(fi == SL - 1 and h2 == 1))
                for dc2 in range(KC // 2):
                    po, bpo = pos_[dc2]
                    S.op("vector", lambda e, po=po, dc2=dc2, c0=c0: e.tensor_tensor(
                        ys[:, 2 * dc2:2 * dc2 + 2, c0:c0 + BS], po[:, 0:2 * BS].rearrange("p (a c) -> p a c", a=2), ys[:, 2 * dc2:2 * dc2 + 2, c0:c0 + BS], ALU.add),
                        reads=[bpo], writes=[bys[b]])

            def chain(f):
                opened = 0
                for b in range(nblk):
                    q = p * nblk + b
                    if q > 0:
                        S.begin_cond(cap, bcnt, BS * q, key=("cap", ex))
                        opened += 1
                    f(b)
                for _ in range(opened):
                    S.end_cond()
            hid_of = {}
            for b in range(nblk):
                q = p * nblk + b
                if q > 0:
                    S.begin_cond(cap, bcnt, BS * q, key=("cap", ex))
                up(b)
                down(b)
                if q > 0:
                    S.end_cond()

    def do_scatter(ex, p):
        cap = cap_of(ex)
        for b in range(nblk):
            q = p * nblk + b
            if q > 0:
                S.begin_cond(cap, bcnt, BS * q, key=("cap", ex))
            ysb, bysb = ysb_r.next()
            S.op("gpsimd", lambda e, ysb=ysb, b=b: e.tensor_copy(ysb[:], ys[:, :, b * BS:(b + 1) * BS]), reads=[bys[b]], writes=[bysb])
            tiles = []
            for st2 in range(BS // 128):
                slot = q * (BS // 128) + st2
                ptr, bptr = C.ps.next()
                ptb = ptr[:].bitcast(BF16)
                for dc in range(KC):
                    S.op("tensor", lambda e, ptb=ptb, dc=dc, ysb=ysb, st2=st2: e.transpose(
                        ptb[:, dc * 128:(dc + 1) * 128], ysb[:, dc, st2 * 128:(st2 + 1) * 128], identb[:]),
                        reads=[bysb, bidb], writes=[bptr], sig=(dc == KC - 1))
                ysm, bysm = ysm_r.next()
                S.op("scalar", lambda e, ysm=ysm, ptb=ptb: e.copy(ysm[:], ptb[:, 0:D]), reads=[bptr], writes=[bysm])
                sg, bsg = sg_r.next()
                S.op("vector", lambda e, sg=sg, slot=slot: e.scalar_tensor_tensor(
                    sg[:], prow[:], jp[:, slot:slot + 1], grow[:], ALU.is_equal, ALU.mult), reads=[brow, btab], writes=[bsg])
                tiles.append((ysm, bysm, sg, bsg))
            for dc in range(KC):
                for tb in range(NT // 512):
                    po, bpo = C.ps.next()
                    for i, (ysm, bysm, sg, bsg) in enumerate(tiles):
                        S.op("tensor", lambda e, po=po, ysm=ysm, sg=sg, dc=dc, tb=tb, i=i: e.matmul(
                            po[:], ysm[:, dc * 128:(dc + 1) * 128], sg[:, tb * 512:(tb + 1) * 512], start=(i == 0), stop=(i == len(tiles) - 1)),
                            reads=[bysm, bsg], writes=[bpo], sig=(i == len(tiles) - 1))
                    if (dc * 4 + tb) % 3 != 2:
                        S.op("vector", lambda e, po=po, dc=dc, tb=tb: e.scalar_tensor_tensor(
                            xT[:, dc, tb * 512:(tb + 1) * 512], po[:], mod1[:, 5 * 8 + dc, 0:1], xT[:, dc, tb * 512:(tb + 1) * 512], ALU.mult, ALU.add),
                            reads=[bpo, bmod1], writes=bx[tb * 4:(tb + 1) * 4])
                    else:
                        sc, bsc = sct_r.next()
                        S.op("scalar", lambda e, sc=sc, po=po, dc=dc: e.activation(sc[:], po[:], AF.Copy, scale=mod1[:, 5 * 8 + dc, 0:1]),
                             reads=[bpo, bmod1], writes=[bsc])
                        S.op("gpsimd", lambda e, sc=sc, dc=dc, tb=tb: e.tensor_tensor(
                            xT[:, dc, tb * 512:(tb + 1) * 512], xT[:, dc, tb * 512:(tb + 1) * 512], sc[:], ALU.add),
                            reads=[bsc], writes=bx[tb * 4:(tb + 1) * 4])
            if q > 0:
                S.end_cond()

    do_rowbcast(0)
    do_gather(0, 0)
    for ex in range(NE):
        do_ffn(ex, 0)
        if ex + 1 < NE:
            prefetch(ex + 1)
            do_gather(ex + 1, 0)
        do_scatter(ex, 0)
        for p in range(1, NT // PASS):
            S.begin_cond(cap_of(ex), bcnt, PASS * p, key=("cap", ex))
            do_gather(ex, p)
            do_ffn(ex, p)
            do_scatter(ex, p)
            if ex + 1 < NE and p == NT // PASS - 1:
                do_gather(ex + 1, 0)
                for s0 in (0, SL):
                    issue_slice(ex + 1, s0, pre[(ex + 1, s0)])
            S.end_cond()
        if ex + 1 < NE:
            do_rowbcast(ex + 1)


GROUPS = [[0, 1, 2, 3], [4, 5, 6, 7]]


def build_fused():
    nc = bass.Bass("TRN2", target_bir_lowering=False)

    def din(name, shape, dt=F32):
        return nc.dram_tensor(name, list(shape), dt, kind="ExternalInput").ap()

    def dscr(name, shape, dt=F32):
        return nc.dram_tensor(name, list(shape), dt).ap()
    xall = din("xall", [TOT, D])
    vecs = din("vecs", [128, VP.n])
    ident = din("ident", [128, 128])
    ada_w = din("ada_w", [2, D, 6 * D])
    w_in = din("w_in", [D, 2 * D])
    w_a = din("w_a", [2, 8, 128, 128])
    w_i = din("w_i", [2, 8, 128, 128])
    w_out = din("w_out", [D, D])
    f_w1 = din("f_w1", [D, DFF])
    f_w3 = din("f_w3", [D, DFF])
    f_w2 = din("f_w2", [DFF, D])
    w_qkv = din("w_qkv", [D, 1536])
    cos_d = din("cos", [128, NT])
    sin_d = din("sin", [128, NT])
    rotm_d = din("rotm", [128, 128])
    w_o = din("w_o", [D, D])
    router = din("router", [D, NE])
    m_w1 = din("m_w1", [NE, D, DFE])
    m_w3 = din("m_w3", [NE, D, DFE])
    m_w2 = din("m_w2", [NE, DFE, D])
    fin_g = din("fin_g", [D])
    cst_d = din("cst", [128, 385])
    out_d = nc.dram_tensor("out", [NT, D], F32, kind="ExternalOutput").ap()
    htm_d = dscr("htm_d", [NT // 128, 128, D], BF16)
    a_sp = dscr("a_sp", [16, 128, NT])
    b_sp = dscr("b_sp", [16, 128, NT])
    summ_loc = dscr("summ_loc", [128, 32])
    summ_all_d = dscr("summ_all_d", [4 * 128, 32])
    klat = [dscr(f"klat{g}", [128, NT], BF16) for g in range(2)]
    kall = [dscr(f"kall{g}", [4 * 128, NT], BF16) for g in range(2)]
    kctx = dscr("kctx", [128, 2 * NCX], BF16)
    vlat = [dscr(f"vlat{h}", [NT // 2, 256], BF16) for h in range(2)]
    vall = [dscr(f"vall{h}", [4 * NT // 2, 256], BF16) for h in range(2)]
    vctx = dscr("vctx", [NCX, 256], BF16)

    with ExitStack() as es:
        C = setup_common(nc, es, vecs, ident)
        S = C.S
        outb = S.buf("out")
        C.ada_queue = "gpsimd"
        C.ada_dt = BF16
        C.ada_ring = 4
        emit_silu_c(C)
        mod0, bmod0 = S.sbuf("mod0", [128, 48, 2], F32), S.buf("mod")
        mod1, bmod1 = S.sbuf("mod1", [128, 48, 2], F32), S.buf("mod")
        gs1 = S.sbuf("gs1", [128, KC, 2], F32)
        gs2 = S.sbuf("gs2", [128, KC, 2], F32)
        gs3 = S.sbuf("gs3", [128, KC, 2], F32)
        gs4 = S.sbuf("gs4", [128, KC, 2], F32)
        bgs1, bgs2, bgs3, bgs4 = S.buf("gs"), S.buf("gs"), S.buf("gs"), S.buf("gs")
        identb = S.sbuf("identb", [128, 128], BF16)
        bidb = S.buf("identb")
        S.op("vector", lambda e: e.tensor_copy(identb[:], C.ident[:]), reads=[C.bident], writes=[bidb])
        cst = S.sbuf("cst", [128, 16], F32)
        bcst = S.buf("cst")
        summ = S.sbuf("summ", [128, 32], F32)
        bsumm = S.buf("summ")
        sall = S.sbuf("sall", [128, 4, 32], F32)
        bsall = S.buf("sall")
        cneg = S.sbuf("cneg", [128, 32], F32)
        bcneg = S.buf("cneg")
        S.op("scalar", lambda e: e.activation(cneg[:, 0:16], V(C, "lam", 0, 16), AF.Exp, scale=-1.0), reads=[C.bvec], writes=[bcneg])
        S.op("scalar", lambda e: e.activation(cneg[:, 0:16], cneg[:, 0:16], AF.Ln, bias=1.0), reads=[bcneg], writes=[bcneg])
        S.op("vector", lambda e: e.tensor_scalar(cneg[:, 16:32], cneg[:, 0:16], -16.0, None, ALU.mult), reads=[bcneg], writes=[bcneg])
        S.op("vector", lambda e: e.tensor_scalar(cneg[:, 0:16], cneg[:, 0:16], -8.0, None, ALU.mult), reads=[bcneg], writes=[bcneg])

        es_l0 = scoped(S)
        hT = S.sbuf("hT", [128, KC, TOT], BF16)
        bh = [S.buf("hT") for _ in range(TOT // 128)]
        es_ada = scoped(S)
        emit_ada(C, ada_w, 0, "ada_b0", mod0, bmod0)
        emit_gs_into(C, gs1, bgs1, mod0, bmod0, 1, "n1g0")
        emit_gs_into(C, gs2, bgs2, mod0, bmod0, 4, "n2g0")

        es1 = scoped(S)
        C.xtile = Ring(S, "xtile", [128, D], F32, 2)
        alloc_norm_tmps(C)
        xblk = Ring(S, "xblk", [128, KC, 512], F32, 2)
        for (c0, n, which) in LAT_BLOCKS + [CTX_BLOCK, HAL_BLOCK]:
            xb_, bxb_ = xblk.next()
            bl = [bxb_] * 4
            emit_load_xT(C, xall, c0, n // 128, xb_, bl, 0)
            emit_norm(C, xb_, bl, 0, hT, bh, c0, n, gs1, bgs1, mod0, bmod0, 0, which)
        S.barrier()
        es1.close()
        es_ada.close()

        es_yc = scoped(S)
        yctx = S.sbuf("yctx", [128, KC, NCX], F32)
        byctx = S.buf("yctx")
        es_mix = scoped(S)
        ada1_r = Ring(S, "adaw1s", [128, KC, 768], BF16, 2)
        win_r = Ring(S, "win", [128, KC, 128], BF16, 3)
        wg_r = Ring(S, "wg", [128, 4, 128], BF16, 3)
        xbe = Ring(S, "xbe", [128, NT + 3 + NCX + 3], F32, 2)
        xc_r = Ring(S, "xc", [128, NSC], F32, 2)
        xcb_r = Ring(S, "xcb", [128, NSC], BF16, 2)
        r_r = Ring(S, "rr", [128, NSC], F32, 2)
        b_r = Ring(S, "bb", [128, NSC], F32, 2)
        a_r = Ring(S, "aa", [128, NSC], F32, 2)
        m_r = Ring(S, "mm", [128, NSC], F32, 2)
        yc_r = Ring(S, "yc", [128, NCX], F32, 2)
        sm_r = Ring(S, "sm", [128, 8], F32, 2)
        bsp = [S.buf("sp") for _ in range(16)]
        LB = NT + 3
        w_in_v = w_in.rearrange("(k p) n -> p k n", p=128)
        ada1_w = {0: emit_ada_dma(C, ada_w, 1, 0, ada1_r)}

        def p2a_loadw(ct):
            wt, bw = win_r.next()
            S.dma("gpsimd", wt[:], w_in_v[:, :, ct * 128:(ct + 1) * 128], writes=[bw])
            wg, bwg = wg_r.next()
            for d in range(2):
                S.dma("gpsimd", wg[:, d, :], w_a[d, ct], writes=[bwg], nowaw=True)
                S.dma("gpsimd", wg[:, 2 + d, :], w_i[d, ct], writes=[bwg], nowaw=True)
            return wt, bw, wg, bwg
        p2a_w = {0: p2a_loadw(0)}

        def p2a_front(ct):
            if ct + 1 < KC:
                ada1_w[ct + 1] = emit_ada_dma(C, ada_w, 1, ct + 1, ada1_r)
            if ct + 1 < KC:
                p2a_w[ct + 1] = p2a_loadw(ct + 1)
            emit_ada_mm(C, ct, ada1_w[ct][0], ada1_w[ct][1], "ada_b1", mod1, bmod1)
            wt, bw, wg, bwg = p2a_w[ct]
            xe, bxe = xbe.next()
            S.op("gpsimd", lambda e, xe=xe: e.memset(xe[:, LB:LB + 2], 0.0), writes=[bxe])
            S.op("gpsimd", lambda e, xe=xe: e.memset(xe[:, LB + 2 + NCX:LB + 3 + NCX], 0.0), writes=[bxe])
            for (c0, n, which) in LAT_BLOCKS + [CTX_BLOCK, (HAL0, 3, 0)]:
                ps, bps = C.ps.next()
                for k in range(KC):
                    S.op("tensor", lambda e, ps=ps, k=k, wt=wt, c0=c0, n=n: e.matmul(
                        ps[:, :n], wt[:, k, :], hT[:, k, c0:c0 + n], start=(k == 0), stop=(k == KC - 1)),
                        reads=tile_bufs(bh, c0, n) + [bw], writes=[bps], sig=(k == KC - 1))
                if c0 < CTX0:
                    copy_op(C, evac_engine(C), xe[:, 2 + c0:2 + c0 + n], ps[:, :n], [bps], [bxe])
                elif c0 == CTX0:
                    copy_op(C, evac_engine(C), xe[:, LB + 2:LB + 2 + NCX], ps[:, :n], [bps], [bxe])
                else:
                    S.op("vector", lambda e, ps=ps, xe=xe: e.tensor_tensor(xe[:, 0:2], ps[:, 0:2], V(C, "hmask", 0, 2), ALU.mult),
                         reads=[bps, C.bvec], writes=[bxe])
                    S.op("vector", lambda e, ps=ps, xe=xe: e.tensor_tensor(xe[:, 2 + NT:3 + NT], ps[:, 2:3], V(C, "hmask", 2, 1), ALU.mult),
                         reads=[bps, C.bvec], writes=[bxe])
            xc, bxc = xc_r.next()
            for (dst0, src0, n) in [(0, 0, NT), (NT, LB, NCX)]:
                S.op("scalar", lambda e, xc=xc, xe=xe, dst0=dst0, src0=src0, n=n, ct=ct: e.activation(
                    xc[:, dst0:dst0 + n], xe[:, src0:src0 + n], AF.Identity,
                    bias=V(C, "convb", ct), scale=V(C, "convw", ct)), reads=[bxe, C.bvec], writes=[bxc])
                for k in range(1, 4):
                    S.op("vector", lambda e, xc=xc, xe=xe, dst0=dst0, src0=src0, n=n, k=k, ct=ct: e.scalar_tensor_tensor(
                        xc[:, dst0:dst0 + n], xe[:, src0 + k:src0 + k + n], V(C, "convw", k * 8 + ct), xc[:, dst0:dst0 + n],
                        ALU.mult, ALU.add), reads=[bxe, C.bvec, bxc], writes=[bxc])
            xcb, bxcb = xcb_r.next()
            S.op("gpsimd", lambda e, xcb=xcb, xc=xc: e.tensor_copy(xcb[:], xc[:]), reads=[bxc], writes=[bxcb])
            return (xc, bxc, xcb, bxcb, wg, bwg)
        def p2a_back(ct, xc, bxc, xcb, bxcb, wg, bwg):
            ycs = []
            for d in range(2):
                rr, brr = r_r.next()
                bb, bbb = b_r.next()
                for (c0, n) in [(0, 512), (512, 512), (1024, 512), (1536, 512), (NT, NCX)]:
                    pr, bpr = C.ps.next()
                    S.op("tensor", lambda e, pr=pr, wg=wg, d=d, xcb=xcb, c0=c0, n=n: e.matmul(
                        pr[:, :n], wg[:, d, :], xcb[:, c0:c0 + n], start=True, stop=True), reads=[bwg, bxcb], writes=[bpr])
                    S.op("scalar", lambda e, pr=pr, rr=rr, c0=c0, n=n, d=d, ct=ct: e.activation(
                        rr[:, c0:c0 + n], pr[:, :n], AF.Sigmoid, bias=V(C, "b_a", d * 8 + ct)), reads=[bpr, C.bvec], writes=[brr])
                    pi, bpi = C.ps.next()
                    S.op("tensor", lambda e, pi=pi, wg=wg, d=d, xcb=xcb, c0=c0, n=n: e.matmul(
                        pi[:, :n], wg[:, 2 + d, :], xcb[:, c0:c0 + n], start=True, stop=True), reads=[bwg, bxcb], writes=[bpi])
                    S.op("scalar", lambda e, pi=pi, bb=bb, c0=c0, n=n, d=d, ct=ct: e.activation(
                        bb[:, c0:c0 + n], pi[:, :n], AF.Sigmoid, bias=V(C, "b_i", d * 8 + ct)), reads=[bpi, C.bvec], writes=[bbb])
                aa, baa = a_r.next()
                mm, bmm = m_r.next()
                cn = d * 8 + ct
                sm, bsm = sm_r.next()
                S.op("scalar", lambda e, aa=aa, rr=rr, cn=cn: e.activation(aa[:], rr[:], AF.Exp, scale=cneg[:, cn:cn + 1]),
                     reads=[brr, bcneg], writes=[baa])
                S.op("scalar", lambda e, mm=mm, rr=rr, cn=cn: e.activation(mm[:], rr[:], AF.Exp, scale=cneg[:, 16 + cn:17 + cn]),
                     reads=[brr, bcneg], writes=[bmm])
                S.op("vector", lambda e, sm=sm, rr=rr: e.tensor_reduce(sm[:, 0:1], rr[:, 0:NT], AX.X, ALU.add), reads=[brr], writes=[bsm])
                S.op("scalar", lambda e, mm=mm: e.activation(mm[:], mm[:], AF.Sqrt, bias=1.0, scale=-1.0), reads=[bmm], writes=[bmm])
                S.op("gpsimd", lambda e, bb=bb, xc=xc: e.tensor_tensor(bb[:], bb[:], xc[:], ALU.mult), reads=[bbb, bxc], writes=[bbb])
                S.op("vector", lambda e, bb=bb, mm=mm: e.tensor_tensor(bb[:], bb[:], mm[:], ALU.mult), reads=[bbb, bmm], writes=[bbb])
                S.dma("sync", a_sp[cn], aa[:, 0:NT], reads=[baa], writes=[bsp[cn]])
                S.dma("sync", b_sp[cn], bb[:, 0:NT], reads=[bbb], writes=[bsp[cn]], nowaw=True)
                yc, byc = yc_r.next()
                o = ct * 4 + d * 2
                if d == 0:
                    S.op("vector", lambda e, yc=yc, aa=aa, bb=bb: e.tensor_tensor_scan(
                        yc[:], aa[:, NT:NSC], bb[:, NT:NSC], 0.0, ALU.mult, ALU.add), reads=[baa, bbb], writes=[byc])
                    S.op("vector", lambda e, mm=mm, aa=aa, bb=bb: e.tensor_tensor_scan(
                        mm[:, 0:NT], aa[:, 0:NT], bb[:, 0:NT], 0.0, ALU.mult, ALU.add), reads=[baa, bbb], writes=[bmm])
                    st0, hend = yc[:, NCX - 1:NCX], mm[:, NT - 1:NT]
                else:
                    S.op("vector", lambda e, yc=yc, aa=aa, bb=bb: e.tensor_tensor_scan(
                        rev_ap(yc[:]), rev_ap(aa[:, NT:NSC]), rev_ap(bb[:, NT:NSC]), 0.0, ALU.mult, ALU.add), reads=[baa, bbb], writes=[byc])
                    S.op("vector", lambda e, mm=mm, aa=aa, bb=bb: e.tensor_tensor_scan(
                        rev_ap(mm[:, 0:NT]), rev_ap(aa[:, 0:NT]), rev_ap(bb[:, 0:NT]), 0.0, ALU.mult, ALU.add), reads=[baa, bbb], writes=[bmm])
                    st0, hend = yc[:, 0:1], mm[:, 0:1]
                S.op("vector", lambda e, st0=st0, cn=cn: e.tensor_copy(cst[:, cn:cn + 1], st0), reads=[byc], writes=[bcst])
                S.op("scalar", lambda e, sm=sm, cn=cn, o=o: e.activation(summ[:, o:o + 1], sm[:, 0:1], AF.Exp, scale=cneg[:, cn:cn + 1]),
                     reads=[bsm, bcneg], writes=[bsumm])
                S.op("vector", lambda e, hend=hend, o=o: e.tensor_copy(summ[:, o + 1:o + 2], hend), reads=[bmm], writes=[bsumm])
                ycs.append((yc, byc))
            (yc0, byc0), (yc1, byc1) = ycs
            S.op("gpsimd", lambda e, yc0=yc0, yc1=yc1, ct=ct: e.tensor_tensor(yctx[:, ct, :], yc0[:], yc1[:], ALU.add), reads=[byc0, byc1], writes=[byctx])
        fr = p2a_front(0)
        for ct in range(KC):
            cur = fr
            if ct + 1 < KC:
                fr = p2a_front(ct + 1)
            p2a_back(ct, *cur)
        emit_gs_into(C, gs3, bgs3, mod1, bmod1, 1, "n1g1")
        emit_gs_into(C, gs4, bgs4, mod1, bmod1, 4, "n2g1")
        bsl, bsa = S.buf("summ_loc"), S.buf("summ_all")
        S.dma("sync", summ_loc, summ[:], reads=[bsumm], writes=[bsl])
        S.collective("AllGather", GROUPS, summ_loc, summ_all_d, [bsl], bsa)
        S.dma("sync", sall[:], summ_all_d.rearrange("(r p) c -> p r c", p=128), reads=[bsa], writes=[bsall])
        S.barrier()
        es_mix.close()

        es_yg = scoped(S)
        ygT = S.sbuf("ygT", [128, KC, NSC], BF16)
        byg = [S.buf("yg") for _ in range(NSC // 128)]
        es_p2b = scoped(S)
        a2_r = Ring(S, "a2", [128, NT], F32, 4)
        b2_r = Ring(S, "b2", [128, NT], F32, 4)
        y_r = Ring(S, "yy", [128, NT], F32, 2)
        sm_r = Ring(S, "sm2", [128, 8], F32, 2)
        gtmp = Ring(S, "gtmp", [128, 512], F32, 3)
        win_r = Ring(S, "win2", [128, KC, 128], BF16, 2)
        carr = S.sbuf("carr", [128, 16], F32)
        ctmp = S.sbuf("ctmp", [128, 8], F32)
        bcarr = S.buf("carr")
        S.op("vector", lambda e: e.tensor_copy(carr[:], cst[:]), reads=[bcst], writes=[bcarr])
        for d in range(2):
            order, sel = ([0, 1, 2, 3], "sel_f") if d == 0 else ([3, 2, 1, 0], "sel_r")
            cs_ = carr[:, d * 8:(d + 1) * 8]
            for i in order:
                Pv = bass.AP(sall[:].tensor, sall[:, i, d * 2:d * 2 + 1].offset, [list(sall[:].ap[0]), [4, 8]])
                Hv = bass.AP(sall[:].tensor, sall[:, i, d * 2 + 1:d * 2 + 2].offset, [list(sall[:].ap[0]), [4, 8]])
                S.op("vector", lambda e, cs_=cs_, Pv=Pv: e.tensor_tensor(ctmp[:], cs_, Pv, ALU.mult), reads=[bcarr, bsall], writes=[bcarr])
                S.op("vector", lambda e, Hv=Hv: e.tensor_tensor(ctmp[:], ctmp[:], Hv, ALU.add), reads=[bcarr, bsall], writes=[bcarr])
                S.op("vector", lambda e, cs_=cs_: e.tensor_tensor(ctmp[:], ctmp[:], cs_, ALU.subtract), reads=[bcarr], writes=[bcarr])
                S.op("vector", lambda e, cs_=cs_, i=i, sel=sel: e.scalar_tensor_tensor(
                    cs_, ctmp[:], V(C, sel, i), cs_, ALU.mult, ALU.add), reads=[bcarr, C.bvec], writes=[bcarr])
        for ct in range(KC):
            wt, bw = win_r.next()
            S.dma("gpsimd", wt[:], w_in_v[:, :, D + ct * 128:D + (ct + 1) * 128], writes=[bw])
            ys = []
            for d in range(2):
                cn = d * 8 + ct
                aa, baa = a2_r.next()
                bb, bbb = b2_r.next()
                S.dma("sync", aa[:], a_sp[cn], reads=[bsp[cn]], writes=[baa])
                S.dma("sync", bb[:], b_sp[cn], reads=[bsp[cn]], writes=[bbb])
                yy, byy = y_r.next()
                if d == 0:
                    S.op("vector", lambda e, yy=yy, aa=aa, bb=bb, cn=cn: e.tensor_tensor_scan(
                        yy[:], aa[:], bb[:], carr[:, cn:cn + 1], ALU.mult, ALU.add), reads=[baa, bbb, bcarr], writes=[byy])
                else:
                    S.op("vector", lambda e, yy=yy, aa=aa, bb=bb, cn=cn: e.tensor_tensor_scan(
                        rev_ap(yy[:]), rev_ap(aa[:]), rev_ap(bb[:]), carr[:, cn:cn + 1], ALU.mult, ALU.add), reads=[baa, bbb, bcarr], writes=[byy])
                ys.append((yy, byy))
            (y0, by0), (y1, by1) = ys
            S.op("gpsimd", lambda e, y0=y0, y1=y1: e.tensor_tensor(y0[:], y0[:], y1[:], ALU.add), reads=[by0, by1], writes=[by0])
            for (c0, n, which) in LAT_BLOCKS + [CTX_BLOCK]:
                pg, bpg = C.ps.next()
                for k in range(KC):
                    S.op("tensor", lambda e, pg=pg, k=k, wt=wt, c0=c0, n=n: e.matmul(
                        pg[:, :n], wt[:, k, :], hT[:, k, c0:c0 + n], start=(k == 0), stop=(k == KC - 1)),
                        reads=tile_bufs(bh, c0, n) + [bw], writes=[bpg], sig=(k == KC - 1))
                t1, bt1 = gtmp.next()
                S.op("scalar", lambda e, t1=t1, pg=pg, n=n: e.activation(t1[:, :n], pg[:, :n], AF.Gelu_apprx_tanh), reads=[bpg], writes=[bt1])
                if which == 0:
                    S.op("vector", lambda e, t1=t1, y0=y0, c0=c0, n=n, ct=ct: e.tensor_tensor(ygT[:, ct, c0:c0 + n], t1[:, :n], y0[:, c0:c0 + n], ALU.mult),
                         reads=[bt1, by0], writes=tile_bufs(byg, c0, n))
                else:
                    S.op("vector", lambda e, t1=t1, c0=c0, n=n, ct=ct: e.tensor_tensor(ygT[:, ct, c0:c0 + n], t1[:, :n], yctx[:, ct, :], ALU.mult),
                         reads=[bt1, byctx], writes=tile_bufs(byg, c0, n))
        S.barrier()
        es_p2b.close()

        S.es = es
        xT = S.sbuf("xT", [128, KC, NSC], F32, side="right")
        bx = [S.buf("xT") for _ in range(NSC // 128)]
        es_o = scoped(S)
        C.xtile = Ring(S, "xtile", [128, D], F32, 2)
        emit_load_xT(C, xall, 0, NSC // 128, xT, bx, 0)
        wo_r = Ring(S, "wo", [128, KC, D], BF16, 1)
        wot, bwo = load_w(C, wo_r, w_out, D, 0, D)
        BL = LAT_BLOCKS + [CTX_BLOCK]
        emit_proj_residual(C, ygT, byg, KC, wot, bwo, xT, bx, BL, mod0, bmod0, 2)
        S.barrier()
        es_o.close()
        es_yg.close()
        es_yc.close()

        es_f = scoped(S)
        alloc_norm_tmps(C)
        emit_norm_blocks(C, xT, bx, hT, bh, BL, gs2, bgs2, mod0, bmod0, 3)
        alloc_ffn(C, 4)
        emit_ffn(C, hT, bh, xT, bx, BL, f_w1, f_w3, f_w2, DFF, mod0, bmod0, 5)
        emit_norm_blocks(C, xT, bx, hT, bh, BL, gs3, bgs3, mod1, bmod1, 0)
        S.barrier()
        es_f.close()

        es_qo = scoped(S)
        qoT = S.sbuf("qoT", [128, 8, NT], BF16, side="right")
        bqo = [[S.buf("qo") for _ in range(NT // 512)] for _ in range(8)]
        es_q = scoped(S)
        wq_r = Ring(S, "wq", [128, KC, 1536], BF16, 1)
        wqt, bwq = load_w(C, wq_r, w_qkv, D, 0, 1536)
        cs_r = Ring(S, "cs", [128, 2, 512], F32, 2)
        rotm = S.sbuf("rotm", [128, 128], F32)
        rotb = S.sbuf("rotb", [128, 128], BF16)
        brot = S.buf("rot")
        S.dma("sync", rotm[:], rotm_d, writes=[brot])
        S.op("vector", lambda e: e.tensor_copy(rotb[:], rotm[:]), reads=[brot], writes=[brot])
        C.rstd = Ring(S, "rstd2", [128, 512], F32, 3)
        qst = Ring(S, "qst", [128, 512], BF16, 2)
        qgb = Ring(S, "qgb", [128, 512], BF16, 3)
        qsq = Ring(S, "qsq", [128, 512], BF16, 3)
        qt1 = Ring(S, "qt1", [128, 512], F32, 4)
        vst = Ring(S, "vst", [128, 256], BF16, 2)
        bklat = [S.buf("klat") for _ in range(2)]
        bkall = [S.buf("kall") for _ in range(2)]
        bkctx = S.buf("kctx")
        bvlat = [S.buf("vlat") for _ in range(2)]
        bvall = [S.buf("vall") for _ in range(2)]
        bvctx = S.buf("vctx")

        def head_mm(hd, c0, n, which):
            rh = tile_bufs(bh, c0, n)
            pq, bpq = C.ps.next()
            for k in range(KC):
                S.op("tensor", lambda e, k=k: e.matmul(
                    pq[:, :n], wqt[:, k, hd * 128:(hd + 1) * 128], hT[:, k, c0:c0 + n], start=(k == 0), stop=(k == KC - 1)),
                    reads=rh + [bwq], writes=[bpq], sig=(k == KC - 1))
            return (hd, c0, n, which, pq, bpq)

        def head_a(hd, c0, n, which, pq, bpq):
            gname = "q_g" if hd < 8 else "k_g"
            sqt, bsqt = qsq.next()
            S.op("scalar", lambda e: e.activation(sqt[:, :n], pq[:, :n], AF.Square), reads=[bpq], writes=[bsqt])
            pss, bpss = C.ps.next()
            S.op("tensor", lambda e: e.matmul(pss[:, :n], C.ones[:], sqt[:, :n], start=True, stop=True),
                 reads=[bsqt, C.bones], writes=[bpss])
            rs, brs = C.rstd.next()
            S.op("scalar", lambda e: e.activation(rs[:, :n], pss[:, :n], AF.Sqrt, bias=C.epsb[:, 0:1], scale=1.0 / 128),
                 reads=[bpss, C.bepsb], writes=[brs])
            S.op("vector", lambda e: e.reciprocal(rs[:, :n], rs[:, :n]), reads=[brs], writes=[brs])
            qn, bqn = qgb.next()
            S.op("vector", lambda e: e.scalar_tensor_tensor(
                qn[:, :n], pq[:, :n], V(C, gname, 0), rs[:, :n], ALU.mult, ALU.mult), reads=[bpq, C.bvec, brs], writes=[bqn])
            pr = bpr = None
            if which == 0:
                pr, bpr = C.ps.next()
                S.op("tensor", lambda e: e.matmul(pr[:, :n], rotb[:], qn[:, :n], start=True, stop=True),
                     reads=[bqn, brot], writes=[bpr])
            return (hd, c0, n, which, qn, bqn, pr, bpr, head_proj.cs if which == 0 else None)

        def head_b(hd, c0, n, which, qn, bqn, pr, bpr, cs_):
            if hd < 8:
                dst, bdst = qoT[:, hd, c0:c0 + n], [bqo[hd][c0 // 512]]
            else:
                qo_, bqo_ = qst.next()
                dst, bdst = qo_[:, :n], [bqo_]
            if which == 0:
                cs, bcs = cs_
                t0_, bt0 = qt1.next()
                t1, bt1 = qt1.next()
                S.op("vector", lambda e: e.tensor_tensor(t0_[:, :n], qn[:, :n], cs[:, 0, :n], ALU.mult), reads=[bqn, bcs], writes=[bt0])
                S.op("vector", lambda e: e.tensor_tensor(t1[:, :n], pr[:, :n], cs[:, 1, :n], ALU.mult), reads=[bpr, bcs], writes=[bt1])
                S.op("vector", lambda e: e.tensor_tensor(dst, t0_[:, :n], t1[:, :n], ALU.add), reads=[bt0, bt1], writes=bdst)
            else:
                S.op("vector", lambda e: e.tensor_copy(dst, qn[:, :n]), reads=[bqn], writes=bdst)
            if hd >= 8:
                g = hd - 8
                if which == 0:
                    S.dma("sync", klat[g][:, c0:c0 + n], dst, reads=bdst, writes=[bklat[g]], nowaw=True)
                else:
                    S.dma("sync", kctx[:, g * NCX:(g + 1) * NCX], dst, reads=bdst, writes=[bkctx], nowaw=True)

        def head_proj(hd, c0, n, which, pq=None, bpq=None):
            if pq is None:
                _, _, _, _, pq, bpq = head_mm(hd, c0, n, which)
            head_b(*head_a(hd, c0, n, which, pq, bpq))

        def load_cs(c0, n):
            cs, bcs = cs_r.next()
            S.dma("sync", cs[:, 0, :n], cos_d[:, c0:c0 + n], writes=[bcs])
            S.dma("sync", cs[:, 1, :n], sin_d[:, c0:c0 + n], writes=[bcs], nowaw=True)
            head_proj.cs = (cs, bcs)
        for (c0, n, which) in BL:
            if which == 0:
                load_cs(c0, n)
            for hd in (8, 9):
                head_proj(hd, c0, n, which)
        for g in range(2):
            S.collective("AllGather", GROUPS, klat[g], kall[g], [bklat[g]], bkall[g])
        for (c0, n, which) in BL:
            rh = tile_bufs(bh, c0, n)
            for t0 in range(0, n, 128):
                pv, bpv = C.ps.next()
                for k in range(KC):
                    S.op("tensor", lambda e, pv=pv, k=k, t0=t0, c0=c0: e.matmul(
                        pv[:, 0:256], hT[:, k, c0 + t0:c0 + t0 + 128], wqt[:, k, 1280:1536], start=(k == 0), stop=(k == KC - 1)),
                        reads=rh + [bwq], writes=[bpv], sig=(k == KC - 1))
                vo, bvo = vst.next()
                copy_op(C, evac_engine(C), vo[:], pv[:, 0:256], [bpv], [bvo])
                if which == 0:
                    t = c0 + t0
                    hh = t // (NT // 2)
                    r0 = t % (NT // 2)
                    S.dma("sync", vlat[hh][r0:r0 + 128, :], vo[:], reads=[bvo], writes=[bvlat[hh]], nowaw=True)
                else:
                    S.dma("sync", vctx[t0:t0 + 128, :], vo[:], reads=[bvo], writes=[bvctx], nowaw=True)
        for hh in range(2):
            S.collective("AllGather", GROUPS, vlat[hh], vall[hh], [bvlat[hh]], bvall[hh])
        for (c0, n, which) in LAT_BLOCKS:
            load_cs(c0, n)
            mm = {0: head_mm(0, c0, n, which), 1: head_mm(1, c0, n, which)}
            st = {0: head_a(*mm[0])}
            for hd in range(8):
                if hd + 2 < 8:
                    mm[hd + 2] = head_mm(hd + 2, c0, n, which)
                if hd + 1 < 8:
                    st[hd + 1] = head_a(*mm[hd + 1])
                head_b(*st[hd])
        S.barrier()
        es_q.close()
        es_l0.close()

        es_a = scoped(S)
        kT = S.sbuf("kT", [128, 2, NKEY], BF16)
        bk = S.buf("kT")
        va = S.sbuf("va", [128, NKT, 2, 130], BF16)
        bva = S.buf("va")
        S.op("vector", lambda e: e.memset(va[:, :, :, 128:130], 1.0), writes=[bva])
        for g in range(2):
            S.dma("sync", kT[:, g, 0:NCX], kctx[:, g * NCX:(g + 1) * NCX], reads=[bkctx], writes=[bk], nowaw=True)
            for r in range(4):
                S.dma("sync", kT[:, g, NCX + r * NT:NCX + (r + 1) * NT], kall[g][r * 128:(r + 1) * 128, :], reads=[bkall[g]], writes=[bk], nowaw=True)
            S.dma("sync", va[:, 0:2, g, 0:128], vctx[:, g * 128:(g + 1) * 128].rearrange("(t p) d -> p t d", p=128), reads=[bvctx], writes=[bva], nowaw=True)
            for hh in range(2):
                for r in range(4):
                    kt0 = 2 + r * 16 + hh * 8
                    S.dma("sync", va[:, kt0:kt0 + 8, g, 0:128],
                          vall[hh][r * (NT // 2):(r + 1) * (NT // 2), g * 128:(g + 1) * 128].rearrange("(t p) d -> p t d", p=128),
                          reads=[bvall[hh]], writes=[bva], nowaw=True)
        pT_r = Ring(S, "pT", [128, 512], BF16, 4)
        on_r = Ring(S, "on", [128, 128], BF16, 2)
        ri_r = Ring(S, "ri", [128, 2], F32, 2)
        SCL = 1.0 / float(np.sqrt(128.0))
        psO = SubRing(C.ps.tiles[0:4])
        psS = SubRing(C.ps.tiles[4:7])
        psT = SubRing(C.ps.tiles[7:8])
        for qb in range(NT // 512):
            for h in range(8):
                g = h // 4
                po = [psO.next(), psO.next()]

                def s_issue(kt, g=g, h=h, qb=qb):
                    ps, bps = psS.next()
                    S.op("tensor", lambda e, ps=ps, kt=kt: e.matmul(
                        ps[:], kT[:, g, kt * 128:(kt + 1) * 128], qoT[:, h, qb * 512:(qb + 1) * 512], start=True, stop=True),
                        reads=[bk, bqo[h][qb]], writes=[bps])
                    pT, bpT = pT_r.next()
                    S.op("scalar", lambda e, pT=pT, ps=ps: e.activation(pT[:], ps[:], AF.Exp, scale=SCL), reads=[bps], writes=[bpT])
                    return pT, bpT
                pend = [s_issue(0), s_issue(1)]
                for kt in range(NKT):
                    pT, bpT = pend.pop(0)
                    if kt + 2 < NKT:
                        pend.append(s_issue(kt + 2))
                    for qt in range(4):
                        pot, bpot = po[qt // 2]
                        c = (qt % 2) * 129
                        S.op("tensor", lambda e, pot=pot, c=c, pT=pT, qt=qt, kt=kt, g=g: e.matmul(
                            pot[:, c:c + 129], pT[:, qt * 128:(qt + 1) * 128], va[:, kt, g, 0:129], start=(kt == 0 and qt % 2 == 0), stop=(kt == NKT - 1),
                            skip_group_check=True),
                            reads=[bpT, bva], writes=[bpot], sig=(qt == 3))
                for qt in range(4):
                    pot, bpot = po[qt // 2]
                    c = (qt % 2) * 129
                    ri, bri = ri_r.next()
                    S.op("vector", lambda e, ri=ri, pot=pot, c=c: e.reciprocal(ri[:, 0:1], pot[:, c + 128:c + 129]), reads=[bpot], writes=[bri])
                    on, bon = on_r.next()
                    S.op("vector", lambda e, on=on, pot=pot, c=c, ri=ri: e.tensor_scalar(on[:], pot[:, c:c + 128], ri[:, 0:1], None, ALU.mult),
                         reads=[bpot, bri], writes=[bon])
                    pt, bpt = psT.next()
                    ptb = pt[:].bitcast(BF16)
                    S.op("tensor", lambda e, ptb=ptb, on=on: e.transpose(ptb[:, 0:128], on[:], identb[:]), reads=[bon, bidb], writes=[bpt])
                    col = qb * 512 + qt * 128
                    S.op("scalar", lambda e, ptb=ptb, h=h, col=col: e.copy(qoT[:, h, col:col + 128], ptb[:, 0:128]), reads=[bpt], writes=[bqo[h][qb]])
        S.barrier()
        es_a.close()

        es_o = scoped(S)
        wo_r = Ring(S, "wo2", [128, KC, D], BF16, 1)
        wot, bwo = load_w(C, wo_r, w_o, D, 0, D)
        bqo_cols = [None] * (NT // 128)
        for (c0, n, which) in LAT_BLOCKS:
            ri = [bqo[h][c0 // 512] for h in range(8)]
            wx = tile_bufs(bx, c0, n)
            for dc in range(KC):
                po_, bpo_ = C.ps.next()
                for k in range(KC):
                    S.op("tensor", lambda e, po_=po_, k=k, dc=dc, c0=c0, n=n: e.matmul(
                        po_[:, :n], wot[:, k, dc * 128:(dc + 1) * 128], qoT[:, k, c0:c0 + n], start=(k == 0), stop=(k == KC - 1)),
                        reads=ri + [bwo], writes=[bpo_], sig=(k == KC - 1))
                S.op("vector", lambda e, po_=po_, dc=dc, c0=c0, n=n: e.scalar_tensor_tensor(
                    xT[:, dc, c0:c0 + n], po_[:, :n], mod1[:, 2 * 8 + dc, 0:1], xT[:, dc, c0:c0 + n], ALU.mult, ALU.add),
                    reads=[bpo_, bmod1], writes=wx)
        S.barrier()
        es_o.close()
        es_qo.close()

        S.es = es
        gates = S.sbuf("gates", [128, NT // 128, NE], F32)
        maskt = S.sbuf("maskt", [128, NT // 128, NE], F32)
        bgates = S.buf("gates")
        es_h2 = scoped(S)
        hT2 = S.sbuf("hT2", [128, KC, NT], BF16)
        bh2 = [S.buf("hT2") for _ in range(NT // 128)]
        es_r = scoped(S)
        alloc_norm_tmps(C)
        hf = S.sbuf("hf", [128, KC, NT], F32)
        bhf = [S.buf("hf") for _ in range(NT // 128)]
        emit_norm_blocks(C, xT, bx, hT2, bh2, LAT_BLOCKS, gs4, bgs4, mod1, bmod1, 3, hf=hf, bhf=bhf)
        emit_router(C, hf, bhf, router, gates, bgates, maskt=maskt)
        S.barrier()
        es_r.close()

        es_t = scoped(S)
        bhtm = emit_htm(C, hT2, bh2, htm_d, identb, bidb)
        S.barrier()
        es_t.close()
        es_h2.close()
        es_m = scoped(S)
        emit_moe_sparse(C, bhtm, xT, bx, maskt, gates, bgates, cst_d, htm_d, m_w1, m_w3, m_w2, mod1, bmod1, identb, bidb)
        S.barrier()
        es_m.close()

        es_z = scoped(S)
        emit_final(C, xT, bx, fin_g, out_d, outb)
        S.finish()
        S.emit()
        es_z.close()
    return nc


def rope_tables():
    nfreq = 32
    t = np.arange(SEQ)
    row = (t // 64).astype(np.float32)
    col = (t % 64).astype(np.float32)
    freqs = (np.float32(10000.0) ** (-np.arange(nfreq, dtype=np.float32) / np.float32(nfreq))).astype(np.float32)
    ang = np.zeros((128, SEQ), np.float32)
    for d in range(128):
        pos = row if d < 64 else col
        ang[d] = pos * freqs[d % 32]
    return np.cos(ang).astype(np.float32), np.sin(ang).astype(np.float32)


def rot_matrix_T():
    R = np.zeros((128, 128), np.float32)
    for m in range(128):
        if m % 64 < 32:
            R[m, m + 32] = -1.0
        else:
            R[m, m - 32] = 1.0
    return np.ascontiguousarray(R.T)


_CACHE = {}
_DEBUG = {}


def _prog(name):
    if name not in _CACHE:
        _CACHE[name] = {"A": lambda: build_l0("A"), "B": lambda: build_l0("B"), "C": build_l1, "F": build_fused}[name]()
    return _CACHE[name]


def kernel_unfused(**inp):
    inp = {k: np.asarray(v) for k, v in inp.items()}
    x = inp["x"].astype(np.float32, copy=False)
    ctx = inp["ctx"].astype(np.float32, copy=False)
    ident = np.eye(128, dtype=np.float32)
    cores = list(range(8))
    maps0 = []
    for c in cores:
        b, j = c // 4, c % 4
        t0 = j * NT
        hal = np.zeros((128, D), np.float32)
        if j > 0:
            hal[0:2] = x[b, t0 - 2:t0]
        if j < 3:
            hal[2] = x[b, t0 + NT]
        xall = np.concatenate([x[b, t0:t0 + NT], ctx[b], hal], axis=0)
        maps0.append({
            "xall": np.ascontiguousarray(xall), "vecs": pack_vecs(inp, b, j), "ident": ident,
            "ada_w": inp["ada_w"], "w_in": inp["rg_w_in"][0], "w_a": inp["rg_w_a"][0], "w_i": inp["rg_w_i"][0],
        })
    resA = run_bass_kernel_spmd(_prog("A"), maps0, core_ids=cores).results
    cosf, sinf = rope_tables()
    rotm = rot_matrix_T()
    maps1 = []
    for c in cores:
        b, j = c // 4, c % 4
        m = dict(maps0[c])
        m["summ_all"] = np.ascontiguousarray(np.concatenate([resA[b * 4 + i]["summ"] for i in range(4)], axis=1))
        m.update({"w_out": inp["rg_w_out"][0], "f_w1": inp["ffn_w1"][0], "f_w3": inp["ffn_w3"][0], "f_w2": inp["ffn_w2"][0],
                  "w_qkv": inp["attn_w_qkv"][0], "cos": np.ascontiguousarray(cosf[:, j * NT:(j + 1) * NT]),
                  "sin": np.ascontiguousarray(sinf[:, j * NT:(j + 1) * NT]), "rotm": rotm})
        maps1.append(m)
    resB = run_bass_kernel_spmd(_prog("B"), maps1, core_ids=cores).results
    if _DEBUG.get("stop") == "B":
        return resA, resB
    maps2 = []
    for c in cores:
        b, j = c // 4, c % 4
        kparts, vparts = [], []
        for g in range(2):
            segs = [resB[b * 4]["kT"][:, g * NSC + NT:(g + 1) * NSC]]
            segs += [resB[b * 4 + i]["kT"][:, g * NSC:g * NSC + NT] for i in range(4)]
            kparts.append(np.concatenate(segs, axis=1))
        kfull = np.ascontiguousarray(np.concatenate(kparts, axis=1))
        vfull = np.ascontiguousarray(np.concatenate([resB[b * 4]["vtm"][NT:NSC]] + [resB[b * 4 + i]["vtm"][0:NT] for i in range(4)], axis=0))
        maps2.append({
            "vecs": maps0[c]["vecs"], "ident": ident, "ada_w": inp["ada_w"],
            "x1T": resB[c]["x1T"], "qT": resB[c]["qT"], "kT": kfull, "vtm": vfull,
            "w_o": inp["attn_w_o"][0], "router": inp["moe_router"][0],
            "m_w1": inp["moe_w1"][0], "m_w3": inp["moe_w3"][0], "m_w2": inp["moe_w2"][0], "fin_g": inp["final_g"],
        })
    resC = run_bass_kernel_spmd(_prog("C"), maps2, core_ids=cores).results
    out = np.zeros((2, SEQ, D), np.float32)
    for c in cores:
        b, j = c // 4, c % 4
        out[b, j * NT:(j + 1) * NT] = resC[c]["out"]
    return out


def kernel(**inp):
    inp = {k: np.asarray(v) for k, v in inp.items()}
    x = inp["x"].astype(np.float32, copy=False)
    ctx = inp["ctx"].astype(np.float32, copy=False)
    ident = np.eye(128, dtype=np.float32)
    cosf, sinf = rope_tables()
    rotm = rot_matrix_T()
    cst = np.zeros((128, 385), np.float32)
    cst[:, 0:128] = np.triu(np.ones((128, 128), np.float32), 1)
    cst[:, 128:384] = np.arange(256, dtype=np.float32)[None, :]
    cst[:, 384] = np.arange(128, dtype=np.float32)
    cores = list(range(8))
    maps = []
    for c in cores:
        b, j = c // 4, c % 4
        t0 = j * NT
        hal = np.zeros((128, D), np.float32)
        if j > 0:
            hal[0:2] = x[b, t0 - 2:t0]
        if j < 3:
            hal[2] = x[b, t0 + NT]
        xall = np.concatenate([x[b, t0:t0 + NT], ctx[b], hal], axis=0)
        maps.append({
            "xall": np.ascontiguousarray(xall), "vecs": pack_vecs(inp, b, j), "ident": ident,
            "ada_w": inp["ada_w"], "w_in": inp["rg_w_in"][0], "w_a": inp["rg_w_a"][0], "w_i": inp["rg_w_i"][0],
            "w_out": inp["rg_w_out"][0], "f_w1": inp["ffn_w1"][0], "f_w3": inp["ffn_w3"][0], "f_w2": inp["ffn_w2"][0],
            "w_qkv": inp["attn_w_qkv"][0], "cos": np.ascontiguousarray(cosf[:, j * NT:(j + 1) * NT]),
            "sin": np.ascontiguousarray(sinf[:, j * NT:(j + 1) * NT]), "rotm": rotm,
            "w_o": inp["attn_w_o"][0], "router": inp["moe_router"][0],
            "m_w1": inp["moe_w1"][0], "m_w3": inp["moe_w3"][0], "m_w2": inp["moe_w2"][0], "fin_g": inp["final_g"],
            "cst": cst,
        })
    res = run_bass_kernel_spmd(_prog("F"), maps, core_ids=cores).results
    out = np.zeros((2, SEQ, D), np.float32)
    for c in cores:
        b, j = c // 4, c % 4
        out[b, j * NT:(j + 1) * NT] = res[c]["out"]
    return out
```

```python
from contextlib import ExitStack
import numpy as np
import ml_dtypes
import concourse.bass as bass
import concourse.mybir as mybir
from concourse.bass_utils import run_bass_kernel_spmd

F32 = mybir.dt.float32
BF16 = mybir.dt.bfloat16
AF = mybir.ActivationFunctionType
ALU = mybir.AluOpType
AX = mybir.AxisListType

D = 1024
KC = 8
NT = 2048
NCX = 256
LAT0, CTX0, HAL0 = 0, NT, NT + NCX
TOT = NT + NCX + 128
DFF = 2816
DFE = 3584
NE = 8
EPS = 1e-6
SEQ = 8192
NKEY = SEQ + NCX
NKT = NKEY // 128


class Buf:
    __slots__ = ("name", "w", "r", "sem", "cnt")

    def __init__(self, name):
        self.name = name
        self.w = None
        self.r = []
        self.sem = None
        self.cnt = 0


class Sched:
    ENG = ("tensor", "vector", "scalar", "gpsimd", "sync")

    def __init__(self, nc, es):
        self.nc = nc
        self.es = es
        self.es_sem = es
        self.ops = {e: [] for e in self.ENG}
        self.cnt = {e: 0 for e in self.ENG}
        self.seen = {e: {} for e in self.ENG}
        self.esem = {e: es.enter_context(nc.semaphore("s_" + e)) for e in self.ENG}
        self.dsems = []
        self.out_tokens = []
        self.nbuf = 0
        self.cond = None
        self.regs = {e: [es.enter_context(getattr(nc, e).register(f"r{i}_" + e)) for i in range(3)] for e in self.ENG}

    def sbuf(self, name, shape, dtype, side=None):
        self.nbuf += 1
        name = f"sb{self.nbuf}_{name}"
        if side is None:
            return self.es.enter_context(self.nc.sbuf_tensor(name, list(shape), dtype))
        return self.es.enter_context(self.nc.sbuf_tensor(name, list(shape), dtype, side=side))

    def psum(self, name, shape, dtype=F32):
        self.nbuf += 1
        name = f"pp{self.nbuf}_{name}"
        return self.es.enter_context(self.nc.psum_tensor(name, list(shape), dtype))

    def buf(self, name="b"):
        self.nbuf += 1
        return Buf(f"{name}{self.nbuf}")

    def _waits(self, engine, deps):
        need = {}
        for tok in deps:
            if tok is None:
                continue
            key, val = tok
            if isinstance(key, str):
                if key == engine and engine in ("tensor", "sync"):
                    continue
                sem = self.esem[key]
                assert val <= self.cnt[key], f"dep on unsignaled op of {key}"
            else:
                sem = key
            k = id(sem)
            if self.seen[engine].get(k, 0) >= val:
                continue
            if k not in need or need[k][1] < val:
                need[k] = (sem, val)
        for k, (sem, val) in need.items():
            self.seen[engine][k] = val
        return list(need.values())

    def op(self, engine, fn, reads=(), writes=(), sig=True):
        deps = []
        for b in reads:
            deps.append(b.w)
        for b in writes:
            deps.append(b.w)
            deps.extend(b.r)
        waits = self._waits(engine, deps)
        if sig:
            self.cnt[engine] += 1
            tok = (engine, self.cnt[engine])
            inc = (self.esem[engine], 1)
        else:
            tok = (engine, self.cnt[engine] + 1)
            inc = None
        self.ops[engine].append((fn, waits, inc, None))
        for b in reads:
            b.r.append(tok)
        for b in writes:
            b.w = tok
            b.r = []
        return tok

    def collective(self, kind, groups, in_ap, out_ap, reads, out_buf):
        deps = [b.w for b in reads] + [out_buf.w] + list(out_buf.r)
        waits = self._waits("gpsimd", deps)
        if out_buf.sem is None:
            out_buf.sem = self.es_sem.enter_context(self.nc.semaphore("c_" + out_buf.name))
            self.dsems.append(out_buf)
        out_buf.cnt += 1
        tok = (out_buf.sem, out_buf.cnt)

        def fn(eng):
            return eng.collective_compute(kind, ALU.bypass, replica_groups=groups, ins=[in_ap], outs=[out_ap])
        self.ops["gpsimd"].append((fn, waits, (out_buf.sem, 1), out_buf))
        for b in reads:
            b.r.append(tok)
        out_buf.w = tok
        out_buf.r = []
        return tok

    def dma(self, queue, out, in_, reads=(), writes=(), out_sem_buf=None, nowaw=False, **kw):
        deps = []
        for b in reads:
            deps.append(b.w)
        for b in writes:
            if not (nowaw and b.w is not None and b.sem is not None and b.w[0] is b.sem):
                deps.append(b.w)
            deps.extend(b.r)
        waits = self._waits(queue, deps)
        holder = writes[0] if writes else out_sem_buf
        if holder.sem is None:
            holder.sem = self.es_sem.enter_context(self.nc.semaphore("d_" + holder.name))
            self.dsems.append(holder)
        holder.cnt += 16
        tok = (holder.sem, holder.cnt)

        def fn(eng, out=out, in_=in_, kw=kw):
            return eng.dma_start(out=out, in_=in_, **kw)
        self.ops[queue].append((fn, waits, (holder.sem, 16), holder))
        for b in reads:
            b.r.append(tok)
        for b in writes:
            b.w = tok
            b.r = []
        if not writes:
            self.out_tokens.append(tok)
        return tok

    def barrier(self):
        toks = [(e, self.cnt[e]) for e in self.ENG if self.cnt[e] > 0]
        toks += [(h.sem, h.cnt) for h in self.dsems]
        for e in self.ENG:
            waits = self._waits(e, toks)
            if waits:
                self.ops[e].append((None, waits, None, None))

    def finish(self):
        waits = self._waits("sync", self.out_tokens)
        self.ops["sync"].append((None, waits, None, None))

    def begin_cond(self, cnt_ap, cnt_buf, thr, key=None):
        if self.cond is None:
            self.cond = []
        self.cond.append(dict(ap=cnt_ap, buf=cnt_buf, thr=thr, key=key, start={e: len(self.ops[e]) for e in self.ENG},
                              cnt0=dict(self.cnt), hcnt0={id(h): h.cnt for h in self.dsems},
                              seen0={e: dict(self.seen[e]) for e in self.ENG}))

    def end_cond(self):
        c = self.cond.pop()

        def collect(body, hinc):
            for (fn, w, inc, holder) in body:
                if fn == "cond":
                    collect(inc[2], hinc)
                elif fn is not None and holder is not None:
                    k = id(holder)
                    if k not in hinc:
                        hinc[k] = [holder, c["hcnt0"].get(k, 0), 0]
                    hinc[k][2] += inc[1]
        for e in self.ENG:
            body = self.ops[e][c["start"][e]:]
            if not body:
                continue
            del self.ops[e][c["start"][e]:]
            self.seen[e] = c["seen0"][e]
            waits = self._waits(e, [c["buf"].w])
            nsig = self.cnt[e] - c["cnt0"][e]
            hinc = {}
            collect(body, hinc)
            self.ops[e].append(("cond", waits, (c["ap"], c["thr"], body, c["cnt0"][e], nsig, list(hinc.values()), c["key"]), None))

    def emit(self):
        with self.nc.Block() as block:
            loaded = {}

            def mk(engine):
                def run(eng, ops, depth=0):
                    for fn, waits, inc, _h in ops:
                        for sem, val in waits:
                            eng.wait_ge(sem, val)
                        if fn is None:
                            continue
                        if fn == "cond":
                            ap, thr, sub, cnt0, nsig, hincs, key = inc
                            reg = self.regs[engine][0]
                            if key is None or loaded.get(engine) != key:
                                eng.reg_load(reg, ap)
                                loaded[engine] = key if depth == 0 else None
                            with eng.If_lt(reg, thr + 1):
                                if nsig:
                                    if cnt0:
                                        eng.wait_ge(self.esem[engine], cnt0)
                                    eng.sem_inc(self.esem[engine], nsig)
                                for holder, before, tot in hincs:
                                    if before:
                                        eng.wait_ge(holder.sem, before)
                                    eng.sem_inc(holder.sem, tot)
                            with eng.Else():
                                run(eng, sub, depth + 1)
                            continue
                        ins = fn(eng)
                        if inc is not None:
                            ins.then_inc(inc[0], inc[1])

                def body(eng):
                    run(eng, self.ops[engine])
                return body
            for e in self.ENG:
                if self.ops[e]:
                    getattr(block, e)(mk(e))


class Ring:
    def __init__(self, S, name, shape, dtype, n, psum=False):
        self.tiles = []
        S.nbuf += 1
        name = f"{name}_{S.nbuf}_"
        for i in range(n):
            t = S.psum(f"{name}{i}", shape, dtype) if psum else S.sbuf(f"{name}{i}", shape, dtype)
            self.tiles.append((t, S.buf(name)))
        self.i = 0

    def next(self):
        t = self.tiles[self.i % len(self.tiles)]
        self.i += 1
        return t


def bcast_mid(ap2d, n):
    a = ap2d.ap
    return bass.AP(ap2d.tensor, ap2d.offset, [list(a[0]), [0, n], list(a[1])])


def rev_ap(ap2d):
    a = [list(p) for p in ap2d.ap]
    n = a[-1][1]
    a[-1] = [-a[-1][0], n]
    return bass.AP(ap2d.tensor, ap2d.offset + (n - 1) * ap2d.ap[-1][0], a)


class VecPack:
    def __init__(self):
        self.cols = {}
        self.n = 0

    def add(self, name, ncols):
        self.cols[name] = (self.n, ncols)
        self.n += ncols

    def sl(self, name, j=0, w=1):
        o, n = self.cols[name]
        assert j + w <= n
        return slice(o + j, o + j + w)


def chunked(v):
    v = np.asarray(v, np.float32)
    return np.ascontiguousarray(v.reshape(-1, 128).T)


VP = VecPack()
for _n, _c in [("cv", 16), ("ada_b0", 48), ("ada_b1", 48), ("n1g0", 8), ("n2g0", 8), ("n1g1", 8), ("n2g1", 8),
               ("convw", 32), ("convb", 8), ("b_a", 16), ("b_i", 16), ("lam", 16), ("q_g", 1), ("k_g", 1),
               ("hmask", 3), ("sel_f", 4), ("sel_r", 4)]:
    VP.add(_n, _c)


def pack_vecs(inp, b, j):
    v = np.zeros((128, VP.n), np.float32)

    def put(name, arr):
        o, n = VP.cols[name]
        arr = np.asarray(arr, np.float32).reshape(128, n)
        v[:, o:o + n] = arr
    cl = chunked(inp["c"][b])
    cc = chunked(inp["c_ctx"])
    put("cv", np.stack([cl, cc], axis=2).reshape(128, 16))
    put("ada_b0", chunked(inp["ada_b"][0]))
    put("ada_b1", chunked(inp["ada_b"][1]))
    put("n1g0", chunked(inp["norm1_g"][0]))
    put("n2g0", chunked(inp["norm2_g"][0]))
    put("n1g1", chunked(inp["norm1_g"][1]))
    put("n2g1", chunked(inp["norm2_g"][1]))
    put("convw", np.concatenate([chunked(inp["rg_conv_w"][0, k]) for k in range(4)], axis=1))
    put("convb", chunked(inp["rg_conv_b"][0]))
    put("b_a", np.concatenate([chunked(inp["rg_b_a"][0, d]) for d in range(2)], axis=1))
    put("b_i", np.concatenate([chunked(inp["rg_b_i"][0, d]) for d in range(2)], axis=1))
    put("lam", np.concatenate([chunked(inp["rg_lam"][0, d]) for d in range(2)], axis=1))
    put("q_g", np.asarray(inp["attn_q_g"][0]).reshape(128, 1))
    put("k_g", np.asarray(inp["attn_k_g"][0]).reshape(128, 1))
    hm = np.array([1.0 if j > 0 else 0.0, 1.0 if j > 0 else 0.0, 1.0 if j < 3 else 0.0], np.float32)
    put("hmask", np.broadcast_to(hm, (128, 3)))
    put("sel_f", np.broadcast_to(np.array([1.0 if i < j else 0.0 for i in range(4)], np.float32), (128, 4)))
    put("sel_r", np.broadcast_to(np.array([1.0 if i > j else 0.0 for i in range(4)], np.float32), (128, 4)))
    return v


class Ctx:
    pass


def setup_common(nc, es, vecs_d, ident_d, ps_ring=True):
    C = Ctx()
    C.nc = nc
    S = C.S = Sched(nc, es)
    if ps_ring:
        C.ps = Ring(S, "ps", [128, 512], F32, 8, psum=True)
    C.vec = S.sbuf("vec", [128, VP.n], F32)
    C.bvec = S.buf("vec")
    S.dma("sync", C.vec[:], vecs_d, writes=[C.bvec])
    C.ident = S.sbuf("ident", [128, 128], F32)
    C.bident = S.buf("ident")
    S.dma("sync", C.ident[:], ident_d, writes=[C.bident])
    C.ones = S.sbuf("ones", [128, 128], BF16)
    C.bones = S.buf("ones")
    S.op("vector", lambda e: e.memset(C.ones[:], 1.0), writes=[C.bones])
    C.epsb = S.sbuf("epsb", [128, 1], F32)
    C.bepsb = S.buf("epsb")
    S.op("vector", lambda e: e.memset(C.epsb[:], EPS), writes=[C.bepsb])
    C.alt = 0
    C.ada_queue = "sync"
    C.ada_dt = F32
    C.ada_ring = 2
    return C


def V(C, name, j=0, w=1):
    return C.vec[:, VP.sl(name, j, w)]


def evac_engine(C):
    C.alt += 1
    return "vector" if C.alt % 2 else "scalar"


def copy_op(C, eng, out, in_, reads, writes):
    S = C.S
    if eng == "scalar":
        S.op("scalar", lambda e: e.copy(out, in_), reads=reads, writes=writes)
    else:
        S.op(eng, lambda e: e.tensor_copy(out, in_), reads=reads, writes=writes)


def emit_ada(C, ada_w_d, layer, bias_name, out_mod, bmod):
    S = C.S
    nc = C.nc
    ps, bps = C.ps.next()
    wr = Ring(S, f"adaw{layer}", [128, KC, 768], C.ada_dt, C.ada_ring)
    for g in range(8):
        wt, bw = wr.next()
        src = ada_w_d[layer].rearrange("(k p) n -> p k n", p=128)[:, :, g * 768:(g + 1) * 768]
        S.dma(C.ada_queue, wt[:], src, writes=[bw])
        for fi in range(6):
            f = g * 6 + fi
            for k in range(KC):
                S.op("tensor", lambda e, wt=wt, fi=fi, k=k, f=f: e.matmul(
                    ps[:, f * 2:f * 2 + 2], wt[:, k, fi * 128:(fi + 1) * 128], C.sv_mm[:, k * 2:k * 2 + 2],
                    start=(k == 0), stop=(k == KC - 1)),
                    reads=[bw, C.bsv], writes=[bps], sig=(k == KC - 1))
    bo, bn = VP.cols[bias_name]
    S.op("vector", lambda e: e.tensor_tensor(
        out_mod[:], ps[:, 0:96].rearrange("p (f c) -> p f c", c=2),
        bass.AP(C.vec[:].tensor, C.vec[:, bo:bo + 48].offset, [list(C.vec[:].ap[0]), [1, 48], [0, 2]]), ALU.add),
        reads=[bps, C.bvec], writes=[bmod])


def emit_silu_c(C):
    S = C.S
    C.sv = S.sbuf("sv", [128, 16], F32)
    C.bsv = S.buf("sv")
    S.op("scalar", lambda e: e.activation(C.sv[:], V(C, "cv", 0, 16), AF.Silu), reads=[C.bvec], writes=[C.bsv])
    C.sv_mm = C.sv
    if C.ada_dt != F32:
        C.sv_mm = S.sbuf("svb", [128, 16], C.ada_dt)
        S.op("vector", lambda e: e.tensor_copy(C.sv_mm[:], C.sv[:]), reads=[C.bsv], writes=[C.bsv])


def emit_gs(C, mod, bmod, sc_idx, gname, name):
    S = C.S
    gs = S.sbuf(name, [128, KC, 2], F32)
    bgs = S.buf(name)
    go, _ = VP.cols[gname]
    gb = bass.AP(C.vec[:].tensor, C.vec[:, go:go + 8].offset, [list(C.vec[:].ap[0]), [1, 8], [0, 2]])
    S.op("vector", lambda e: e.scalar_tensor_tensor(
        gs[:], mod[:, sc_idx * 8:(sc_idx + 1) * 8, :], 1.0, gb, ALU.add, ALU.mult),
        reads=[bmod, C.bvec], writes=[bgs])
    return gs, bgs


def emit_ada_dma(C, ada_w_d, layer, g, wr):
    wt, bw = wr.next()
    src = ada_w_d[layer].rearrange("(k p) n -> p k n", p=128)[:, :, g * 768:(g + 1) * 768]
    C.S.dma(C.ada_queue, wt[:], src, writes=[bw])
    return wt, bw


def emit_ada_mm(C, g, wt, bw, bias_name, out_mod, bmod):
    S = C.S
    ps, bps = C.ps.next()
    for fi in range(6):
        for k in range(KC):
            S.op("tensor", lambda e, fi=fi, k=k: e.matmul(
                ps[:, fi * 2:fi * 2 + 2], wt[:, k, fi * 128:(fi + 1) * 128], C.sv_mm[:, k * 2:k * 2 + 2],
                start=(k == 0), stop=(k == KC - 1)),
                reads=[bw, C.bsv], writes=[bps], sig=(k == KC - 1))
    bo, bn = VP.cols[bias_name]
    S.op("vector", lambda e: e.tensor_tensor(
        out_mod[:, g * 6:(g + 1) * 6, :], ps[:, 0:12].rearrange("p (f c) -> p f c", c=2),
        bass.AP(C.vec[:].tensor, C.vec[:, bo + g * 6:bo + g * 6 + 6].offset, [list(C.vec[:].ap[0]), [1, 6], [0, 2]]), ALU.add),
        reads=[bps, C.bvec], writes=[bmod])


def emit_gs_into(C, gs, bgs, mod, bmod, sc_idx, gname):
    S = C.S
    go, _ = VP.cols[gname]
    gb = bass.AP(C.vec[:].tensor, C.vec[:, go:go + 8].offset, [list(C.vec[:].ap[0]), [1, 8], [0, 2]])
    S.op("vector", lambda e: e.scalar_tensor_tensor(
        gs[:], mod[:, sc_idx * 8:(sc_idx + 1) * 8, :], 1.0, gb, ALU.add, ALU.mult),
        reads=[bmod, C.bvec], writes=[bgs])


def tile_bufs(bufs, c0, n):
    return bufs[c0 // 128:(c0 + n + 127) // 128]


def bcast_cols(ap_col, n):
    return bass.AP(ap_col.tensor, ap_col.offset, [list(ap_col.ap[0]), [0, n]])


def scoped(S):
    es = ExitStack()
    S.es = es
    return es


def alloc_norm_tmps(C):
    S = C.S
    C.sq = Ring(S, "sq", [128, 512], BF16, 2)
    C.rstd = Ring(S, "rstd", [128, 512], F32, 3)
    C.ntmp = Ring(S, "ntmp", [128, 512], F32, 2)


def emit_norm_stats(C, xs, bxs, s0, n):
    S = C.S
    rx = tile_bufs(bxs, s0, n)
    ps, bps = C.ps.next()
    for k in range(KC):
        sq, bsq = C.sq.next()
        if k % 2 == 0:
            S.op("gpsimd", lambda e, sq=sq, k=k: e.tensor_tensor(sq[:, :n], xs[:, k, s0:s0 + n], xs[:, k, s0:s0 + n], ALU.mult),
                 reads=rx, writes=[bsq])
        else:
            S.op("scalar", lambda e, sq=sq, k=k: e.activation(sq[:, :n], xs[:, k, s0:s0 + n], AF.Square), reads=rx, writes=[bsq])
        S.op("tensor", lambda e, sq=sq, k=k: e.matmul(ps[:, :n], C.ones[:], sq[:, :n], start=(k == 0), stop=(k == KC - 1)),
             reads=[bsq, C.bones], writes=[bps])
    rs, brs = C.rstd.next()
    S.op("scalar", lambda e: e.activation(rs[:, :n], ps[:, :n], AF.Sqrt, bias=C.epsb[:, 0:1], scale=1.0 / D), reads=[bps, C.bepsb], writes=[brs])
    S.op("vector", lambda e: e.reciprocal(rs[:, :n], rs[:, :n]), reads=[brs], writes=[brs])
    return rs, brs


def emit_norm_apply(C, rs, brs, xs, bxs, s0, hT, bh, c0, n, gs, bgs, mod, bmod, sh_idx, which, hf=None, bhf=None):
    S = C.S
    rx = tile_bufs(bxs, s0, n)
    wh = tile_bufs(bh, c0, n)
    for k in range(KC):
        tmp, btmp = C.ntmp.next()
        S.op("vector", lambda e, tmp=tmp, k=k: e.tensor_tensor(tmp[:, :n], xs[:, k, s0:s0 + n], rs[:, :n], ALU.mult),
             reads=rx + [brs], writes=[btmp])
        S.op("scalar", lambda e, tmp=tmp, k=k: e.activation(
            hT[:, k, c0:c0 + n], tmp[:, :n], AF.Identity,
            bias=mod[:, sh_idx * 8 + k, which:which + 1], scale=gs[:, k, which:which + 1]),
            reads=[btmp, bgs, bmod], writes=wh)
        if hf is not None:
            S.op("vector", lambda e, tmp=tmp, k=k: e.scalar_tensor_tensor(
                hf[:, k, c0:c0 + n], tmp[:, :n], gs[:, k, which:which + 1],
                bcast_cols(mod[:, sh_idx * 8 + k, which:which + 1], n), ALU.mult, ALU.add),
                reads=[btmp, bgs, bmod], writes=tile_bufs(bhf, c0, n))


def emit_norm(C, xs, bxs, s0, hT, bh, c0, n, gs, bgs, mod, bmod, sh_idx, which, hf=None, bhf=None):
    rs, brs = emit_norm_stats(C, xs, bxs, s0, n)
    emit_norm_apply(C, rs, brs, xs, bxs, s0, hT, bh, c0, n, gs, bgs, mod, bmod, sh_idx, which, hf, bhf)


def emit_norm_blocks(C, xs, bxs, hT, bh, blocks, gs, bgs, mod, bmod, sh_idx, hf=None, bhf=None):
    st = emit_norm_stats(C, xs, bxs, blocks[0][0], blocks[0][1])
    for i, (c0, n, which) in enumerate(blocks):
        cur = st
        if i + 1 < len(blocks):
            st = emit_norm_stats(C, xs, bxs, blocks[i + 1][0], blocks[i + 1][1])
        emit_norm_apply(C, cur[0], cur[1], xs, bxs, c0, hT, bh, c0, n, gs, bgs, mod, bmod, sh_idx, which, hf, bhf)


def emit_load_xT(C, x_d, row0, ntiles, dst, bdst, dcol0):
    S = C.S
    for i in range(ntiles):
        xt, bxt = C.xtile.next()
        S.dma("sync", xt[:], x_d[row0 + i * 128:row0 + (i + 1) * 128, :], writes=[bxt])
        c0 = dcol0 + i * 128
        for half in range(2):
            ps, bps = C.ps.next()
            for j in range(4):
                kc = half * 4 + j
                S.op("tensor", lambda e, ps=ps, j=j, kc=kc, xt=xt: e.transpose(
                    ps[:, j * 128:(j + 1) * 128], xt[:, kc * 128:(kc + 1) * 128], C.ident[:]),
                    reads=[bxt, C.bident], writes=[bps], sig=(j == 3))
            copy_op(C, evac_engine(C), dst[:, half * 4:half * 4 + 4, c0:c0 + 128],
                    ps[:].rearrange("p (j t) -> p j t", j=4), [bps], [bdst[c0 // 128]])


def load_w(C, ring, w_d, rows, c0, ncols, queue="gpsimd"):
    wt, bw = ring.next()
    kc = rows // 128
    src = w_d.rearrange("(k p) n -> p k n", p=128)[:, :, c0:c0 + ncols]
    C.S.dma(queue, wt[:, :kc, :ncols], src, writes=[bw])
    return wt, bw


def alloc_ffn(C, SL):
    S = C.S
    C.SL = SL
    C.w13 = Ring(S, "w13", [128, KC, SL * 128], BF16, 4)
    C.w2r = Ring(S, "w2", [128, SL, D], BF16, 2)
    C.hid = Ring(S, "hid", [128, SL, 512], BF16, 2)
    C.sil = Ring(S, "sil", [128, 512], F32, 3)


def emit_ffn(C, hT, bh, xT, bx, blocks, w1_d, w3_d, w2_d, dff, mod, bmod, g_idx, gate=None):
    S = C.S
    nfc = dff // 128
    SL = C.SL
    pending = [None]
    for s0 in range(0, nfc, SL):
        sl = min(SL, nfc - s0)
        w1t, bw1 = load_w(C, C.w13, w1_d, D, s0 * 128, sl * 128)
        w3t, bw3 = load_w(C, C.w13, w3_d, D, s0 * 128, sl * 128)
        w2t, bw2 = C.w2r.next()
        S.dma("gpsimd", w2t[:, :sl, :], w2_d[s0 * 128:(s0 + sl) * 128, :].rearrange("(f p) n -> p f n", p=128), writes=[bw2])
        for (c0, n, which) in blocks:
            rh = tile_bufs(bh, c0, n)
            hid, bhid = C.hid.next()
            for fi in range(sl):
                pa, bpa = C.ps.next()
                for k in range(KC):
                    S.op("tensor", lambda e, pa=pa, k=k, fi=fi, w1t=w1t, c0=c0, n=n: e.matmul(
                        pa[:, :n], w1t[:, k, fi * 128:(fi + 1) * 128], hT[:, k, c0:c0 + n], start=(k == 0), stop=(k == KC - 1)),
                        reads=rh + [bw1], writes=[bpa], sig=(k == KC - 1))
                pb, bpb = C.ps.next()
                for k in range(KC):
                    S.op("tensor", lambda e, pb=pb, k=k, fi=fi, w3t=w3t, c0=c0, n=n: e.matmul(
                        pb[:, :n], w3t[:, k, fi * 128:(fi + 1) * 128], hT[:, k, c0:c0 + n], start=(k == 0), stop=(k == KC - 1)),
                        reads=rh + [bw3], writes=[bpb], sig=(k == KC - 1))
                sa, bsa = C.sil.next()
                S.op("scalar", lambda e, sa=sa, pa=pa, n=n: e.activation(sa[:, :n], pa[:, :n], AF.Silu), reads=[bpa], writes=[bsa])
                if gate is None:
                    S.op("vector", lambda e, sa=sa, pb=pb, hid=hid, fi=fi, n=n: e.tensor_tensor(hid[:, fi, :n], pb[:, :n], sa[:, :n], ALU.mult),
                         reads=[bpb, bsa], writes=[bhid])
                else:
                    gt, bgt = gate
                    S.op("vector", lambda e, sa=sa, pb=pb, n=n: e.tensor_tensor(sa[:, :n], pb[:, :n], sa[:, :n], ALU.mult),
                         reads=[bpb, bsa], writes=[bsa])
                    S.op("gpsimd", lambda e, sa=sa, hid=hid, fi=fi, gt=gt, c0=c0, n=n: e.tensor_tensor(hid[:, fi, :n], sa[:, :n], gt[:, c0:c0 + n], ALU.mult),
                         reads=[bsa, bgt], writes=[bhid])
            def down(c0=c0, n=n, which=which, hid=hid, bhid=bhid, w2t=w2t, bw2=bw2, sl=sl):
                wx = tile_bufs(bx, c0, n)
                for dc in range(KC):
                    po, bpo = C.ps.next()
                    for fi in range(sl):
                        S.op("tensor", lambda e, po=po, fi=fi, dc=dc: e.matmul(
                            po[:, :n], w2t[:, fi, dc * 128:(dc + 1) * 128], hid[:, fi, :n], start=(fi == 0), stop=(fi == sl - 1)),
                            reads=[bhid, bw2], writes=[bpo], sig=(fi == sl - 1))
                    S.op("vector", lambda e, po=po, dc=dc: e.scalar_tensor_tensor(
                        xT[:, dc, c0:c0 + n], po[:, :n], mod[:, g_idx * 8 + dc, which:which + 1], xT[:, dc, c0:c0 + n], ALU.mult, ALU.add),
                        reads=[bpo, bmod], writes=wx)
            if pending[0] is not None:
                pending[0]()
            pending[0] = down
    if pending[0] is not None:
        pending[0]()


def emit_proj_residual(C, inT, bin_, nk, w_t, bw, xT, bx, blocks, mod, bmod, g_idx):
    S = C.S
    for (c0, n, which) in blocks:
        ri = tile_bufs(bin_, c0, n)
        wx = tile_bufs(bx, c0, n)
        for dc in range(KC):
            po, bpo = C.ps.next()
            for k in range(nk):
                S.op("tensor", lambda e, po=po, k=k, dc=dc, c0=c0, n=n: e.matmul(
                    po[:, :n], w_t[:, k, dc * 128:(dc + 1) * 128], inT[:, k, c0:c0 + n], start=(k == 0), stop=(k == nk - 1)),
                    reads=ri + [bw], writes=[bpo], sig=(k == nk - 1))
            S.op("vector", lambda e, po=po, dc=dc, c0=c0, n=n, which=which: e.scalar_tensor_tensor(
                xT[:, dc, c0:c0 + n], po[:, :n], mod[:, g_idx * 8 + dc, which:which + 1], xT[:, dc, c0:c0 + n], ALU.mult, ALU.add),
                reads=[bpo, bmod], writes=wx)


def emit_mods(C, es, ada_w, layers):
    S = C.S
    nc = C.nc
    emit_silu_c(C)
    out = {}
    for l in layers:
        out[l] = (es.enter_context(nc.sbuf_tensor(f"mod{l}", [128, 48, 2], F32)), S.buf("mod"))
    es_ada = scoped(S)
    for l in layers:
        emit_ada(C, ada_w, l, f"ada_b{l}", out[l][0], out[l][1])
    S.barrier()
    es_ada.close()
    S.es = es
    return out


LAT_BLOCKS = [(0, 512, 0), (512, 512, 0), (1024, 512, 0), (1536, 512, 0)]
CTX_BLOCK = (CTX0, NCX, 1)
HAL_BLOCK = (HAL0, 128, 0)
NSC = NT + NCX


def build_l0(phase):
    nc = bass.Bass("TRN2", target_bir_lowering=False)

    def din(name, shape, dt=F32):
        return nc.dram_tensor(name, list(shape), dt, kind="ExternalInput").ap()

    def dout(name, shape, dt=F32):
        return nc.dram_tensor(name, list(shape), dt, kind="ExternalOutput").ap()
    xall = din("xall", [TOT, D])
    vecs = din("vecs", [128, VP.n])
    ident = din("ident", [128, 128])
    ada_w = din("ada_w", [2, D, 6 * D])
    w_in = din("w_in", [D, 2 * D])
    w_a = din("w_a", [2, 8, 128, 128])
    w_i = din("w_i", [2, 8, 128, 128])
    if phase == "A":
        summ_o = dout("summ", [128, 32])
    else:
        summ_all = din("summ_all", [128, 4 * 32])
        w_out = din("w_out", [D, D])
        f_w1 = din("f_w1", [D, DFF])
        f_w3 = din("f_w3", [D, DFF])
        f_w2 = din("f_w2", [DFF, D])
        w_qkv = din("w_qkv", [D, 1536])
        cos_d = din("cos", [128, NT])
        sin_d = din("sin", [128, NT])
        rotm_d = din("rotm", [128, 128])
        x1_o = dout("x1T", [128, KC * NT])
        q_o = dout("qT", [128, 8 * NT], BF16)
        k_o = dout("kT", [128, 2 * NSC], BF16)
        v_o = dout("vtm", [NSC, 256], BF16)

    with ExitStack() as es:
        C = setup_common(nc, es, vecs, ident)
        S = C.S
        outb = S.buf("out")
        mods = emit_mods(C, es, ada_w, [0] if phase == "A" else [0, 1])
        mod0, bmod0 = mods[0]
        gs1, bgs1 = emit_gs(C, mod0, bmod0, 1, "n1g0", "gs1")
        hT = S.sbuf("hT", [128, KC, TOT], BF16)
        bh = [S.buf("hT") for _ in range(TOT // 128)]

        es1 = scoped(S)
        C.xtile = Ring(S, "xtile", [128, D], F32, 2)
        alloc_norm_tmps(C)
        xblk = Ring(S, "xblk", [128, KC, 512], F32, 2)
        for (c0, n, which) in LAT_BLOCKS + [CTX_BLOCK, HAL_BLOCK]:
            xb_, bxb_ = xblk.next()
            bl = [bxb_] * 4
            emit_load_xT(C, xall, c0, n // 128, xb_, bl, 0)
            emit_norm(C, xb_, bl, 0, hT, bh, c0, n, gs1, bgs1, mod0, bmod0, 0, which)
        S.barrier()
        es1.close()

        es_yg = scoped(S)
        if phase == "B":
            ygT = S.sbuf("ygT", [128, KC, NSC], BF16)
            byg = [S.buf("yg") for _ in range(NSC // 128)]
        es_mix = scoped(S)
        win_r = Ring(S, "win", [128, KC, 256], BF16, 2)
        wg_r = Ring(S, "wg", [128, 4, 128], BF16, 2)
        xbe = Ring(S, "xbe", [128, NT + 3 + NCX + 3], F32, 1)
        xc_r = Ring(S, "xc", [128, NSC], F32, 1)
        xcb_r = Ring(S, "xcb", [128, NSC], BF16, 1)
        r_r = Ring(S, "rr", [128, NSC], F32, 1)
        b_r = Ring(S, "bb", [128, NSC], F32, 1)
        a_r = Ring(S, "aa", [128, NSC], F32, 1)
        m_r = Ring(S, "mm", [128, NSC], F32, 1)
        y_r = Ring(S, "yy", [128, NSC], F32, 2)
        sm_r = Ring(S, "sm", [128, 8], F32, 2)
        gtmp = Ring(S, "gtmp", [128, 512], F32, 3)
        cneg = S.sbuf("cneg", [128, 32], F32)
        bcneg = S.buf("cneg")
        S.op("scalar", lambda e: e.activation(cneg[:, 0:16], V(C, "lam", 0, 16), AF.Exp, scale=-1.0), reads=[C.bvec], writes=[bcneg])
        S.op("scalar", lambda e: e.activation(cneg[:, 0:16], cneg[:, 0:16], AF.Ln, bias=1.0), reads=[bcneg], writes=[bcneg])
        S.op("vector", lambda e: e.tensor_scalar(cneg[:, 16:32], cneg[:, 0:16], -16.0, None, ALU.mult), reads=[bcneg], writes=[bcneg])
        S.op("vector", lambda e: e.tensor_scalar(cneg[:, 0:16], cneg[:, 0:16], -8.0, None, ALU.mult), reads=[bcneg], writes=[bcneg])
        if phase == "A":
            summ = S.sbuf("summ", [128, 32], F32)
            bsumm = S.buf("summ")
        else:
            sall = S.sbuf("sall", [128, 4 * 32], F32)
            bsall = S.buf("sall")
            S.dma("sync", sall[:], summ_all, writes=[bsall])
        LB = NT + 3
        w_in_v = w_in.rearrange("(k p) n -> p k n", p=128)
        for ct in range(KC):
            wt, bw = win_r.next()
            S.dma("gpsimd", wt[:, :, 0:128], w_in_v[:, :, ct * 128:(ct + 1) * 128], writes=[bw])
            S.dma("gpsimd", wt[:, :, 128:256], w_in_v[:, :, D + ct * 128:D + (ct + 1) * 128], writes=[bw])
            wg, bwg = wg_r.next()
            for d in range(2):
                S.dma("gpsimd", wg[:, d, :], w_a[d, ct], writes=[bwg])
                S.dma("gpsimd", wg[:, 2 + d, :], w_i[d, ct], writes=[bwg])
            xe, bxe = xbe.next()
            S.op("gpsimd", lambda e, xe=xe: e.memset(xe[:, LB:LB + 2], 0.0), writes=[bxe])
            S.op("gpsimd", lambda e, xe=xe: e.memset(xe[:, LB + 2 + NCX:LB + 3 + NCX], 0.0), writes=[bxe])
            for (c0, n, which) in LAT_BLOCKS + [CTX_BLOCK, (HAL0, 3, 0)]:
                ps, bps = C.ps.next()
                for k in range(KC):
                    S.op("tensor", lambda e, ps=ps, k=k, wt=wt, c0=c0, n=n: e.matmul(
                        ps[:, :n], wt[:, k, 0:128], hT[:, k, c0:c0 + n], start=(k == 0), stop=(k == KC - 1)),
                        reads=tile_bufs(bh, c0, n) + [bw], writes=[bps], sig=(k == KC - 1))
                if c0 < CTX0:
                    copy_op(C, evac_engine(C), xe[:, 2 + c0:2 + c0 + n], ps[:, :n], [bps], [bxe])
                elif c0 == CTX0:
                    copy_op(C, evac_engine(C), xe[:, LB + 2:LB + 2 + NCX], ps[:, :n], [bps], [bxe])
                else:
                    S.op("vector", lambda e, ps=ps, xe=xe: e.tensor_tensor(xe[:, 0:2], ps[:, 0:2], V(C, "hmask", 0, 2), ALU.mult),
                         reads=[bps, C.bvec], writes=[bxe])
                    S.op("vector", lambda e, ps=ps, xe=xe: e.tensor_tensor(xe[:, 2 + NT:3 + NT], ps[:, 2:3], V(C, "hmask", 2, 1), ALU.mult),
                         reads=[bps, C.bvec], writes=[bxe])
            xc, bxc = xc_r.next()
            for (dst0, src0, n) in [(0, 0, NT), (NT, LB, NCX)]:
                S.op("scalar", lambda e, xc=xc, xe=xe, dst0=dst0, src0=src0, n=n, ct=ct: e.activation(
                    xc[:, dst0:dst0 + n], xe[:, src0:src0 + n], AF.Identity,
                    bias=V(C, "convb", ct), scale=V(C, "convw", ct)), reads=[bxe, C.bvec], writes=[bxc])
                for k in range(1, 4):
                    S.op("vector", lambda e, xc=xc, xe=xe, dst0=dst0, src0=src0, n=n, k=k, ct=ct: e.scalar_tensor_tensor(
                        xc[:, dst0:dst0 + n], xe[:, src0 + k:src0 + k + n], V(C, "convw", k * 8 + ct), xc[:, dst0:dst0 + n],
                        ALU.mult, ALU.add), reads=[bxe, C.bvec, bxc], writes=[bxc])
            xcb, bxcb = xcb_r.next()
            S.op("gpsimd", lambda e, xcb=xcb, xc=xc: e.tensor_copy(xcb[:], xc[:]), reads=[bxc], writes=[bxcb])
            ys = []
            for d in range(2):
                rr, brr = r_r.next()
                bb, bbb = b_r.next()
                for (c0, n) in [(0, 512), (512, 512), (1024, 512), (1536, 512), (NT, NCX)]:
                    pr, bpr = C.ps.next()
                    S.op("tensor", lambda e, pr=pr, wg=wg, d=d, xcb=xcb, c0=c0, n=n: e.matmul(
                        pr[:, :n], wg[:, d, :], xcb[:, c0:c0 + n], start=True, stop=True), reads=[bwg, bxcb], writes=[bpr])
                    S.op("scalar", lambda e, pr=pr, rr=rr, c0=c0, n=n, d=d, ct=ct: e.activation(
                        rr[:, c0:c0 + n], pr[:, :n], AF.Sigmoid, bias=V(C, "b_a", d * 8 + ct)), reads=[bpr, C.bvec], writes=[brr])
                    pi, bpi = C.ps.next()
                    S.op("tensor", lambda e, pi=pi, wg=wg, d=d, xcb=xcb, c0=c0, n=n: e.matmul(
                        pi[:, :n], wg[:, 2 + d, :], xcb[:, c0:c0 + n], start=True, stop=True), reads=[bwg, bxcb], writes=[bpi])
                    S.op("scalar", lambda e, pi=pi, bb=bb, c0=c0, n=n, d=d, ct=ct: e.activation(
                        bb[:, c0:c0 + n], pi[:, :n], AF.Sigmoid, bias=V(C, "b_i", d * 8 + ct)), reads=[bpi, C.bvec], writes=[bbb])
                aa, baa = a_r.next()
                mm, bmm = m_r.next()
                cn = d * 8 + ct
                S.op("scalar", lambda e, aa=aa, rr=rr, cn=cn: e.activation(aa[:], rr[:], AF.Exp, scale=cneg[:, cn:cn + 1]),
                     reads=[brr, bcneg], writes=[baa])
                S.op("scalar", lambda e, mm=mm, rr=rr, cn=cn: e.activation(mm[:], rr[:], AF.Exp, scale=cneg[:, 16 + cn:17 + cn]),
                     reads=[brr, bcneg], writes=[bmm])
                S.op("scalar", lambda e, mm=mm: e.activation(mm[:], mm[:], AF.Sqrt, bias=1.0, scale=-1.0), reads=[bmm], writes=[bmm])
                S.op("gpsimd", lambda e, bb=bb, xc=xc: e.tensor_tensor(bb[:], bb[:], xc[:], ALU.mult), reads=[bbb, bxc], writes=[bbb])
                S.op("vector", lambda e, bb=bb, mm=mm: e.tensor_tensor(bb[:], bb[:], mm[:], ALU.mult), reads=[bbb, bmm], writes=[bbb])
                yy, byy = y_r.next()
                sm, bsm = sm_r.next()
                if phase == "A":
                    if d == 0:
                        S.op("vector", lambda e, yy=yy, aa=aa, bb=bb: e.tensor_tensor_scan(
                            yy[:, 0:NT], aa[:, 0:NT], bb[:, 0:NT], 0.0, ALU.mult, ALU.add), reads=[baa, bbb], writes=[byy])
                        hend = yy[:, NT - 1:NT]
                    else:
                        S.op("vector", lambda e, yy=yy, aa=aa, bb=bb: e.tensor_tensor_scan(
                            rev_ap(yy[:, 0:NT]), rev_ap(aa[:, 0:NT]), rev_ap(bb[:, 0:NT]), 0.0, ALU.mult, ALU.add),
                            reads=[baa, bbb], writes=[byy])
                        hend = yy[:, 0:1]
                    S.op("vector", lambda e, sm=sm, rr=rr: e.tensor_reduce(sm[:, 0:1], rr[:, 0:NT], AX.X, ALU.add), reads=[brr], writes=[bsm])
                    o = ct * 4 + d * 2
                    S.op("scalar", lambda e, sm=sm, cn=cn, o=o: e.activation(summ[:, o:o + 1], sm[:, 0:1], AF.Exp, scale=cneg[:, cn:cn + 1]),
                         reads=[bsm, bcneg], writes=[bsumm])
                    S.op("vector", lambda e, hend=hend, o=o: e.tensor_copy(summ[:, o + 1:o + 2], hend), reads=[byy], writes=[bsumm])
                else:
                    if d == 0:
                        S.op("vector", lambda e, yy=yy, aa=aa, bb=bb: e.tensor_tensor_scan(
                            yy[:, NT:NSC], aa[:, NT:NSC], bb[:, NT:NSC], 0.0, ALU.mult, ALU.add), reads=[baa, bbb], writes=[byy])
                        st0 = yy[:, NSC - 1:NSC]
                        order = [0, 1, 2, 3]
                        sel = "sel_f"
                    else:
                        S.op("vector", lambda e, yy=yy, aa=aa, bb=bb: e.tensor_tensor_scan(
                            rev_ap(yy[:, NT:NSC]), rev_ap(aa[:, NT:NSC]), rev_ap(bb[:, NT:NSC]), 0.0, ALU.mult, ALU.add),
                            reads=[baa, bbb], writes=[byy])
                        st0 = yy[:, NT:NT + 1]
                        order = [3, 2, 1, 0]
                        sel = "sel_r"
                    S.op("vector", lambda e, sm=sm, st0=st0: e.tensor_copy(sm[:, 0:1], st0), reads=[byy], writes=[bsm])
                    for i in order:
                        o = i * 32 + ct * 4 + d * 2
                        S.op("vector", lambda e, sm=sm, o=o: e.scalar_tensor_tensor(
                            sm[:, 1:2], sm[:, 0:1], sall[:, o:o + 1], sall[:, o + 1:o + 2], ALU.mult, ALU.add), reads=[bsm, bsall], writes=[bsm])
                        S.op("vector", lambda e, sm=sm: e.tensor_tensor(sm[:, 1:2], sm[:, 1:2], sm[:, 0:1], ALU.subtract), reads=[bsm], writes=[bsm])
                        S.op("vector", lambda e, sm=sm, i=i, sel=sel: e.scalar_tensor_tensor(
                            sm[:, 0:1], sm[:, 1:2], V(C, sel, i), sm[:, 0:1], ALU.mult, ALU.add), reads=[bsm, C.bvec], writes=[bsm])
                    if d == 0:
                        S.op("vector", lambda e, yy=yy, aa=aa, bb=bb, sm=sm: e.tensor_tensor_scan(
                            yy[:, 0:NT], aa[:, 0:NT], bb[:, 0:NT], sm[:, 0:1], ALU.mult, ALU.add), reads=[baa, bbb, bsm], writes=[byy])
                    else:
                        S.op("vector", lambda e, yy=yy, aa=aa, bb=bb, sm=sm: e.tensor_tensor_scan(
                            rev_ap(yy[:, 0:NT]), rev_ap(aa[:, 0:NT]), rev_ap(bb[:, 0:NT]), sm[:, 0:1], ALU.mult, ALU.add),
                            reads=[baa, bbb, bsm], writes=[byy])
                    ys.append((yy, byy))
            if phase == "B":
                (y0, by0), (y1, by1) = ys
                S.op("gpsimd", lambda e, y0=y0, y1=y1: e.tensor_tensor(y0[:], y0[:], y1[:], ALU.add), reads=[by0, by1], writes=[by0])
                for (c0, n, which) in LAT_BLOCKS + [CTX_BLOCK]:
                    pg, bpg = C.ps.next()
                    for k in range(KC):
                        S.op("tensor", lambda e, pg=pg, k=k, wt=wt, c0=c0, n=n: e.matmul(
                            pg[:, :n], wt[:, k, 128:256], hT[:, k, c0:c0 + n], start=(k == 0), stop=(k == KC - 1)),
                            reads=tile_bufs(bh, c0, n) + [bw], writes=[bpg], sig=(k == KC - 1))
                    t1, bt1 = gtmp.next()
                    S.op("scalar", lambda e, t1=t1, pg=pg, n=n: e.activation(t1[:, :n], pg[:, :n], AF.Gelu_apprx_tanh), reads=[bpg], writes=[bt1])
                    S.op("vector", lambda e, t1=t1, y0=y0, c0=c0, n=n, ct=ct: e.tensor_tensor(ygT[:, ct, c0:c0 + n], t1[:, :n], y0[:, c0:c0 + n], ALU.mult),
                         reads=[bt1, by0], writes=tile_bufs(byg, c0, n))
        if phase == "A":
            S.dma("sync", summ_o, summ[:], reads=[bsumm], out_sem_buf=outb)
            S.finish()
            S.emit()
            es_mix.close()
            es_yg.close()
            return nc
        S.barrier()
        es_mix.close()

        S.es = es
        xT = S.sbuf("xT", [128, KC, NSC], F32, side="right")
        bx = [S.buf("xT") for _ in range(NSC // 128)]
        es_o = scoped(S)
        C.xtile = Ring(S, "xtile", [128, D], F32, 2)
        emit_load_xT(C, xall, 0, NSC // 128, xT, bx, 0)
        wo_r = Ring(S, "wo", [128, KC, D], BF16, 1)
        wot, bwo = load_w(C, wo_r, w_out, D, 0, D)
        BL = LAT_BLOCKS + [CTX_BLOCK]
        emit_proj_residual(C, ygT, byg, KC, wot, bwo, xT, bx, BL, mod0, bmod0, 2)
        S.barrier()
        es_o.close()
        es_yg.close()

        es_f = scoped(S)
        gs2, bgs2 = emit_gs(C, mod0, bmod0, 4, "n2g0", "gs2")
        alloc_norm_tmps(C)
        for (c0, n, which) in BL:
            emit_norm(C, xT, bx, c0, hT, bh, c0, n, gs2, bgs2, mod0, bmod0, 3, which)
        alloc_ffn(C, 4)
        emit_ffn(C, hT, bh, xT, bx, BL, f_w1, f_w3, f_w2, DFF, mod0, bmod0, 5)
        for k in range(KC):
            S.dma("sync", x1_o[:, k * NT:(k + 1) * NT], xT[:, k, 0:NT], reads=bx[0:NT // 128], out_sem_buf=outb)
        mod1, bmod1 = mods[1]
        gs3, bgs3 = emit_gs(C, mod1, bmod1, 1, "n1g1", "gs3")
        for (c0, n, which) in BL:
            emit_norm(C, xT, bx, c0, hT, bh, c0, n, gs3, bgs3, mod1, bmod1, 0, which)
        S.barrier()
        es_f.close()

        es_q = scoped(S)
        wq_r = Ring(S, "wq", [128, KC, 1536], BF16, 1)
        wqt, bwq = load_w(C, wq_r, w_qkv, D, 0, 1536)
        cosT = S.sbuf("cosT", [128, NT], F32)
        sinT = S.sbuf("sinT", [128, NT], F32)
        bcs = S.buf("cs")
        S.dma("sync", cosT[:], cos_d, writes=[bcs])
        S.dma("sync", sinT[:], sin_d, writes=[bcs])
        rotm = S.sbuf("rotm", [128, 128], F32)
        rotb = S.sbuf("rotb", [128, 128], BF16)
        brot = S.buf("rot")
        S.dma("sync", rotm[:], rotm_d, writes=[brot])
        S.op("vector", lambda e: e.tensor_copy(rotb[:], rotm[:]), reads=[brot], writes=[brot])
        C.rstd = Ring(S, "rstd2", [128, 512], F32, 3)
        qst = Ring(S, "qst", [128, 512], BF16, 3)
        qg = Ring(S, "qg", [128, 512], F32, 2)
        qgb = Ring(S, "qgb", [128, 512], BF16, 2)
        qsq = Ring(S, "qsq", [128, 512], BF16, 2)
        qt1 = Ring(S, "qt1", [128, 512], F32, 2)
        vst = Ring(S, "vst", [128, 256], BF16, 2)
        for (c0, n, which) in BL:
            rh = tile_bufs(bh, c0, n)
            for hd in range(10):
                if hd < 8 and which == 1:
                    continue
                gname = "q_g" if hd < 8 else "k_g"
                pq, bpq = C.ps.next()
                for k in range(KC):
                    S.op("tensor", lambda e, pq=pq, k=k, hd=hd, c0=c0, n=n: e.matmul(
                        pq[:, :n], wqt[:, k, hd * 128:(hd + 1) * 128], hT[:, k, c0:c0 + n], start=(k == 0), stop=(k == KC - 1)),
                        reads=rh + [bwq], writes=[bpq], sig=(k == KC - 1))
                sqt, bsqt = qsq.next()
                S.op("scalar", lambda e, sqt=sqt, pq=pq, n=n: e.activation(sqt[:, :n], pq[:, :n], AF.Square), reads=[bpq], writes=[bsqt])
                pss, bpss = C.ps.next()
                S.op("tensor", lambda e, pss=pss, sqt=sqt, n=n: e.matmul(pss[:, :n], C.ones[:], sqt[:, :n], start=True, stop=True),
                     reads=[bsqt, C.bones], writes=[bpss])
                rs, brs = C.rstd.next()
                S.op("scalar", lambda e, rs=rs, pss=pss, n=n: e.activation(rs[:, :n], pss[:, :n], AF.Sqrt, bias=C.epsb[:, 0:1], scale=1.0 / 128),
                     reads=[bpss, C.bepsb], writes=[brs])
                S.op("vector", lambda e, rs=rs, n=n: e.reciprocal(rs[:, :n], rs[:, :n]), reads=[brs], writes=[brs])
                qn, bqn = qg.next()
                S.op("vector", lambda e, qn=qn, pq=pq, rs=rs, gname=gname, n=n: e.scalar_tensor_tensor(
                    qn[:, :n], pq[:, :n], V(C, gname, 0), rs[:, :n], ALU.mult, ALU.mult), reads=[bpq, C.bvec, brs], writes=[bqn])
                qo, bqo = qst.next()
                if which == 0:
                    qb, bqb = qgb.next()
                    S.op("gpsimd", lambda e, qb=qb, qn=qn, n=n: e.tensor_copy(qb[:, :n], qn[:, :n]), reads=[bqn], writes=[bqb])
                    pr, bpr = C.ps.next()
                    S.op("tensor", lambda e, pr=pr, qb=qb, n=n: e.matmul(pr[:, :n], rotb[:], qb[:, :n], start=True, stop=True),
                         reads=[bqb, brot], writes=[bpr])
                    t1, bt1 = qt1.next()
                    S.op("vector", lambda e, t1=t1, pr=pr, c0=c0, n=n: e.tensor_tensor(t1[:, :n], pr[:, :n], sinT[:, c0:c0 + n], ALU.mult), reads=[bpr, bcs], writes=[bt1])
                    S.op("gpsimd", lambda e, qn=qn, c0=c0, n=n: e.tensor_tensor(qn[:, :n], qn[:, :n], cosT[:, c0:c0 + n], ALU.mult), reads=[bqn, bcs], writes=[bqn])
                    S.op("vector", lambda e, qo=qo, qn=qn, t1=t1, n=n: e.tensor_tensor(qo[:, :n], qn[:, :n], t1[:, :n], ALU.add), reads=[bqn, bt1], writes=[bqo])
                else:
                    S.op("vector", lambda e, qo=qo, qn=qn, n=n: e.tensor_copy(qo[:, :n], qn[:, :n]), reads=[bqn], writes=[bqo])
                if hd < 8:
                    S.dma("sync", q_o[:, hd * NT + c0:hd * NT + c0 + n], qo[:, :n], reads=[bqo], out_sem_buf=outb)
                else:
                    S.dma("sync", k_o[:, (hd - 8) * NSC + c0:(hd - 8) * NSC + c0 + n], qo[:, :n], reads=[bqo], out_sem_buf=outb)
            for t0 in range(0, n, 128):
                pv, bpv = C.ps.next()
                for k in range(KC):
                    S.op("tensor", lambda e, pv=pv, k=k, t0=t0, c0=c0: e.matmul(
                        pv[:, 0:256], hT[:, k, c0 + t0:c0 + t0 + 128], wqt[:, k, 1280:1536], start=(k == 0), stop=(k == KC - 1)),
                        reads=rh + [bwq], writes=[bpv], sig=(k == KC - 1))
                vo, bvo = vst.next()
                copy_op(C, evac_engine(C), vo[:], pv[:, 0:256], [bpv], [bvo])
                S.dma("sync", v_o[c0 + t0:c0 + t0 + 128, :], vo[:], reads=[bvo], out_sem_buf=outb)
        S.finish()
        S.emit()
        es_q.close()
    return nc


def build_l1():
    nc = bass.Bass("TRN2", target_bir_lowering=False)

    def din(name, shape, dt=F32):
        return nc.dram_tensor(name, list(shape), dt, kind="ExternalInput").ap()

    def dout(name, shape, dt=F32):
        return nc.dram_tensor(name, list(shape), dt, kind="ExternalOutput").ap()
    vecs = din("vecs", [128, VP.n])
    ident = din("ident", [128, 128])
    ada_w = din("ada_w", [2, D, 6 * D])
    x1_d = din("x1T", [128, KC * NT])
    q_d = din("qT", [128, 8 * NT], BF16)
    k_d = din("kT", [128, 2 * NKEY], BF16)
    v_d = din("vtm", [NKEY, 256], BF16)
    w_o = din("w_o", [D, D])
    router = din("router", [D, NE])
    m_w1 = din("m_w1", [NE, D, DFE])
    m_w3 = din("m_w3", [NE, D, DFE])
    m_w2 = din("m_w2", [NE, DFE, D])
    fin_g = din("fin_g", [D])
    out_d = dout("out", [NT, D])

    with ExitStack() as es:
        C = setup_common(nc, es, vecs, ident, ps_ring=False)
        S = C.S
        outb = S.buf("out")
        identb = S.sbuf("identb", [128, 128], BF16)
        bidb = S.buf("identb")
        S.op("vector", lambda e: e.tensor_copy(identb[:], C.ident[:]), reads=[C.bident], writes=[bidb])
        es_oT = scoped(S)
        oT = S.sbuf("oT", [128, 8, NT], BF16, side="right")
        boT = [S.buf("oT") for _ in range(NT // 128)]
        S.es = es

        es_a = scoped(S)
        psS = Ring(S, "psS", [128, 512], F32, 3, psum=True)
        psO = Ring(S, "psO", [128, 512], F32, 4, psum=True)
        psT = Ring(S, "psT", [128, 1024], BF16, 1, psum=True)
        kT = S.sbuf("kT", [128, 2, NKEY], BF16)
        bk = S.buf("kT")
        S.dma("sync", kT[:], k_d.rearrange("p (g s) -> p g s", g=2), writes=[bk])
        va = S.sbuf("va", [128, NKT, 2, 130], BF16)
        bva = S.buf("va")
        S.op("vector", lambda e: e.memset(va[:, :, :, 128:130], 1.0), writes=[bva])
        vsrc = v_d.rearrange("(t p) (g d) -> p t g d", p=128, g=2)
        for g in range(2):
            S.dma("sync", va[:, :, g, 0:128], vsrc[:, :, g, :], writes=[bva])
        qT = S.sbuf("qT", [128, 8, NT], BF16)
        bq = S.buf("qT")
        S.dma("sync", qT[:], q_d.rearrange("p (h t) -> p h t", h=8), writes=[bq])
        pT_r = Ring(S, "pT", [128, 512], BF16, 3)
        on_r = Ring(S, "on", [128, 128], BF16, 2)
        ri_r = Ring(S, "ri", [128, 2], F32, 2)
        SCL = 1.0 / float(np.sqrt(128.0))
        for qb in range(NT // 512):
            for h in range(8):
                g = h // 4
                po = [psO.next(), psO.next()]
                for kt in range(NKT):
                    ps, bps = psS.next()
                    S.op("tensor", lambda e, ps=ps, kt=kt, g=g, h=h, qb=qb: e.matmul(
                        ps[:], kT[:, g, kt * 128:(kt + 1) * 128], qT[:, h, qb * 512:(qb + 1) * 512], start=True, stop=True),
                        reads=[bk, bq], writes=[bps])
                    pT, bpT = pT_r.next()
                    S.op("scalar", lambda e, pT=pT, ps=ps: e.activation(pT[:], ps[:], AF.Exp, scale=SCL), reads=[bps], writes=[bpT])
                    for qt in range(4):
                        pot, bpot = po[qt // 2]
                        c = (qt % 2) * 129
                        S.op("tensor", lambda e, pot=pot, c=c, pT=pT, qt=qt, kt=kt, g=g: e.matmul(
                            pot[:, c:c + 129], pT[:, qt * 128:(qt + 1) * 128], va[:, kt, g, 0:129], start=(kt == 0 and qt % 2 == 0), stop=(kt == NKT - 1),
                            skip_group_check=True),
                            reads=[bpT, bva], writes=[bpot], sig=(qt == 3))
                for qt in range(4):
                    pot, bpot = po[qt // 2]
                    c = (qt % 2) * 129
                    ri, bri = ri_r.next()
                    S.op("vector", lambda e, ri=ri, pot=pot, c=c: e.reciprocal(ri[:, 0:1], pot[:, c + 128:c + 129]), reads=[bpot], writes=[bri])
                    on, bon = on_r.next()
                    S.op("vector", lambda e, on=on, pot=pot, c=c, ri=ri: e.tensor_scalar(on[:], pot[:, c:c + 128], ri[:, 0:1], None, ALU.mult),
                         reads=[bpot, bri], writes=[bon])
                    pt, bpt = psT.next()
                    S.op("tensor", lambda e, pt=pt, on=on: e.transpose(pt[:, 0:128], on[:], identb[:]), reads=[bon, bidb], writes=[bpt])
                    col = qb * 512 + qt * 128
                    S.op("scalar", lambda e, pt=pt, h=h, col=col: e.copy(oT[:, h, col:col + 128], pt[:, 0:128]), reads=[bpt], writes=[boT[col // 128]])
        S.barrier()
        es_a.close()

        S.es = es
        C.ps = Ring(S, "ps", [128, 512], F32, 8, psum=True)
        mods = emit_mods(C, es, ada_w, [1])
        mod1, bmod1 = mods[1]
        xT = S.sbuf("xT", [128, KC, NT], F32)
        bx = [S.buf("xT") for _ in range(NT // 128)]
        for k in range(KC):
            S.dma("sync", xT[:, k, :], x1_d[:, k * NT:(k + 1) * NT], writes=bx)
        hT = S.sbuf("hT", [128, KC, NT], BF16)
        bh = [S.buf("hT") for _ in range(NT // 128)]
        es_o = scoped(S)
        wo_r = Ring(S, "wo", [128, KC, D], BF16, 1)
        wot, bwo = load_w(C, wo_r, w_o, D, 0, D)
        emit_proj_residual(C, oT, boT, KC, wot, bwo, xT, bx, LAT_BLOCKS, mod1, bmod1, 2)
        S.barrier()
        es_o.close()
        es_oT.close()

        gates = es.enter_context(nc.sbuf_tensor("gates", [128, NT // 128, NE], F32))
        bgates = S.buf("gates")
        es_r = scoped(S)
        gs2, bgs2 = emit_gs(C, mod1, bmod1, 4, "n2g1", "gs2")
        alloc_norm_tmps(C)
        hf = S.sbuf("hf", [128, KC, NT], F32)
        bhf = [S.buf("hf") for _ in range(NT // 128)]
        for (c0, n, which) in LAT_BLOCKS:
            emit_norm(C, xT, bx, c0, hT, bh, c0, n, gs2, bgs2, mod1, bmod1, 3, which, hf=hf, bhf=bhf)
        rw = S.sbuf("rw", [128, KC, NE], F32)
        brw = S.buf("rw")
        S.dma("sync", rw[:], router.rearrange("(k p) e -> p k e", p=128), writes=[brw])
        lg_r = Ring(S, "lg", [128, 32], F32, 2)
        for t in range(NT // 128):
            pl, bpl = C.ps.next()
            for k in range(KC):
                S.op("tensor", lambda e, pl=pl, k=k, t=t: e.matmul(
                    pl[:, 0:NE], hf[:, k, t * 128:(t + 1) * 128], rw[:, k, :], start=(k == 0), stop=(k == KC - 1)),
                    reads=[bhf[t], brw], writes=[bpl], sig=(k == KC - 1))
            lg, blg = lg_r.next()
            S.op("vector", lambda e, lg=lg, pl=pl: e.tensor_copy(lg[:, 0:8], pl[:, 0:8]), reads=[bpl], writes=[blg])
            S.op("vector", lambda e, lg=lg: e.max(lg[:, 8:16], lg[:, 0:8]), reads=[blg], writes=[blg])
            S.op("vector", lambda e, lg=lg: e.tensor_scalar(lg[:, 16:24], lg[:, 0:8], lg[:, 8:9], None, ALU.subtract), reads=[blg], writes=[blg])
            S.op("scalar", lambda e, lg=lg: e.activation(lg[:, 16:24], lg[:, 16:24], AF.Exp), reads=[blg], writes=[blg])
            S.op("vector", lambda e, lg=lg: e.tensor_tensor(lg[:, 24:25], lg[:, 9:10], lg[:, 8:9], ALU.subtract), reads=[blg], writes=[blg])
            S.op("scalar", lambda e, lg=lg: e.activation(lg[:, 24:25], lg[:, 24:25], AF.Exp), reads=[blg], writes=[blg])
            S.op("vector", lambda e, lg=lg: e.tensor_scalar(lg[:, 24:25], lg[:, 24:25], 1.0, None, ALU.add), reads=[blg], writes=[blg])
            S.op("vector", lambda e, lg=lg: e.reciprocal(lg[:, 24:25], lg[:, 24:25]), reads=[blg], writes=[blg])
            S.op("vector", lambda e, lg=lg: e.tensor_scalar(lg[:, 0:8], lg[:, 0:8], lg[:, 9:10], None, ALU.is_ge), reads=[blg], writes=[blg])
            S.op("vector", lambda e, lg=lg: e.tensor_tensor(lg[:, 0:8], lg[:, 0:8], lg[:, 16:24], ALU.mult), reads=[blg], writes=[blg])
            S.op("vector", lambda e, lg=lg, t=t: e.tensor_scalar(gates[:, t, :], lg[:, 0:8], lg[:, 24:25], None, ALU.mult), reads=[blg], writes=[bgates])
        S.barrier()
        es_r.close()

        es_m = scoped(S)
        alloc_ffn(C, 4)
        gB_r = Ring(S, "gB", [128, NT], F32, 2)
        gl_r = Ring(S, "gl", [128, 128], F32, 2)
        for ex in range(NE):
            gB, bgB = gB_r.next()
            for t4 in range(NT // 512):
                pg, bpg = C.ps.next()
                for j in range(4):
                    t = t4 * 4 + j
                    gl, bgl = gl_r.next()
                    S.op("gpsimd", lambda e, gl=gl, t=t, ex=ex: e.tensor_copy(gl[:], bcast_cols(gates[:, t, ex:ex + 1], 128)), reads=[bgates], writes=[bgl])
                    S.op("tensor", lambda e, pg=pg, gl=gl, j=j: e.matmul(pg[:, j * 128:(j + 1) * 128], gl[:], C.ident[:], start=True, stop=True),
                         reads=[bgl, C.bident], writes=[bpg])
                copy_op(C, "scalar", gB[:, t4 * 512:(t4 + 1) * 512], pg[:], [bpg], [bgB])
            emit_ffn(C, hT, bh, xT, bx, LAT_BLOCKS, m_w1[ex], m_w3[ex], m_w2[ex], DFE, mod1, bmod1, 5, gate=(gB, bgB))
        S.barrier()
        es_m.close()

        es_z = scoped(S)
        gfin = S.sbuf("gfin", [128, D], F32)
        bgf = S.buf("gfin")
        S.dma("sync", gfin[:], bass.AP(fin_g.tensor, fin_g.offset, [[0, 128], [1, D]]), writes=[bgf])
        ot_r = Ring(S, "ot", [128, D], F32, 2)
        sq_r = Ring(S, "fsq", [128, 512], F32, 2)
        ss_r = Ring(S, "fss", [128, 4], F32, 2)
        for t in range(NT // 128):
            pp = [C.ps.next(), C.ps.next()]
            for half in range(2):
                ph, bph = pp[half]
                for j in range(4):
                    kc = half * 4 + j
                    S.op("tensor", lambda e, ph=ph, j=j, kc=kc, t=t: e.transpose(
                        ph[:, j * 128:(j + 1) * 128], xT[:, kc, t * 128:(t + 1) * 128], C.ident[:]),
                        reads=[bx[t], C.bident], writes=[bph], sig=(j == 3))
            ss, bss = ss_r.next()
            S.op("vector", lambda e, ss=ss: e.memset(ss[:], 0.0), writes=[bss])
            for half in range(2):
                ph, bph = pp[half]
                sq, bsq = sq_r.next()
                S.op("scalar", lambda e, sq=sq, ph=ph, ss=ss, half=half: e.activation(sq[:], ph[:], AF.Square, accum_out=ss[:, half:half + 1]),
                     reads=[bph], writes=[bsq, bss])
            S.op("vector", lambda e, ss=ss: e.tensor_tensor(ss[:, 2:3], ss[:, 0:1], ss[:, 1:2], ALU.add), reads=[bss], writes=[bss])
            S.op("scalar", lambda e, ss=ss: e.activation(ss[:, 2:3], ss[:, 2:3], AF.Sqrt, bias=C.epsb[:, 0:1], scale=1.0 / D), reads=[bss, C.bepsb], writes=[bss])
            S.op("vector", lambda e, ss=ss: e.reciprocal(ss[:, 3:4], ss[:, 2:3]), reads=[bss], writes=[bss])
            ot, bot = ot_r.next()
            for half in range(2):
                ph, bph = pp[half]
                S.op("vector", lambda e, ot=ot, ph=ph, ss=ss, half=half: e.scalar_tensor_tensor(
                    ot[:, half * 512:(half + 1) * 512], ph[:], ss[:, 3:4], gfin[:, half * 512:(half + 1) * 512], ALU.mult, ALU.mult),
                    reads=[bph, bss, bgf], writes=[bot])
            S.dma("sync", out_d[t * 128:(t + 1) * 128, :], ot[:], reads=[bot], out_sem_buf=outb)
        S.finish()
        S.emit()
        es_z.close()
    return nc


def emit_router(C, hf, bhf, router, gates, bgates, maskt=None):
    S = C.S
    rw = S.sbuf("rw", [128, KC, NE], F32)
    brw = S.buf("rw")
    S.dma("sync", rw[:], router.rearrange("(k p) e -> p k e", p=128), writes=[brw])
    lg_r = Ring(S, "lg", [128, 32], F32, 2)
    for t in range(NT // 128):
        pl, bpl = C.ps.next()
        for k in range(KC):
            S.op("tensor", lambda e, pl=pl, k=k, t=t: e.matmul(
                pl[:, 0:NE], hf[:, k, t * 128:(t + 1) * 128], rw[:, k, :], start=(k == 0), stop=(k == KC - 1)),
                reads=[bhf[t], brw], writes=[bpl], sig=(k == KC - 1))
        lg, blg = lg_r.next()
        S.op("vector", lambda e, lg=lg, pl=pl: e.tensor_copy(lg[:, 0:8], pl[:, 0:8]), reads=[bpl], writes=[blg])
        S.op("vector", lambda e, lg=lg: e.max(lg[:, 8:16], lg[:, 0:8]), reads=[blg], writes=[blg])
        S.op("vector", lambda e, lg=lg: e.tensor_scalar(lg[:, 16:24], lg[:, 0:8], lg[:, 8:9], None, ALU.subtract), reads=[blg], writes=[blg])
        S.op("scalar", lambda e, lg=lg: e.activation(lg[:, 16:24], lg[:, 16:24], AF.Exp), reads=[blg], writes=[blg])
        S.op("vector", lambda e, lg=lg: e.tensor_tensor(lg[:, 24:25], lg[:, 9:10], lg[:, 8:9], ALU.subtract), reads=[blg], writes=[blg])
        S.op("scalar", lambda e, lg=lg: e.activation(lg[:, 24:25], lg[:, 24:25], AF.Exp), reads=[blg], writes=[blg])
        S.op("vector", lambda e, lg=lg: e.tensor_scalar(lg[:, 24:25], lg[:, 24:25], 1.0, None, ALU.add), reads=[blg], writes=[blg])
        S.op("vector", lambda e, lg=lg: e.reciprocal(lg[:, 24:25], lg[:, 24:25]), reads=[blg], writes=[blg])
        S.op("vector", lambda e, lg=lg: e.tensor_scalar(lg[:, 0:8], lg[:, 0:8], lg[:, 9:10], None, ALU.is_ge), reads=[blg], writes=[blg])
        if maskt is not None:
            S.op("vector", lambda e, lg=lg, t=t: e.tensor_copy(maskt[:, t, :], lg[:, 0:8]), reads=[blg], writes=[bgates])
        S.op("vector", lambda e, lg=lg: e.tensor_tensor(lg[:, 0:8], lg[:, 0:8], lg[:, 16:24], ALU.mult), reads=[blg], writes=[blg])
        S.op("vector", lambda e, lg=lg, t=t: e.tensor_scalar(gates[:, t, :], lg[:, 0:8], lg[:, 24:25], None, ALU.mult), reads=[blg], writes=[bgates])


def emit_final(C, xT, bx, fin_g, out_d, outb):
    S = C.S
    gfin = S.sbuf("gfin", [128, D], F32)
    bgf = S.buf("gfin")
    S.dma("sync", gfin[:], bass.AP(fin_g.tensor, fin_g.offset, [[0, 128], [1, D]]), writes=[bgf])
    ot_r = Ring(S, "ot", [128, D], F32, 2)
    sq_r = Ring(S, "fsq", [128, 512], F32, 2)
    ss_r = Ring(S, "fss", [128, 4], F32, 2)
    for t in range(NT // 128):
        pp = [C.ps.next(), C.ps.next()]
        for half in range(2):
            ph, bph = pp[half]
            for j in range(4):
                kc = half * 4 + j
                S.op("tensor", lambda e, ph=ph, j=j, kc=kc, t=t: e.transpose(
                    ph[:, j * 128:(j + 1) * 128], xT[:, kc, t * 128:(t + 1) * 128], C.ident[:]),
                    reads=[bx[t], C.bident], writes=[bph], sig=(j == 3))
        ss, bss = ss_r.next()
        S.op("vector", lambda e, ss=ss: e.memset(ss[:], 0.0), writes=[bss])
        for half in range(2):
            ph, bph = pp[half]
            sq, bsq = sq_r.next()
            S.op("scalar", lambda e, sq=sq, ph=ph, ss=ss, half=half: e.activation(sq[:], ph[:], AF.Square, accum_out=ss[:, half:half + 1]),
                 reads=[bph], writes=[bsq, bss])
        S.op("vector", lambda e, ss=ss: e.tensor_tensor(ss[:, 2:3], ss[:, 0:1], ss[:, 1:2], ALU.add), reads=[bss], writes=[bss])
        S.op("scalar", lambda e, ss=ss: e.activation(ss[:, 2:3], ss[:, 2:3], AF.Sqrt, bias=C.epsb[:, 0:1], scale=1.0 / D), reads=[bss, C.bepsb], writes=[bss])
        S.op("vector", lambda e, ss=ss: e.reciprocal(ss[:, 3:4], ss[:, 2:3]), reads=[bss], writes=[bss])
        ot, bot = ot_r.next()
        for half in range(2):
            ph, bph = pp[half]
            S.op("vector", lambda e, ot=ot, ph=ph, ss=ss, half=half: e.scalar_tensor_tensor(
                ot[:, half * 512:(half + 1) * 512], ph[:], ss[:, 3:4], gfin[:, half * 512:(half + 1) * 512], ALU.mult, ALU.mult),
                reads=[bph, bss, bgf], writes=[bot])
        S.dma("sync", out_d[t * 128:(t + 1) * 128, :], ot[:], reads=[bot], out_sem_buf=outb)


class SubRing:
    def __init__(self, tiles):
        self.tiles = list(tiles)
        self.i = 0

    def next(self):
        t = self.tiles[self.i % len(self.tiles)]
        self.i += 1
        return t


BS = 256
PASS = 1024
NTT = NT // 128


def emit_htm(C, hT2, bh2, htm_d, identb, bidb):
    S = C.S
    bhtm = S.buf("htm")
    htw = Ring(S, "htw", [128, D], BF16, 2)
    for tt in range(NTT):
        ptr, bptr = C.ps.next()
        ptb = ptr[:].bitcast(BF16)
        for k in range(KC):
            S.op("tensor", lambda e, ptb=ptb, k=k, tt=tt: e.transpose(ptb[:, k * 128:(k + 1) * 128], hT2[:, k, tt * 128:(tt + 1) * 128], identb[:]),
                 reads=[bh2[tt], bidb], writes=[bptr], sig=(k == KC - 1))
        ht, bht = htw.next()
        copy_op(C, evac_engine(C), ht[:], ptb[:, 0:D], [bptr], [bht])
        S.dma("sync", htm_d[tt], ht[:], reads=[bht], writes=[bhtm], nowaw=True)

    return bhtm


def emit_moe_sparse(C, bhtm, xT, bx, maskt, gates, bgates, cst_d, htm_d, m_w1, m_w3, m_w2, mod1, bmod1, identb, bidb):
    S = C.S
    cst = S.sbuf("cst", [128, 385], F32)
    bcst_ = S.buf("cst")
    S.dma("sync", cst[:], cst_d, writes=[bcst_])
    utri, iota_row, iota_p = cst[:, 0:128], cst[:, 128:384], cst[:, 384:385]
    ones_f = S.sbuf("ones_f", [128, 128], F32)
    bof = S.buf("ones_f")
    S.op("vector", lambda e: e.memset(ones_f[:], 1.0), writes=[bof])
    tot = S.sbuf("tot", [128, NTT, NE], F32)
    incl = S.sbuf("incl", [128, NTT, NE], F32)
    pos = S.sbuf("pos", [128, NTT, NE], F32)
    posq = S.sbuf("posq", [128, 8, NTT, NE], F32)
    jp = S.sbuf("jp", [128, 16], F32)
    cnti = S.sbuf("cnti", [128, NE], mybir.dt.int32)
    btab = S.buf("tab")
    bcnt = S.buf("cnt")
    mflat = maskt[:].rearrange("p t e -> p (t e)")
    pw, bpw = C.ps.next()
    S.op("tensor", lambda e: e.matmul(pw[:, 0:128], utri, mflat, start=True, stop=True), reads=[bcst_, bgates], writes=[bpw])
    pt_, bpt_ = C.ps.next()
    S.op("tensor", lambda e: e.matmul(pt_[:, 0:128], ones_f[:], mflat, start=True, stop=True), reads=[bof, bgates], writes=[bpt_])
    S.op("vector", lambda e: e.tensor_copy(tot[:].rearrange("p t e -> p (t e)"), pt_[:, 0:128]), reads=[bpt_], writes=[btab])
    for ex in range(NE):
        S.op("vector", lambda e, ex=ex: e.tensor_tensor_scan(incl[:, :, ex], ones_f[:, 0:NTT], tot[:, :, ex], 0.0, ALU.mult, ALU.add),
             reads=[btab, bof], writes=[btab])
    S.op("vector", lambda e: e.tensor_tensor(pos[:].rearrange("p t e -> p (t e)"), pw[:, 0:128], incl[:].rearrange("p t e -> p (t e)"), ALU.add),
         reads=[bpw, btab], writes=[btab])
    S.op("vector", lambda e: e.tensor_tensor(pos[:], pos[:], tot[:], ALU.subtract), reads=[btab], writes=[btab])
    S.op("vector", lambda e: e.scalar_tensor_tensor(pos[:], pos[:], 1.0, maskt[:], ALU.add, ALU.mult), reads=[btab, bgates], writes=[btab])
    for q in range(8):
        S.op("vector", lambda e, q=q: e.tensor_scalar(posq[:, q], pos[:], -1.0 - BS * q, None, ALU.add), reads=[btab], writes=[btab])
    S.op("vector", lambda e: e.tensor_scalar(pos[:], pos[:], -1.0, None, ALU.add), reads=[btab], writes=[btab])
    for st in range(16):
        S.op("vector", lambda e, st=st: e.tensor_scalar(jp[:, st:st + 1], iota_p, 128.0 * st, None, ALU.add), reads=[bcst_], writes=[btab])
    S.op("vector", lambda e: e.tensor_copy(cnti[:], incl[:, NTT - 1, :]), reads=[btab], writes=[bcnt])

    hg = S.sbuf("hg", [128, KC, PASS], BF16)
    bhg = [S.buf("hg") for _ in range(PASS // BS)]
    ys = S.sbuf("ys", [128, KC, PASS], F32)
    bys = [S.buf("ys") for _ in range(PASS // BS)]
    prow = S.sbuf("prow", [128, NT], F32)
    grow = S.sbuf("grow", [128, NT], F32)
    brow = S.buf("rows")
    gl_r = Ring(S, "gl", [128, 128], F32, 3)
    htr = Ring(S, "htr", [128, D], BF16, 3)
    sct_r = Ring(S, "sct", [128, 512], F32, 2)
    sel_r = Ring(S, "sel", [128, BS], BF16, 3)
    ysb_r = Ring(S, "ysb", [128, KC, BS], BF16, 1)
    ysm_r = Ring(S, "ysm", [128, D], BF16, 2)
    sg_r = Ring(S, "sg", [128, NT], BF16, 2)
    SL = 2
    w13 = Ring(S, "w13s", [128, KC, SL * 128], BF16, 4)
    w2r = Ring(S, "w2s", [128, SL, D], BF16, 2)
    hid_r = [Ring(S, "hids", [128, BS], BF16, PASS // BS) for _ in range(SL)]
    sil_r = Ring(S, "sils", [128, BS], F32, 2)
    nfc = DFE // 128
    nblk = PASS // BS

    def cap_of(ex):
        return cnti[0:1, ex:ex + 1]

    pre = {}

    def issue_slice(ex, s0, tiles):
        w1t, bw1, w3t, bw3, w2t, bw2 = tiles
        S.dma("gpsimd", w1t[:], m_w1[ex].rearrange("(k p) n -> p k n", p=128)[:, :, s0 * 128:(s0 + SL) * 128], writes=[bw1])
        S.dma("gpsimd", w3t[:], m_w3[ex].rearrange("(k p) n -> p k n", p=128)[:, :, s0 * 128:(s0 + SL) * 128], writes=[bw3])
        S.dma("gpsimd", w2t[:], m_w2[ex][s0 * 128:(s0 + SL) * 128, :].rearrange("(f p) n -> p f n", p=128), writes=[bw2])

    def load_slice(ex, s0):
        w1t, bw1 = w13.next()
        w3t, bw3 = w13.next()
        w2t, bw2 = w2r.next()
        tiles = (w1t, bw1, w3t, bw3, w2t, bw2)
        issue_slice(ex, s0, tiles)
        return tiles

    def prefetch(ex):
        for s0 in (0, SL):
            pre[(ex, s0)] = load_slice(ex, s0)

    def do_rowbcast(ex):
        for src, dstrow in ((pos, prow), (gates, grow)):
            for t4 in range(NT // 512):
                pg, bpg = C.ps.next()
                for j in range(4):
                    t = t4 * 4 + j
                    gl, bgl = gl_r.next()
                    S.op("scalar", lambda e, gl=gl, t=t, ex=ex, src=src: e.copy(gl[:], bcast_cols(src[:, t, ex:ex + 1], 128)),
                         reads=[btab, bgates], writes=[bgl])
                    S.op("tensor", lambda e, pg=pg, gl=gl, j=j: e.matmul(pg[:, j * 128:(j + 1) * 128], gl[:], C.ident[:], start=True, stop=True),
                         reads=[bgl, C.bident], writes=[bpg])
                copy_op(C, "vector", dstrow[:, t4 * 512:(t4 + 1) * 512], pg[:], [bpg], [brow])

    def do_gather(ex, p):
        cap = cap_of(ex)
        for b in range(nblk):
            q = p * nblk + b
            if q > 0:
                S.begin_cond(cap, bcnt, BS * q, key=("cap", ex))
            pgk = [C.ps.next() for _ in range(4)]
            for tt in range(NTT):
                ht, bht = htr.next()
                S.dma("sync", ht[:], htm_d[tt], reads=[bhtm], writes=[bht])
                sel, bsel = sel_r.next()
                S.op("vector", lambda e, sel=sel, q=q, tt=tt, ex=ex: e.tensor_scalar(
                    sel[:], iota_row, posq[:, q, tt, ex:ex + 1], None, ALU.is_equal), reads=[bcst_, btab], writes=[bsel])
                for k in range(KC):
                    pk, bpk = pgk[k // 2]
                    S.op("tensor", lambda e, pk=pk, k=k, ht=ht, sel=sel, tt=tt: e.matmul(
                        pk[:, (k % 2) * BS:(k % 2 + 1) * BS], ht[:, k * 128:(k + 1) * 128], sel[:], start=(tt == 0 and k % 2 == 0), stop=(tt == NTT - 1),
                        skip_group_check=True),
                        reads=[bht, bsel], writes=[bpk], sig=(k == KC - 1))
            for k2 in range(4):
                pk, bpk = pgk[k2]
                copy_op(C, evac_engine(C), hg[:, 2 * k2:2 * k2 + 2, b * BS:(b + 1) * BS],
                        pk[:].rearrange("p (a c) -> p a c", a=2), [bpk], [bhg[b]])
            if q > 0:
                S.end_cond()

    def do_ffn(ex, p):
        cap = cap_of(ex)
        for b in range(nblk):
            S.op("gpsimd", lambda e, b=b: e.memset(ys[:, :, b * BS:(b + 1) * BS], 0.0), writes=[bys[b]])
        for s0 in range(0, nfc, SL):
            if p == 0 and (ex, s0) in pre:
                w1t, bw1, w3t, bw3, w2t, bw2 = pre[(ex, s0)]
            else:
                w1t, bw1, w3t, bw3, w2t, bw2 = load_slice(ex, s0)
            def up(b, w1t=w1t, w3t=w3t, bw1=bw1, bw3=bw3):
                c0 = b * BS
                hids = [r.next() for r in hid_r]
                for fi in range(SL):
                    pa, bpa = C.ps.next()
                    for k in range(KC):
                        S.op("tensor", lambda e, pa=pa, k=k, fi=fi, c0=c0: e.matmul(
                            pa[:, :BS], w1t[:, k, fi * 128:(fi + 1) * 128], hg[:, k, c0:c0 + BS], start=(k == 0), stop=(k == KC - 1)),
                            reads=[bhg[b], bw1], writes=[bpa], sig=(k == KC - 1))
                    pb, bpb = C.ps.next()
                    for k in range(KC):
                        S.op("tensor", lambda e, pb=pb, k=k, fi=fi, c0=c0: e.matmul(
                            pb[:, :BS], w3t[:, k, fi * 128:(fi + 1) * 128], hg[:, k, c0:c0 + BS], start=(k == 0), stop=(k == KC - 1)),
                            reads=[bhg[b], bw3], writes=[bpb], sig=(k == KC - 1))
                    sa, bsa = sil_r.next()
                    S.op("scalar", lambda e, sa=sa, pa=pa: e.activation(sa[:], pa[:, :BS], AF.Silu), reads=[bpa], writes=[bsa])
                    S.op("vector", lambda e, sa=sa, pb=pb, hf_=hids[fi][0]: e.tensor_tensor(hf_[:], pb[:, :BS], sa[:], ALU.mult),
                         reads=[bpb, bsa], writes=[hids[fi][1]])
                hid_of[b] = hids

            def down(b, w2t=w2t, bw2=bw2):
                c0 = b * BS
                hids = hid_of[b]
                pos_ = [C.ps.next() for _ in range(KC // 2)]
                for fi in range(SL):
                    for dc in range(KC):
                        po, bpo = pos_[dc // 2]
                        h2 = dc % 2
                        S.op("tensor", lambda e, po=po, fi=fi, dc=dc, h2=h2, hf_=hids[fi][0]: e.matmul(
                            po[:, h2 * BS:(h2 + 1) * BS], w2t[:, fi, dc * 128:(dc + 1) * 128], hf_[:],
                            start=(fi == 0 and h2 == 0), stop=(fi == SL - 1), skip_group_check=True),
                            reads=[hids[fi][1], bw2], writes=[bpo], sig=(fi == SL - 1 and h2 == 1))
                for dc2 in range(KC // 2):
                    po, bpo = pos_[dc2]
                    S.op("vector", lambda e, po=po, dc2=dc2, c0=c0: e.tensor_tensor(
                        ys[:, 2 * dc2:2 * dc2 + 2, c0:c0 + BS], po[:, 0:2 * BS].rearrange("p (a c) -> p a c", a=2), ys[:, 2 * dc2:2 * dc2 + 2, c0:c0 + BS], ALU.add),
                        reads=[bpo], writes=[bys[b]])

            def chain(f):
                opened = 0
                for b in range(nblk):
                    q = p * nblk + b
                    if q > 0:
                        S.begin_cond(cap, bcnt, BS * q, key=("cap", ex))
                        opened += 1
                    f(b)
                for _ in range(opened):
                    S.end_cond()
            hid_of = {}
            for b in range(nblk):
                q = p * nblk + b
                if q > 0:
                    S.begin_cond(cap, bcnt, BS * q, key=("cap", ex))
                up(b)
                down(b)
                if q > 0:
                    S.end_cond()

    def do_scatter(ex, p):
        cap = cap_of(ex)
        for b in range(nblk):
            q = p * nblk + b
            if q > 0:
                S.begin_cond(cap, bcnt, BS * q, key=("cap", ex))
            ysb, bysb = ysb_r.next()
            S.op("gpsimd", lambda e, ysb=ysb, b=b: e.tensor_copy(ysb[:], ys[:, :, b * BS:(b + 1) * BS]), reads=[bys[b]], writes=[bysb])
            tiles = []
            for st2 in range(BS // 128):
                slot = q * (BS // 128) + st2
                ptr, bptr = C.ps.next()
                ptb = ptr[:].bitcast(BF16)
                for dc in range(KC):
                    S.op("tensor", lambda e, ptb=ptb, dc=dc, ysb=ysb, st2=st2: e.transpose(
                        ptb[:, dc * 128:(dc + 1) * 128], ysb[:, dc, st2 * 128:(st2 + 1) * 128], identb[:]),
                        reads=[bysb, bidb], writes=[bptr], sig=(dc == KC - 1))
                ysm, bysm = ysm_r.next()
                S.op("scalar", lambda e, ysm=ysm, ptb=ptb: e.copy(ysm[:], ptb[:, 0:D]), reads=[bptr], writes=[bysm])
                sg, bsg = sg_r.next()
                S.op("vector", lambda e, sg=sg, slot=slot: e.scalar_tensor_tensor(
                    sg[:], prow[:], jp[:, slot:slot + 1], grow[:], ALU.is_equal, ALU.mult), reads=[brow, btab], writes=[bsg])
                tiles.append((ysm, bysm, sg, bsg))
            for dc in range(KC):
                for tb in range(NT // 512):
                    po, bpo = C.ps.next()
                    for i, (ysm, bysm, sg, bsg) in enumerate(tiles):
                        S.op("tensor", lambda e, po=po, ysm=ysm, sg=sg, dc=dc, tb=tb, i=i: e.matmul(
                            po[:], ysm[:, dc * 128:(dc + 1) * 128], sg[:, tb * 512:(tb + 1) * 512], start=(i == 0), stop=(i == len(tiles) - 1)),
                            reads=[bysm, bsg], writes=[bpo], sig=(i == len(tiles) - 1))
                    if (dc * 4 + tb) % 3 != 2:
                        S.op("vector", lambda e, po=po, dc=dc, tb=tb: e.scalar_tensor_tensor(
                            xT[:, dc, tb * 512:(tb + 1) * 512], po[:], mod1[:, 5 * 8 + dc, 0:1], xT[:, dc, tb * 512:(tb + 1) * 512], ALU.mult, ALU.add),
                            reads=[bpo, bmod1], writes=bx[tb * 4:(tb + 1) * 4])
                    else:
                        sc, bsc = sct_r.next()
                        S.op("scalar", lambda e, sc=sc, po=po, dc=dc: e.activation(sc[:], po[:], AF.Copy, scale=mod1[:, 5 * 8 + dc, 0:1]),
                             reads=[bpo, bmod1], writes=[bsc])
                        S.op("gpsimd", lambda e, sc=sc, dc=dc, tb=tb: e.tensor_tensor(
                            xT[:, dc, tb * 512:(tb + 1) * 512], xT[:, dc, tb * 512:(tb + 1) * 512], sc[:], ALU.add),
                            reads=[bsc], writes=bx[tb * 4:(tb + 1) * 4])
            if q > 0:
                S.end_cond()

    prefetch(0)
    do_rowbcast(0)
    do_gather(0, 0)
    for ex in range(NE):
        do_ffn(ex, 0)
        if ex + 1 < NE:
            prefetch(ex + 1)
            do_gather(ex + 1, 0)
        do_scatter(ex, 0)
        for p in range(1, NT // PASS):
            S.begin_cond(cap_of(ex), bcnt, PASS * p, key=("cap", ex))
            do_gather(ex, p)
            do_ffn(ex, p)
            do_scatter(ex, p)
            if ex + 1 < NE and p == NT // PASS - 1:
                do_gather(ex + 1, 0)
                for s0 in (0, SL):
                    issue_slice(ex + 1, s0, pre[(ex + 1, s0)])
            S.end_cond()
        if ex + 1 < NE:
            do_rowbcast(ex + 1)


GROUPS = [[0, 1, 2, 3], [4, 5, 6, 7]]


def build_fused():
    nc = bass.Bass("TRN2", target_bir_lowering=False)

    def din(name, shape, dt=F32):
        return nc.dram_tensor(name, list(shape), dt, kind="ExternalInput").ap()

    def dscr(name, shape, dt=F32):
        return nc.dram_tensor(name, list(shape), dt).ap()
    xall = din("xall", [TOT, D])
    vecs = din("vecs", [128, VP.n])
    ident = din("ident", [128, 128])
    ada_w = din("ada_w", [2, D, 6 * D])
    w_in = din("w_in", [D, 2 * D])
    w_a = din("w_a", [2, 8, 128, 128])
    w_i = din("w_i", [2, 8, 128, 128])
    w_out = din("w_out", [D, D])
    f_w1 = din("f_w1", [D, DFF])
    f_w3 = din("f_w3", [D, DFF])
    f_w2 = din("f_w2", [DFF, D])
    w_qkv = din("w_qkv", [D, 1536])
    cos_d = din("cos", [128, NT])
    sin_d = din("sin", [128, NT])
    rotm_d = din("rotm", [128, 128])
    w_o = din("w_o", [D, D])
    router = din("router", [D, NE])
    m_w1 = din("m_w1", [NE, D, DFE])
    m_w3 = din("m_w3", [NE, D, DFE])
    m_w2 = din("m_w2", [NE, DFE, D])
    fin_g = din("fin_g", [D])
    cst_d = din("cst", [128, 385])
    out_d = nc.dram_tensor("out", [NT, D], F32, kind="ExternalOutput").ap()
    htm_d = dscr("htm_d", [NT // 128, 128, D], BF16)
    a_sp = dscr("a_sp", [16, 128, NT])
    b_sp = dscr("b_sp", [16, 128, NT])
    summ_loc = dscr("summ_loc", [128, 32])
    summ_all_d = dscr("summ_all_d", [4 * 128, 32])
    klat = [dscr(f"klat{g}", [128, NT], BF16) for g in range(2)]
    kall = [dscr(f"kall{g}", [4 * 128, NT], BF16) for g in range(2)]
    kctx = dscr("kctx", [128, 2 * NCX], BF16)
    vlat = [dscr(f"vlat{h}", [NT // 2, 256], BF16) for h in range(2)]
    vall = [dscr(f"vall{h}", [4 * NT // 2, 256], BF16) for h in range(2)]
    vctx = dscr("vctx", [NCX, 256], BF16)

    with ExitStack() as es:
        C = setup_common(nc, es, vecs, ident)
        S = C.S
        outb = S.buf("out")
        C.ada_queue = "gpsimd"
        C.ada_dt = BF16
        C.ada_ring = 4
        emit_silu_c(C)
        mod0, bmod0 = S.sbuf("mod0", [128, 48, 2], F32), S.buf("mod")
        mod1, bmod1 = S.sbuf("mod1", [128, 48, 2], F32), S.buf("mod")
        gs1 = S.sbuf("gs1", [128, KC, 2], F32)
        gs2 = S.sbuf("gs2", [128, KC, 2], F32)
        gs3 = S.sbuf("gs3", [128, KC, 2], F32)
        gs4 = S.sbuf("gs4", [128, KC, 2], F32)
        bgs1, bgs2, bgs3, bgs4 = S.buf("gs"), S.buf("gs"), S.buf("gs"), S.buf("gs")
        identb = S.sbuf("identb", [128, 128], BF16)
        bidb = S.buf("identb")
        S.op("vector", lambda e: e.tensor_copy(identb[:], C.ident[:]), reads=[C.bident], writes=[bidb])
        cst = S.sbuf("cst", [128, 16], F32)
        bcst = S.buf("cst")
        summ = S.sbuf("summ", [128, 32], F32)
        bsumm = S.buf("summ")
        sall = S.sbuf("sall", [128, 4, 32], F32)
        bsall = S.buf("sall")
        cneg = S.sbuf("cneg", [128, 32], F32)
        bcneg = S.buf("cneg")
        S.op("scalar", lambda e: e.activation(cneg[:, 0:16], V(C, "lam", 0, 16), AF.Exp, scale=-1.0), reads=[C.bvec], writes=[bcneg])
        S.op("scalar", lambda e: e.activation(cneg[:, 0:16], cneg[:, 0:16], AF.Ln, bias=1.0), reads=[bcneg], writes=[bcneg])
        S.op("vector", lambda e: e.tensor_scalar(cneg[:, 16:32], cneg[:, 0:16], -16.0, None, ALU.mult), reads=[bcneg], writes=[bcneg])
        S.op("vector", lambda e: e.tensor_scalar(cneg[:, 0:16], cneg[:, 0:16], -8.0, None, ALU.mult), reads=[bcneg], writes=[bcneg])

        es_l0 = scoped(S)
        hT = S.sbuf("hT", [128, KC, TOT], BF16)
        bh = [S.buf("hT") for _ in range(TOT // 128)]
        es_ada = scoped(S)
        emit_ada(C, ada_w, 0, "ada_b0", mod0, bmod0)
        emit_gs_into(C, gs1, bgs1, mod0, bmod0, 1, "n1g0")
        emit_gs_into(C, gs2, bgs2, mod0, bmod0, 4, "n2g0")

        es1 = scoped(S)
        C.xtile = Ring(S, "xtile", [128, D], F32, 2)
        alloc_norm_tmps(C)
        xblk = Ring(S, "xblk", [128, KC, 512], F32, 2)
        for (c0, n, which) in LAT_BLOCKS + [CTX_BLOCK, HAL_BLOCK]:
            xb_, bxb_ = xblk.next()
            bl = [bxb_] * 4
            emit_load_xT(C, xall, c0, n // 128, xb_, bl, 0)
            emit_norm(C, xb_, bl, 0, hT, bh, c0, n, gs1, bgs1, mod0, bmod0, 0, which)
        S.barrier()
        es1.close()
        es_ada.close()

        es_yc = scoped(S)
        yctx = S.sbuf("yctx", [128, KC, NCX], F32)
        byctx = S.buf("yctx")
        es_mix = scoped(S)
        ada1_r = Ring(S, "adaw1s", [128, KC, 768], BF16, 2)
        win_r = Ring(S, "win", [128, KC, 128], BF16, 3)
        wg_r = Ring(S, "wg", [128, 4, 128], BF16, 3)
        xbe = Ring(S, "xbe", [128, NT + 3 + NCX + 3], F32, 1)
        xc_r = Ring(S, "xc", [128, NSC], F32, 2)
        xcb_r = Ring(S, "xcb", [128, NSC], BF16, 2)
        r_r = Ring(S, "rr", [128, NSC], F32, 2)
        b_r = Ring(S, "bb", [128, NSC], F32, 3)
        a_r = Ring(S, "aa", [128, NSC], F32, 2)
        m_r = Ring(S, "mm", [128, NSC], F32, 2)
        yc_r = Ring(S, "yc", [128, NCX], F32, 2)
        sm_r = Ring(S, "sm", [128, 8], F32, 2)
        bsp = [S.buf("sp") for _ in range(16)]
        LB = NT + 3
        w_in_v = w_in.rearrange("(k p) n -> p k n", p=128)
        ada1_w = {0: emit_ada_dma(C, ada_w, 1, 0, ada1_r)}

        def p2a_loadw(ct):
            wt, bw = win_r.next()
            S.dma("gpsimd", wt[:], w_in_v[:, :, ct * 128:(ct + 1) * 128], writes=[bw])
            wg, bwg = wg_r.next()
            for d in range(2):
                S.dma("gpsimd", wg[:, d, :], w_a[d, ct], writes=[bwg], nowaw=True)
                S.dma("gpsimd", wg[:, 2 + d, :], w_i[d, ct], writes=[bwg], nowaw=True)
            return wt, bw, wg, bwg
        p2a_w = {0: p2a_loadw(0)}

        def p2a_front(ct):
            if ct + 1 < KC:
                ada1_w[ct + 1] = emit_ada_dma(C, ada_w, 1, ct + 1, ada1_r)
            if ct + 1 < KC:
                p2a_w[ct + 1] = p2a_loadw(ct + 1)
            emit_ada_mm(C, ct, ada1_w[ct][0], ada1_w[ct][1], "ada_b1", mod1, bmod1)
            wt, bw, wg, bwg = p2a_w[ct]
            xe, bxe = xbe.next()
            S.op("gpsimd", lambda e, xe=xe: e.memset(xe[:, LB:LB + 2], 0.0), writes=[bxe])
            S.op("gpsimd", lambda e, xe=xe: e.memset(xe[:, LB + 2 + NCX:LB + 3 + NCX], 0.0), writes=[bxe])
            for (c0, n, which) in LAT_BLOCKS + [CTX_BLOCK, (HAL0, 3, 0)]:
                ps, bps = C.ps.next()
                for k in range(KC):
                    S.op("tensor", lambda e, ps=ps, k=k, wt=wt, c0=c0, n=n: e.matmul(
                        ps[:, :n], wt[:, k, :], hT[:, k, c0:c0 + n], start=(k == 0), stop=(k == KC - 1)),
                        reads=tile_bufs(bh, c0, n) + [bw], writes=[bps], sig=(k == KC - 1))
                if c0 < CTX0:
                    copy_op(C, evac_engine(C), xe[:, 2 + c0:2 + c0 + n], ps[:, :n], [bps], [bxe])
                elif c0 == CTX0:
                    copy_op(C, evac_engine(C), xe[:, LB + 2:LB + 2 + NCX], ps[:, :n], [bps], [bxe])
                else:
                    S.op("vector", lambda e, ps=ps, xe=xe: e.tensor_tensor(xe[:, 0:2], ps[:, 0:2], V(C, "hmask", 0, 2), ALU.mult),
                         reads=[bps, C.bvec], writes=[bxe])
                    S.op("vector", lambda e, ps=ps, xe=xe: e.tensor_tensor(xe[:, 2 + NT:3 + NT], ps[:, 2:3], V(C, "hmask", 2, 1), ALU.mult),
                         reads=[bps, C.bvec], writes=[bxe])
            xc, bxc = xc_r.next()
            for (dst0, src0, n) in [(0, 0, NT), (NT, LB, NCX)]:
                S.op("scalar", lambda e, xc=xc, xe=xe, dst0=dst0, src0=src0, n=n, ct=ct: e.activation(
                    xc[:, dst0:dst0 + n], xe[:, src0:src0 + n], AF.Identity,
                    bias=V(C, "convb", ct), scale=V(C, "convw", ct)), reads=[bxe, C.bvec], writes=[bxc])
                for k in range(1, 4):
                    S.op("vector", lambda e, xc=xc, xe=xe, dst0=dst0, src0=src0, n=n, k=k, ct=ct: e.scalar_tensor_tensor(
                        xc[:, dst0:dst0 + n], xe[:, src0 + k:src0 + k + n], V(C, "convw", k * 8 + ct), xc[:, dst0:dst0 + n],
                        ALU.mult, ALU.add), reads=[bxe, C.bvec, bxc], writes=[bxc])
            xcb, bxcb = xcb_r.next()
            S.op("gpsimd", lambda e, xcb=xcb, xc=xc: e.tensor_copy(xcb[:], xc[:]), reads=[bxc], writes=[bxcb])
            return (xc, bxc, xcb, bxcb, wg, bwg)
        def p2a_back(ct, xc, bxc, xcb, bxcb, wg, bwg):
            ycs = []
            for d in range(2):
                rr, brr = r_r.next()
                bb, bbb = b_r.next()
                for (c0, n) in [(0, 512), (512, 512), (1024, 512), (1536, 512), (NT, NCX)]:
                    pr, bpr = C.ps.next()
                    S.op("tensor", lambda e, pr=pr, wg=wg, d=d, xcb=xcb, c0=c0, n=n: e.matmul(
                        pr[:, :n], wg[:, d, :], xcb[:, c0:c0 + n], start=True, stop=True), reads=[bwg, bxcb], writes=[bpr])
                    S.op("scalar", lambda e, pr=pr, rr=rr, c0=c0, n=n, d=d, ct=ct: e.activation(
                        rr[:, c0:c0 + n], pr[:, :n], AF.Sigmoid, bias=V(C, "b_a", d * 8 + ct)), reads=[bpr, C.bvec], writes=[brr])
                    pi, bpi = C.ps.next()
                    S.op("tensor", lambda e, pi=pi, wg=wg, d=d, xcb=xcb, c0=c0, n=n: e.matmul(
                        pi[:, :n], wg[:, 2 + d, :], xcb[:, c0:c0 + n], start=True, stop=True), reads=[bwg, bxcb], writes=[bpi])
                    S.op("scalar", lambda e, pi=pi, bb=bb, c0=c0, n=n, d=d, ct=ct: e.activation(
                        bb[:, c0:c0 + n], pi[:, :n], AF.Sigmoid, bias=V(C, "b_i", d * 8 + ct)), reads=[bpi, C.bvec], writes=[bbb])
                aa, baa = a_r.next()
                mm, bmm = m_r.next()
                cn = d * 8 + ct
                sm, bsm = sm_r.next()
                S.op("scalar", lambda e, aa=aa, rr=rr, cn=cn: e.activation(aa[:], rr[:], AF.Exp, scale=cneg[:, cn:cn + 1]),
                     reads=[brr, bcneg], writes=[baa])
                S.op("scalar", lambda e, mm=mm, rr=rr, cn=cn: e.activation(mm[:], rr[:], AF.Exp, scale=cneg[:, 16 + cn:17 + cn]),
                     reads=[brr, bcneg], writes=[bmm])
                S.op("vector", lambda e, sm=sm, rr=rr: e.tensor_reduce(sm[:, 0:1], rr[:, 0:NT], AX.X, ALU.add), reads=[brr], writes=[bsm])
                S.op("scalar", lambda e, mm=mm: e.activation(mm[:], mm[:], AF.Sqrt, bias=1.0, scale=-1.0), reads=[bmm], writes=[bmm])
                S.op("gpsimd", lambda e, bb=bb, xc=xc: e.tensor_tensor(bb[:], bb[:], xc[:], ALU.mult), reads=[bbb, bxc], writes=[bbb])
                S.op("vector", lambda e, bb=bb, mm=mm: e.tensor_tensor(bb[:], bb[:], mm[:], ALU.mult), reads=[bbb, bmm], writes=[bbb])
                S.dma("sync", a_sp[cn], aa[:, 0:NT], reads=[baa], writes=[bsp[cn]])
                S.dma("sync", b_sp[cn], bb[:, 0:NT], reads=[bbb], writes=[bsp[cn]], nowaw=True)
                yc, byc = yc_r.next()
                o = ct * 4 + d * 2
                if d == 0:
                    S.op("vector", lambda e, yc=yc, aa=aa, bb=bb: e.tensor_tensor_scan(
                        yc[:], aa[:, NT:NSC], bb[:, NT:NSC], 0.0, ALU.mult, ALU.add), reads=[baa, bbb], writes=[byc])
                    S.op("vector", lambda e, mm=mm, aa=aa, bb=bb: e.tensor_tensor_scan(
                        mm[:, 0:NT], aa[:, 0:NT], bb[:, 0:NT], 0.0, ALU.mult, ALU.add), reads=[baa, bbb], writes=[bmm])
                    st0, hend = yc[:, NCX - 1:NCX], mm[:, NT - 1:NT]
                else:
                    S.op("vector", lambda e, yc=yc, aa=aa, bb=bb: e.tensor_tensor_scan(
                        rev_ap(yc[:]), rev_ap(aa[:, NT:NSC]), rev_ap(bb[:, NT:NSC]), 0.0, ALU.mult, ALU.add), reads=[baa, bbb], writes=[byc])
                    S.op("vector", lambda e, mm=mm, aa=aa, bb=bb: e.tensor_tensor_scan(
                        rev_ap(mm[:, 0:NT]), rev_ap(aa[:, 0:NT]), rev_ap(bb[:, 0:NT]), 0.0, ALU.mult, ALU.add), reads=[baa, bbb], writes=[bmm])
                    st0, hend = yc[:, 0:1], mm[:, 0:1]
                S.op("vector", lambda e, st0=st0, cn=cn: e.tensor_copy(cst[:, cn:cn + 1], st0), reads=[byc], writes=[bcst])
                S.op("scalar", lambda e, sm=sm, cn=cn, o=o: e.activation(summ[:, o:o + 1], sm[:, 0:1], AF.Exp, scale=cneg[:, cn:cn + 1]),
                     reads=[bsm, bcneg], writes=[bsumm])
                S.op("vector", lambda e, hend=hend, o=o: e.tensor_copy(summ[:, o + 1:o + 2], hend), reads=[bmm], writes=[bsumm])
                ycs.append((yc, byc))
            (yc0, byc0), (yc1, byc1) = ycs
            S.op("gpsimd", lambda e, yc0=yc0, yc1=yc1, ct=ct: e.tensor_tensor(yctx[:, ct, :], yc0[:], yc1[:], ALU.add), reads=[byc0, byc1], writes=[byctx])
        fr = p2a_front(0)
        for ct in range(KC):
            cur = fr
            if ct + 1 < KC:
                fr = p2a_front(ct + 1)
            p2a_back(ct, *cur)
        emit_gs_into(C, gs3, bgs3, mod1, bmod1, 1, "n1g1")
        emit_gs_into(C, gs4, bgs4, mod1, bmod1, 4, "n2g1")
        bsl, bsa = S.buf("summ_loc"), S.buf("summ_all")
        S.dma("sync", summ_loc, summ[:], reads=[bsumm], writes=[bsl])
        S.collective("AllGather", GROUPS, summ_loc, summ_all_d, [bsl], bsa)
        S.dma("sync", sall[:], summ_all_d.rearrange("(r p) c -> p r c", p=128), reads=[bsa], writes=[bsall])
        S.barrier()
        es_mix.close()

        es_yg = scoped(S)
        ygT = S.sbuf("ygT", [128, KC, NSC], BF16)
        byg = [S.buf("yg") for _ in range(NSC // 128)]
        es_p2b = scoped(S)
        a2_r = Ring(S, "a2", [128, NT], F32, 4)
        b2_r = Ring(S, "b2", [128, NT], F32, 4)
        y_r = Ring(S, "yy", [128, NT], F32, 2)
        sm_r = Ring(S, "sm2", [128, 8], F32, 2)
        gtmp = Ring(S, "gtmp", [128, 512], F32, 3)
        win_r = Ring(S, "win2", [128, KC, 128], BF16, 2)
        carr = S.sbuf("carr", [128, 16], F32)
        ctmp = S.sbuf("ctmp", [128, 8], F32)
        bcarr = S.buf("carr")
        S.op("vector", lambda e: e.tensor_copy(carr[:], cst[:]), reads=[bcst], writes=[bcarr])
        for d in range(2):
            order, sel = ([0, 1, 2, 3], "sel_f") if d == 0 else ([3, 2, 1, 0], "sel_r")
            cs_ = carr[:, d * 8:(d + 1) * 8]
            for i in order:
                Pv = bass.AP(sall[:].tensor, sall[:, i, d * 2:d * 2 + 1].offset, [list(sall[:].ap[0]), [4, 8]])
                Hv = bass.AP(sall[:].tensor, sall[:, i, d * 2 + 1:d * 2 + 2].offset, [list(sall[:].ap[0]), [4, 8]])
                S.op("vector", lambda e, cs_=cs_, Pv=Pv: e.tensor_tensor(ctmp[:], cs_, Pv, ALU.mult), reads=[bcarr, bsall], writes=[bcarr])
                S.op("vector", lambda e, Hv=Hv: e.tensor_tensor(ctmp[:], ctmp[:], Hv, ALU.add), reads=[bcarr, bsall], writes=[bcarr])
                S.op("vector", lambda e, cs_=cs_: e.tensor_tensor(ctmp[:], ctmp[:], cs_, ALU.subtract), reads=[bcarr], writes=[bcarr])
                S.op("vector", lambda e, cs_=cs_, i=i, sel=sel: e.scalar_tensor_tensor(
                    cs_, ctmp[:], V(C, sel, i), cs_, ALU.mult, ALU.add), reads=[bcarr, C.bvec], writes=[bcarr])
        for ct in range(KC):
            wt, bw = win_r.next()
            S.dma("gpsimd", wt[:], w_in_v[:, :, D + ct * 128:D + (ct + 1) * 128], writes=[bw])
            ys = []
            for d in range(2):
                cn = d * 8 + ct
                aa, baa = a2_r.next()
                bb, bbb = b2_r.next()
                S.dma("sync", aa[:], a_sp[cn], reads=[bsp[cn]], writes=[baa])
                S.dma("sync", bb[:], b_sp[cn], reads=[bsp[cn]], writes=[bbb])
                yy, byy = y_r.next()
                if d == 0:
                    S.op("vector", lambda e, yy=yy, aa=aa, bb=bb, cn=cn: e.tensor_tensor_scan(
                        yy[:], aa[:], bb[:], carr[:, cn:cn + 1], ALU.mult, ALU.add), reads=[baa, bbb, bcarr], writes=[byy])
                else:
                    S.op("vector", lambda e, yy=yy, aa=aa, bb=bb, cn=cn: e.tensor_tensor_scan(
                        rev_ap(yy[:]), rev_ap(aa[:]), rev_ap(bb[:]), carr[:, cn:cn + 1], ALU.mult, ALU.add), reads=[baa, bbb, bcarr], writes=[byy])
                ys.append((yy, byy))
            (y0, by0), (y1, by1) = ys
            S.op("gpsimd", lambda e, y0=y0, y1=y1: e.tensor_tensor(y0[:], y0[:], y1[:], ALU.add), reads=[by0, by1], writes=[by0])
            for (c0, n, which) in LAT_BLOCKS + [CTX_BLOCK]:
                pg, bpg = C.ps.next()
                for k in range(KC):
                    S.op("tensor", lambda e, pg=pg, k=k, wt=wt, c0=c0, n=n: e.matmul(
                        pg[:, :n], wt[:, k, :], hT[:, k, c0:c0 + n], start=(k == 0), stop=(k == KC - 1)),
                        reads=tile_bufs(bh, c0, n) + [bw], writes=[bpg], sig=(k == KC - 1))
                t1, bt1 = gtmp.next()
                S.op("scalar", lambda e, t1=t1, pg=pg, n=n: e.activation(t1[:, :n], pg[:, :n], AF.Gelu_apprx_tanh), reads=[bpg], writes=[bt1])
                if which == 0:
                    S.op("vector", lambda e, t1=t1, y0=y0, c0=c0, n=n, ct=ct: e.tensor_tensor(ygT[:, ct, c0:c0 + n], t1[:, :n], y0[:, c0:c0 + n], ALU.mult),
                         reads=[bt1, by0], writes=tile_bufs(byg, c0, n))
                else:
                    S.op("vector", lambda e, t1=t1, c0=c0, n=n, ct=ct: e.tensor_tensor(ygT[:, ct, c0:c0 + n], t1[:, :n], yctx[:, ct, :], ALU.mult),
                         reads=[bt1, byctx], writes=tile_bufs(byg, c0, n))
        S.barrier()
        es_p2b.close()

        S.es = es
        xT = S.sbuf("xT", [128, KC, NSC], F32, side="right")
        bx = [S.buf("xT") for _ in range(NSC // 128)]
        es_o = scoped(S)
        C.xtile = Ring(S, "xtile", [128, D], F32, 2)
        emit_load_xT(C, xall, 0, NSC // 128, xT, bx, 0)
        wo_r = Ring(S, "wo", [128, KC, D], BF16, 1)
        wot, bwo = load_w(C, wo_r, w_out, D, 0, D)
        BL = LAT_BLOCKS + [CTX_BLOCK]
        emit_proj_residual(C, ygT, byg, KC, wot, bwo, xT, bx, BL, mod0, bmod0, 2)
        S.barrier()
        es_o.close()
        es_yg.close()
        es_yc.close()

        es_f = scoped(S)
        alloc_norm_tmps(C)
        emit_norm_blocks(C, xT, bx, hT, bh, BL, gs2, bgs2, mod0, bmod0, 3)
        alloc_ffn(C, 4)
        emit_ffn(C, hT, bh, xT, bx, BL, f_w1, f_w3, f_w2, DFF, mod0, bmod0, 5)
        emit_norm_blocks(C, xT, bx, hT, bh, BL, gs3, bgs3, mod1, bmod1, 0)
        S.barrier()
        es_f.close()

        es_qo = scoped(S)
        qoT = S.sbuf("qoT", [128, 8, NT], BF16, side="right")
        bqo = [[S.buf("qo") for _ in range(NT // 512)] for _ in range(8)]
        es_q = scoped(S)
        wq_r = Ring(S, "wq", [128, KC, 1536], BF16, 1)
        wqt, bwq = load_w(C, wq_r, w_qkv, D, 0, 1536)
        cs_r = Ring(S, "cs", [128, 2, 512], F32, 2)
        rotm = S.sbuf("rotm", [128, 128], F32)
        rotb = S.sbuf("rotb", [128, 128], BF16)
        brot = S.buf("rot")
        S.dma("sync", rotm[:], rotm_d, writes=[brot])
        S.op("vector", lambda e: e.tensor_copy(rotb[:], rotm[:]), reads=[brot], writes=[brot])
        C.rstd = Ring(S, "rstd2", [128, 512], F32, 3)
        qst = Ring(S, "qst", [128, 512], BF16, 2)
        qgb = Ring(S, "qgb", [128, 512], BF16, 3)
        qsq = Ring(S, "qsq", [128, 512], BF16, 3)
        qt1 = Ring(S, "qt1", [128, 512], F32, 4)
        vst = Ring(S, "vst", [128, 256], BF16, 2)
        bklat = [S.buf("klat") for _ in range(2)]
        bkall = [S.buf("kall") for _ in range(2)]
        bkctx = S.buf("kctx")
        bvlat = [S.buf("vlat") for _ in range(2)]
        bvall = [S.buf("vall") for _ in range(2)]
        bvctx = S.buf("vctx")

        def head_mm(hd, c0, n, which):
            rh = tile_bufs(bh, c0, n)
            pq, bpq = C.ps.next()
            for k in range(KC):
                S.op("tensor", lambda e, k=k: e.matmul(
                    pq[:, :n], wqt[:, k, hd * 128:(hd + 1) * 128], hT[:, k, c0:c0 + n], start=(k == 0), stop=(k == KC - 1)),
                    reads=rh + [bwq], writes=[bpq], sig=(k == KC - 1))
            return (hd, c0, n, which, pq, bpq)

        def head_a(hd, c0, n, which, pq, bpq):
            gname = "q_g" if hd < 8 else "k_g"
            sqt, bsqt = qsq.next()
            S.op("scalar", lambda e: e.activation(sqt[:, :n], pq[:, :n], AF.Square), reads=[bpq], writes=[bsqt])
            pss, bpss = C.ps.next()
            S.op("tensor", lambda e: e.matmul(pss[:, :n], C.ones[:], sqt[:, :n], start=True, stop=True),
                 reads=[bsqt, C.bones], writes=[bpss])
            rs, brs = C.rstd.next()
            S.op("scalar", lambda e: e.activation(rs[:, :n], pss[:, :n], AF.Sqrt, bias=C.epsb[:, 0:1], scale=1.0 / 128),
                 reads=[bpss, C.bepsb], writes=[brs])
            S.op("vector", lambda e: e.reciprocal(rs[:, :n], rs[:, :n]), reads=[brs], writes=[brs])
            qn, bqn = qgb.next()
            S.op("vector", lambda e: e.scalar_tensor_tensor(
                qn[:, :n], pq[:, :n], V(C, gname, 0), rs[:, :n], ALU.mult, ALU.mult), reads=[bpq, C.bvec, brs], writes=[bqn])
            pr = bpr = None
            if which == 0:
                pr, bpr = C.ps.next()
                S.op("tensor", lambda e: e.matmul(pr[:, :n], rotb[:], qn[:, :n], start=True, stop=True),
                     reads=[bqn, brot], writes=[bpr])
            return (hd, c0, n, which, qn, bqn, pr, bpr, head_proj.cs if which == 0 else None)

        def head_b(hd, c0, n, which, qn, bqn, pr, bpr, cs_):
            if hd < 8:
                dst, bdst = qoT[:, hd, c0:c0 + n], [bqo[hd][c0 // 512]]
            else:
                qo_, bqo_ = qst.next()
                dst, bdst = qo_[:, :n], [bqo_]
            if which == 0:
                cs, bcs = cs_
                t0_, bt0 = qt1.next()
                t1, bt1 = qt1.next()
                S.op("vector", lambda e: e.tensor_tensor(t0_[:, :n], qn[:, :n], cs[:, 0, :n], ALU.mult), reads=[bqn, bcs], writes=[bt0])
                S.op("vector", lambda e: e.tensor_tensor(t1[:, :n], pr[:, :n], cs[:, 1, :n], ALU.mult), reads=[bpr, bcs], writes=[bt1])
                S.op("vector", lambda e: e.tensor_tensor(dst, t0_[:, :n], t1[:, :n], ALU.add), reads=[bt0, bt1], writes=bdst)
            else:
                S.op("vector", lambda e: e.tensor_copy(dst, qn[:, :n]), reads=[bqn], writes=bdst)
            if hd >= 8:
                g = hd - 8
                if which == 0:
                    S.dma("sync", klat[g][:, c0:c0 + n], dst, reads=bdst, writes=[bklat[g]], nowaw=True)
                else:
                    S.dma("sync", kctx[:, g * NCX:(g + 1) * NCX], dst, reads=bdst, writes=[bkctx], nowaw=True)

        def head_proj(hd, c0, n, which, pq=None, bpq=None):
            if pq is None:
                _, _, _, _, pq, bpq = head_mm(hd, c0, n, which)
            head_b(*head_a(hd, c0, n, which, pq, bpq))

        def load_cs(c0, n):
            cs, bcs = cs_r.next()
            S.dma("sync", cs[:, 0, :n], cos_d[:, c0:c0 + n], writes=[bcs])
            S.dma("sync", cs[:, 1, :n], sin_d[:, c0:c0 + n], writes=[bcs], nowaw=True)
            head_proj.cs = (cs, bcs)
        for (c0, n, which) in BL:
            if which == 0:
                load_cs(c0, n)
            for hd in (8, 9):
                head_proj(hd, c0, n, which)
        for g in range(2):
            S.collective("AllGather", GROUPS, klat[g], kall[g], [bklat[g]], bkall[g])
        for (c0, n, which) in BL:
            rh = tile_bufs(bh, c0, n)
            for t0 in range(0, n, 128):
                pv, bpv = C.ps.next()
                for k in range(KC):
                    S.op("tensor", lambda e, pv=pv, k=k, t0=t0, c0=c0: e.matmul(
                        pv[:, 0:256], hT[:, k, c0 + t0:c0 + t0 + 128], wqt[:, k, 1280:1536], start=(k == 0), stop=(k == KC - 1)),
                        reads=rh + [bwq], writes=[bpv], sig=(k == KC - 1))
                vo, bvo = vst.next()
                copy_op(C, evac_engine(C), vo[:], pv[:, 0:256], [bpv], [bvo])
                if which == 0:
                    t = c0 + t0
                    hh = t // (NT // 2)
                    r0 = t % (NT // 2)
                    S.dma("sync", vlat[hh][r0:r0 + 128, :], vo[:], reads=[bvo], writes=[bvlat[hh]], nowaw=True)
                else:
                    S.dma("sync", vctx[t0:t0 + 128, :], vo[:], reads=[bvo], writes=[bvctx], nowaw=True)
        for hh in range(2):
            S.collective("AllGather", GROUPS, vlat[hh], vall[hh], [bvlat[hh]], bvall[hh])
        for (c0, n, which) in LAT_BLOCKS:
            load_cs(c0, n)
            mm = {0: head_mm(0, c0, n, which), 1: head_mm(1, c0, n, which)}
            st = {0: head_a(*mm[0])}
            for hd in range(8):
                if hd + 2 < 8:
                    mm[hd + 2] = head_mm(hd + 2, c0, n, which)
                if hd + 1 < 8:
                    st[hd + 1] = head_a(*mm[hd + 1])
                head_b(*st[hd])
        S.barrier()
        es_q.close()
        es_l0.close()

        es_a = scoped(S)
        kT = S.sbuf("kT", [128, 2, NKEY], BF16)
        bk = S.buf("kT")
        va = S.sbuf("va", [128, NKT, 2, 130], BF16)
        bva = S.buf("va")
        S.op("vector", lambda e: e.memset(va[:, :, :, 128:130], 1.0), writes=[bva])
        for g in range(2):
            S.dma("sync", kT[:, g, 0:NCX], kctx[:, g * NCX:(g + 1) * NCX], reads=[bkctx], writes=[bk], nowaw=True)
            for r in range(4):
                S.dma("sync", kT[:, g, NCX + r * NT:NCX + (r + 1) * NT], kall[g][r * 128:(r + 1) * 128, :], reads=[bkall[g]], writes=[bk], nowaw=True)
            S.dma("sync", va[:, 0:2, g, 0:128], vctx[:, g * 128:(g + 1) * 128].rearrange("(t p) d -> p t d", p=128), reads=[bvctx], writes=[bva], nowaw=True)
            for hh in range(2):
                for r in range(4):
                    kt0 = 2 + r * 16 + hh * 8
                    S.dma("sync", va[:, kt0:kt0 + 8, g, 0:128],
                          vall[hh][r * (NT // 2):(r + 1) * (NT // 2), g * 128:(g + 1) * 128].rearrange("(t p) d -> p t d", p=128),
                          reads=[bvall[hh]], writes=[bva], nowaw=True)
        pT_r = Ring(S, "pT", [128, 512], BF16, 4)
        on_r = Ring(S, "on", [128, 128], BF16, 2)
        ri_r = Ring(S, "ri", [128, 2], F32, 2)
        SCL = 1.0 / float(np.sqrt(128.0))
        psO = SubRing(C.ps.tiles[0:4])
        psS = SubRing(C.ps.tiles[4:7])
        psT = SubRing(C.ps.tiles[7:8])
        for qb in range(NT // 512):
            for h in range(8):
                g = h // 4
                po = [psO.next(), psO.next()]

                def s_issue(kt, g=g, h=h, qb=qb):
                    ps, bps = psS.next()
                    S.op("tensor", lambda e, ps=ps, kt=kt: e.matmul(
                        ps[:], kT[:, g, kt * 128:(kt + 1) * 128], qoT[:, h, qb * 512:(qb + 1) * 512], start=True, stop=True),
                        reads=[bk, bqo[h][qb]], writes=[bps])
                    pT, bpT = pT_r.next()
                    S.op("scalar", lambda e, pT=pT, ps=ps: e.activation(pT[:], ps[:], AF.Exp, scale=SCL), reads=[bps], writes=[bpT])
                    return pT, bpT
                pend = [s_issue(0), s_issue(1)]
                for kt in range(NKT):
                    pT, bpT = pend.pop(0)
                    if kt + 2 < NKT:
                        pend.append(s_issue(kt + 2))
                    for qt in range(4):
                        pot, bpot = po[qt // 2]
                        c = (qt % 2) * 129
                        S.op("tensor", lambda e, pot=pot, c=c, pT=pT, qt=qt, kt=kt, g=g: e.matmul(
                            pot[:, c:c + 129], pT[:, qt * 128:(qt + 1) * 128], va[:, kt, g, 0:129], start=(kt == 0 and qt % 2 == 0), stop=(kt == NKT - 1),
                            skip_group_check=True),
                            reads=[bpT, bva], writes=[bpot], sig=(qt == 3))
                for qt in range(4):
                    pot, bpot = po[qt // 2]
                    c = (qt % 2) * 129
                    ri, bri = ri_r.next()
                    S.op("vector", lambda e, ri=ri, pot=pot, c=c: e.reciprocal(ri[:, 0:1], pot[:, c + 128:c + 129]), reads=[bpot], writes=[bri])
                    on, bon = on_r.next()
                    S.op("vector", lambda e, on=on, pot=pot, c=c, ri=ri: e.tensor_scalar(on[:], pot[:, c:c + 128], ri[:, 0:1], None, ALU.mult),
                         reads=[bpot, bri], writes=[bon])
                    pt, bpt = psT.next()
                    ptb = pt[:].bitcast(BF16)
                    S.op("tensor", lambda e, ptb=ptb, on=on: e.transpose(ptb[:, 0:128], on[:], identb[:]), reads=[bon, bidb], writes=[bpt])
                    col = qb * 512 + qt * 128
                    S.op("scalar", lambda e, ptb=ptb, h=h, col=col: e.copy(qoT[:, h, col:col + 128], ptb[:, 0:128]), reads=[bpt], writes=[bqo[h][qb]])
        S.barrier()
        es_a.close()

        es_o = scoped(S)
        wo_r = Ring(S, "wo2", [128, KC, D], BF16, 1)
        wot, bwo = load_w(C, wo_r, w_o, D, 0, D)
        bqo_cols = [None] * (NT // 128)
        for (c0, n, which) in LAT_BLOCKS:
            ri = [bqo[h][c0 // 512] for h in range(8)]
            wx = tile_bufs(bx, c0, n)
            for dc in range(KC):
                po_, bpo_ = C.ps.next()
                for k in range(KC):
                    S.op("tensor", lambda e, po_=po_, k=k, dc=dc, c0=c0, n=n: e.matmul(
                        po_[:, :n], wot[:, k, dc * 128:(dc + 1) * 128], qoT[:, k, c0:c0 + n], start=(k == 0), stop=(k == KC - 1)),
                        reads=ri + [bwo], writes=[bpo_], sig=(k == KC - 1))
                S.op("vector", lambda e, po_=po_, dc=dc, c0=c0, n=n: e.scalar_tensor_tensor(
                    xT[:, dc, c0:c0 + n], po_[:, :n], mod1[:, 2 * 8 + dc, 0:1], xT[:, dc, c0:c0 + n], ALU.mult, ALU.add),
                    reads=[bpo_, bmod1], writes=wx)
        S.barrier()
        es_o.close()
        es_qo.close()

        S.es = es
        gates = S.sbuf("gates", [128, NT // 128, NE], F32)
        maskt = S.sbuf("maskt", [128, NT // 128, NE], F32)
        bgates = S.buf("gates")
        es_h2 = scoped(S)
        hT2 = S.sbuf("hT2", [128, KC, NT], BF16)
        bh2 = [S.buf("hT2") for _ in range(NT // 128)]
        es_r = scoped(S)
        alloc_norm_tmps(C)
        hf = S.sbuf("hf", [128, KC, NT], F32)
        bhf = [S.buf("hf") for _ in range(NT // 128)]
        emit_norm_blocks(C, xT, bx, hT2, bh2, LAT_BLOCKS, gs4, bgs4, mod1, bmod1, 3, hf=hf, bhf=bhf)
        emit_router(C, hf, bhf, router, gates, bgates, maskt=maskt)
        S.barrier()
        es_r.close()

        es_t = scoped(S)
        bhtm = emit_htm(C, hT2, bh2, htm_d, identb, bidb)
        S.barrier()
        es_t.close()
        es_h2.close()
        es_m = scoped(S)
        emit_moe_sparse(C, bhtm, xT, bx, maskt, gates, bgates, cst_d, htm_d, m_w1, m_w3, m_w2, mod1, bmod1, identb, bidb)
        S.barrier()
        es_m.close()

        es_z = scoped(S)
        emit_final(C, xT, bx, fin_g, out_d, outb)
        S.finish()
        S.emit()
        es_z.close()
    return nc


def rope_tables():
    nfreq = 32
    t = np.arange(SEQ)
    row = (t // 64).astype(np.float32)
    col = (t % 64).astype(np.float32)
    freqs = (np.float32(10000.0) ** (-np.arange(nfreq, dtype=np.float32) / np.float32(nfreq))).astype(np.float32)
    ang = np.zeros((128, SEQ), np.float32)
    for d in range(128):
        pos = row if d < 64 else col
        ang[d] = pos * freqs[d % 32]
    return np.cos(ang).astype(np.float32), np.sin(ang).astype(np.float32)


def rot_matrix_T():
    R = np.zeros((128, 128), np.float32)
    for m in range(128):
        if m % 64 < 32:
            R[m, m + 32] = -1.0
        else:
            R[m, m - 32] = 1.0
    return np.ascontiguousarray(R.T)


_CACHE = {}
_DEBUG = {}


def _prog(name):
    if name not in _CACHE:
        _CACHE[name] = {"A": lambda: build_l0("A"), "B": lambda: build_l0("B"), "C": build_l1, "F": build_fused}[name]()
    return _CACHE[name]


def kernel_unfused(**inp):
    inp = {k: np.asarray(v) for k, v in inp.items()}
    x = inp["x"].astype(np.float32, copy=False)
    ctx = inp["ctx"].astype(np.float32, copy=False)
    ident = np.eye(128, dtype=np.float32)
    cores = list(range(8))
    maps0 = []
    for c in cores:
        b, j = c // 4, c % 4
        t0 = j * NT
        hal = np.zeros((128, D), np.float32)
        if j > 0:
            hal[0:2] = x[b, t0 - 2:t0]
        if j < 3:
            hal[2] = x[b, t0 + NT]
        xall = np.concatenate([x[b, t0:t0 + NT], ctx[b], hal], axis=0)
        maps0.append({
            "xall": np.ascontiguousarray(xall), "vecs": pack_vecs(inp, b, j), "ident": ident,
            "ada_w": inp["ada_w"], "w_in": inp["rg_w_in"][0], "w_a": inp["rg_w_a"][0], "w_i": inp["rg_w_i"][0],
        })
    resA = run_bass_kernel_spmd(_prog("A"), maps0, core_ids=cores).results
    cosf, sinf = rope_tables()
    rotm = rot_matrix_T()
    maps1 = []
    for c in cores:
        b, j = c // 4, c % 4
        m = dict(maps0[c])
        m["summ_all"] = np.ascontiguousarray(np.concatenate([resA[b * 4 + i]["summ"] for i in range(4)], axis=1))
        m.update({"w_out": inp["rg_w_out"][0], "f_w1": inp["ffn_w1"][0], "f_w3": inp["ffn_w3"][0], "f_w2": inp["ffn_w2"][0],
                  "w_qkv": inp["attn_w_qkv"][0], "cos": np.ascontiguousarray(cosf[:, j * NT:(j + 1) * NT]),
                  "sin": np.ascontiguousarray(sinf[:, j * NT:(j + 1) * NT]), "rotm": rotm})
        maps1.append(m)
    resB = run_bass_kernel_spmd(_prog("B"), maps1, core_ids=cores).results
    if _DEBUG.get("stop") == "B":
        return resA, resB
    maps2 = []
    for c in cores:
        b, j = c // 4, c % 4
        kparts, vparts = [], []
        for g in range(2):
            segs = [resB[b * 4]["kT"][:, g * NSC + NT:(g + 1) * NSC]]
            segs += [resB[b * 4 + i]["kT"][:, g * NSC:g * NSC + NT] for i in range(4)]
            kparts.append(np.concatenate(segs, axis=1))
        kfull = np.ascontiguousarray(np.concatenate(kparts, axis=1))
        vfull = np.ascontiguousarray(np.concatenate([resB[b * 4]["vtm"][NT:NSC]] + [resB[b * 4 + i]["vtm"][0:NT] for i in range(4)], axis=0))
        maps2.append({
            "vecs": maps0[c]["vecs"], "ident": ident, "ada_w": inp["ada_w"],
            "x1T": resB[c]["x1T"], "qT": resB[c]["qT"], "kT": kfull, "vtm": vfull,
            "w_o": inp["attn_w_o"][0], "router": inp["moe_router"][0],
            "m_w1": inp["moe_w1"][0], "m_w3": inp["moe_w3"][0], "m_w2": inp["moe_w2"][0], "fin_g": inp["final_g"],
        })
    resC = run_bass_kernel_spmd(_prog("C"), maps2, core_ids=cores).results
    out = np.zeros((2, SEQ, D), np.float32)
    for c in cores:
        b, j = c // 4, c % 4
        out[b, j * NT:(j + 1) * NT] = resC[c]["out"]
    return out


def kernel(**inp):
    inp = {k: np.asarray(v) for k, v in inp.items()}
    x = inp["x"].astype(np.float32, copy=False)
    ctx = inp["ctx"].astype(np.float32, copy=False)
    ident = np.eye(128, dtype=np.float32)
    cosf, sinf = rope_tables()
    rotm = rot_matrix_T()
    cst = np.zeros((128, 385), np.float32)
    cst[:, 0:128] = np.triu(np.ones((128, 128), np.float32), 1)
    cst[:, 128:384] = np.arange(256, dtype=np.float32)[None, :]
    cst[:, 384] = np.arange(128, dtype=np.float32)
    cores = list(range(8))
    maps = []
    for c in cores:
        b, j = c // 4, c % 4
        t0 = j * NT
        hal = np.zeros((128, D), np.float32)
        if j > 0:
            hal[0:2] = x[b, t0 - 2:t0]
        if j < 3:
            hal[2] = x[b, t0 + NT]
        xall = np.concatenate([x[b, t0:t0 + NT], ctx[b], hal], axis=0)
        maps.append({
            "xall": np.ascontiguousarray(xall), "vecs": pack_vecs(inp, b, j), "ident": ident,
            "ada_w": inp["ada_w"], "w_in": inp["rg_w_in"][0], "w_a": inp["rg_w_a"][0], "w_i": inp["rg_w_i"][0],
            "w_out": inp["rg_w_out"][0], "f_w1": inp["ffn_w1"][0], "f_w3": inp["ffn_w3"][0], "f_w2": inp["ffn_w2"][0],
            "w_qkv": inp["attn_w_qkv"][0], "cos": np.ascontiguousarray(cosf[:, j * NT:(j + 1) * NT]),
            "sin": np.ascontiguousarray(sinf[:, j * NT:(j + 1) * NT]), "rotm": rotm,
            "w_o": inp["attn_w_o"][0], "router": inp["moe_router"][0],
            "m_w1": inp["moe_w1"][0], "m_w3": inp["moe_w3"][0], "m_w2": inp["moe_w2"][0], "fin_g": inp["final_g"],
            "cst": cst,
        })
    res = run_bass_kernel_spmd(_prog("F"), maps, core_ids=cores).results
    out = np.zeros((2, SEQ, D), np.float32)
    for c in cores:
        b, j = c // 4, c % 4
        out[b, j * NT:(j + 1) * NT] = res[c]["out"]
    return out
```
